# Optimizing a Trainium2 kernel written in Bass

```python
import math
import jax, jax.numpy as jnp
from jax import lax
import numpy as np

D_MODEL = 1024
BATCH = 2
SEQ = 8192
DEPTH = 2

CHUNK = 64
Q_BLOCK = 128
MEM_LEN = 256
GMLP_BLOCK = 128
A_GROUPS = 4
A_CH = 128
A_WIDTH = A_GROUPS * A_CH
B_HEADS = 4
B_HEAD_DIM = 128
B_WIDTH = B_HEADS * B_HEAD_DIM
C_HEADS = 4
C_QK_DIM = 64
C_V_DIM = 2 * C_QK_DIM
C_WIDTH = C_HEADS * C_V_DIM
D_GROUPS = 8
D_WIDTH = 512
CONV_W = 3
EVEN_IN = 2 * A_WIDTH + 3 * B_WIDTH + B_HEADS
ODD_IN = 2 * (2 * C_HEADS * C_QK_DIM) + C_WIDTH + 3 * D_WIDTH
EVEN_MIX = A_WIDTH + B_WIDTH
ODD_MIX = C_WIDTH + D_WIDTH
M_HEADS = 4
M_HEAD_DIM = D_MODEL // M_HEADS
D_FF = 2816
N_EXPERTS = 8
TOP_K = 2
D_FF_EXPERT = 3584
REL_BUCKETS = 32
REL_MAX_DIST = 128
EPS = 1e-6
N_EVEN = (DEPTH + 1) // 2
N_ODD = DEPTH // 2

kernel_name = 'hybrid_streaming_encoder_gmlp_fox_diffattn_shortconv_moe'


def rmsnorm(x, g):
    xf = x.astype(jnp.float32)
    y = xf * lax.rsqrt(jnp.mean(xf * xf, axis=-1, keepdims=True) + EPS)
    return (y * g.astype(jnp.float32)).astype(x.dtype)


def split_cols(z, widths):
    offs = np.cumsum([0] + list(widths))
    return [z[..., int(a):int(b)] for a, b in zip(offs[:-1], offs[1:])]


def to_blocks(t):
    b, h, s, d = t.shape
    return t.reshape(b, h, s // Q_BLOCK, Q_BLOCK, d).transpose(2, 0, 1, 3, 4)


def from_blocks(t):
    nb, b, h, q, d = t.shape
    return t.transpose(1, 2, 0, 3, 4).reshape(b, h, nb * q, d)


def swiglu(h, wg, wu, wd):
    return (jax.nn.silu(h @ wg) * (h @ wu)) @ wd


def gmlp_spatial_gate(u, v, v_gain, w_s, b_s):
    bsz, s, _ = u.shape
    nblk = s // GMLP_BLOCK
    u = jax.nn.gelu(u).reshape(bsz, nblk, GMLP_BLOCK, A_GROUPS, A_CH)
    v = jax.nn.gelu(v).reshape(bsz, nblk, GMLP_BLOCK, A_GROUPS, A_CH)
    v = rmsnorm(v, v_gain.reshape(A_GROUPS, A_CH))
    pos = jnp.arange(GMLP_BLOCK)
    mask = (pos[None, :] // CHUNK) <= (pos[:, None] // CHUNK)
    w = jnp.where(mask[None], w_s, 0)
    mixed = jnp.einsum('gij,bnjgc->bnigc', w, v) + b_s.T[None, None, :, :, None]
    return (u * mixed).reshape(bsz, s, A_WIDTH)


def forgetting_attention(q, k, v, f_logit):
    b, h, s, _ = q.shape
    nb = s // Q_BLOCK
    c = jnp.cumsum(jax.nn.log_sigmoid(f_logit.astype(jnp.float32)), axis=-1)
    cb = c.reshape(b, h, nb, Q_BLOCK).transpose(2, 0, 1, 3)
    kpos = jnp.arange(s)
    scale = B_HEAD_DIM ** -0.5

    def block(args):
        qi, ci, bi = args
        qpos = bi * Q_BLOCK + jnp.arange(Q_BLOCK)
        lg = jnp.einsum('bhqd,bhkd->bhqk', qi, k).astype(jnp.float32) * scale
        lg = lg + ci[..., :, None] - c[:, :, None, :]
        lg = jnp.where(kpos[None, :] <= qpos[:, None], lg, -jnp.inf)
        p = jax.nn.softmax(lg, axis=-1).astype(v.dtype)
        return jnp.einsum('bhqk,bhkd->bhqd', p, v)

    return from_blocks(lax.map(block, (to_blocks(q), cb, jnp.arange(nb))))


def rel_bucket(rel):
    n_half = REL_BUCKETS // 2
    max_exact = n_half // 2
    ret = jnp.where(rel > 0, n_half, 0)
    n = jnp.abs(rel)
    nf = jnp.maximum(n, 1).astype(jnp.float32)
    large = max_exact + (jnp.log(nf / max_exact) / math.log(REL_MAX_DIST / max_exact)
                         * (n_half - max_exact)).astype(jnp.int32)
    large = jnp.minimum(large, n_half - 1)
    return ret + jnp.where(n < max_exact, n, large)


def differential_attention(q1, q2, k1, k2, v, lam, rel_table):
    s = q1.shape[2]
    nb = s // Q_BLOCK
    kpos = jnp.arange(s)
    scale = C_QK_DIM ** -0.5

    def block(args):
        q1i, q2i, bi = args
        qpos = bi * Q_BLOCK + jnp.arange(Q_BLOCK)
        bias = rel_table[rel_bucket(kpos[None, :] - qpos[:, None])]
        bias = bias.transpose(2, 0, 1).astype(jnp.float32)[None]
        mask = (kpos[None, :] // CHUNK) <= (qpos[:, None] // CHUNK)

        def probs(qi, ki):
            lg = jnp.einsum('bhqd,bhkd->bhqk', qi, ki).astype(jnp.float32) * scale + bias
            return jax.nn.softmax(jnp.where(mask, lg, -jnp.inf), axis=-1)

        p = probs(q1i, k1) - lam * probs(q2i, k2)
        return jnp.einsum('bhqk,bhkd->bhqd', p.astype(v.dtype), v)

    return from_blocks(lax.map(block, (to_blocks(q1), to_blocks(q2), jnp.arange(nb))))


def short_conv_mixer(h, gb, gc, conv_w):
    y = gc * h
    y = lax.conv_general_dilated(y, conv_w[:, None, :].astype(y.dtype), window_strides=(1,),
                                 padding=[(CONV_W - 1, 0)], dimension_numbers=('NWC', 'WIO', 'NWC'),
                                 feature_group_count=D_WIDTH)
    return gb * y


def even_mixer(xn, w_in, b_f, v_gain, w_s, b_s, w_out):
    b, s, _ = xn.shape
    u, v, q, k, vv, f = split_cols(xn @ w_in, [A_WIDTH, A_WIDTH, B_WIDTH, B_WIDTH, B_WIDTH, B_HEADS])
    ya = gmlp_spatial_gate(u, v, v_gain, w_s, b_s)
    heads = lambda t: t.reshape(b, s, B_HEADS, B_HEAD_DIM).transpose(0, 2, 1, 3)
    yb = forgetting_attention(heads(q), heads(k), heads(vv), (f + b_f).transpose(0, 2, 1))
    yb = yb.transpose(0, 2, 1, 3).reshape(b, s, B_WIDTH)
    return jnp.concatenate([ya, yb], axis=-1) @ w_out


def odd_mixer(xn, w_in, lq1, lk1, lq2, lk2, subln_gain, conv_w, rel_table, w_out, lam_init):
    b, s, _ = xn.shape
    qk_w = 2 * C_HEADS * C_QK_DIM
    q, k, v, h, gb, gc = split_cols(xn @ w_in, [qk_w, qk_w, C_WIDTH, D_WIDTH, D_WIDTH, D_WIDTH])
    q = q.reshape(b, s, 2, C_HEADS, C_QK_DIM).transpose(2, 0, 3, 1, 4)
    k = k.reshape(b, s, 2, C_HEADS, C_QK_DIM).transpose(2, 0, 3, 1, 4)
    v = v.reshape(b, s, C_HEADS, C_V_DIM).transpose(0, 2, 1, 3)
    lam = (jnp.exp(jnp.sum(lq1.astype(jnp.float32) * lk1.astype(jnp.float32)))
           - jnp.exp(jnp.sum(lq2.astype(jnp.float32) * lk2.astype(jnp.float32))) + lam_init)
    o = differential_attention(q[0], q[1], k[0], k[1], v, lam, rel_table)
    o = rmsnorm(o, subln_gain.reshape(C_HEADS, 1, C_V_DIM)) * (1.0 - lam_init)
    yc = o.transpose(0, 2, 1, 3).reshape(b, s, C_WIDTH)
    yd = short_conv_mixer(h, gb, gc, conv_w)
    return jnp.concatenate([yc, yd], axis=-1) @ w_out


def memory_cross_attention(xn, memn, w_q, w_kv, w_o):
    b, s, _ = xn.shape
    m = memn.shape[1]
    q = (xn @ w_q).reshape(b, s, M_HEADS, M_HEAD_DIM)
    kv = (memn @ w_kv).reshape(b, m, 2, M_HEADS, M_HEAD_DIM)
    k, v = kv[:, :, 0], kv[:, :, 1]
    lg = jnp.einsum('bshd,bmhd->bhsm', q, k).astype(jnp.float32) * M_HEAD_DIM ** -0.5
    p = jax.nn.softmax(lg, axis=-1).astype(v.dtype)
    o = jnp.einsum('bhsm,bmhd->bshd', p, v).reshape(b, s, D_MODEL)
    return o @ w_o


def moe_swiglu(h, w_router, wg, wu, wd):
    b, s, d = h.shape
    t = h.reshape(b * s, d)
    logits = (t @ w_router).astype(jnp.float32)
    top_v, top_i = lax.top_k(logits, TOP_K)
    gates = jax.nn.softmax(top_v, axis=-1)
    combine = jnp.sum(jax.nn.one_hot(top_i, N_EXPERTS, dtype=jnp.float32) * gates[..., None], axis=1)
    out = jnp.zeros_like(t)
    for e in range(N_EXPERTS):
        out = out + combine[:, e:e + 1].astype(t.dtype) * swiglu(t, wg[e], wu[e], wd[e])
    return out.reshape(b, s, d)


def setup_inputs(seed: int = 0) -> dict:
    key = jax.random.key(seed)
    ks = iter(jax.random.split(key, 48))
    f32 = jnp.float32

    def nrm(shape, fan_in):
        return jax.random.normal(next(ks), shape, f32) * fan_in ** -0.5

    def gain(shape):
        return 1.0 + 0.05 * jax.random.normal(next(ks), shape, f32)

    return {
        'x': jax.random.normal(next(ks), (BATCH, SEQ, D_MODEL), f32),
        'mem': jax.random.normal(next(ks), (BATCH, MEM_LEN, D_MODEL), f32),
        'norm_mix': gain((DEPTH, D_MODEL)),
        'norm_mem_q': gain((DEPTH, D_MODEL)),
        'norm_mem_kv': gain((DEPTH, D_MODEL)),
        'norm_ffn': gain((DEPTH, D_MODEL)),
        'norm_final': gain((D_MODEL,)),
        'even_w_in': nrm((N_EVEN, D_MODEL, EVEN_IN), D_MODEL),
        'fox_b_f': jax.random.uniform(next(ks), (N_EVEN, B_HEADS), f32, 1.0, 4.0),
        'gmlp_v_gain': gain((N_EVEN, A_WIDTH)),
        'gmlp_w_s': nrm((N_EVEN, A_GROUPS, GMLP_BLOCK, GMLP_BLOCK), GMLP_BLOCK),
        'gmlp_b_s': 1.0 + 0.1 * jax.random.normal(next(ks), (N_EVEN, A_GROUPS, GMLP_BLOCK), f32),
        'even_w_out': nrm((N_EVEN, EVEN_MIX, D_MODEL), EVEN_MIX),
        'odd_w_in': nrm((N_ODD, D_MODEL, ODD_IN), D_MODEL),
        'diff_lambda_q1': 0.1 * jax.random.normal(next(ks), (N_ODD, C_QK_DIM), f32),
        'diff_lambda_k1': 0.1 * jax.random.normal(next(ks), (N_ODD, C_QK_DIM), f32),
        'diff_lambda_q2': 0.1 * jax.random.normal(next(ks), (N_ODD, C_QK_DIM), f32),
        'diff_lambda_k2': 0.1 * jax.random.normal(next(ks), (N_ODD, C_QK_DIM), f32),
        'diff_subln_gain': gain((N_ODD, C_WIDTH)),
        'conv_w': nrm((N_ODD, CONV_W, D_WIDTH), CONV_W),
        'odd_w_out': nrm((N_ODD, ODD_MIX, D_MODEL), ODD_MIX),
        'rel_bias': 0.2 * jax.random.normal(next(ks), (REL_BUCKETS, C_HEADS), f32),
        'mem_w_q': nrm((DEPTH, D_MODEL, D_MODEL), D_MODEL),
        'mem_w_kv': nrm((DEPTH, D_MODEL, 2 * D_MODEL), D_MODEL),
        'mem_w_o': nrm((DEPTH, D_MODEL, D_MODEL), D_MODEL),
        'ffn_w_gate': nrm((N_EVEN, D_MODEL, D_FF), D_MODEL),
        'ffn_w_up': nrm((N_EVEN, D_MODEL, D_FF), D_MODEL),
        'ffn_w_down': nrm((N_EVEN, D_FF, D_MODEL), D_FF),
        'router_w': nrm((N_ODD, D_MODEL, N_EXPERTS), D_MODEL),
        'moe_w_gate': nrm((N_ODD, N_EXPERTS, D_MODEL, D_FF_EXPERT), D_MODEL),
        'moe_w_up': nrm((N_ODD, N_EXPERTS, D_MODEL, D_FF_EXPERT), D_MODEL),
        'moe_w_down': nrm((N_ODD, N_EXPERTS, D_FF_EXPERT, D_MODEL), D_FF_EXPERT),
    }


def reference(x, mem, norm_mix, norm_mem_q, norm_mem_kv, norm_ffn, norm_final,
              even_w_in, fox_b_f, gmlp_v_gain, gmlp_w_s, gmlp_b_s, even_w_out,
              odd_w_in, diff_lambda_q1, diff_lambda_k1, diff_lambda_q2, diff_lambda_k2,
              diff_subln_gain, conv_w, odd_w_out, rel_bias,
              mem_w_q, mem_w_kv, mem_w_o,
              ffn_w_gate, ffn_w_up, ffn_w_down,
              router_w, moe_w_gate, moe_w_up, moe_w_down):
    for l in range(DEPTH):
        i = l // 2
        xn = rmsnorm(x, norm_mix[l])
        if l % 2 == 0:
            x = x + even_mixer(xn, even_w_in[i], fox_b_f[i], gmlp_v_gain[i], gmlp_w_s[i],
                               gmlp_b_s[i], even_w_out[i])
        else:
            lam_init = 0.8 - 0.6 * math.exp(-0.3 * l)
            x = x + odd_mixer(xn, odd_w_in[i], diff_lambda_q1[i], diff_lambda_k1[i],
                              diff_lambda_q2[i], diff_lambda_k2[i], diff_subln_gain[i],
                              conv_w[i], rel_bias, odd_w_out[i], lam_init)
        x = x + memory_cross_attention(rmsnorm(x, norm_mem_q[l]), rmsnorm(mem, norm_mem_kv[l]),
                                       mem_w_q[l], mem_w_kv[l], mem_w_o[l])
        h = rmsnorm(x, norm_ffn[l])
        if l % 2 == 0:
            x = x + swiglu(h, ffn_w_gate[i], ffn_w_up[i], ffn_w_down[i])
        else:
            x = x + moe_swiglu(h, router_w[i], moe_w_gate[i], moe_w_up[i], moe_w_down[i])
    return rmsnorm(x, norm_final)
```

```python
import contextlib
from concourse.bass_utils import run_bass_kernel_spmd
import numpy as np
import concourse.bass as bass
import concourse.mybir as mybir

F32 = mybir.dt.float32
BF16 = mybir.dt.bfloat16
F32R = mybir.dt.float32r
I32 = mybir.dt.int32
U32 = mybir.dt.uint32
AF = mybir.ActivationFunctionType
ALU = mybir.AluOpType
AX = mybir.AxisListType

ENGS = ("pe", "act", "dve", "pool", "sp")


class Phase:
    def __init__(self, nc, name):
        self.nc = nc
        self.name = name
        self.ops = []
        self.last_writer = {}
        self.readers = {}
        self.dma_keys = {}

    def _add(self, eng, fn, reads, writes, dma_key=None, ndma=0):
        idx = len(self.ops)
        deps = set()
        for r in reads:
            w = self.last_writer.get(r)
            if w is not None:
                deps.add(w)
        for r in writes:
            w = self.last_writer.get(r)
            if w is not None:
                deps.add(w)
            for rd in self.readers.get(r, ()):
                deps.add(rd)
        deps.discard(idx)
        for r in writes:
            self.last_writer[r] = idx
            self.readers[r] = []
        for r in reads:
            if r not in writes:
                self.readers.setdefault(r, []).append(idx)
        self.ops.append(dict(eng=eng, fn=fn, deps=deps, dma_key=dma_key, ndma=ndma,
                             signal=False))
        return idx

    def op(self, eng, fn, reads=(), writes=()):
        return self._add(eng, fn, tuple(reads), tuple(writes))

    def I(self, eng, meth, *a, reads=(), writes=(), **kw):
        return self.op(eng, lambda e: getattr(e, meth)(*a, **kw), reads, writes)

    def dma(self, queue, fn, key, reads=(), writes=(), n=1):
        return self._add(queue, fn, tuple(reads), tuple(writes), dma_key=key, ndma=n)

    def emit(self):
        nc = self.nc
        ops = self.ops
        pos = {}
        cnt = {e: 0 for e in ENGS}
        for i, o in enumerate(ops):
            pos[i] = cnt[o["eng"]]
            cnt[o["eng"]] += 1
        for i, o in enumerate(ops):
            need = set()
            for d in o["deps"]:
                po = ops[d]
                if po["dma_key"] is not None:
                    need.add(d)
                elif po["eng"] == o["eng"]:
                    if o["eng"] != "pe" and pos[i] - pos[d] <= 2 and o["dma_key"] is None:
                        need.add(d)
                    elif o["dma_key"] is not None:
                        need.add(d)
                else:
                    need.add(d)
            o["need"] = need
            for d in need:
                if ops[d]["dma_key"] is None:
                    ops[d]["signal"] = True
        sigcnt = {e: 0 for e in ENGS}
        for o in ops:
            if o["dma_key"] is None and o["signal"]:
                sigcnt[o["eng"]] += 1
                o["sigval"] = sigcnt[o["eng"]]
        dmacnt = {}
        for o in ops:
            if o["dma_key"] is not None:
                dmacnt[o["dma_key"]] = dmacnt.get(o["dma_key"], 0) + o["ndma"]
                o["sigval"] = 16 * dmacnt[o["dma_key"]]
        keys = sorted(dmacnt.keys(), key=str)
        import contextlib
        esem = {e: nc.alloc_semaphore(name=f"{self.name}_{e}") for e in ENGS}
        dsem = {k: nc.alloc_semaphore(name=f"{self.name}_d{j}") for j, k in enumerate(keys)}
        with contextlib.ExitStack() as st:
            block = st.enter_context(nc.Block())
            per_eng = {e: [] for e in ENGS}
            for i, o in enumerate(ops):
                per_eng[o["eng"]].append(i)

            def run(engname, eng):
                seen = {}
                for i in per_eng[engname]:
                    o = ops[i]
                    waits = {}
                    for d in o["need"]:
                        po = ops[d]
                        if po["dma_key"] is not None:
                            k = ("d", po["dma_key"])
                        else:
                            k = ("e", po["eng"])
                        waits[k] = max(waits.get(k, 0), po["sigval"])
                    for k, v in waits.items():
                        if seen.get(k, 0) >= v:
                            continue
                        seen[k] = v
                        s = dsem[k[1]] if k[0] == "d" else esem[k[1]]
                        eng.wait_ge(s, v)
                    if o["dma_key"] is not None:
                        o["fn"](eng, dsem[o["dma_key"]])
                    else:
                        ins = o["fn"](eng)
                        if o["signal"]:
                            ins.then_inc(esem[engname], 1)
                fin = {}
                for i in per_eng[engname]:
                    o = ops[i]
                    if o["dma_key"] is not None:
                        fin[o["dma_key"]] = max(fin.get(o["dma_key"], 0), o["sigval"])
                for k, v in fin.items():
                    if seen.get(("d", k), 0) < v:
                        eng.wait_ge(dsem[k], v)

            @block.tensor
            def _(e):
                run("pe", e)

            @block.scalar
            def _(e):
                run("act", e)

            @block.vector
            def _(e):
                run("dve", e)

            @block.gpsimd
            def _(e):
                run("pool", e)

            @block.sync
            def _(e):
                run("sp", e)
        nc.clear_and_free_semaphores(list(esem.values()) + list(dsem.values()))
        nc.all_engine_barrier()


T = 2048
CH = 512
NCH = T // CH
EPS = 1e-6
NEG = -30000.0


class Ctx:
    def __init__(self, name, nc=None):
        self.nc = nc if nc is not None else bass.Bass("TRN2", target_bir_lowering=False)
        self.name = name
        self.st = contextlib.ExitStack()
        self.ph = Phase(self.nc, name)
        self.n = 0

    def din(self, name, shape, dt=F32):
        return self.nc.dram_tensor(name, list(shape), dt, kind="ExternalInput").ap()

    def dout(self, name, shape, dt=F32):
        return self.nc.dram_tensor(name, list(shape), dt, kind="ExternalOutput").ap()

    def sb(self, name, shape, dt=F32):
        return self.st.enter_context(self.nc.sbuf_tensor("s_" + self.name + "_" + name, list(shape), dt))

    def psum(self, name):
        return self.st.enter_context(self.nc.psum_tensor("p_" + self.name + "_" + name, [128, 512], F32))

    def finish(self):
        self.ph.emit()
        self.st.close()
        return self.nc


def ld(ph, queue, key, out_ap, in_ap, writes, reads=()):
    ph.dma(queue, lambda e, s: e.dma_start(out=out_ap, in_=in_ap).then_inc(s, 16), key,
           reads=reads, writes=writes)


def rmsnorm_fm(ph, tag, src, cs, gcol, xn, ones_bf, sq, psN, rstd_s, rstd, src_res, xn_res,
               nk=8, dim=1024.0):
    for k in range(nk):
        sqk = sq[k % 2]
        ph.op("act", (lambda k, sqk: lambda e: e.activation(out=sqk[:], in_=src[:, k, cs], func=AF.Square))(k, sqk),
              reads=[src_res], writes=[f"{tag}sq{k%2}"])
        ph.op("pe", (lambda k, sqk: lambda e: e.matmul(psN[:], lhsT=ones_bf[:], rhs=sqk[:], start=(k == 0), stop=(k == nk - 1)))(k, sqk),
              reads=[f"{tag}sq{k%2}", "ones"], writes=[f"{tag}psN"])
    ph.op("act", lambda e: e.activation(out=rstd_s[:], in_=psN[:], func=AF.Sqrt, scale=1.0 / dim, bias=EPS),
          reads=[f"{tag}psN"], writes=[f"{tag}rstd_s"])
    ph.op("dve", lambda e: e.reciprocal(out=rstd[:], in_=rstd_s[:]), reads=[f"{tag}rstd_s"], writes=[f"{tag}rstd"])
    for k in range(nk):
        ph.op("dve", (lambda k: lambda e: e.scalar_tensor_tensor(out=xn[:, k, :], in0=src[:, k, cs], scalar=gcol[:, k:k + 1],
                                                                  in1=rstd[:], op0=ALU.mult, op1=ALU.mult))(k),
              reads=[src_res, f"{tag}rstd", "gcol"], writes=[xn_res])


def build_A(c=None, io=None):
    if c is None:
        c = Ctx("A")
        io = dict(xT=c.din("xT", [1024, T]), g=c.din("g", [128, 8]), w=c.din("w", [1024, 2564]), vgain=c.din("vgain", [128, 512]),
                  ws=c.din("ws", [128, 4, 128]), bs=c.din("bs", [128, 4, 512]),
                  ya=c.dout("ya", [512, T], BF16), q=c.dout("q", [512, T], BF16), k=c.dout("k", [512, T], BF16),
                  v=c.dout("v", [T, 512], BF16), f=c.dout("f", [4, T], F32))
    nc, ph = c.nc, c.ph
    xT_d, g_d, w_d, vg_d, ws_d, bs_d = io["xT"], io["g"], io["w"], io["vgain"], io["ws"], io["bs"]
    ya_d, q_d, k_d, v_d, f_d = io["ya"], io["q"], io["k"], io["v"], io["f"]

    xT = c.sb("xT", [128, 8, T])
    w = c.sb("w", [128, 8, 2564], BF16)
    gcol = c.sb("gcol", [128, 8])
    ones_bf = c.sb("ones", [128, 128], BF16)
    sq = [c.sb(f"sq{i}", [128, CH], BF16) for i in range(2)]
    rstd_s = c.sb("rstd_s", [128, CH])
    rstd = c.sb("rstd", [128, CH])
    xn = c.sb("xn", [128, 8, CH], BF16)
    uT = c.sb("uT", [128, 4, CH])
    vgain = c.sb("vgain", [128, 512])
    wsm = c.sb("wsm", [128, 4, 128], BF16)
    bsb = c.sb("bsb", [128, 4, 512])
    vg = c.sb("vg", [128, 512])
    vscr = c.sb("vscr", [128, 128])
    ss = c.sb("ss", [128, 4])
    ssr = c.sb("ssr", [128, 4])
    vn = [c.sb(f"vn{i}", [128, 512], BF16) for i in range(2)]
    tmix = c.sb("tmix", [128, 512])
    yast = c.sb("yast", [128, 4, CH], BF16)
    qst = c.sb("qst", [128, 4, CH], BF16)
    kst = c.sb("kst", [128, 4, CH], BF16)
    vst = c.sb("vst", [128, 4, 512], BF16)
    fst = c.sb("fst", [4, CH])
    psN = c.psum("psN")
    psP = [c.psum(f"psP{i}") for i in range(3)]
    psM = [c.psum(f"psM{i}") for i in range(4)]

    ld(ph, "sp", "lx", xT[:], xT_d.rearrange("(k p) t -> p k t", p=128), ["xT"])
    ld(ph, "sp", "lg", gcol[:], g_d, ["gcol"])
    ld(ph, "sp", "lvg", vgain[:], vg_d, ["vgain"])
    ld(ph, "sp", "lbs", bsb[:], bs_d, ["bsb"])
    wv = w_d.rearrange("(k p) c -> p k c", p=128)
    for gi, (a, b) in enumerate([(0, 512), (512, 1024), (1024, 1536), (1536, 2048), (2048, 2564)]):
        ld(ph, "pool", f"lw{gi}", w[:, :, a:b], wv[:, :, a:b], [f"w{gi}"])
    ld(ph, "pool", "lws", wsm[:], ws_d, ["wsm"])
    ph.op("dve", lambda e: e.memset(ones_bf[:], 1.0), writes=["ones"])
    ph.op("dve", lambda e: e.memset(wsm[64:128, :, 0:64], 0.0), reads=["wsm"], writes=["wsm"])

    pi = [0]

    def nextps():
        pi[0] = (pi[0] + 1) % 3
        return psP[pi[0]], f"psP{pi[0]}"

    def proj_fm(col0, m, wres):
        ps, pres = nextps()
        for k in range(8):
            ph.op("pe", (lambda k, ps: lambda e: e.matmul(ps[:], lhsT=w[:, k, col0 + m * 128: col0 + (m + 1) * 128], rhs=xn[:, k, :],
                                                           start=(k == 0), stop=(k == 7)))(k, ps),
                  reads=["xn", wres], writes=[pres])
        return ps, pres

    for tc in range(NCH):
        cs = slice(tc * CH, (tc + 1) * CH)
        rmsnorm_fm(ph, "n", xT, cs, gcol, xn, ones_bf, sq, psN, rstd_s, rstd, "xT", "xn")
        for m in range(4):
            ps, pres = proj_fm(0, m, "w0")
            ph.op("act", (lambda m, ps: lambda e: e.activation(out=uT[:, m, :], in_=ps[:], func=AF.Gelu_apprx_tanh))(m, ps),
                  reads=[pres], writes=[f"uT{m}"])
        for m in range(4):
            ps, pres = proj_fm(1024, m, "w2")
            ph.op("dve", (lambda m, ps: lambda e: e.tensor_copy(out=qst[:, m, :], in_=ps[:]))(m, ps), reads=[pres], writes=["qst"])
        ld(ph, "sp", "sq", q_d.rearrange("(m p) t -> p m t", p=128)[:, :, cs], qst[:], [], reads=["qst"])
        for m in range(4):
            ps, pres = proj_fm(1536, m, "w3")
            ph.op("act", (lambda m, ps: lambda e: e.activation(out=kst[:, m, :], in_=ps[:], func=AF.Copy))(m, ps), reads=[pres], writes=["kst"])
        ld(ph, "sp", "sk", k_d.rearrange("(m p) t -> p m t", p=128)[:, :, cs], kst[:], [], reads=["kst"])
        ps, pres = nextps()
        for k in range(8):
            ph.op("pe", (lambda k, ps: lambda e: e.matmul(ps[0:4, :], lhsT=w[:, k, 2560:2564], rhs=xn[:, k, :], start=(k == 0), stop=(k == 7)))(k, ps),
                  reads=["xn", "w4"], writes=[pres])
        ph.op("dve", (lambda ps: lambda e: e.tensor_copy(out=fst[:], in_=ps[0:4, :]))(ps), reads=[pres], writes=["fst"])
        ld(ph, "sp", "sf", f_d[:, cs], fst[:], [], reads=["fst"])
        for s in range(4):
            ts_ = slice(s * 128, (s + 1) * 128)
            ps, pres = nextps()
            for k in range(8):
                ph.op("pe", (lambda k, ps, ts_: lambda e: e.matmul(ps[:], lhsT=xn[:, k, ts_], rhs=w[:, k, 512:1024], start=(k == 0), stop=(k == 7)))(k, ps, ts_),
                      reads=["xn", "w1"], writes=[pres])
            ph.op("act", (lambda ps: lambda e: e.activation(out=vg[:], in_=ps[:], func=AF.Gelu_apprx_tanh))(ps), reads=[pres], writes=["vg"])
            for g in range(4):
                ph.op("act", (lambda g: lambda e: e.activation(out=vscr[:], in_=vg[:, g * 128:(g + 1) * 128], func=AF.Square,
                                                               accum_out=ss[:, g:g + 1]))(g),
                      reads=["vg"], writes=["vscr", f"ss{g}"])
            ph.op("act", lambda e: e.activation(out=ssr[:], in_=ss[:], func=AF.Sqrt, scale=1.0 / 128.0, bias=EPS),
                  reads=[f"ss{g}" for g in range(4)], writes=["ssr"])
            ph.op("dve", lambda e: e.reciprocal(out=ss[:], in_=ssr[:]), reads=["ssr"], writes=[f"ss{g}" for g in range(4)])
            vnb = vn[s % 2]
            for g in range(4):
                gs = slice(g * 128, (g + 1) * 128)
                ph.op("dve", (lambda g, gs, vnb: lambda e: e.scalar_tensor_tensor(out=vnb[:, gs], in0=vg[:, gs], scalar=ss[:, g:g + 1], in1=vgain[:, gs],
                                                                                   op0=ALU.mult, op1=ALU.mult))(g, gs, vnb),
                      reads=["vg", f"ss{g}", "vgain"], writes=[f"vn{s%2}"])
            for g in range(4):
                gs = slice(g * 128, (g + 1) * 128)
                ph.op("pe", (lambda g, gs, vnb, ts_: lambda e: e.matmul(psM[g][:, ts_], lhsT=vnb[:, gs], rhs=wsm[:, g, :], start=True, stop=True))(g, gs, vnb, ts_),
                      reads=[f"vn{s%2}", "wsm"], writes=[f"psM{g}"])
            ps, pres = nextps()
            for k in range(8):
                ph.op("pe", (lambda k, ps, ts_: lambda e: e.matmul(ps[:], lhsT=xn[:, k, ts_], rhs=w[:, k, 2048:2560], start=(k == 0), stop=(k == 7)))(k, ps, ts_),
                      reads=["xn", "w4"], writes=[pres])
            ph.op("dve", (lambda ps, s: lambda e: e.tensor_copy(out=vst[:, s, :], in_=ps[:]))(ps, s), reads=[pres], writes=["vst"])
        ld(ph, "sp", "sv", v_d.rearrange("(n p) c -> p n c", p=128)[:, tc * 4:(tc + 1) * 4, :], vst[:], [], reads=["vst"])
        for g in range(4):
            ph.op("dve", (lambda g: lambda e: e.tensor_tensor(out=tmix[:], in0=psM[g][:], in1=bsb[:, g, :], op=ALU.add))(g),
                  reads=[f"psM{g}", "bsb"], writes=["tmix"])
            ph.op("dve", (lambda g: lambda e: e.tensor_tensor(out=yast[:, g, :], in0=tmix[:], in1=uT[:, g, :], op=ALU.mult))(g),
                  reads=["tmix", f"uT{g}"], writes=["yast"])
        ld(ph, "sp", "sy", ya_d.rearrange("(m p) t -> p m t", p=128)[:, :, cs], yast[:], [], reads=["yast"])
    return c.finish()


def _c(a):
    return np.ascontiguousarray(a)


def pk(vec):
    return _c(np.asarray(vec).reshape(-1, 128).T)


def rep(vec, n=128):
    return _c(np.broadcast_to(np.asarray(vec)[None], (n,) + tuple(np.asarray(vec).shape)))


def core_tokens(xfull, core):
    b, i = core // 4, core % 4
    return xfull[b, i * T:(i + 1) * T]


_NC_CACHE = {}


def get_nc(name, builder):
    if name not in _NC_CACHE:
        _NC_CACHE[name] = builder()
    return _NC_CACHE[name]


def run(nc, in_maps):
    res = run_bass_kernel_spmd(nc, in_maps, core_ids=list(range(8)))
    return res.results


def host_A(inp):
    x = np.asarray(inp["x"], np.float32)
    maps = []
    ws = _c(np.transpose(np.asarray(inp["gmlp_w_s"][0], np.float32), (2, 0, 1)))
    bs = np.asarray(inp["gmlp_b_s"][0], np.float32)
    bs4 = rep(np.tile(bs, (1, 4)))
    for core in range(8):
        maps.append(dict(
            xT=_c(core_tokens(x, core).T),
            g=pk(inp["norm_mix"][0]),
            w=_c(np.asarray(inp["even_w_in"][0], np.float32)),
            vgain=rep(np.asarray(inp["gmlp_v_gain"][0], np.float32)),
            ws=ws, bs=bs4))
    return maps


S = 8192
NT = S // 128
NG = S // CH


def build_B(c=None, io=None):
    if c is None:
        c = Ctx("B")
        io = dict(q=c.din("q", [128, S], BF16), k=c.din("k", [128, S], BF16), v=c.din("v", [128, NT, 128], BF16),
                  f2=c.din("f2", [64, 128]), bf=c.din("bf", [64, 1]), ident=c.din("ident", [64, 64]), lmat=c.din("lmat", [64, 64]),
                  tri=c.din("tri", [128, 128]), y=c.dout("y", [128, S], BF16))
    nc, ph = c.nc, c.ph
    q_d, k_d, v_d, f_d, bf_d, id_d, L_d, tri_d, y_d = (io[n] for n in ("q", "k", "v", "f2", "bf", "ident", "lmat", "tri", "y"))

    qT = c.sb("qT", [128, S], BF16)
    kT = c.sb("kT", [128, S], BF16)
    V = c.sb("V", [128, NT, 128], BF16)
    f2 = c.sb("f2", [64, 128])
    bfc = c.sb("bfc", [64, 1])
    ident = c.sb("ident", [64, 64])
    lmat = c.sb("lmat", [64, 64])
    tri = c.sb("tri", [128, 128], BF16)
    ones_bf = c.sb("ones", [128, 128], BF16)
    ones64 = c.sb("ones64", [64, 128])
    cs2 = c.sb("cs2", [64, 128])
    off = c.sb("off", [64, 1])
    dg = c.sb("dg", [64, 64])
    dcol = c.sb("dcol", [128, 64])
    dref = c.sb("dref", [128, 64])
    biasm = c.sb("biasm", [128, NT, NT])
    PT = [c.sb(f"PT{i}", [128, CH], BF16) for i in range(3)]
    rl = c.sb("rl", [128, CH])
    ost = [c.sb(f"ost{i}", [128, CH], BF16) for i in range(2)]
    psS = [c.psum(f"psS{i}") for i in range(3)]
    psO = [c.psum(f"psO{i}") for i in range(2)]
    psL = [c.psum(f"psL{i}") for i in range(2)]
    psX = c.psum("psX")

    ld(ph, "sp", "l1", f2[:], f_d, ["f2"])
    ld(ph, "sp", "l2", bfc[:], bf_d, ["bfc"])
    ld(ph, "sp", "l3", ident[:], id_d, ["ident"])
    ld(ph, "sp", "l4", lmat[:], L_d, ["lmat"])
    ld(ph, "pool", "l5", tri[:], tri_d, ["tri"])
    ld(ph, "sp", "lq", qT[:], q_d, ["qT"])
    ld(ph, "act", "lk", kT[:], k_d, ["kT"])
    ld(ph, "sp", "lv", V[:], v_d, ["V"])
    ph.op("dve", lambda e: e.memset(ones_bf[:], 1.0), writes=["ones"])
    ph.op("dve", lambda e: e.memset(ones64[:], 1.0), writes=["ones64"])

    ph.op("dve", lambda e: e.tensor_scalar(out=bfc[:], in0=bfc[:], scalar1=-1.0, scalar2=None, op0=ALU.mult), reads=["bfc"], writes=["bfc"])
    ph.op("act", lambda e: e.activation(out=f2[:], in_=f2[:], func=AF.Exp, scale=-1.0, bias=bfc[:, 0:1]), reads=["f2", "bfc"], writes=["f2"])
    ph.op("act", lambda e: e.activation(out=f2[:], in_=f2[:], func=AF.Ln, scale=1.0, bias=1.0), reads=["f2"], writes=["f2"])
    ph.op("dve", lambda e: e.tensor_tensor_scan(out=cs2[:], data0=f2[:], data1=f2[:], initial=0.0, op0=ALU.add, op1=ALU.max),
          reads=["f2"], writes=["cs2"])
    ph.op("pe", lambda e: e.matmul(psX[0:64, 0:1], lhsT=lmat[:], rhs=cs2[:, 127:128], start=True, stop=True), reads=["cs2", "lmat"], writes=["psX"])
    ph.op("dve", lambda e: e.tensor_copy(out=off[:], in_=psX[0:64, 0:1]), reads=["psX"], writes=["off"])
    ph.op("dve", lambda e: e.tensor_scalar(out=cs2[:], in0=cs2[:], scalar1=off[:, 0:1], scalar2=None, op0=ALU.add), reads=["cs2", "off"], writes=["cs2"])
    ph.op("pe", lambda e: e.transpose(psX[:, 0:64], cs2[:], ident[:]), reads=["cs2", "ident", "off"], writes=["psX"])
    ph.op("dve", lambda e: e.tensor_copy(out=dcol[:], in_=psX[:, 0:64]), reads=["psX"], writes=["dcol"])
    ph.op("dve", lambda e: e.tensor_scalar(out=dg[:], in0=ident[:], scalar1=cs2[:, 64:65], scalar2=None, op0=ALU.mult), reads=["cs2", "ident"], writes=["dg"])
    ph.op("pe", lambda e: e.matmul(psX[:, 64:128], lhsT=ones64[:], rhs=dg[:], start=True, stop=True), reads=["dg", "ones64", "dcol"], writes=["psX"])
    ph.op("dve", lambda e: e.tensor_copy(out=dref[:], in_=psX[:, 64:128]), reads=["psX"], writes=["dref"])
    for i in range(NT):
        ph.op("dve", (lambda i: lambda e: e.tensor_scalar(out=biasm[:, i, 0:i + 1], in0=dcol[:, 0:i + 1], scalar1=dref[:, i:i + 1], scalar2=None,
                                                          op0=ALU.subtract))(i),
              reads=["dcol", "dref"], writes=["biasm"])

    scale = 128.0 ** -0.5
    it = 0
    for I in range(NG):
        qs = slice(I * CH, (I + 1) * CH)
        pO, pL = psO[I % 2], psL[I % 2]
        last = 4 * I + 3
        for j in range(last + 1):
            pS = psS[it % 3]
            P = PT[it % 3]
            pr, sr = f"psS{it%3}", f"PT{it%3}"
            it += 1
            ph.op("pe", (lambda j, pS, qs: lambda e: e.matmul(pS[:], lhsT=kT[:, j * 128:(j + 1) * 128], rhs=qT[:, qs], start=True, stop=True))(j, pS, qs),
                  reads=["kT", "qT"], writes=[pr])
            jj = max(j - 4 * I, 0)
            for ii in range(jj, 4):
                i = 4 * I + ii
                bs_ = slice(ii * 128, (ii + 1) * 128)
                ph.op("act", (lambda i, j, bs_, pS, P: lambda e: e.activation(out=P[:, bs_], in_=pS[:, bs_], func=AF.Exp, scale=scale,
                                                                              bias=biasm[:, i, j:j + 1]))(i, j, bs_, pS, P),
                      reads=[pr, "biasm"], writes=[sr])
                if i == j:
                    ph.op("pool", (lambda bs_, P: lambda e: e.tensor_tensor(out=P[:, bs_], in0=P[:, bs_], in1=tri[:], op=ALU.mult))(bs_, P),
                          reads=[sr, "tri"], writes=[sr])
            c0 = jj * 128
            ph.op("pe", (lambda j, P, c0, pO, last: lambda e: e.matmul(pO[:, c0:], lhsT=V[:, j, :], rhs=P[:, c0:], start=(j == 0), stop=(j == last)))(j, P, c0, pO, last),
                  reads=[sr, "V"], writes=[f"psO{I%2}"])
            ph.op("pe", (lambda j, P, c0, pL, last: lambda e: e.matmul(pL[:, c0:], lhsT=ones_bf[:], rhs=P[:, c0:], start=(j == 0), stop=(j == last)))(j, P, c0, pL, last),
                  reads=[sr, "ones"], writes=[f"psL{I%2}"])
        o = ost[I % 2]
        ph.op("dve", (lambda pL: lambda e: e.reciprocal(out=rl[:], in_=pL[:]))(pL), reads=[f"psL{I%2}"], writes=["rl"])
        ph.op("dve", (lambda o, pO: lambda e: e.tensor_tensor(out=o[:], in0=pO[:], in1=rl[:], op=ALU.mult))(o, pO), reads=[f"psO{I%2}", "rl"], writes=[f"ost{I%2}"])
        ld(ph, "sp", f"so{I%2}", y_d[:, qs], o[:], [], reads=[f"ost{I%2}"])
    return c.finish()


def host_B(inp, resA):
    maps = []
    ident = np.eye(64, dtype=np.float32)
    lmat = np.triu(np.ones((64, 64), np.float32), 1)
    tri = np.triu(np.ones((128, 128), np.float32), 0)
    for core in range(8):
        b, h = core // 4, core % 4
        q = np.concatenate([resA[b * 4 + i]["q"][h * 128:(h + 1) * 128] for i in range(4)], axis=1)
        k = np.concatenate([resA[b * 4 + i]["k"][h * 128:(h + 1) * 128] for i in range(4)], axis=1)
        v = np.concatenate([resA[b * 4 + i]["v"][:, h * 128:(h + 1) * 128] for i in range(4)], axis=0)
        f = np.concatenate([resA[b * 4 + i]["f"][h] for i in range(4)], axis=0)
        maps.append(dict(q=_c(q), k=_c(k), v=_c(v.reshape(NT, 128, 128).transpose(1, 0, 2)),
                         f2=_c(f.reshape(64, 128)), bf=np.full((64, 1), inp["fox_b_f"][0][h], np.float32),
                         ident=ident, lmat=lmat, tri=tri))
    return maps


MC = 1024
M_SCALE = 256.0 ** -0.5


def wtiles(W, ncols):
    W = np.asarray(W, np.float32)
    K_, N_ = W.shape
    return _c(W.reshape(K_ // 128, 128, N_ // ncols, ncols).transpose(2, 1, 0, 3))


class Tok(Ctx):
    def __init__(self, name, nwb=4, wb_elems=4096, arena_words=11264, nc=None):
        super().__init__(name, nc=nc)
        c = self
        self.xT = c.sb("xT", [128, 8, T])
        self.ones_bf = c.sb("ones", [128, 128], BF16)
        self.sq = [c.sb(f"sq{i}", [128, CH], BF16) for i in range(2)]
        self.rstd_s = c.sb("rstd_s", [128, CH])
        self.rstd = c.sb("rstd", [128, CH])
        self.gcols = c.sb("gcols", [128, 4, 8])
        self.wb = [c.sb(f"wb{i}", [128, wb_elems], BF16) for i in range(nwb)]
        self.wi = 0
        self.arena = c.sb("arena", [128, arena_words])
        self.actT = self.arena[:].bitcast(BF16).rearrange("p (k t) -> p k t", t=MC)
        self.ymb = [c.sb(f"ymb{i}", [128, 8, CH], BF16) for i in range(2)]
        self.hT = c.sb("hT", [128, 8, MC], BF16)
        self.sg = [c.sb(f"sg{i}", [128, CH], BF16) for i in range(2)]
        self.pp = [c.psum(f"pp{i}") for i in range(6)]
        self.psN = c.psum("psN")
        self.psL = c.psum("psL")
        self.pi = 0
        self.ph.I("dve", "memset", self.ones_bf[:], 1.0, writes=["ones"])

    def nextps(self):
        self.pi = (self.pi + 1) % len(self.pp)
        return self.pp[self.pi], f"pp{self.pi}"

    def wload(self, dram_tile_ap, nk, ncols):
        i = self.wi
        self.wi = (self.wi + 1) % len(self.wb)
        view = self.wb[i][:, 0:nk * ncols].rearrange("p (k c) -> p k c", c=ncols)
        ld(self.ph, "pool", f"wb{i}", view, dram_tile_ap, [f"wb{i}"])
        return view, f"wb{i}"

    def norm(self, gi, cs, xn, xn_res, n=CH):
        rmsnorm_fm(self.ph, "n", self.xT, cs, self.gcols[:, gi, :], xn, self.ones_bf, self.sq, self.psN,
                   self.rstd_s, self.rstd, "xT", xn_res)

    def linear_fm(self, Wt_d, nk, ncols, src, src_res, ntok, evac):
        ph = self.ph
        ncg = Wt_d.shape[0]
        for cg in range(ncg):
            wv, wres = self.wload(Wt_d[cg], nk, ncols)
            for mi in range(ncols // 128):
                for ts in range(ntok // CH):
                    ps, pres = self.nextps()
                    for k in range(nk):
                        ph.I("pe", "matmul", ps[:], lhsT=wv[:, k, mi * 128:(mi + 1) * 128], rhs=src[:, k, ts * CH:(ts + 1) * CH],
                             start=(k == 0), stop=(k == nk - 1), reads=[wres, src_res], writes=[pres])
                    evac(cg * (ncols // 128) + mi, ts, ps, pres)

    def resid_add(self, t0):
        ph, xT = self.ph, self.xT

        def ev(m, ts, ps, pres):
            sl = slice(t0 + ts * CH, t0 + (ts + 1) * CH)
            ph.I("dve", "tensor_tensor", out=xT[:, m, sl], in0=xT[:, m, sl], in1=ps[:], op=ALU.add, reads=[pres, "xT"], writes=["xT"])
        return ev

    def mem_setup(self, memT_d, gkv_d, Wkv_k_d, Wkv_v_d):
        c, ph = self, self.ph
        self.memT = self.arena[:, 0:2048].rearrange("p (k t) -> p k t", t=256)
        self.memn = self.arena[:, 2048:3072].bitcast(BF16).rearrange("p (k t) -> p k t", t=256)
        self.gkv = c.sb("gkv", [128, 8])
        self.KmT = self.arena[:, 3072:4096].bitcast(BF16).rearrange("p (k t) -> p k t", t=256)
        self.Vm = self.arena[:, 4096:5120].bitcast(BF16).rearrange("p (k t) -> p k t", t=1024)
        self.qmT = self.ymb[0]
        self.omT = self.ymb[1]
        self.PmT = [self.arena[:, 9728 + 256 * i: 9728 + 256 * (i + 1)].bitcast(BF16) for i in range(2)]
        self.rl = self.arena[:, 10240:10752]
        self.xq = c.sb("xq", [128, 8, CH], BF16)
        ld(ph, "sp", "lmem", self.memT, memT_d.rearrange("(k p) t -> p k t", p=128), ["memT"])
        ld(ph, "sp", "lgkv", self.gkv[:], gkv_d, ["gkv"])
        for k in range(8):
            sqk = self.sq[k % 2]
            ph.I("act", "activation", out=sqk[:, 0:256], in_=self.memT[:, k, :], func=AF.Square, reads=["memT"], writes=[f"nsq{k%2}"])
            ph.I("pe", "matmul", self.psN[:, 0:256], lhsT=self.ones_bf[:], rhs=sqk[:, 0:256], start=(k == 0), stop=(k == 7),
                 reads=[f"nsq{k%2}", "ones"], writes=["npsN"])
        ph.I("act", "activation", out=self.rstd_s[:, 0:256], in_=self.psN[:, 0:256], func=AF.Sqrt, scale=1.0 / 1024.0, bias=EPS,
             reads=["npsN"], writes=["nrstd_s"])
        ph.I("dve", "reciprocal", out=self.rstd[:, 0:256], in_=self.rstd_s[:, 0:256], reads=["nrstd_s"], writes=["nrstd"])
        for k in range(8):
            ph.I("dve", "scalar_tensor_tensor", out=self.memn[:, k, :], in0=self.memT[:, k, :], scalar=self.gkv[:, k:k + 1],
                 in1=self.rstd[:, 0:256], op0=ALU.mult, op1=ALU.mult, reads=["memT", "nrstd", "gkv"], writes=["memn"])
        for cg in range(2):
            wv, wres = self.wload(Wkv_k_d[cg], 8, 512)
            for mi in range(4):
                ps, pres = self.nextps()
                for k in range(8):
                    ph.I("pe", "matmul", ps[:, 0:256], lhsT=wv[:, k, mi * 128:(mi + 1) * 128], rhs=self.memn[:, k, :],
                         start=(k == 0), stop=(k == 7), reads=[wres, "memn"], writes=[pres])
                ph.I("act", "activation", out=self.KmT[:, cg * 4 + mi, :], in_=ps[:, 0:256], func=AF.Copy, reads=[pres], writes=["KmT"])
        for cg in range(2):
            wv, wres = self.wload(Wkv_v_d[cg], 8, 512)
            for mt in range(2):
                ps, pres = self.nextps()
                for k in range(8):
                    ph.I("pe", "matmul", ps[:], lhsT=self.memn[:, k, mt * 128:(mt + 1) * 128], rhs=wv[:, k, :],
                         start=(k == 0), stop=(k == 7), reads=[wres, "memn"], writes=[pres])
                ph.I("dve", "tensor_copy", out=self.Vm[:, mt, cg * 512:(cg + 1) * 512], in_=ps[:], reads=[pres], writes=["Vm"])

    def mem_attn(self, gi, Wq_d, Wo_d):
        ph = self.ph
        for tc in range(NCH):
            cs = slice(tc * CH, (tc + 1) * CH)
            self.norm(gi, cs, self.xq, "xq")

            def ev_q(m, ts, ps, pres):
                eng = "act" if m % 2 else "dve"
                if eng == "act":
                    ph.I("act", "activation", out=self.qmT[:, m, :], in_=ps[:], func=AF.Copy, reads=[pres], writes=["ymb0"])
                else:
                    ph.I("dve", "tensor_copy", out=self.qmT[:, m, :], in_=ps[:], reads=[pres], writes=["ymb0"])
            self.linear_fm(Wq_d, 8, 512, self.xq, "xq", CH, ev_q)
            for hh in range(4):
                for mt in range(2):
                    ps, pres = self.nextps()
                    for c2 in range(2):
                        ph.I("pe", "matmul", ps[:], lhsT=self.KmT[:, hh * 2 + c2, mt * 128:(mt + 1) * 128], rhs=self.qmT[:, hh * 2 + c2, :],
                             start=(c2 == 0), stop=(c2 == 1), reads=["KmT", "ymb0"], writes=[pres])
                    ph.I("act", "activation", out=self.PmT[mt], in_=ps[:], func=AF.Exp, scale=M_SCALE, reads=[pres], writes=[f"PmT{mt}"])
                for mt in range(2):
                    ph.I("pe", "matmul", self.psL[:], lhsT=self.ones_bf[:], rhs=self.PmT[mt], start=(mt == 0), stop=(mt == 1),
                         reads=[f"PmT{mt}", "ones"], writes=["psL"])
                ph.I("dve", "reciprocal", out=self.rl, in_=self.psL[:], reads=["psL"], writes=["rl"])
                for c2 in range(2):
                    ps, pres = self.nextps()
                    for mt in range(2):
                        ph.I("pe", "matmul", ps[:], lhsT=self.Vm[:, mt, hh * 256 + c2 * 128: hh * 256 + (c2 + 1) * 128], rhs=self.PmT[mt],
                             start=(mt == 0), stop=(mt == 1), reads=["Vm", f"PmT{mt}"], writes=[pres])
                    ph.I("dve", "tensor_tensor", out=self.omT[:, hh * 2 + c2, :], in0=ps[:], in1=self.rl, op=ALU.mult,
                         reads=[pres, "rl"], writes=["ymb1"])
            self.linear_fm(Wo_d, 8, 512, self.omT, "ymb1", CH, self.resid_add(tc * CH))


def out_proj(c, ymix_d, Wout_d):
    ph = c.ph
    for tc in range(NCH):
        yb = c.ymb[tc % 2]
        if isinstance(ymix_d, tuple):
            for hi, part in enumerate(ymix_d):
                ld(ph, "sp", f"lym{tc%2}", yb[:, 4 * hi:4 * hi + 4, :], part.rearrange("(k p) t -> p k t", p=128)[:, :, tc * CH:(tc + 1) * CH],
                   [f"ymb{tc%2}"])
        else:
            ymv = ymix_d.rearrange("(k p) t -> p k t", p=128)
            ld(ph, "sp", f"lym{tc%2}", yb[:], ymv[:, :, tc * CH:(tc + 1) * CH], [f"ymb{tc%2}"])
        c.linear_fm(Wout_d, 8, 512, yb, f"ymb{tc%2}", CH, c.resid_add(tc * CH))


def ffn_swiglu(c, gi, Wg_d, Wu_d, Wd_d, nf, post=None):
    ph = c.ph
    ncg = nf // 2
    for mc in range(T // MC):
        for ts in range(MC // CH):
            cs = slice(mc * MC + ts * CH, mc * MC + (ts + 1) * CH)
            c.norm(gi, cs, c.hT[:, :, ts * CH:(ts + 1) * CH], "hT")
        for cg in range(ncg):
            wg, wgr = c.wload(Wg_d[cg], 8, 256)
            wu, wur = c.wload(Wu_d[cg], 8, 256)
            for mi in range(2):
                for ts in range(MC // CH):
                    psG, pgr = c.nextps()
                    psU, pur = c.nextps()
                    for k in range(8):
                        ph.I("pe", "matmul", psG[:], lhsT=wg[:, k, mi * 128:(mi + 1) * 128], rhs=c.hT[:, k, ts * CH:(ts + 1) * CH],
                             start=(k == 0), stop=(k == 7), reads=[wgr, "hT"], writes=[pgr])
                    for k in range(8):
                        ph.I("pe", "matmul", psU[:], lhsT=wu[:, k, mi * 128:(mi + 1) * 128], rhs=c.hT[:, k, ts * CH:(ts + 1) * CH],
                             start=(k == 0), stop=(k == 7), reads=[wur, "hT"], writes=[pur])
                    sg = c.sg[(mi * 2 + ts) % 2]
                    sgr = f"sg{(mi * 2 + ts) % 2}"
                    ph.I("act", "activation", out=sg[:], in_=psG[:], func=AF.Silu, reads=[pgr], writes=[sgr])
                    ph.I("dve", "tensor_tensor", out=c.actT[:, cg * 2 + mi, ts * CH:(ts + 1) * CH], in0=sg[:], in1=psU[:], op=ALU.mult,
                         reads=[sgr, pur], writes=["actT"])
        c.linear_fm(Wd_d, nf, 128, c.actT, "actT", MC, c.resid_add(mc * MC))


C_IN = dict(xT=([1024, T], F32), ymix=([1024, T], BF16), gcols=([128, 4, 8], F32), Wout=([2, 128, 8, 512], F32), memT=([1024, 256], F32),
            gkv=([128, 8], F32), Wkvk=([2, 128, 8, 512], F32), Wkvv=([2, 128, 8, 512], F32), Wq=([2, 128, 8, 512], F32),
            Wo=([2, 128, 8, 512], F32), Wg=([11, 128, 8, 256], F32), Wu=([11, 128, 8, 256], F32), Wd=([8, 128, 22, 128], F32),
            Win=([6, 128, 8, 512], F32))


def build_C(c=None, io=None):
    if c is None:
        c = Tok("C")
        io = {n: c.din(n, sh, dt) for n, (sh, dt) in C_IN.items()}
        io.update(xo=c.dout("xo", [1024, T]), qk=c.dout("qk", [1024, T], BF16), v=c.dout("v", [T, 512], BF16),
                  z=c.dout("z", [512, T]), gb=c.dout("gb", [512, T]))
    nc, ph = c.nc, c.ph
    xT_d, ymix_d, gcols_d, Wout_d, memT_d, gkv_d, Wkvk_d, Wkvv_d, Wq_d, Wo_d, Wg_d, Wu_d, Wd_d, Win_d = (io[n] for n in C_IN)
    xo_d, qk_d, v_d, z_d, gb_d = io["xo"], io["qk"], io["v"], io["z"], io["gb"]

    ld(ph, "sp", "lx", c.xT[:], xT_d.rearrange("(k p) t -> p k t", p=128), ["xT"])
    ld(ph, "sp", "lg", c.gcols[:], gcols_d, ["gcol"])

    out_proj(c, ymix_d, Wout_d)
    c.mem_setup(memT_d, gkv_d, Wkvk_d, Wkvv_d)
    c.mem_attn(0, Wq_d, Wo_d)
    ffn_swiglu(c, 1, Wg_d, Wu_d, Wd_d, 22)

    xn1 = c.xq
    ar = c.arena
    gbst = ar[:, 0:2048].rearrange("p (m t) -> p m t", t=CH)
    zst = ar[:, 2048:4096].rearrange("p (m t) -> p m t", t=CH)
    vst = ar[:, 4096:5120].bitcast(BF16).rearrange("p (m t) -> p m t", t=512)
    qkst = ar[:, 5120:7168].bitcast(BF16).rearrange("p (m t) -> p m t", t=CH)
    hst = ar[:, 7168:9216].rearrange("p (m t) -> p m t", t=CH)
    dummy = c.sb("dummy", [128, 1])
    ph.I("dve", "memset", dummy[:], 0.0, reads=["actT"], writes=["qkst", "gbst", "zst", "vst"] + [f"hst{m}" for m in range(4)])
    for tc in range(NCH):
        cs = slice(tc * CH, (tc + 1) * CH)
        c.norm(2, cs, xn1, "xq")

        def ev_qk(m, ts, ps, pres):
            if m % 2:
                ph.I("act", "activation", out=qkst[:, m, :], in_=ps[:], func=AF.Copy, reads=[pres], writes=["qkst"])
            else:
                ph.I("dve", "tensor_copy", out=qkst[:, m, :], in_=ps[:], reads=[pres], writes=["qkst"])
        c.linear_fm(Win_d[0:2], 8, 512, xn1, "xq", CH, ev_qk)
        ld(ph, "sp", "sqk", qk_d.rearrange("(m p) t -> p m t", p=128)[:, :, cs], qkst, [], reads=["qkst"])
        wv, wres = c.wload(Win_d[2], 8, 512)
        for s in range(4):
            ps, pres = c.nextps()
            for k in range(8):
                ph.I("pe", "matmul", ps[:], lhsT=xn1[:, k, s * 128:(s + 1) * 128], rhs=wv[:, k, :], start=(k == 0), stop=(k == 7),
                     reads=[wres, "xq"], writes=[pres])
            ph.I("act", "activation", out=vst[:, s, :], in_=ps[:], func=AF.Copy, reads=[pres], writes=["vst"])
        ld(ph, "sp", "sv", v_d.rearrange("(n p) c -> p n c", p=128)[:, tc * 4:(tc + 1) * 4, :], vst, [], reads=["vst"])

        def ev_conv(m, ts, ps, pres):
            if m < 4:
                ph.I("act", "activation", out=hst[:, m, :], in_=ps[:], func=AF.Copy, reads=[pres], writes=[f"hst{m}"])
            elif m < 8:
                ph.I("act", "activation", out=gbst[:, m - 4, :], in_=ps[:], func=AF.Copy, reads=[pres], writes=["gbst"])
            else:
                ph.I("dve", "tensor_tensor", out=zst[:, m - 8, :], in0=hst[:, m - 8, :], in1=ps[:], op=ALU.mult,
                     reads=[pres, f"hst{m-8}"], writes=["zst"])
        c.linear_fm(Win_d[3:6], 8, 512, xn1, "xq", CH, ev_conv)
        ld(ph, "sp", "sgb", gb_d.rearrange("(m p) t -> p m t", p=128)[:, :, cs], gbst, [], reads=["gbst"])
        ld(ph, "sp", "sz", z_d.rearrange("(m p) t -> p m t", p=128)[:, :, cs], zst, [], reads=["zst"])
    ld(ph, "sp", "sx", xo_d.rearrange("(k p) t -> p k t", p=128), c.xT[:], [], reads=["xT"])
    return c.finish()


def mem_inputs(inp, l, b):
    wkv = np.asarray(inp["mem_w_kv"][l], np.float32)
    return dict(memT=_c(np.asarray(inp["mem"][b], np.float32).T), gkv=pk(inp["norm_mem_kv"][l]),
                Wkvk=wtiles(wkv[:, :1024], 512), Wkvv=wtiles(wkv[:, 1024:], 512),
                Wq=wtiles(inp["mem_w_q"][l], 512), Wo=wtiles(inp["mem_w_o"][l], 512))


def host_C(inp, resA, resB, xT_cores):
    maps = []
    gcols = _c(np.stack([pk(inp["norm_mem_q"][0]), pk(inp["norm_ffn"][0]), pk(inp["norm_mix"][1]), pk(inp["norm_mix"][1])], axis=1))
    shared = dict(gcols=gcols, Wout=wtiles(inp["even_w_out"][0], 512), Wg=wtiles(inp["ffn_w_gate"][0], 256),
                  Wu=wtiles(inp["ffn_w_up"][0], 256), Wd=wtiles(inp["ffn_w_down"][0], 128), Win=wtiles(inp["odd_w_in"][0], 512))
    mem = [mem_inputs(inp, 0, b) for b in range(2)]
    for core in range(8):
        b, i = core // 4, core % 4
        yb = np.concatenate([resB[b * 4 + h]["y"][:, i * T:(i + 1) * T] for h in range(4)], axis=0)
        ymix = np.concatenate([resA[core]["ya"], yb], axis=0)
        m = dict(xT=xT_cores[core], ymix=_c(ymix))
        m.update(shared)
        m.update(mem[b])
        maps.append(m)
    return maps


LAM_INIT = 0.8 - 0.6 * float(np.exp(-0.3 * 1))


def rel_bucket_static(rel):
    import jax.numpy as jnp
    import jax
    with jax.default_device(jax.devices("cpu")[0]):
        rel = jnp.asarray(rel, jnp.int32)
        n_half, max_exact = 16, 8
        ret = jnp.where(rel > 0, n_half, 0)
        n = jnp.abs(rel)
        nf = jnp.maximum(n, 1).astype(jnp.float32)
        large = max_exact + (jnp.log(nf / max_exact) / float(np.log(128 / max_exact)) * (n_half - max_exact)).astype(jnp.int32)
        large = jnp.minimum(large, n_half - 1)
        return np.asarray(ret + jnp.where(n < max_exact, n, large))


def diff_masks():
    p = np.arange(128)[:, None]
    f = np.arange(128)[None, :]
    out = {}
    for delta in (0, -1):
        rel = 128 * delta + p - f
        bk = rel_bucket_static(rel)
        ids = sorted(set(bk.reshape(-1).tolist()))
        masks = np.stack([(bk == b).astype(np.float32) for b in ids])
        out[delta] = (ids, masks)
    notallowed = ((p // 64) > (f // 64)).astype(np.float32)
    return out, notallowed


_DM = {}


def get_diff_masks():
    if "m" not in _DM:
        _DM["m"] = diff_masks()
    return _DM["m"]


def build_D(c=None, io=None):
    (dm, notallowed) = get_diff_masks()
    ids0, ids1 = dm[0][0], dm[-1][0]
    n0, n1 = len(ids0), len(ids1)
    if c is None:
        c = Ctx("D")
        io = dict(q=c.din("q", [128, S], BF16), k=c.din("k", [128, S], BF16), v=c.din("v", [128, NT, 128], BF16),
                  masks=c.din("masks", [128, n0 + n1 + 1, 128]), tab=c.din("tab", [128, 32]), lam=c.din("lam", [128, 4, 64]),
                  gain=c.din("gain", [128, 1]), y=c.dout("y", [128, S], BF16))
    nc, ph = c.nc, c.ph
    q_d, k_d, v_d, mk_d, tab_d, lam_d, gain_d, y_d = (io[n] for n in ("q", "k", "v", "masks", "tab", "lam", "gain", "y"))

    QT = c.sb("QT", [128, S], BF16)
    KT = [c.sb(f"KT{i}", [128, S], BF16) for i in range(2)]
    V = c.sb("V", [128, NT, 128], BF16)
    mk = c.sb("mk", [128, n0 + n1 + 1, 128])
    tab = c.sb("tab", [128, 32])
    lamp = c.sb("lamp", [128, 4, 64])
    gsc = c.sb("gsc", [128, 1])
    B0 = c.sb("B0", [128, 128])
    B1 = c.sb("B1", [128, 128])
    lprod = c.sb("lprod", [128, 2, 64])
    lsum = c.sb("lsum", [128, 2])
    neglam = c.sb("neglam", [128, 1])
    ones_bf = c.sb("ones", [128, 128], BF16)
    tb = [c.sb(f"tb{i}", [128, 128]) for i in range(2)]
    PT = [[c.sb(f"PT{s}{i}", [128, CH], BF16) for i in range(2)] for s in range(2)]
    rl = c.sb("rl", [128, CH])
    o1 = c.sb("o1", [128, CH])
    o2 = c.sb("o2", [128, CH])
    sqo = c.sb("sqo", [128, CH], BF16)
    rs = c.sb("rs", [128, CH])
    ost = [c.sb(f"ost{i}", [128, CH], BF16) for i in range(2)]
    psS = [[c.psum(f"psS{s}{i}") for i in range(2)] for s in range(2)]
    psO = [c.psum(f"psO{s}") for s in range(2)]
    psL = [c.psum(f"psL{s}") for s in range(2)]

    ld(ph, "sp", "l1", mk[:], mk_d, ["mk"])
    ld(ph, "sp", "l2", tab[:], tab_d, ["tab"])
    ld(ph, "sp", "l3", lamp[:], lam_d, ["lamp"])
    ld(ph, "sp", "l4", gsc[:], gain_d, ["gsc"])
    if isinstance(q_d, tuple):
        ld(ph, "sp", "lq", QT[0:64, :], q_d[0], ["QT"])
        ld(ph, "sp", "lq", QT[64:128, :], q_d[1], ["QT"])
        ld(ph, "act", "lk0", KT[0][0:64, :], k_d[0], ["KT0"])
        ld(ph, "act", "lk1", KT[1][64:128, :], k_d[1], ["KT1"])
    else:
        ld(ph, "sp", "lq", QT[:], q_d, ["QT"])
        ld(ph, "act", "lk0", KT[0][0:64, :], k_d[0:64, :], ["KT0"])
        ld(ph, "act", "lk1", KT[1][64:128, :], k_d[64:128, :], ["KT1"])
    ph.I("pool", "memset", KT[0][64:128, :], 0.0, writes=["KT0z"])
    ph.I("pool", "memset", KT[1][0:64, :], 0.0, writes=["KT1z"])
    ld(ph, "sp", "lv", V[:], v_d, ["V"])
    ph.I("dve", "memset", ones_bf[:], 1.0, writes=["ones"])
    ph.I("dve", "tensor_scalar", out=B0[:], in0=mk[:, n0 + n1, :], scalar1=NEG, scalar2=None, op0=ALU.mult, reads=["mk"], writes=["B0"])
    for n, b in enumerate(ids0):
        ph.I("dve", "scalar_tensor_tensor", out=B0[:], in0=mk[:, n, :], scalar=tab[:, b:b + 1], in1=B0[:], op0=ALU.mult, op1=ALU.add,
             reads=["mk", "tab", "B0"], writes=["B0"])
    ph.I("dve", "tensor_scalar", out=B1[:], in0=mk[:, n0, :], scalar1=tab[:, ids1[0]:ids1[0] + 1], scalar2=None, op0=ALU.mult,
         reads=["mk", "tab"], writes=["B1"])
    for n, b in enumerate(ids1[1:]):
        ph.I("dve", "scalar_tensor_tensor", out=B1[:], in0=mk[:, n0 + 1 + n, :], scalar=tab[:, b:b + 1], in1=B1[:], op0=ALU.mult, op1=ALU.add,
             reads=["mk", "tab", "B1"], writes=["B1"])
    ph.I("dve", "tensor_tensor", out=lprod[:, 0, :], in0=lamp[:, 0, :], in1=lamp[:, 1, :], op=ALU.mult, reads=["lamp"], writes=["lprod"])
    ph.I("dve", "tensor_tensor", out=lprod[:, 1, :], in0=lamp[:, 2, :], in1=lamp[:, 3, :], op=ALU.mult, reads=["lamp"], writes=["lprod"])
    ph.I("dve", "reduce_sum", out=lsum[:], in_=lprod[:], axis=AX.X, reads=["lprod"], writes=["lsum"])
    ph.I("act", "activation", out=lsum[:], in_=lsum[:], func=AF.Exp, reads=["lsum"], writes=["lsum"])
    ph.I("dve", "tensor_tensor", out=neglam[:], in0=lsum[:, 1:2], in1=lsum[:, 0:1], op=ALU.subtract, reads=["lsum"], writes=["neglam"])
    ph.I("dve", "tensor_scalar", out=neglam[:], in0=neglam[:], scalar1=-LAM_INIT, scalar2=None, op0=ALU.add, reads=["neglam"], writes=["neglam"])
    ph.I("dve", "tensor_scalar", out=gsc[:], in0=gsc[:], scalar1=1.0 - LAM_INIT, scalar2=None, op0=ALU.mult, reads=["gsc"], writes=["gsc"])

    scale = 64.0 ** -0.5
    it = 0
    for I in range(NG):
        qs = slice(I * CH, (I + 1) * CH)
        last = 4 * I + 3
        for j in range(last + 1):
            jj = max(j - 4 * I, 0)
            c0 = jj * 128
            for s in range(2):
                pS, pr = psS[s][it % 2], f"psS{s}{it%2}"
                P, sr = PT[s][it % 2], f"PT{s}{it%2}"
                ph.I("pe", "matmul", pS[:], lhsT=KT[s][:, j * 128:(j + 1) * 128], rhs=QT[:, qs], start=True, stop=True,
                     reads=[f"KT{s}", f"KT{s}z", "QT"], writes=[pr])
                if j <= 4 * I - 2:
                    ph.I("act", "activation", out=P[:], in_=pS[:], func=AF.Exp, scale=scale, bias=tab[:, 15:16], reads=[pr, "tab"], writes=[sr])
                else:
                    for ii in range(jj, 4):
                        delta = j - (4 * I + ii)
                        bs_ = slice(ii * 128, (ii + 1) * 128)
                        if delta <= -2:
                            ph.I("act", "activation", out=P[:, bs_], in_=pS[:, bs_], func=AF.Exp, scale=scale, bias=tab[:, 15:16],
                                 reads=[pr, "tab"], writes=[sr])
                        else:
                            Bt, br = (B0, "B0") if delta == 0 else (B1, "B1")
                            t = tb[(s + ii) % 2]
                            tr = f"tb{(s + ii) % 2}"
                            ph.I("dve", "scalar_tensor_tensor", out=t[:], in0=pS[:, bs_], scalar=scale, in1=Bt[:], op0=ALU.mult, op1=ALU.add,
                                 reads=[pr, br], writes=[tr])
                            ph.I("act", "activation", out=P[:, bs_], in_=t[:], func=AF.Exp, reads=[tr], writes=[sr])
                ph.I("pe", "matmul", psO[s][:, c0:], lhsT=V[:, j, :], rhs=P[:, c0:], start=(j == 0), stop=(j == last),
                     reads=[sr, "V"], writes=[f"psO{s}"])
                ph.I("pe", "matmul", psL[s][:, c0:], lhsT=ones_bf[:], rhs=P[:, c0:], start=(j == 0), stop=(j == last),
                     reads=[sr, "ones"], writes=[f"psL{s}"])
            it += 1
        ph.I("dve", "reciprocal", out=rl[:], in_=psL[0][:], reads=["psL0"], writes=["rl"])
        ph.I("dve", "tensor_tensor", out=o1[:], in0=psO[0][:], in1=rl[:], op=ALU.mult, reads=["psO0", "rl"], writes=["o1"])
        ph.I("dve", "reciprocal", out=rl[:], in_=psL[1][:], reads=["psL1"], writes=["rl"])
        ph.I("dve", "tensor_tensor", out=o2[:], in0=psO[1][:], in1=rl[:], op=ALU.mult, reads=["psO1", "rl"], writes=["o2"])
        ph.I("dve", "scalar_tensor_tensor", out=o1[:], in0=o2[:], scalar=neglam[:, 0:1], in1=o1[:], op0=ALU.mult, op1=ALU.add,
             reads=["o1", "o2", "neglam"], writes=["o1"])
        ph.I("act", "activation", out=sqo[:], in_=o1[:], func=AF.Square, reads=["o1"], writes=["sqo"])
        ph.I("pe", "matmul", psL[0][:], lhsT=ones_bf[:], rhs=sqo[:], start=True, stop=True, reads=["sqo", "ones"], writes=["psL0"])
        ph.I("act", "activation", out=rs[:], in_=psL[0][:], func=AF.Sqrt, scale=1.0 / 128.0, bias=EPS, reads=["psL0"], writes=["rs"])
        ph.I("dve", "reciprocal", out=rs[:], in_=rs[:], reads=["rs"], writes=["rs"])
        o = ost[I % 2]
        ph.I("dve", "scalar_tensor_tensor", out=o[:], in0=o1[:], scalar=gsc[:, 0:1], in1=rs[:], op0=ALU.mult, op1=ALU.mult,
             reads=["o1", "rs", "gsc"], writes=[f"ost{I%2}"])
        ld(ph, "sp", f"so{I%2}", y_d[:, qs], o[:], [], reads=[f"ost{I%2}"])
    return c.finish()


def host_D(inp, resC):
    (dm, notallowed) = get_diff_masks()
    masks = np.concatenate([dm[0][1], dm[-1][1], notallowed[None]], axis=0)
    masks = _c(masks.transpose(1, 0, 2))
    lam = _c(np.stack([rep(inp["diff_lambda_q1"][0]), rep(inp["diff_lambda_k1"][0]),
                       rep(inp["diff_lambda_q2"][0]), rep(inp["diff_lambda_k2"][0])], axis=1).astype(np.float32))
    maps = []
    for core in range(8):
        b, h = core // 4, core % 4
        qk = np.concatenate([resC[b * 4 + i]["qk"] for i in range(4)], axis=1)
        q = np.concatenate([qk[h * 64:(h + 1) * 64], qk[256 + h * 64:256 + (h + 1) * 64]], axis=0)
        k = np.concatenate([qk[512 + h * 64:512 + (h + 1) * 64], qk[768 + h * 64:768 + (h + 1) * 64]], axis=0)
        v = np.concatenate([resC[b * 4 + i]["v"][:, h * 128:(h + 1) * 128] for i in range(4)], axis=0)
        maps.append(dict(q=_c(q), k=_c(k), v=_c(v.reshape(NT, 128, 128).transpose(1, 0, 2)), masks=masks,
                         tab=rep(np.asarray(inp["rel_bias"], np.float32)[:, h]), lam=lam,
                         gain=_c(np.asarray(inp["diff_subln_gain"][0], np.float32)[h * 128:(h + 1) * 128, None])))
    return maps


NE = 8
NFE = 28


E_IN = dict(xT=([1024, T], F32), yc=([512, T], BF16), zext=([512, T + 2], F32), gb=([512, T], F32), convw=([128, 4, 3], F32),
            gcols=([128, 4, 8], F32), Wout=([2, 128, 8, 512], F32), memT=([1024, 256], F32), gkv=([128, 8], F32),
            Wkvk=([2, 128, 8, 512], F32), Wkvv=([2, 128, 8, 512], F32), Wq=([2, 128, 8, 512], F32), Wo=([2, 128, 8, 512], F32),
            Wr=([128, 8, 8], F32), ident=([128, 128], F32), Wg=([NE, 14, 128, 8, 256], F32), Wu=([NE, 14, 128, 8, 256], F32),
            Wd=([NE, 8, 128, NFE, 128], F32))


def build_E(stage=4, c=None, io=None):
    if c is None:
        c = Tok("E", arena_words=14336)
        io = {n: c.din(n, sh, dt) for n, (sh, dt) in E_IN.items()}
        io["out"] = c.dout("out", [1024, T])
    nc, ph = c.nc, c.ph
    (xT_d, yc_d, zx_d, gb_d, cw_d, gcols_d, Wout_d, memT_d, gkv_d, Wkvk_d, Wkvv_d, Wq_d, Wo_d, Wr_d, id_d, Wg_d, Wu_d, Wd_d) = (io[n] for n in E_IN)
    out_d = io["out"]
    sel_d = io.get("sel")
    fused = sel_d is not None

    cw = c.sb("cw", [128, 4, 3])
    zc = c.arena[:, 5120:5120 + 4 * (CH + 2)].rearrange("p (m t) -> p m t", t=CH + 2)
    gbc = c.arena[:, 7680:9728].rearrange("p (m t) -> p m t", t=CH)
    acc = c.arena[:, 10752:11264]
    ident = c.sb("ident", [128, 128])
    ones32 = c.sb("ones32", [128, 128])
    wr32 = c.sb("wr32", [128, 8, 8])
    wrh = c.sb("wrh", [128, 8, 8], BF16)
    wrl = c.sb("wrl", [128, 8, 8], BF16)
    y1w = c.ymb[1][:].rearrange("p k t -> p (k t)").bitcast(F32)
    y0w = c.ymb[0][:].rearrange("p k t -> p (k t)").bitcast(F32)
    t32 = [y1w[:, i * CH:(i + 1) * CH] for i in range(2)]
    lg = c.sb("lg", [128, 8])
    mx8 = c.sb("mx8", [128, 8])
    nv1 = c.sb("nv1", [128, 1])
    ex = c.sb("ex", [128, 8])
    msk = c.sb("msk", [128, 8])
    den = c.sb("den", [128, 1])
    comb = c.sb("comb", [128, MC // 128, 8])
    dg = [c.sb(f"dg{i}", [128, 128]) for i in range(2)]
    cbc = [y0w[:, i * MC:(i + 1) * MC] for i in range(2)]
    tmp = [y1w[:, (2 + i) * CH:(3 + i) * CH] for i in range(2)]

    def sel_load(dst, srcs, stg, tag, res, queue="sp"):
        for b_ in range(4):
            sb_ = stg[b_ % len(stg)]
            sr_ = f"{tag}stg{b_ % len(stg)}"
            ld(ph, queue, f"l{tag}{b_ % len(stg)}", sb_, srcs[b_], [sr_])
            if b_ == 0:
                ph.I("dve", "tensor_scalar", out=dst, in0=sb_, scalar1=sel[:, 0:1], scalar2=None, op0=ALU.mult, reads=[sr_, "sel"], writes=[res])
            else:
                ph.I("dve", "scalar_tensor_tensor", out=dst, in0=sb_, scalar=sel[:, b_:b_ + 1], in1=dst, op0=ALU.mult, op1=ALU.add,
                     reads=[sr_, "sel", res], writes=[res])

    if fused:
        sel = c.sb("sel", [128, 4])
        ld(ph, "sp", "lsel", sel[:], sel_d, ["sel"])
        xstg = [c.arena[:, i * T:(i + 1) * T] for i in range(2)]
        for k in range(8):
            sel_load(c.xT[:, k, :], [xb.rearrange("(k p) t -> p k t", p=128)[:, k, :] for xb in xT_d], xstg, "x", "xT")
    else:
        ld(ph, "sp", "lx", c.xT[:], xT_d.rearrange("(k p) t -> p k t", p=128), ["xT"])
    ld(ph, "sp", "lg", c.gcols[:], gcols_d, ["gcol"])
    ld(ph, "sp", "lcw", cw[:], cw_d, ["cw"])
    ld(ph, "sp", "lid", ident[:], id_d, ["ident"])
    ld(ph, "sp", "lwr", wr32[:], Wr_d, ["wr32"])
    ph.I("dve", "memset", ones32[:], 1.0, writes=["ones32"])
    ph.I("dve", "tensor_copy", out=wrh[:], in_=wr32[:], reads=["wr32"], writes=["wrh"])
    ph.I("dve", "tensor_tensor", out=wrl[:], in0=wr32[:], in1=wrh[:], op=ALU.subtract, reads=["wr32", "wrh"], writes=["wrl"])

    fm = lambda ap: ap.rearrange("(k p) t -> p k t", p=128)
    if fused:
        zstg = [c.arena[:, i * 2056:(i + 1) * 2056].rearrange("p (m t) -> p m t", t=CH + 2) for i in range(2)]
        gstg = [c.arena[:, 11264:13312].rearrange("p (m t) -> p m t", t=CH)]
        ystg = [c.arena[:, 13312:14336].bitcast(BF16).rearrange("p (m t) -> p m t", t=CH)]
        dmy = c.sb("dmy", [128, 1])
        ph.I("dve", "memset", dmy[:], 0.0, reads=["xstg0", "xstg1"], writes=["zstg0", "zstg1"])
    for tc in range(NCH):
        yb = c.ymb[tc % 2]
        yr = f"ymb{tc%2}"
        if fused:
            sel_load(yb[:, 0:4, :], [fm(a)[:, :, tc * CH:(tc + 1) * CH] for a in yc_d], ystg, "y", yr)
            sel_load(zc, [fm(a)[:, :, tc * CH: tc * CH + CH + 2] for a in zx_d], zstg, "z", "zc")
            sel_load(gbc, [fm(a)[:, :, tc * CH:(tc + 1) * CH] for a in gb_d], gstg, "g", "gbc")
        else:
            ld(ph, "sp", f"lym{tc%2}", yb[:, 0:4, :], fm(yc_d)[:, :, tc * CH:(tc + 1) * CH], [yr])
            ld(ph, "sp", "lz", zc, fm(zx_d)[:, :, tc * CH: tc * CH + CH + 2], ["zc"])
            ld(ph, "sp", "lgb", gbc, fm(gb_d)[:, :, tc * CH:(tc + 1) * CH], ["gbc"])
        for m in range(4):
            ph.I("dve", "tensor_scalar", out=acc, in0=zc[:, m, 2:CH + 2], scalar1=cw[:, m, 2:3], scalar2=None, op0=ALU.mult,
                 reads=["zc", "cw"], writes=["acc"])
            ph.I("dve", "scalar_tensor_tensor", out=acc, in0=zc[:, m, 1:CH + 1], scalar=cw[:, m, 1:2], in1=acc, op0=ALU.mult, op1=ALU.add,
                 reads=["zc", "cw", "acc"], writes=["acc"])
            ph.I("dve", "scalar_tensor_tensor", out=acc, in0=zc[:, m, 0:CH], scalar=cw[:, m, 0:1], in1=acc, op0=ALU.mult, op1=ALU.add,
                 reads=["zc", "cw", "acc"], writes=["acc"])
            ph.I("dve", "tensor_tensor", out=yb[:, 4 + m, :], in0=acc, in1=gbc[:, m, :], op=ALU.mult, reads=["acc", "gbc"], writes=[yr])
        c.linear_fm(Wout_d, 8, 512, yb, yr, CH, c.resid_add(tc * CH))

    if stage >= 2:
        c.mem_setup(memT_d, gkv_d, Wkvk_d, Wkvv_d)
        c.mem_attn(0, Wq_d, Wo_d)

    hlo = c.xq if stage >= 2 else None
    for mc in range(T // MC if stage >= 3 else 0):
        for ts in range(MC // CH):
            cs = slice(mc * MC + ts * CH, mc * MC + (ts + 1) * CH)
            hs = slice(ts * CH, (ts + 1) * CH)
            for k in range(8):
                sqk = c.sq[k % 2]
                ph.I("act", "activation", out=sqk[:], in_=c.xT[:, k, cs], func=AF.Square, reads=["xT"], writes=[f"nsq{k%2}"])
                ph.I("pe", "matmul", c.psN[:], lhsT=c.ones_bf[:], rhs=sqk[:], start=(k == 0), stop=(k == 7), reads=[f"nsq{k%2}", "ones"], writes=["npsN"])
            ph.I("act", "activation", out=c.rstd_s[:], in_=c.psN[:], func=AF.Sqrt, scale=1.0 / 1024.0, bias=EPS, reads=["npsN"], writes=["nrstd_s"])
            ph.I("dve", "reciprocal", out=c.rstd[:], in_=c.rstd_s[:], reads=["nrstd_s"], writes=["nrstd"])
            for k in range(8):
                t = t32[k % 2]
                tr = f"t32{k%2}"
                ph.I("dve", "scalar_tensor_tensor", out=t, in0=c.xT[:, k, cs], scalar=c.gcols[:, 1, k:k + 1], in1=c.rstd[:], op0=ALU.mult, op1=ALU.mult,
                     reads=["xT", "nrstd", "gcol"], writes=[tr])
                ph.I("act", "activation", out=c.hT[:, k, hs], in_=t, func=AF.Copy, reads=[tr], writes=["hT"])
                ph.I("dve", "tensor_tensor", out=hlo[:, k, :], in0=t, in1=c.hT[:, k, hs], op=ALU.subtract, reads=[tr, "hT"], writes=["xq"])
            for s4 in range(4):
                sub = slice(ts * CH + s4 * 128, ts * CH + (s4 + 1) * 128)
                subl = slice(s4 * 128, (s4 + 1) * 128)
                ps, pres = c.nextps()
                n = 0
                for (a_, ar_, sl_, b_, br_) in ((c.hT, "hT", sub, wrh, "wrh"), (hlo, "xq", subl, wrh, "wrh"), (c.hT, "hT", sub, wrl, "wrl")):
                    for k in range(8):
                        ph.I("pe", "matmul", ps[:, 0:8], lhsT=a_[:, k, sl_], rhs=b_[:, k, :], start=(n == 0), stop=(n == 23), reads=[ar_, br_], writes=[pres])
                        n += 1
                ph.I("dve", "tensor_copy", out=lg[:], in_=ps[:, 0:8], reads=[pres], writes=["lg"])
                ph.I("dve", "max", out=mx8[:], in_=lg[:], reads=["lg"], writes=["mx8"])
                ph.I("dve", "tensor_scalar", out=nv1[:], in0=mx8[:, 0:1], scalar1=-1.0, scalar2=None, op0=ALU.mult, reads=["mx8"], writes=["nv1"])
                ph.I("act", "activation", out=ex[:], in_=lg[:], func=AF.Exp, bias=nv1[:, 0:1], reads=["lg", "nv1"], writes=["ex"])
                ph.I("dve", "tensor_scalar", out=msk[:], in0=lg[:], scalar1=mx8[:, 1:2], scalar2=None, op0=ALU.is_ge, reads=["lg", "mx8"], writes=["msk"])
                ph.I("dve", "tensor_tensor", out=ex[:], in0=ex[:], in1=msk[:], op=ALU.mult, reads=["ex", "msk"], writes=["ex"])
                ph.I("dve", "reduce_sum", out=den[:], in_=ex[:], axis=AX.X, reads=["ex"], writes=["den"])
                ph.I("dve", "reciprocal", out=den[:], in_=den[:], reads=["den"], writes=["den"])
                ph.I("dve", "tensor_scalar", out=comb[:, ts * 4 + s4, :], in0=ex[:], scalar1=den[:, 0:1], scalar2=None, op0=ALU.mult,
                     reads=["ex", "den"], writes=["comb"])
        for e in range(NE):
            cb = cbc[e % 2]
            cbr = f"cbc{e%2}"
            for s8 in range(MC // 128):
                d_ = dg[s8 % 2]
                dr = f"dg{s8%2}"
                ph.I("dve", "tensor_scalar", out=d_[:], in0=ident[:], scalar1=comb[:, s8, e:e + 1], scalar2=None, op0=ALU.mult,
                     reads=["ident", "comb"], writes=[dr])
                ph.I("pe", "matmul", c.psL[:, (s8 % 4) * 128:(s8 % 4 + 1) * 128], lhsT=ones32[:], rhs=d_[:], start=True, stop=True,
                     reads=[dr, "ones32"], writes=["psL"])
                if s8 % 4 == 3:
                    ph.I("act", "activation", out=cb[:, (s8 // 4) * CH:(s8 // 4 + 1) * CH], in_=c.psL[:], func=AF.Copy, reads=["psL"], writes=[cbr])
            for cg in range(14):
                wg, wgr = c.wload(Wg_d[e, cg], 8, 256)
                wu, wur = c.wload(Wu_d[e, cg], 8, 256)
                for mi in range(2):
                    for ts in range(MC // CH):
                        psG, pgr = c.nextps()
                        psU, pur = c.nextps()
                        for k in range(8):
                            ph.I("pe", "matmul", psG[:], lhsT=wg[:, k, mi * 128:(mi + 1) * 128], rhs=c.hT[:, k, ts * CH:(ts + 1) * CH],
                                 start=(k == 0), stop=(k == 7), reads=[wgr, "hT"], writes=[pgr])
                        for k in range(8):
                            ph.I("pe", "matmul", psU[:], lhsT=wu[:, k, mi * 128:(mi + 1) * 128], rhs=c.hT[:, k, ts * CH:(ts + 1) * CH],
                                 start=(k == 0), stop=(k == 7), reads=[wur, "hT"], writes=[pur])
                        sg = c.sg[(mi * 2 + ts) % 2]
                        sgr = f"sg{(mi * 2 + ts) % 2}"
                        ph.I("act", "activation", out=sg[:], in_=psG[:], func=AF.Silu, reads=[pgr], writes=[sgr])
                        ph.I("dve", "tensor_tensor", out=c.actT[:, cg * 2 + mi, ts * CH:(ts + 1) * CH], in0=sg[:], in1=psU[:], op=ALU.mult,
                             reads=[sgr, pur], writes=["actT"])

            def ev_down(m, ts, ps, pres, cb=cb, cbr=cbr, mc=mc):
                tm = tmp[(m * 2 + ts) % 2]
                tmr = f"tmp{(m * 2 + ts) % 2}"
                sl = slice(mc * MC + ts * CH, mc * MC + (ts + 1) * CH)
                ph.I("dve", "tensor_tensor", out=tm, in0=ps[:], in1=cb[:, ts * CH:(ts + 1) * CH], op=ALU.mult, reads=[pres, cbr], writes=[tmr])
                ph.I("pool", "tensor_tensor", out=c.xT[:, m, sl], in0=c.xT[:, m, sl], in1=tm, op=ALU.add, reads=[tmr, "xT"], writes=["xT"])
            c.linear_fm(Wd_d[e], NFE, 128, c.actT, "actT", MC, ev_down)

    fo = c.arena[:, 0:4096].rearrange("p (k t) -> p k t", t=CH)
    ph.I("dve", "memset", acc[:, 0:1], 0.0, reads=["actT"], writes=["fo"])
    if stage < 4:
        ld(ph, "sp", "sout", out_d.rearrange("(k p) t -> p k t", p=128), c.xT[:], [], reads=["xT"])
    for tc in range(NCH if stage >= 4 else 0):
        cs = slice(tc * CH, (tc + 1) * CH)
        c.norm(2, cs, fo, "fo")
        ld(ph, "sp", "sout", out_d.rearrange("(k p) t -> p k t", p=128)[:, :, cs], fo, [], reads=["fo"])
    return c.finish()


def host_E(inp, resC, resD):
    gcols = _c(np.stack([pk(inp["norm_mem_q"][1]), pk(inp["norm_ffn"][1]), pk(inp["norm_final"]), pk(inp["norm_final"])], axis=1))
    cw = np.asarray(inp["conv_w"][0], np.float32)
    convw = _c(cw.reshape(3, 4, 128).transpose(2, 1, 0))
    Wg = np.stack([wtiles(inp["moe_w_gate"][0][e], 256) for e in range(NE)])
    Wu = np.stack([wtiles(inp["moe_w_up"][0][e], 256) for e in range(NE)])
    Wd = np.stack([wtiles(inp["moe_w_down"][0][e], 128) for e in range(NE)])
    shared = dict(gcols=gcols, convw=convw, Wout=wtiles(inp["odd_w_out"][0], 512),
                  Wr=_c(np.asarray(inp["router_w"][0], np.float32).reshape(8, 128, 8).transpose(1, 0, 2)),
                  ident=np.eye(128, dtype=np.float32), Wg=Wg, Wu=Wu, Wd=Wd)
    mem = [mem_inputs(inp, 1, b) for b in range(2)]
    maps = []
    for core in range(8):
        b, i = core // 4, core % 4
        yc = np.concatenate([resD[b * 4 + h]["y"][:, i * T:(i + 1) * T] for h in range(4)], axis=0)
        z = resC[core]["z"]
        halo = resC[core - 1]["z"][:, T - 2:] if i > 0 else np.zeros((512, 2), np.float32)
        m = dict(xT=resC[core]["xo"], yc=_c(yc), zext=_c(np.concatenate([halo, z], axis=1)), gb=resC[core]["gb"])
        m.update(shared)
        m.update(mem[b])
        maps.append(m)
    return maps


def build_fused():
    nc = bass.Bass("TRN2", target_bir_lowering=False)
    (dm, notallowed) = get_diff_masks()
    nmask = len(dm[0][0]) + len(dm[-1][0]) + 1

    def din(name, shape, dt=F32):
        return nc.dram_tensor(name, list(shape), dt, kind="ExternalInput").ap()

    def scr(name, shape, dt=F32):
        return nc.dram_tensor(name, list(shape), dt, kind="Internal").ap()

    x_d = din("x", [1024, S])
    A = dict(g=din("A_g", [128, 8]), w=din("A_w", [1024, 2564]), vgain=din("A_vgain", [128, 512]), ws=din("A_ws", [128, 4, 128]),
             bs=din("A_bs", [128, 4, 512]))
    B = dict(bf=din("B_bf", [4, 64, 1]), ident=din("B_ident", [64, 64]), lmat=din("B_lmat", [64, 64]), tri=din("B_tri", [128, 128]))
    C = {n: din("C_" + n, sh, dt) for n, (sh, dt) in C_IN.items() if n not in ("xT", "ymix")}
    D = dict(masks=din("D_masks", [128, nmask, 128]), tab=din("D_tab", [4, 128, 32]), lam=din("D_lam", [128, 4, 64]),
             gain=din("D_gain", [4, 128, 1]))
    E = {n: din("E_" + n, sh, dt) for n, (sh, dt) in E_IN.items() if n not in ("xT", "yc", "zext", "gb")}
    sel_d = din("E_sel", [128, 4])
    out_d = nc.dram_tensor("out", [1024, T], F32, kind="ExternalOutput").ap()

    q_s, k_s, ya_s, yb_s, yc_s = (scr(n, [512, S], BF16) for n in ("q_s", "k_s", "ya_s", "yb_s", "yc_s"))
    v_s, v1_s = scr("v_s", [S, 512], BF16), scr("v1_s", [S, 512], BF16)
    f_s = scr("f_s", [4, S])
    x1_s = scr("x1_s", [1024, S])
    qk_s = scr("qk_s", [1024, S], BF16)
    z_s = scr("z_s", [512, S + 2])
    gb_s = scr("gb_s", [512, S])

    c0 = Ctx("Z", nc=nc)
    zt = c0.sb("zt", [128, 4, 2])
    c0.ph.I("dve", "memset", zt[:], 0.0, writes=["zt"])
    ld(c0.ph, "sp", "sz", z_s.rearrange("(m p) t -> p m t", p=128)[:, :, 0:2], zt[:], [], reads=["zt"])
    c0.finish()

    for blk in range(4):
        cols = slice(blk * T, (blk + 1) * T)
        io = dict(A)
        io.update(xT=x_d[:, cols], ya=ya_s[:, cols], q=q_s[:, cols], k=k_s[:, cols], v=v_s[cols, :], f=f_s[:, cols])
        build_A(Ctx(f"A{blk}", nc=nc), io)
    for h in range(4):
        rows = slice(h * 128, (h + 1) * 128)
        io = dict(q=q_s[rows, :], k=k_s[rows, :], v=v_s.rearrange("(j p) c -> p j c", p=128)[:, :, rows],
                  f2=f_s[h].rearrange("(j p) -> j p", p=128), bf=B["bf"][h], ident=B["ident"], lmat=B["lmat"], tri=B["tri"], y=yb_s[rows, :])
        build_B(Ctx(f"B{h}", nc=nc), io)
    for blk in range(4):
        cols = slice(blk * T, (blk + 1) * T)
        io = dict(C)
        io.update(xT=x_d[:, cols], ymix=(ya_s[:, cols], yb_s[:, cols]), xo=x1_s[:, cols], qk=qk_s[:, cols], v=v1_s[cols, :],
                  z=z_s[:, 2 + blk * T: 2 + (blk + 1) * T], gb=gb_s[:, cols])
        build_C(Tok(f"C{blk}", nc=nc), io)
    for h in range(4):
        r64 = lambda base: qk_s[base + h * 64: base + (h + 1) * 64, :]
        io = dict(q=(r64(0), r64(256)), k=(r64(512), r64(768)), v=v1_s.rearrange("(j p) c -> p j c", p=128)[:, :, h * 128:(h + 1) * 128],
                  masks=D["masks"], tab=D["tab"][h], lam=D["lam"], gain=D["gain"][h], y=yc_s[h * 128:(h + 1) * 128, :])
        build_D(Ctx(f"D{h}", nc=nc), io)
    io = dict(E)
    io.update(xT=[x1_s[:, b * T:(b + 1) * T] for b in range(4)], yc=[yc_s[:, b * T:(b + 1) * T] for b in range(4)],
              zext=[z_s[:, b * T: b * T + T + 2] for b in range(4)], gb=[gb_s[:, b * T:(b + 1) * T] for b in range(4)],
              sel=sel_d, out=out_d)
    build_E(4, Tok("E", arena_words=14336, nc=nc), io)
    return nc


def host_fused(inp):
    x = np.asarray(inp["x"], np.float32)
    (dm, notallowed) = get_diff_masks()
    masks = _c(np.concatenate([dm[0][1], dm[-1][1], notallowed[None]], axis=0).transpose(1, 0, 2))
    bs4 = rep(np.tile(np.asarray(inp["gmlp_b_s"][0], np.float32), (1, 4)))
    sh = {
        "A_g": pk(inp["norm_mix"][0]), "A_w": _c(np.asarray(inp["even_w_in"][0], np.float32)),
        "A_vgain": rep(np.asarray(inp["gmlp_v_gain"][0], np.float32)),
        "A_ws": _c(np.transpose(np.asarray(inp["gmlp_w_s"][0], np.float32), (2, 0, 1))), "A_bs": bs4,
        "B_bf": _c(np.broadcast_to(np.asarray(inp["fox_b_f"][0], np.float32)[:, None, None], (4, 64, 1))),
        "B_ident": np.eye(64, dtype=np.float32), "B_lmat": np.triu(np.ones((64, 64), np.float32), 1),
        "B_tri": np.triu(np.ones((128, 128), np.float32), 0),
        "C_gcols": _c(np.stack([pk(inp["norm_mem_q"][0]), pk(inp["norm_ffn"][0]), pk(inp["norm_mix"][1]), pk(inp["norm_mix"][1])], axis=1)),
        "C_Wout": wtiles(inp["even_w_out"][0], 512), "C_Wg": wtiles(inp["ffn_w_gate"][0], 256), "C_Wu": wtiles(inp["ffn_w_up"][0], 256),
        "C_Wd": wtiles(inp["ffn_w_down"][0], 128), "C_Win": wtiles(inp["odd_w_in"][0], 512),
        "D_masks": masks,
        "D_tab": _c(np.stack([rep(np.asarray(inp["rel_bias"], np.float32)[:, h]) for h in range(4)])),
        "D_lam": _c(np.stack([rep(inp["diff_lambda_q1"][0]), rep(inp["diff_lambda_k1"][0]),
                              rep(inp["diff_lambda_q2"][0]), rep(inp["diff_lambda_k2"][0])], axis=1).astype(np.float32)),
        "D_gain": _c(np.asarray(inp["diff_subln_gain"][0], np.float32).reshape(4, 128, 1)),
        "E_convw": _c(np.asarray(inp["conv_w"][0], np.float32).reshape(3, 4, 128).transpose(2, 1, 0)),
        "E_gcols": _c(np.stack([pk(inp["norm_mem_q"][1]), pk(inp["norm_ffn"][1]), pk(inp["norm_final"]), pk(inp["norm_final"])], axis=1)),
        "E_Wout": wtiles(inp["odd_w_out"][0], 512),
        "E_Wr": _c(np.asarray(inp["router_w"][0], np.float32).reshape(8, 128, 8).transpose(1, 0, 2)),
        "E_ident": np.eye(128, dtype=np.float32),
        "E_Wg": np.stack([wtiles(inp["moe_w_gate"][0][e], 256) for e in range(NE)]),
        "E_Wu": np.stack([wtiles(inp["moe_w_up"][0][e], 256) for e in range(NE)]),
        "E_Wd": np.stack([wtiles(inp["moe_w_down"][0][e], 128) for e in range(NE)]),
    }
    memC = [mem_inputs(inp, 0, b) for b in range(2)]
    memE = [mem_inputs(inp, 1, b) for b in range(2)]
    xT = [_c(x[b].T) for b in range(2)]
    maps = []
    for core in range(8):
        b, i = core // 4, core % 4
        m = dict(sh)
        m["x"] = xT[b]
        for k_, v_ in memC[b].items():
            m["C_" + k_] = v_
        for k_, v_ in memE[b].items():
            m["E_" + k_] = v_
        selv = np.zeros((128, 4), np.float32)
        selv[:, i] = 1.0
        m["E_sel"] = selv
        maps.append(m)
    return maps


def kernel(**inp):
    inp = {k: np.asarray(v) for k, v in inp.items()}
    res = run(get_nc("F", build_fused), host_fused(inp))
    out = np.empty((2, 8192, 1024), np.float32)
    for core in range(8):
        b, i = core // 4, core % 4
        out[b, i * T:(i + 1) * T] = res[core]["out"].T
    return out
```

```python
import contextlib
from concourse.bass_utils import run_bass_kernel_spmd
import numpy as np
import concourse.bass as bass
import concourse.mybir as mybir

F32 = mybir.dt.float32
BF16 = mybir.dt.bfloat16
F32R = mybir.dt.float32r
I32 = mybir.dt.int32
U32 = mybir.dt.uint32
AF = mybir.ActivationFunctionType
ALU = mybir.AluOpType
AX = mybir.AxisListType

ENGS = ("pe", "act", "dve", "pool", "sp")


class Phase:
    def __init__(self, nc, name):
        self.nc = nc
        self.name = name
        self.ops = []
        self.last_writer = {}
        self.readers = {}
        self.dma_keys = {}

    def _add(self, eng, fn, reads, writes, dma_key=None, ndma=0):
        idx = len(self.ops)
        deps = set()
        for r in reads:
            w = self.last_writer.get(r)
            if w is not None:
                deps.add(w)
        for r in writes:
            w = self.last_writer.get(r)
            if w is not None:
                deps.add(w)
            for rd in self.readers.get(r, ()):
                deps.add(rd)
        deps.discard(idx)
        for r in writes:
            self.last_writer[r] = idx
            self.readers[r] = []
        for r in reads:
            if r not in writes:
                self.readers.setdefault(r, []).append(idx)
        self.ops.append(dict(eng=eng, fn=fn, deps=deps, dma_key=dma_key, ndma=ndma,
                             signal=False))
        return idx

    def op(self, eng, fn, reads=(), writes=()):
        return self._add(eng, fn, tuple(reads), tuple(writes))

    def I(self, eng, meth, *a, reads=(), writes=(), **kw):
        return self.op(eng, lambda e: getattr(e, meth)(*a, **kw), reads, writes)

    def dma(self, queue, fn, key, reads=(), writes=(), n=1):
        return self._add(queue, fn, tuple(reads), tuple(writes), dma_key=key, ndma=n)

    def emit(self):
        nc = self.nc
        ops = self.ops
        pos = {}
        cnt = {e: 0 for e in ENGS}
        for i, o in enumerate(ops):
            pos[i] = cnt[o["eng"]]
            cnt[o["eng"]] += 1
        for i, o in enumerate(ops):
            need = set()
            for d in o["deps"]:
                po = ops[d]
                if po["dma_key"] is not None:
                    need.add(d)
                elif po["eng"] == o["eng"]:
                    if o["eng"] != "pe" and pos[i] - pos[d] <= 2 and o["dma_key"] is None:
                        need.add(d)
                    elif o["dma_key"] is not None:
                        need.add(d)
                else:
                    need.add(d)
            o["need"] = need
            for d in need:
                if ops[d]["dma_key"] is None:
                    ops[d]["signal"] = True
        sigcnt = {e: 0 for e in ENGS}
        for o in ops:
            if o["dma_key"] is None and o["signal"]:
                sigcnt[o["eng"]] += 1
                o["sigval"] = sigcnt[o["eng"]]
        dmacnt = {}
        for o in ops:
            if o["dma_key"] is not None:
                dmacnt[o["dma_key"]] = dmacnt.get(o["dma_key"], 0) + o["ndma"]
                o["sigval"] = 16 * dmacnt[o["dma_key"]]
        keys = sorted(dmacnt.keys(), key=str)
        import contextlib
        esem = {e: nc.alloc_semaphore(name=f"{self.name}_{e}") for e in ENGS}
        dsem = {k: nc.alloc_semaphore(name=f"{self.name}_d{j}") for j, k in enumerate(keys)}
        with contextlib.ExitStack() as st:
            block = st.enter_context(nc.Block())
            per_eng = {e: [] for e in ENGS}
            for i, o in enumerate(ops):
                per_eng[o["eng"]].append(i)

            def run(engname, eng):
                seen = {}
                for i in per_eng[engname]:
                    o = ops[i]
                    waits = {}
                    for d in o["need"]:
                        po = ops[d]
                        if po["dma_key"] is not None:
                            k = ("d", po["dma_key"])
                        else:
                            k = ("e", po["eng"])
                        waits[k] = max(waits.get(k, 0), po["sigval"])
                    for k, v in waits.items():
                        if seen.get(k, 0) >= v:
                            continue
                        seen[k] = v
                        s = dsem[k[1]] if k[0] == "d" else esem[k[1]]
                        eng.wait_ge(s, v)
                    if o["dma_key"] is not None:
                        o["fn"](eng, dsem[o["dma_key"]])
                    else:
                        ins = o["fn"](eng)
                        if o["signal"]:
                            ins.then_inc(esem[engname], 1)
                fin = {}
                for i in per_eng[engname]:
                    o = ops[i]
                    if o["dma_key"] is not None:
                        fin[o["dma_key"]] = max(fin.get(o["dma_key"], 0), o["sigval"])
                for k, v in fin.items():
                    if seen.get(("d", k), 0) < v:
                        eng.wait_ge(dsem[k], v)

            @block.tensor
            def _(e):
                run("pe", e)

            @block.scalar
            def _(e):
                run("act", e)

            @block.vector
            def _(e):
                run("dve", e)

            @block.gpsimd
            def _(e):
                run("pool", e)

            @block.sync
            def _(e):
                run("sp", e)
        nc.clear_and_free_semaphores(list(esem.values()) + list(dsem.values()))
        nc.all_engine_barrier()


T = 2048
CH = 512
NCH = T // CH
EPS = 1e-6
NEG = -30000.0


class Ctx:
    def __init__(self, name, nc=None):
        self.nc = nc if nc is not None else bass.Bass("TRN2", target_bir_lowering=False)
        self.name = name
        self.st = contextlib.ExitStack()
        self.ph = Phase(self.nc, name)
        self.n = 0

    def din(self, name, shape, dt=F32):
        return self.nc.dram_tensor(name, list(shape), dt, kind="ExternalInput").ap()

    def dout(self, name, shape, dt=F32):
        return self.nc.dram_tensor(name, list(shape), dt, kind="ExternalOutput").ap()

    def sb(self, name, shape, dt=F32):
        return self.st.enter_context(self.nc.sbuf_tensor("s_" + self.name + "_" + name, list(shape), dt))

    def psum(self, name):
        return self.st.enter_context(self.nc.psum_tensor("p_" + self.name + "_" + name, [128, 512], F32))

    def finish(self):
        self.ph.emit()
        self.st.close()
        return self.nc


def ld(ph, queue, key, out_ap, in_ap, writes, reads=()):
    ph.dma(queue, lambda e, s: e.dma_start(out=out_ap, in_=in_ap).then_inc(s, 16), key,
           reads=reads, writes=writes)


def rmsnorm_fm(ph, tag, src, cs, gcol, xn, ones_bf, sq, psN, rstd_s, rstd, src_res, xn_res,
               nk=8, dim=1024.0):
    for k in range(nk):
        sqk = sq[k % 2]
        ph.op("act", (lambda k, sqk: lambda e: e.activation(out=sqk[:], in_=src[:, k, cs], func=AF.Square))(k, sqk),
              reads=[src_res], writes=[f"{tag}sq{k%2}"])
        ph.op("pe", (lambda k, sqk: lambda e: e.matmul(psN[:], lhsT=ones_bf[:], rhs=sqk[:], start=(k == 0), stop=(k == nk - 1)))(k, sqk),
              reads=[f"{tag}sq{k%2}", "ones"], writes=[f"{tag}psN"])
    ph.op("act", lambda e: e.activation(out=rstd_s[:], in_=psN[:], func=AF.Sqrt, scale=1.0 / dim, bias=EPS),
          reads=[f"{tag}psN"], writes=[f"{tag}rstd_s"])
    ph.op("dve", lambda e: e.reciprocal(out=rstd[:], in_=rstd_s[:]), reads=[f"{tag}rstd_s"], writes=[f"{tag}rstd"])
    for k in range(nk):
        ph.op("dve", (lambda k: lambda e: e.scalar_tensor_tensor(out=xn[:, k, :], in0=src[:, k, cs], scalar=gcol[:, k:k + 1],
                                                                  in1=rstd[:], op0=ALU.mult, op1=ALU.mult))(k),
              reads=[src_res, f"{tag}rstd", "gcol"], writes=[xn_res])


def build_A(c=None, io=None):
    if c is None:
        c = Ctx("A")
        io = dict(xT=c.din("xT", [1024, T]), g=c.din("g", [128, 8]), w=c.din("w", [1024, 2564]), vgain=c.din("vgain", [128, 512]),
                  ws=c.din("ws", [128, 4, 128]), bs=c.din("bs", [128, 4, 512]),
                  ya=c.dout("ya", [512, T], BF16), q=c.dout("q", [512, T], BF16), k=c.dout("k", [512, T], BF16),
                  v=c.dout("v", [T, 512], BF16), f=c.dout("f", [4, T], F32))
    nc, ph = c.nc, c.ph
    xT_d, g_d, w_d, vg_d, ws_d, bs_d = io["xT"], io["g"], io["w"], io["vgain"], io["ws"], io["bs"]
    ya_d, q_d, k_d, v_d, f_d = io["ya"], io["q"], io["k"], io["v"], io["f"]

    xT = c.sb("xT", [128, 8, T])
    w = c.sb("w", [128, 8, 2564], BF16)
    gcol = c.sb("gcol", [128, 8])
    ones_bf = c.sb("ones", [128, 128], BF16)
    sq = [c.sb(f"sq{i}", [128, CH], BF16) for i in range(2)]
    rstd_s = c.sb("rstd_s", [128, CH])
    rstd = c.sb("rstd", [128, CH])
    xn = c.sb("xn", [128, 8, CH], BF16)
    uT = c.sb("uT", [128, 4, CH])
    vgain = c.sb("vgain", [128, 512])
    wsm = c.sb("wsm", [128, 4, 128], BF16)
    bsb = c.sb("bsb", [128, 4, 512])
    vg = c.sb("vg", [128, 512])
    vscr = c.sb("vscr", [128, 128])
    ss = c.sb("ss", [128, 4])
    ssr = c.sb("ssr", [128, 4])
    vn = [c.sb(f"vn{i}", [128, 512], BF16) for i in range(2)]
    tmix = c.sb("tmix", [128, 512])
    yast = c.sb("yast", [128, 4, CH], BF16)
    qst = c.sb("qst", [128, 4, CH], BF16)
    kst = c.sb("kst", [128, 4, CH], BF16)
    vst = c.sb("vst", [128, 4, 512], BF16)
    fst = c.sb("fst", [4, CH])
    psN = c.psum("psN")
    psP = [c.psum(f"psP{i}") for i in range(3)]
    psM = [c.psum(f"psM{i}") for i in range(4)]

    ld(ph, "sp", "lx", xT[:], xT_d.rearrange("(k p) t -> p k t", p=128), ["xT"])
    ld(ph, "sp", "lg", gcol[:], g_d, ["gcol"])
    ld(ph, "sp", "lvg", vgain[:], vg_d, ["vgain"])
    ld(ph, "sp", "lbs", bsb[:], bs_d, ["bsb"])
    wv = w_d.rearrange("(k p) c -> p k c", p=128)
    for gi, (a, b) in enumerate([(0, 512), (512, 1024), (1024, 1536), (1536, 2048), (2048, 2564)]):
        ld(ph, "pool", f"lw{gi}", w[:, :, a:b], wv[:, :, a:b], [f"w{gi}"])
    ld(ph, "pool", "lws", wsm[:], ws_d, ["wsm"])
    ph.op("dve", lambda e: e.memset(ones_bf[:], 1.0), writes=["ones"])
    ph.op("dve", lambda e: e.memset(wsm[64:128, :, 0:64], 0.0), reads=["wsm"], writes=["wsm"])

    pi = [0]

    def nextps():
        pi[0] = (pi[0] + 1) % 3
        return psP[pi[0]], f"psP{pi[0]}"

    def proj_fm(col0, m, wres):
        ps, pres = nextps()
        for k in range(8):
            ph.op("pe", (lambda k, ps: lambda e: e.matmul(ps[:], lhsT=w[:, k, col0 + m * 128: col0 + (m + 1) * 128], rhs=xn[:, k, :],
                                                           start=(k == 0), stop=(k == 7)))(k, ps),
                  reads=["xn", wres], writes=[pres])
        return ps, pres

    for tc in range(NCH):
        cs = slice(tc * CH, (tc + 1) * CH)
        rmsnorm_fm(ph, "n", xT, cs, gcol, xn, ones_bf, sq, psN, rstd_s, rstd, "xT", "xn")
        for m in range(4):
            ps, pres = proj_fm(0, m, "w0")
            ph.op("act", (lambda m, ps: lambda e: e.activation(out=uT[:, m, :], in_=ps[:], func=AF.Gelu_apprx_tanh))(m, ps),
                  reads=[pres], writes=[f"uT{m}"])
        for m in range(4):
            ps, pres = proj_fm(1024, m, "w2")
            ph.op("dve", (lambda m, ps: lambda e: e.tensor_copy(out=qst[:, m, :], in_=ps[:]))(m, ps), reads=[pres], writes=["qst"])
        ld(ph, "sp", "sq", q_d.rearrange("(m p) t -> p m t", p=128)[:, :, cs], qst[:], [], reads=["qst"])
        for m in range(4):
            ps, pres = proj_fm(1536, m, "w3")
            ph.op("act", (lambda m, ps: lambda e: e.activation(out=kst[:, m, :], in_=ps[:], func=AF.Copy))(m, ps), reads=[pres], writes=["kst"])
        ld(ph, "sp", "sk", k_d.rearrange("(m p) t -> p m t", p=128)[:, :, cs], kst[:], [], reads=["kst"])
        ps, pres = nextps()
        for k in range(8):
            ph.op("pe", (lambda k, ps: lambda e: e.matmul(ps[0:4, :], lhsT=w[:, k, 2560:2564], rhs=xn[:, k, :], start=(k == 0), stop=(k == 7)))(k, ps),
                  reads=["xn", "w4"], writes=[pres])
        ph.op("dve", (lambda ps: lambda e: e.tensor_copy(out=fst[:], in_=ps[0:4, :]))(ps), reads=[pres], writes=["fst"])
        ld(ph, "sp", "sf", f_d[:, cs], fst[:], [], reads=["fst"])
        for s in range(4):
            ts_ = slice(s * 128, (s + 1) * 128)
            ps, pres = nextps()
            for k in range(8):
                ph.op("pe", (lambda k, ps, ts_: lambda e: e.matmul(ps[:], lhsT=xn[:, k, ts_], rhs=w[:, k, 512:1024], start=(k == 0), stop=(k == 7)))(k, ps, ts_),
                      reads=["xn", "w1"], writes=[pres])
            ph.op("act", (lambda ps: lambda e: e.activation(out=vg[:], in_=ps[:], func=AF.Gelu_apprx_tanh))(ps), reads=[pres], writes=["vg"])
            for g in range(4):
                ph.op("act", (lambda g: lambda e: e.activation(out=vscr[:], in_=vg[:, g * 128:(g + 1) * 128], func=AF.Square,
                                                               accum_out=ss[:, g:g + 1]))(g),
                      reads=["vg"], writes=["vscr", f"ss{g}"])
            ph.op("act", lambda e: e.activation(out=ssr[:], in_=ss[:], func=AF.Sqrt, scale=1.0 / 128.0, bias=EPS),
                  reads=[f"ss{g}" for g in range(4)], writes=["ssr"])
            ph.op("dve", lambda e: e.reciprocal(out=ss[:], in_=ssr[:]), reads=["ssr"], writes=[f"ss{g}" for g in range(4)])
            vnb = vn[s % 2]
            for g in range(4):
                gs = slice(g * 128, (g + 1) * 128)
                ph.op("dve", (lambda g, gs, vnb: lambda e: e.scalar_tensor_tensor(out=vnb[:, gs], in0=vg[:, gs], scalar=ss[:, g:g + 1], in1=vgain[:, gs],
                                                                                   op0=ALU.mult, op1=ALU.mult))(g, gs, vnb),
                      reads=["vg", f"ss{g}", "vgain"], writes=[f"vn{s%2}"])
            for g in range(4):
                gs = slice(g * 128, (g + 1) * 128)
                ph.op("pe", (lambda g, gs, vnb, ts_: lambda e: e.matmul(psM[g][:, ts_], lhsT=vnb[:, gs], rhs=wsm[:, g, :], start=True, stop=True))(g, gs, vnb, ts_),
                      reads=[f"vn{s%2}", "wsm"], writes=[f"psM{g}"])
            ps, pres = nextps()
            for k in range(8):
                ph.op("pe", (lambda k, ps, ts_: lambda e: e.matmul(ps[:], lhsT=xn[:, k, ts_], rhs=w[:, k, 2048:2560], start=(k == 0), stop=(k == 7)))(k, ps, ts_),
                      reads=["xn", "w4"], writes=[pres])
            ph.op("dve", (lambda ps, s: lambda e: e.tensor_copy(out=vst[:, s, :], in_=ps[:]))(ps, s), reads=[pres], writes=["vst"])
        ld(ph, "sp", "sv", v_d.rearrange("(n p) c -> p n c", p=128)[:, tc * 4:(tc + 1) * 4, :], vst[:], [], reads=["vst"])
        for g in range(4):
            ph.op("dve", (lambda g: lambda e: e.tensor_tensor(out=tmix[:], in0=psM[g][:], in1=bsb[:, g, :], op=ALU.add))(g),
                  reads=[f"psM{g}", "bsb"], writes=["tmix"])
            ph.op("dve", (lambda g: lambda e: e.tensor_tensor(out=yast[:, g, :], in0=tmix[:], in1=uT[:, g, :], op=ALU.mult))(g),
                  reads=["tmix", f"uT{g}"], writes=["yast"])
        ld(ph, "sp", "sy", ya_d.rearrange("(m p) t -> p m t", p=128)[:, :, cs], yast[:], [], reads=["yast"])
    return c.finish()


def _c(a):
    return np.ascontiguousarray(a)


def pk(vec):
    return _c(np.asarray(vec).reshape(-1, 128).T)


def rep(vec, n=128):
    return _c(np.broadcast_to(np.asarray(vec)[None], (n,) + tuple(np.asarray(vec).shape)))


def core_tokens(xfull, core):
    b, i = core // 4, core % 4
    return xfull[b, i * T:(i + 1) * T]


_NC_CACHE = {}


def get_nc(name, builder):
    if name not in _NC_CACHE:
        _NC_CACHE[name] = builder()
    return _NC_CACHE[name]


def run(nc, in_maps):
    res = run_bass_kernel_spmd(nc, in_maps, core_ids=list(range(8)))
    return res.results


def host_A(inp):
    x = np.asarray(inp["x"], np.float32)
    maps = []
    ws = _c(np.transpose(np.asarray(inp["gmlp_w_s"][0], np.float32), (2, 0, 1)))
    bs = np.asarray(inp["gmlp_b_s"][0], np.float32)
    bs4 = rep(np.tile(bs, (1, 4)))
    for core in range(8):
        maps.append(dict(
            xT=_c(core_tokens(x, core).T),
            g=pk(inp["norm_mix"][0]),
            w=_c(np.asarray(inp["even_w_in"][0], np.float32)),
            vgain=rep(np.asarray(inp["gmlp_v_gain"][0], np.float32)),
            ws=ws, bs=bs4))
    return maps


S = 8192
NT = S // 128
NG = S // CH


def build_B(c=None, io=None):
    if c is None:
        c = Ctx("B")
        io = dict(q=c.din("q", [128, S], BF16), k=c.din("k", [128, S], BF16), v=c.din("v", [128, NT, 128], BF16),
                  f2=c.din("f2", [64, 128]), bf=c.din("bf", [64, 1]), ident=c.din("ident", [64, 64]), lmat=c.din("lmat", [64, 64]),
                  tri=c.din("tri", [128, 128]), y=c.dout("y", [128, S], BF16))
    nc, ph = c.nc, c.ph
    q_d, k_d, v_d, f_d, bf_d, id_d, L_d, tri_d, y_d = (io[n] for n in ("q", "k", "v", "f2", "bf", "ident", "lmat", "tri", "y"))

    qT = c.sb("qT", [128, S], BF16)
    kT = c.sb("kT", [128, S], BF16)
    V = c.sb("V", [128, NT, 128], BF16)
    f2 = c.sb("f2", [64, 128])
    bfc = c.sb("bfc", [64, 1])
    ident = c.sb("ident", [64, 64])
    lmat = c.sb("lmat", [64, 64])
    tri = c.sb("tri", [128, 128], BF16)
    ones_bf = c.sb("ones", [128, 128], BF16)
    ones64 = c.sb("ones64", [64, 128])
    cs2 = c.sb("cs2", [64, 128])
    off = c.sb("off", [64, 1])
    dg = c.sb("dg", [64, 64])
    dcol = c.sb("dcol", [128, 64])
    dref = c.sb("dref", [128, 64])
    biasm = c.sb("biasm", [128, NT, NT])
    PT = [c.sb(f"PT{i}", [128, CH], BF16) for i in range(4)]
    rl = c.sb("rl", [128, CH])
    ost = [c.sb(f"ost{i}", [128, CH], BF16) for i in range(2)]
    psS = [c.psum(f"psS{i}") for i in range(3)]
    psO = [c.psum(f"psO{i}") for i in range(2)]
    psL = [c.psum(f"psL{i}") for i in range(2)]
    psX = c.psum("psX")

    ld(ph, "sp", "l1", f2[:], f_d, ["f2"])
    ld(ph, "sp", "l2", bfc[:], bf_d, ["bfc"])
    ld(ph, "sp", "l3", ident[:], id_d, ["ident"])
    ld(ph, "sp", "l4", lmat[:], L_d, ["lmat"])
    ld(ph, "pool", "l5", tri[:], tri_d, ["tri"])
    ld(ph, "sp", "lq", qT[:], q_d, ["qT"])
    ld(ph, "act", "lk", kT[:], k_d, ["kT"])
    ld(ph, "sp", "lv", V[:], v_d, ["V"])
    ph.op("dve", lambda e: e.memset(ones_bf[:], 1.0), writes=["ones"])
    ph.op("dve", lambda e: e.memset(ones64[:], 1.0), writes=["ones64"])

    ph.op("dve", lambda e: e.tensor_scalar(out=bfc[:], in0=bfc[:], scalar1=-1.0, scalar2=None, op0=ALU.mult), reads=["bfc"], writes=["bfc"])
    ph.op("act", lambda e: e.activation(out=f2[:], in_=f2[:], func=AF.Exp, scale=-1.0, bias=bfc[:, 0:1]), reads=["f2", "bfc"], writes=["f2"])
    ph.op("act", lambda e: e.activation(out=f2[:], in_=f2[:], func=AF.Ln, scale=1.0, bias=1.0), reads=["f2"], writes=["f2"])
    ph.op("dve", lambda e: e.tensor_tensor_scan(out=cs2[:], data0=f2[:], data1=f2[:], initial=0.0, op0=ALU.add, op1=ALU.max),
          reads=["f2"], writes=["cs2"])
    ph.op("pe", lambda e: e.matmul(psX[0:64, 0:1], lhsT=lmat[:], rhs=cs2[:, 127:128], start=True, stop=True), reads=["cs2", "lmat"], writes=["psX"])
    ph.op("dve", lambda e: e.tensor_copy(out=off[:], in_=psX[0:64, 0:1]), reads=["psX"], writes=["off"])
    ph.op("dve", lambda e: e.tensor_scalar(out=cs2[:], in0=cs2[:], scalar1=off[:, 0:1], scalar2=None, op0=ALU.add), reads=["cs2", "off"], writes=["cs2"])
    ph.op("pe", lambda e: e.transpose(psX[:, 0:64], cs2[:], ident[:]), reads=["cs2", "ident", "off"], writes=["psX"])
    ph.op("dve", lambda e: e.tensor_copy(out=dcol[:], in_=psX[:, 0:64]), reads=["psX"], writes=["dcol"])
    ph.op("dve", lambda e: e.tensor_scalar(out=dg[:], in0=ident[:], scalar1=cs2[:, 64:65], scalar2=None, op0=ALU.mult), reads=["cs2", "ident"], writes=["dg"])
    ph.op("pe", lambda e: e.matmul(psX[:, 64:128], lhsT=ones64[:], rhs=dg[:], start=True, stop=True), reads=["dg", "ones64", "dcol"], writes=["psX"])
    ph.op("dve", lambda e: e.tensor_copy(out=dref[:], in_=psX[:, 64:128]), reads=["psX"], writes=["dref"])
    for i in range(NT):
        ph.op("dve", (lambda i: lambda e: e.tensor_scalar(out=biasm[:, i, 0:i + 1], in0=dcol[:, 0:i + 1], scalar1=dref[:, i:i + 1], scalar2=None,
                                                          op0=ALU.subtract))(i),
              reads=["dcol", "dref"], writes=["biasm"])

    scale = 128.0 ** -0.5
    LA = 2
    its = [(I, j) for I in range(NG) for j in range(4 * I + 4)]
    NIT = len(its)
    NPT = len(PT)

    def front(n):
        I, j = its[n]
        qs = slice(I * CH, (I + 1) * CH)
        pS, pr = psS[n % 3], f"psS{n%3}"
        P, sr = PT[n % NPT], f"PT{n%NPT}"
        ph.I("pe", "matmul", pS[:], lhsT=kT[:, j * 128:(j + 1) * 128], rhs=qT[:, qs], start=True, stop=True, reads=["kT", "qT"], writes=[pr])
        jj = max(j - 4 * I, 0)
        for ii in range(jj, 4):
            i = 4 * I + ii
            bs_ = slice(ii * 128, (ii + 1) * 128)
            ph.I("act", "activation", out=P[:, bs_], in_=pS[:, bs_], func=AF.Exp, scale=scale, bias=biasm[:, i, j:j + 1],
                 reads=[pr, "biasm"], writes=[sr])
            if i == j:
                ph.I("pool", "tensor_tensor", out=P[:, bs_], in0=P[:, bs_], in1=tri[:], op=ALU.mult, reads=[sr, "tri"], writes=[sr])

    def back(n):
        I, j = its[n]
        qs = slice(I * CH, (I + 1) * CH)
        last = 4 * I + 3
        P, sr = PT[n % NPT], f"PT{n%NPT}"
        pO, pL = psO[I % 2], psL[I % 2]
        c0 = max(j - 4 * I, 0) * 128
        ph.I("pe", "matmul", pO[:, c0:], lhsT=V[:, j, :], rhs=P[:, c0:], start=(j == 0), stop=(j == last), reads=[sr, "V"], writes=[f"psO{I%2}"])
        ph.I("pe", "matmul", pL[:, c0:], lhsT=ones_bf[:], rhs=P[:, c0:], start=(j == 0), stop=(j == last), reads=[sr, "ones"], writes=[f"psL{I%2}"])
        if j == last:
            o = ost[I % 2]
            ph.I("dve", "reciprocal", out=rl[:], in_=pL[:], reads=[f"psL{I%2}"], writes=["rl"])
            ph.I("dve", "tensor_tensor", out=o[:], in0=pO[:], in1=rl[:], op=ALU.mult, reads=[f"psO{I%2}", "rl"], writes=[f"ost{I%2}"])
            ld(ph, "sp", f"so{I%2}", y_d[:, qs], o[:], [], reads=[f"ost{I%2}"])

    for n in range(NIT + LA):
        if n < NIT:
            front(n)
        if n - LA >= 0:
            back(n - LA)
    return c.finish()


def host_B(inp, resA):
    maps = []
    ident = np.eye(64, dtype=np.float32)
    lmat = np.triu(np.ones((64, 64), np.float32), 1)
    tri = np.triu(np.ones((128, 128), np.float32), 0)
    for core in range(8):
        b, h = core // 4, core % 4
        q = np.concatenate([resA[b * 4 + i]["q"][h * 128:(h + 1) * 128] for i in range(4)], axis=1)
        k = np.concatenate([resA[b * 4 + i]["k"][h * 128:(h + 1) * 128] for i in range(4)], axis=1)
        v = np.concatenate([resA[b * 4 + i]["v"][:, h * 128:(h + 1) * 128] for i in range(4)], axis=0)
        f = np.concatenate([resA[b * 4 + i]["f"][h] for i in range(4)], axis=0)
        maps.append(dict(q=_c(q), k=_c(k), v=_c(v.reshape(NT, 128, 128).transpose(1, 0, 2)),
                         f2=_c(f.reshape(64, 128)), bf=np.full((64, 1), inp["fox_b_f"][0][h], np.float32),
                         ident=ident, lmat=lmat, tri=tri))
    return maps


MC = 1024
M_SCALE = 256.0 ** -0.5


def wtiles(W, ncols):
    W = np.asarray(W, np.float32)
    K_, N_ = W.shape
    return _c(W.reshape(K_ // 128, 128, N_ // ncols, ncols).transpose(2, 1, 0, 3))


class Tok(Ctx):
    def __init__(self, name, nwb=4, wb_elems=4096, arena_words=11264, nc=None):
        super().__init__(name, nc=nc)
        c = self
        self.xT = c.sb("xT", [128, 8, T])
        self.ones_bf = c.sb("ones", [128, 128], BF16)
        self.sq = [c.sb(f"sq{i}", [128, CH], BF16) for i in range(2)]
        self.rstd_s = c.sb("rstd_s", [128, CH])
        self.rstd = c.sb("rstd", [128, CH])
        self.gcols = c.sb("gcols", [128, 4, 8])
        self.wb = [c.sb(f"wb{i}", [128, wb_elems], BF16) for i in range(nwb)]
        self.wi = 0
        self.arena = c.sb("arena", [128, arena_words])
        self.actT = self.arena[:].bitcast(BF16).rearrange("p (k t) -> p k t", t=MC)
        self.ymb = [c.sb(f"ymb{i}", [128, 8, CH], BF16) for i in range(2)]
        self.hT = c.sb("hT", [128, 8, MC], BF16)
        self.sg = [c.sb(f"sg{i}", [128, CH], BF16) for i in range(2)]
        self.pp = [c.psum(f"pp{i}") for i in range(6)]
        self.psN = c.psum("psN")
        self.psL = c.psum("psL")
        self.pi = 0
        self.ph.I("dve", "memset", self.ones_bf[:], 1.0, writes=["ones"])

    def nextps(self):
        self.pi = (self.pi + 1) % len(self.pp)
        return self.pp[self.pi], f"pp{self.pi}"

    def wload(self, dram_tile_ap, nk, ncols):
        i = self.wi
        self.wi = (self.wi + 1) % len(self.wb)
        view = self.wb[i][:, 0:nk * ncols].rearrange("p (k c) -> p k c", c=ncols)
        ld(self.ph, "pool", f"wb{i}", view, dram_tile_ap, [f"wb{i}"])
        return view, f"wb{i}"

    def norm(self, gi, cs, xn, xn_res, n=CH):
        rmsnorm_fm(self.ph, "n", self.xT, cs, self.gcols[:, gi, :], xn, self.ones_bf, self.sq, self.psN,
                   self.rstd_s, self.rstd, "xT", xn_res)

    def linear_fm(self, Wt_d, nk, ncols, src, src_res, ntok, evac):
        ph = self.ph
        ncg = Wt_d.shape[0]
        for cg in range(ncg):
            wv, wres = self.wload(Wt_d[cg], nk, ncols)
            for mi in range(ncols // 128):
                for ts in range(ntok // CH):
                    ps, pres = self.nextps()
                    for k in range(nk):
                        ph.I("pe", "matmul", ps[:], lhsT=wv[:, k, mi * 128:(mi + 1) * 128], rhs=src[:, k, ts * CH:(ts + 1) * CH],
                             start=(k == 0), stop=(k == nk - 1), reads=[wres, src_res], writes=[pres])
                    evac(cg * (ncols // 128) + mi, ts, ps, pres)

    def resid_add(self, t0):
        ph, xT = self.ph, self.xT

        def ev(m, ts, ps, pres):
            sl = slice(t0 + ts * CH, t0 + (ts + 1) * CH)
            ph.I("dve", "tensor_tensor", out=xT[:, m, sl], in0=xT[:, m, sl], in1=ps[:], op=ALU.add, reads=[pres, "xT"], writes=["xT"])
        return ev

    def mem_setup(self, memT_d, gkv_d, Wkv_k_d, Wkv_v_d):
        c, ph = self, self.ph
        self.memT = self.arena[:, 0:2048].rearrange("p (k t) -> p k t", t=256)
        self.memn = self.arena[:, 2048:3072].bitcast(BF16).rearrange("p (k t) -> p k t", t=256)
        self.gkv = c.sb("gkv", [128, 8])
        self.KmT = self.arena[:, 3072:4096].bitcast(BF16).rearrange("p (k t) -> p k t", t=256)
        self.Vm = self.arena[:, 4096:5120].bitcast(BF16).rearrange("p (k t) -> p k t", t=1024)
        self.qmT = self.ymb[0]
        self.omT = self.ymb[1]
        self.PmT = [self.arena[:, 9728 + 256 * i: 9728 + 256 * (i + 1)].bitcast(BF16) for i in range(2)]
        self.rl = self.arena[:, 10240:10752]
        self.xq = c.sb("xq", [128, 8, CH], BF16)
        ld(ph, "sp", "lmem", self.memT, memT_d.rearrange("(k p) t -> p k t", p=128), ["memT"])
        ld(ph, "sp", "lgkv", self.gkv[:], gkv_d, ["gkv"])
        for k in range(8):
            sqk = self.sq[k % 2]
            ph.I("act", "activation", out=sqk[:, 0:256], in_=self.memT[:, k, :], func=AF.Square, reads=["memT"], writes=[f"nsq{k%2}"])
            ph.I("pe", "matmul", self.psN[:, 0:256], lhsT=self.ones_bf[:], rhs=sqk[:, 0:256], start=(k == 0), stop=(k == 7),
                 reads=[f"nsq{k%2}", "ones"], writes=["npsN"])
        ph.I("act", "activation", out=self.rstd_s[:, 0:256], in_=self.psN[:, 0:256], func=AF.Sqrt, scale=1.0 / 1024.0, bias=EPS,
             reads=["npsN"], writes=["nrstd_s"])
        ph.I("dve", "reciprocal", out=self.rstd[:, 0:256], in_=self.rstd_s[:, 0:256], reads=["nrstd_s"], writes=["nrstd"])
        for k in range(8):
            ph.I("dve", "scalar_tensor_tensor", out=self.memn[:, k, :], in0=self.memT[:, k, :], scalar=self.gkv[:, k:k + 1],
                 in1=self.rstd[:, 0:256], op0=ALU.mult, op1=ALU.mult, reads=["memT", "nrstd", "gkv"], writes=["memn"])
        for cg in range(2):
            wv, wres = self.wload(Wkv_k_d[cg], 8, 512)
            for mi in range(4):
                ps, pres = self.nextps()
                for k in range(8):
                    ph.I("pe", "matmul", ps[:, 0:256], lhsT=wv[:, k, mi * 128:(mi + 1) * 128], rhs=self.memn[:, k, :],
                         start=(k == 0), stop=(k == 7), reads=[wres, "memn"], writes=[pres])
                ph.I("act", "activation", out=self.KmT[:, cg * 4 + mi, :], in_=ps[:, 0:256], func=AF.Copy, reads=[pres], writes=["KmT"])
        for cg in range(2):
            wv, wres = self.wload(Wkv_v_d[cg], 8, 512)
            for mt in range(2):
                ps, pres = self.nextps()
                for k in range(8):
                    ph.I("pe", "matmul", ps[:], lhsT=self.memn[:, k, mt * 128:(mt + 1) * 128], rhs=wv[:, k, :],
                         start=(k == 0), stop=(k == 7), reads=[wres, "memn"], writes=[pres])
                ph.I("dve", "tensor_copy", out=self.Vm[:, mt, cg * 512:(cg + 1) * 512], in_=ps[:], reads=[pres], writes=["Vm"])

    def mem_attn(self, gi, Wq_d, Wo_d):
        ph = self.ph
        for tc in range(NCH):
            cs = slice(tc * CH, (tc + 1) * CH)
            self.norm(gi, cs, self.xq, "xq")

            def ev_q(m, ts, ps, pres):
                eng = "act" if m % 2 else "dve"
                if eng == "act":
                    ph.I("act", "activation", out=self.qmT[:, m, :], in_=ps[:], func=AF.Copy, reads=[pres], writes=["ymb0"])
                else:
                    ph.I("dve", "tensor_copy", out=self.qmT[:, m, :], in_=ps[:], reads=[pres], writes=["ymb0"])
            self.linear_fm(Wq_d, 8, 512, self.xq, "xq", CH, ev_q)
            for hh in range(4):
                for mt in range(2):
                    ps, pres = self.nextps()
                    for c2 in range(2):
                        ph.I("pe", "matmul", ps[:], lhsT=self.KmT[:, hh * 2 + c2, mt * 128:(mt + 1) * 128], rhs=self.qmT[:, hh * 2 + c2, :],
                             start=(c2 == 0), stop=(c2 == 1), reads=["KmT", "ymb0"], writes=[pres])
                    ph.I("act", "activation", out=self.PmT[mt], in_=ps[:], func=AF.Exp, scale=M_SCALE, reads=[pres], writes=[f"PmT{mt}"])
                for mt in range(2):
                    ph.I("pe", "matmul", self.psL[:], lhsT=self.ones_bf[:], rhs=self.PmT[mt], start=(mt == 0), stop=(mt == 1),
                         reads=[f"PmT{mt}", "ones"], writes=["psL"])
                ph.I("dve", "reciprocal", out=self.rl, in_=self.psL[:], reads=["psL"], writes=["rl"])
                for c2 in range(2):
                    ps, pres = self.nextps()
                    for mt in range(2):
                        ph.I("pe", "matmul", ps[:], lhsT=self.Vm[:, mt, hh * 256 + c2 * 128: hh * 256 + (c2 + 1) * 128], rhs=self.PmT[mt],
                             start=(mt == 0), stop=(mt == 1), reads=["Vm", f"PmT{mt}"], writes=[pres])
                    ph.I("dve", "tensor_tensor", out=self.omT[:, hh * 2 + c2, :], in0=ps[:], in1=self.rl, op=ALU.mult,
                         reads=[pres, "rl"], writes=["ymb1"])
            self.linear_fm(Wo_d, 8, 512, self.omT, "ymb1", CH, self.resid_add(tc * CH))


def out_proj(c, ymix_d, Wout_d):
    ph = c.ph
    for tc in range(NCH):
        yb = c.ymb[tc % 2]
        if isinstance(ymix_d, tuple):
            for hi, part in enumerate(ymix_d):
                ld(ph, "sp", f"lym{tc%2}", yb[:, 4 * hi:4 * hi + 4, :], part.rearrange("(k p) t -> p k t", p=128)[:, :, tc * CH:(tc + 1) * CH],
                   [f"ymb{tc%2}"])
        else:
            ymv = ymix_d.rearrange("(k p) t -> p k t", p=128)
            ld(ph, "sp", f"lym{tc%2}", yb[:], ymv[:, :, tc * CH:(tc + 1) * CH], [f"ymb{tc%2}"])
        c.linear_fm(Wout_d, 8, 512, yb, f"ymb{tc%2}", CH, c.resid_add(tc * CH))


def ffn_swiglu(c, gi, Wg_d, Wu_d, Wd_d, nf, post=None):
    ph = c.ph
    ncg = nf // 2
    for mc in range(T // MC):
        for ts in range(MC // CH):
            cs = slice(mc * MC + ts * CH, mc * MC + (ts + 1) * CH)
            c.norm(gi, cs, c.hT[:, :, ts * CH:(ts + 1) * CH], "hT")
        for cg in range(ncg):
            wg, wgr = c.wload(Wg_d[cg], 8, 256)
            wu, wur = c.wload(Wu_d[cg], 8, 256)
            for mi in range(2):
                for ts in range(MC // CH):
                    psG, pgr = c.nextps()
                    psU, pur = c.nextps()
                    for k in range(8):
                        ph.I("pe", "matmul", psG[:], lhsT=wg[:, k, mi * 128:(mi + 1) * 128], rhs=c.hT[:, k, ts * CH:(ts + 1) * CH],
                             start=(k == 0), stop=(k == 7), reads=[wgr, "hT"], writes=[pgr])
                    for k in range(8):
                        ph.I("pe", "matmul", psU[:], lhsT=wu[:, k, mi * 128:(mi + 1) * 128], rhs=c.hT[:, k, ts * CH:(ts + 1) * CH],
                             start=(k == 0), stop=(k == 7), reads=[wur, "hT"], writes=[pur])
                    sg = c.sg[(mi * 2 + ts) % 2]
                    sgr = f"sg{(mi * 2 + ts) % 2}"
                    ph.I("act", "activation", out=sg[:], in_=psG[:], func=AF.Silu, reads=[pgr], writes=[sgr])
                    ph.I("dve", "tensor_tensor", out=c.actT[:, cg * 2 + mi, ts * CH:(ts + 1) * CH], in0=sg[:], in1=psU[:], op=ALU.mult,
                         reads=[sgr, pur], writes=["actT"])
        c.linear_fm(Wd_d, nf, 128, c.actT, "actT", MC, c.resid_add(mc * MC))


C_IN = dict(xT=([1024, T], F32), ymix=([1024, T], BF16), gcols=([128, 4, 8], F32), Wout=([2, 128, 8, 512], F32), memT=([1024, 256], F32),
            gkv=([128, 8], F32), Wkvk=([2, 128, 8, 512], F32), Wkvv=([2, 128, 8, 512], F32), Wq=([2, 128, 8, 512], F32),
            Wo=([2, 128, 8, 512], F32), Wg=([11, 128, 8, 256], F32), Wu=([11, 128, 8, 256], F32), Wd=([8, 128, 22, 128], F32),
            Win=([6, 128, 8, 512], F32))


def build_C(c=None, io=None):
    if c is None:
        c = Tok("C")
        io = {n: c.din(n, sh, dt) for n, (sh, dt) in C_IN.items()}
        io.update(xo=c.dout("xo", [1024, T]), qk=c.dout("qk", [1024, T], BF16), v=c.dout("v", [T, 512], BF16),
                  z=c.dout("z", [512, T]), gb=c.dout("gb", [512, T]))
    nc, ph = c.nc, c.ph
    xT_d, ymix_d, gcols_d, Wout_d, memT_d, gkv_d, Wkvk_d, Wkvv_d, Wq_d, Wo_d, Wg_d, Wu_d, Wd_d, Win_d = (io[n] for n in C_IN)
    xo_d, qk_d, v_d, z_d, gb_d = io["xo"], io["qk"], io["v"], io["z"], io["gb"]

    ld(ph, "sp", "lx", c.xT[:], xT_d.rearrange("(k p) t -> p k t", p=128), ["xT"])
    ld(ph, "sp", "lg", c.gcols[:], gcols_d, ["gcol"])

    out_proj(c, ymix_d, Wout_d)
    c.mem_setup(memT_d, gkv_d, Wkvk_d, Wkvv_d)
    c.mem_attn(0, Wq_d, Wo_d)
    ffn_swiglu(c, 1, Wg_d, Wu_d, Wd_d, 22)

    xn1 = c.xq
    ar = c.arena
    gbst = ar[:, 0:2048].rearrange("p (m t) -> p m t", t=CH)
    zst = ar[:, 2048:4096].rearrange("p (m t) -> p m t", t=CH)
    vst = ar[:, 4096:5120].bitcast(BF16).rearrange("p (m t) -> p m t", t=512)
    qkst = ar[:, 5120:7168].bitcast(BF16).rearrange("p (m t) -> p m t", t=CH)
    hst = ar[:, 7168:9216].rearrange("p (m t) -> p m t", t=CH)
    dummy = c.sb("dummy", [128, 1])
    ph.I("dve", "memset", dummy[:], 0.0, reads=["actT"], writes=["qkst", "gbst", "zst", "vst"] + [f"hst{m}" for m in range(4)])
    for tc in range(NCH):
        cs = slice(tc * CH, (tc + 1) * CH)
        c.norm(2, cs, xn1, "xq")

        def ev_qk(m, ts, ps, pres):
            if m % 2:
                ph.I("act", "activation", out=qkst[:, m, :], in_=ps[:], func=AF.Copy, reads=[pres], writes=["qkst"])
            else:
                ph.I("dve", "tensor_copy", out=qkst[:, m, :], in_=ps[:], reads=[pres], writes=["qkst"])
        c.linear_fm(Win_d[0:2], 8, 512, xn1, "xq", CH, ev_qk)
        ld(ph, "sp", "sqk", qk_d.rearrange("(m p) t -> p m t", p=128)[:, :, cs], qkst, [], reads=["qkst"])
        wv, wres = c.wload(Win_d[2], 8, 512)
        for s in range(4):
            ps, pres = c.nextps()
            for k in range(8):
                ph.I("pe", "matmul", ps[:], lhsT=xn1[:, k, s * 128:(s + 1) * 128], rhs=wv[:, k, :], start=(k == 0), stop=(k == 7),
                     reads=[wres, "xq"], writes=[pres])
            ph.I("act", "activation", out=vst[:, s, :], in_=ps[:], func=AF.Copy, reads=[pres], writes=["vst"])
        ld(ph, "sp", "sv", v_d.rearrange("(n p) c -> p n c", p=128)[:, tc * 4:(tc + 1) * 4, :], vst, [], reads=["vst"])

        def ev_conv(m, ts, ps, pres):
            if m < 4:
                ph.I("act", "activation", out=hst[:, m, :], in_=ps[:], func=AF.Copy, reads=[pres], writes=[f"hst{m}"])
            elif m < 8:
                ph.I("act", "activation", out=gbst[:, m - 4, :], in_=ps[:], func=AF.Copy, reads=[pres], writes=["gbst"])
            else:
                ph.I("dve", "tensor_tensor", out=zst[:, m - 8, :], in0=hst[:, m - 8, :], in1=ps[:], op=ALU.mult,
                     reads=[pres, f"hst{m-8}"], writes=["zst"])
        c.linear_fm(Win_d[3:6], 8, 512, xn1, "xq", CH, ev_conv)
        ld(ph, "sp", "sgb", gb_d.rearrange("(m p) t -> p m t", p=128)[:, :, cs], gbst, [], reads=["gbst"])
        ld(ph, "sp", "sz", z_d.rearrange("(m p) t -> p m t", p=128)[:, :, cs], zst, [], reads=["zst"])
    ld(ph, "sp", "sx", xo_d.rearrange("(k p) t -> p k t", p=128), c.xT[:], [], reads=["xT"])
    return c.finish()


def mem_inputs(inp, l, b):
    wkv = np.asarray(inp["mem_w_kv"][l], np.float32)
    return dict(memT=_c(np.asarray(inp["mem"][b], np.float32).T), gkv=pk(inp["norm_mem_kv"][l]),
                Wkvk=wtiles(wkv[:, :1024], 512), Wkvv=wtiles(wkv[:, 1024:], 512),
                Wq=wtiles(inp["mem_w_q"][l], 512), Wo=wtiles(inp["mem_w_o"][l], 512))


def host_C(inp, resA, resB, xT_cores):
    maps = []
    gcols = _c(np.stack([pk(inp["norm_mem_q"][0]), pk(inp["norm_ffn"][0]), pk(inp["norm_mix"][1]), pk(inp["norm_mix"][1])], axis=1))
    shared = dict(gcols=gcols, Wout=wtiles(inp["even_w_out"][0], 512), Wg=wtiles(inp["ffn_w_gate"][0], 256),
                  Wu=wtiles(inp["ffn_w_up"][0], 256), Wd=wtiles(inp["ffn_w_down"][0], 128), Win=wtiles(inp["odd_w_in"][0], 512))
    mem = [mem_inputs(inp, 0, b) for b in range(2)]
    for core in range(8):
        b, i = core // 4, core % 4
        yb = np.concatenate([resB[b * 4 + h]["y"][:, i * T:(i + 1) * T] for h in range(4)], axis=0)
        ymix = np.concatenate([resA[core]["ya"], yb], axis=0)
        m = dict(xT=xT_cores[core], ymix=_c(ymix))
        m.update(shared)
        m.update(mem[b])
        maps.append(m)
    return maps


LAM_INIT = 0.8 - 0.6 * float(np.exp(-0.3 * 1))


def rel_bucket_static(rel):
    import jax.numpy as jnp
    import jax
    with jax.default_device(jax.devices("cpu")[0]):
        rel = jnp.asarray(rel, jnp.int32)
        n_half, max_exact = 16, 8
        ret = jnp.where(rel > 0, n_half, 0)
        n = jnp.abs(rel)
        nf = jnp.maximum(n, 1).astype(jnp.float32)
        large = max_exact + (jnp.log(nf / max_exact) / float(np.log(128 / max_exact)) * (n_half - max_exact)).astype(jnp.int32)
        large = jnp.minimum(large, n_half - 1)
        return np.asarray(ret + jnp.where(n < max_exact, n, large))


def diff_masks():
    p = np.arange(128)[:, None]
    f = np.arange(128)[None, :]
    out = {}
    for delta in (0, -1):
        rel = 128 * delta + p - f
        bk = rel_bucket_static(rel)
        ids = sorted(set(bk.reshape(-1).tolist()))
        masks = np.stack([(bk == b).astype(np.float32) for b in ids])
        out[delta] = (ids, masks)
    notallowed = ((p // 64) > (f // 64)).astype(np.float32)
    return out, notallowed


_DM = {}


def get_diff_masks():
    if "m" not in _DM:
        _DM["m"] = diff_masks()
    return _DM["m"]


def build_D(c=None, io=None):
    (dm, notallowed) = get_diff_masks()
    ids0, ids1 = dm[0][0], dm[-1][0]
    n0, n1 = len(ids0), len(ids1)
    if c is None:
        c = Ctx("D")
        io = dict(q=c.din("q", [128, S], BF16), k=c.din("k", [128, S], BF16), v=c.din("v", [128, NT, 128], BF16),
                  masks=c.din("masks", [128, n0 + n1 + 1, 128]), tab=c.din("tab", [128, 32]), lam=c.din("lam", [128, 4, 64]),
                  gain=c.din("gain", [128, 1]), y=c.dout("y", [128, S], BF16))
    nc, ph = c.nc, c.ph
    q_d, k_d, v_d, mk_d, tab_d, lam_d, gain_d, y_d = (io[n] for n in ("q", "k", "v", "masks", "tab", "lam", "gain", "y"))

    QT = c.sb("QT", [128, S], BF16)
    KT = [c.sb(f"KT{i}", [128, S], BF16) for i in range(2)]
    V = c.sb("V", [128, NT, 128], BF16)
    mk = c.sb("mk", [128, n0 + n1 + 1, 128])
    tab = c.sb("tab", [128, 32])
    lamp = c.sb("lamp", [128, 4, 64])
    gsc = c.sb("gsc", [128, 1])
    B0 = c.sb("B0", [128, 128])
    B1 = c.sb("B1", [128, 128])
    lprod = c.sb("lprod", [128, 2, 64])
    lsum = c.sb("lsum", [128, 2])
    neglam = c.sb("neglam", [128, 1])
    ones_bf = c.sb("ones", [128, 128], BF16)
    tb = [c.sb(f"tb{i}", [128, 128]) for i in range(2)]
    PT = [[c.sb(f"PT{s}{i}", [128, CH], BF16) for i in range(4)] for s in range(2)]
    l1c = c.sb("l1c", [128, CH])
    l2c = c.sb("l2c", [128, CH])
    o1 = c.sb("o1", [128, CH])
    o2 = c.sb("o2", [128, CH])
    sqo = c.sb("sqo", [128, CH], BF16)
    rs = c.sb("rs", [128, CH])
    ost = [c.sb(f"ost{i}", [128, CH], BF16) for i in range(2)]
    psS3 = [c.psum(f"psS{i}") for i in range(3)]
    psN = c.psum("psN")
    psO = [c.psum(f"psO{s}") for s in range(2)]
    psL = [c.psum(f"psL{s}") for s in range(2)]

    ld(ph, "sp", "l1", mk[:], mk_d, ["mk"])
    ld(ph, "sp", "l2", tab[:], tab_d, ["tab"])
    ld(ph, "sp", "l3", lamp[:], lam_d, ["lamp"])
    ld(ph, "sp", "l4", gsc[:], gain_d, ["gsc"])
    if isinstance(q_d, tuple):
        ld(ph, "sp", "lq", QT[0:64, :], q_d[0], ["QT"])
        ld(ph, "sp", "lq", QT[64:128, :], q_d[1], ["QT"])
        ld(ph, "act", "lk0", KT[0][0:64, :], k_d[0], ["KT0"])
        ld(ph, "act", "lk1", KT[1][64:128, :], k_d[1], ["KT1"])
    else:
        ld(ph, "sp", "lq", QT[:], q_d, ["QT"])
        ld(ph, "act", "lk0", KT[0][0:64, :], k_d[0:64, :], ["KT0"])
        ld(ph, "act", "lk1", KT[1][64:128, :], k_d[64:128, :], ["KT1"])
    ph.I("pool", "memset", KT[0][64:128, :], 0.0, writes=["KT0z"])
    ph.I("pool", "memset", KT[1][0:64, :], 0.0, writes=["KT1z"])
    ld(ph, "sp", "lv", V[:], v_d, ["V"])
    ph.I("dve", "memset", ones_bf[:], 1.0, writes=["ones"])
    ph.I("dve", "tensor_scalar", out=B0[:], in0=mk[:, n0 + n1, :], scalar1=NEG, scalar2=None, op0=ALU.mult, reads=["mk"], writes=["B0"])
    for n, b in enumerate(ids0):
        ph.I("dve", "scalar_tensor_tensor", out=B0[:], in0=mk[:, n, :], scalar=tab[:, b:b + 1], in1=B0[:], op0=ALU.mult, op1=ALU.add,
             reads=["mk", "tab", "B0"], writes=["B0"])
    ph.I("dve", "tensor_scalar", out=B1[:], in0=mk[:, n0, :], scalar1=tab[:, ids1[0]:ids1[0] + 1], scalar2=None, op0=ALU.mult,
         reads=["mk", "tab"], writes=["B1"])
    for n, b in enumerate(ids1[1:]):
        ph.I("dve", "scalar_tensor_tensor", out=B1[:], in0=mk[:, n0 + 1 + n, :], scalar=tab[:, b:b + 1], in1=B1[:], op0=ALU.mult, op1=ALU.add,
             reads=["mk", "tab", "B1"], writes=["B1"])
    ph.I("dve", "tensor_tensor", out=lprod[:, 0, :], in0=lamp[:, 0, :], in1=lamp[:, 1, :], op=ALU.mult, reads=["lamp"], writes=["lprod"])
    ph.I("dve", "tensor_tensor", out=lprod[:, 1, :], in0=lamp[:, 2, :], in1=lamp[:, 3, :], op=ALU.mult, reads=["lamp"], writes=["lprod"])
    ph.I("dve", "reduce_sum", out=lsum[:], in_=lprod[:], axis=AX.X, reads=["lprod"], writes=["lsum"])
    ph.I("act", "activation", out=lsum[:], in_=lsum[:], func=AF.Exp, reads=["lsum"], writes=["lsum"])
    ph.I("dve", "tensor_tensor", out=neglam[:], in0=lsum[:, 1:2], in1=lsum[:, 0:1], op=ALU.subtract, reads=["lsum"], writes=["neglam"])
    ph.I("dve", "tensor_scalar", out=neglam[:], in0=neglam[:], scalar1=-LAM_INIT, scalar2=None, op0=ALU.add, reads=["neglam"], writes=["neglam"])
    ph.I("dve", "tensor_scalar", out=gsc[:], in0=gsc[:], scalar1=1.0 - LAM_INIT, scalar2=None, op0=ALU.mult, reads=["gsc"], writes=["gsc"])

    scale = 64.0 ** -0.5
    LA = 2
    its = [(I, j) for I in range(NG) for j in range(4 * I + 4)]
    NIT = len(its)
    NPT = len(PT[0])

    def front(n):
        I, j = its[n]
        qs = slice(I * CH, (I + 1) * CH)
        jj = max(j - 4 * I, 0)
        for s in range(2):
            bi = (2 * n + s) % 3
            pS, pr = psS3[bi], f"psS{bi}"
            P, sr = PT[s][n % NPT], f"PT{s}{n%NPT}"
            ph.I("pe", "matmul", pS[:], lhsT=KT[s][:, j * 128:(j + 1) * 128], rhs=QT[:, qs], start=True, stop=True,
                 reads=[f"KT{s}", f"KT{s}z", "QT"], writes=[pr])
            if j <= 4 * I - 2:
                ph.I("act", "activation", out=P[:], in_=pS[:], func=AF.Exp, scale=scale, bias=tab[:, 15:16], reads=[pr, "tab"], writes=[sr])
            else:
                for ii in range(jj, 4):
                    delta = j - (4 * I + ii)
                    bs_ = slice(ii * 128, (ii + 1) * 128)
                    if delta <= -2:
                        ph.I("act", "activation", out=P[:, bs_], in_=pS[:, bs_], func=AF.Exp, scale=scale, bias=tab[:, 15:16],
                             reads=[pr, "tab"], writes=[sr])
                    else:
                        Bt, br = (B0, "B0") if delta == 0 else (B1, "B1")
                        t = tb[(s + ii) % 2]
                        tr = f"tb{(s + ii) % 2}"
                        ph.I("dve", "scalar_tensor_tensor", out=t[:], in0=pS[:, bs_], scalar=scale, in1=Bt[:], op0=ALU.mult, op1=ALU.add,
                             reads=[pr, br], writes=[tr])
                        ph.I("act", "activation", out=P[:, bs_], in_=t[:], func=AF.Exp, reads=[tr], writes=[sr])

    def back(n):
        I, j = its[n]
        qs = slice(I * CH, (I + 1) * CH)
        last = 4 * I + 3
        c0 = max(j - 4 * I, 0) * 128
        for s in range(2):
            P, sr = PT[s][n % NPT], f"PT{s}{n%NPT}"
            ph.I("pe", "matmul", psO[s][:, c0:], lhsT=V[:, j, :], rhs=P[:, c0:], start=(j == 0), stop=(j == last),
                 reads=[sr, "V"], writes=[f"psO{s}"])
            ph.I("pe", "matmul", psL[s][:, c0:], lhsT=ones_bf[:], rhs=P[:, c0:], start=(j == 0), stop=(j == last),
                 reads=[sr, "ones"], writes=[f"psL{s}"])
        if j != last:
            return
        ph.I("dve", "tensor_copy", out=l1c[:], in_=psL[0][:], reads=["psL0"], writes=["l1c"])
        ph.I("dve", "tensor_copy", out=o1[:], in_=psO[0][:], reads=["psO0"], writes=["o1"])
        ph.I("dve", "tensor_copy", out=l2c[:], in_=psL[1][:], reads=["psL1"], writes=["l2c"])
        ph.I("dve", "tensor_copy", out=o2[:], in_=psO[1][:], reads=["psO1"], writes=["o2"])
        ph.I("dve", "reciprocal", out=l1c[:], in_=l1c[:], reads=["l1c"], writes=["l1c"])
        ph.I("dve", "reciprocal", out=l2c[:], in_=l2c[:], reads=["l2c"], writes=["l2c"])
        ph.I("pool", "tensor_tensor", out=o1[:], in0=o1[:], in1=l1c[:], op=ALU.mult, reads=["o1", "l1c"], writes=["o1"])
        ph.I("pool", "tensor_tensor", out=o2[:], in0=o2[:], in1=l2c[:], op=ALU.mult, reads=["o2", "l2c"], writes=["o2"])
        ph.I("dve", "scalar_tensor_tensor", out=o1[:], in0=o2[:], scalar=neglam[:, 0:1], in1=o1[:], op0=ALU.mult, op1=ALU.add,
             reads=["o1", "o2", "neglam"], writes=["o1"])
        ph.I("act", "activation", out=sqo[:], in_=o1[:], func=AF.Square, reads=["o1"], writes=["sqo"])
        ph.I("pe", "matmul", psN[:], lhsT=ones_bf[:], rhs=sqo[:], start=True, stop=True, reads=["sqo", "ones"], writes=["psN"])
        ph.I("act", "activation", out=rs[:], in_=psN[:], func=AF.Sqrt, scale=1.0 / 128.0, bias=EPS, reads=["psN"], writes=["rs"])
        ph.I("dve", "reciprocal", out=rs[:], in_=rs[:], reads=["rs"], writes=["rs"])
        o = ost[I % 2]
        ph.I("dve", "scalar_tensor_tensor", out=o[:], in0=o1[:], scalar=gsc[:, 0:1], in1=rs[:], op0=ALU.mult, op1=ALU.mult,
             reads=["o1", "rs", "gsc"], writes=[f"ost{I%2}"])
        ld(ph, "sp", f"so{I%2}", y_d[:, qs], o[:], [], reads=[f"ost{I%2}"])

    for n in range(NIT + LA):
        if n < NIT:
            front(n)
        if n - LA >= 0:
            back(n - LA)
    return c.finish()


def host_D(inp, resC):
    (dm, notallowed) = get_diff_masks()
    masks = np.concatenate([dm[0][1], dm[-1][1], notallowed[None]], axis=0)
    masks = _c(masks.transpose(1, 0, 2))
    lam = _c(np.stack([rep(inp["diff_lambda_q1"][0]), rep(inp["diff_lambda_k1"][0]),
                       rep(inp["diff_lambda_q2"][0]), rep(inp["diff_lambda_k2"][0])], axis=1).astype(np.float32))
    maps = []
    for core in range(8):
        b, h = core // 4, core % 4
        qk = np.concatenate([resC[b * 4 + i]["qk"] for i in range(4)], axis=1)
        q = np.concatenate([qk[h * 64:(h + 1) * 64], qk[256 + h * 64:256 + (h + 1) * 64]], axis=0)
        k = np.concatenate([qk[512 + h * 64:512 + (h + 1) * 64], qk[768 + h * 64:768 + (h + 1) * 64]], axis=0)
        v = np.concatenate([resC[b * 4 + i]["v"][:, h * 128:(h + 1) * 128] for i in range(4)], axis=0)
        maps.append(dict(q=_c(q), k=_c(k), v=_c(v.reshape(NT, 128, 128).transpose(1, 0, 2)), masks=masks,
                         tab=rep(np.asarray(inp["rel_bias"], np.float32)[:, h]), lam=lam,
                         gain=_c(np.asarray(inp["diff_subln_gain"][0], np.float32)[h * 128:(h + 1) * 128, None])))
    return maps


NE = 8
NFE = 28


E_IN = dict(xT=([1024, T], F32), yc=([512, T], BF16), zext=([512, T + 2], F32), gb=([512, T], F32), convw=([128, 4, 3], F32),
            gcols=([128, 4, 8], F32), Wout=([2, 128, 8, 512], F32), memT=([1024, 256], F32), gkv=([128, 8], F32),
            Wkvk=([2, 128, 8, 512], F32), Wkvv=([2, 128, 8, 512], F32), Wq=([2, 128, 8, 512], F32), Wo=([2, 128, 8, 512], F32),
            Wr=([128, 8, 8], F32), ident=([128, 128], F32), Wg=([NE, 14, 128, 8, 256], F32), Wu=([NE, 14, 128, 8, 256], F32),
            Wd=([NE, 8, 128, NFE, 128], F32))


def build_E(stage=4, c=None, io=None):
    if c is None:
        c = Tok("E", arena_words=14336)
        io = {n: c.din(n, sh, dt) for n, (sh, dt) in E_IN.items()}
        io["out"] = c.dout("out", [1024, T])
    nc, ph = c.nc, c.ph
    (xT_d, yc_d, zx_d, gb_d, cw_d, gcols_d, Wout_d, memT_d, gkv_d, Wkvk_d, Wkvv_d, Wq_d, Wo_d, Wr_d, id_d, Wg_d, Wu_d, Wd_d) = (io[n] for n in E_IN)
    out_d = io["out"]
    sel_d = io.get("sel")
    fused = sel_d is not None

    cw = c.sb("cw", [128, 4, 3])
    zc = c.arena[:, 5120:5120 + 4 * (CH + 2)].rearrange("p (m t) -> p m t", t=CH + 2)
    gbc = c.arena[:, 7680:9728].rearrange("p (m t) -> p m t", t=CH)
    acc = c.arena[:, 10752:11264]
    ident = c.sb("ident", [128, 128])
    ones32 = c.sb("ones32", [128, 128])
    wr32 = c.sb("wr32", [128, 8, 8])
    wrh = c.sb("wrh", [128, 8, 8], BF16)
    wrl = c.sb("wrl", [128, 8, 8], BF16)
    y1w = c.ymb[1][:].rearrange("p k t -> p (k t)").bitcast(F32)
    y0w = c.ymb[0][:].rearrange("p k t -> p (k t)").bitcast(F32)
    t32 = [y1w[:, i * CH:(i + 1) * CH] for i in range(2)]
    lg = c.sb("lg", [128, 8])
    mx8 = c.sb("mx8", [128, 8])
    nv1 = c.sb("nv1", [128, 1])
    ex = c.sb("ex", [128, 8])
    msk = c.sb("msk", [128, 8])
    den = c.sb("den", [128, 1])
    comb = c.sb("comb", [128, MC // 128, 8])
    dg = [c.sb(f"dg{i}", [128, 128]) for i in range(2)]
    cbc = [y0w[:, i * MC:(i + 1) * MC] for i in range(2)]
    tmp = [y1w[:, (2 + i) * CH:(3 + i) * CH] for i in range(2)]

    def sel_load(dst, srcs, stg, tag, res, queue="sp"):
        for b_ in range(4):
            sb_ = stg[b_ % len(stg)]
            sr_ = f"{tag}stg{b_ % len(stg)}"
            ld(ph, queue, f"l{tag}{b_ % len(stg)}", sb_, srcs[b_], [sr_])
            if b_ == 0:
                ph.I("dve", "tensor_scalar", out=dst, in0=sb_, scalar1=sel[:, 0:1], scalar2=None, op0=ALU.mult, reads=[sr_, "sel"], writes=[res])
            else:
                ph.I("dve", "scalar_tensor_tensor", out=dst, in0=sb_, scalar=sel[:, b_:b_ + 1], in1=dst, op0=ALU.mult, op1=ALU.add,
                     reads=[sr_, "sel", res], writes=[res])

    if fused:
        sel = c.sb("sel", [128, 4])
        ld(ph, "sp", "lsel", sel[:], sel_d, ["sel"])
        xstg = [c.arena[:, i * T:(i + 1) * T] for i in range(2)]
        for k in range(8):
            sel_load(c.xT[:, k, :], [xb.rearrange("(k p) t -> p k t", p=128)[:, k, :] for xb in xT_d], xstg, "x", "xT")
    else:
        ld(ph, "sp", "lx", c.xT[:], xT_d.rearrange("(k p) t -> p k t", p=128), ["xT"])
    ld(ph, "sp", "lg", c.gcols[:], gcols_d, ["gcol"])
    ld(ph, "sp", "lcw", cw[:], cw_d, ["cw"])
    ld(ph, "sp", "lid", ident[:], id_d, ["ident"])
    ld(ph, "sp", "lwr", wr32[:], Wr_d, ["wr32"])
    ph.I("dve", "memset", ones32[:], 1.0, writes=["ones32"])
    ph.I("dve", "tensor_copy", out=wrh[:], in_=wr32[:], reads=["wr32"], writes=["wrh"])
    ph.I("dve", "tensor_tensor", out=wrl[:], in0=wr32[:], in1=wrh[:], op=ALU.subtract, reads=["wr32", "wrh"], writes=["wrl"])

    fm = lambda ap: ap.rearrange("(k p) t -> p k t", p=128)
    if fused:
        zstg = [c.arena[:, i * 2056:(i + 1) * 2056].rearrange("p (m t) -> p m t", t=CH + 2) for i in range(2)]
        gstg = [c.arena[:, 11264:13312].rearrange("p (m t) -> p m t", t=CH)]
        ystg = [c.arena[:, 13312:14336].bitcast(BF16).rearrange("p (m t) -> p m t", t=CH)]
        dmy = c.sb("dmy", [128, 1])
        ph.I("dve", "memset", dmy[:], 0.0, reads=["xstg0", "xstg1"], writes=["zstg0", "zstg1"])
    for tc in range(NCH):
        yb = c.ymb[tc % 2]
        yr = f"ymb{tc%2}"
        if fused:
            sel_load(yb[:, 0:4, :], [fm(a)[:, :, tc * CH:(tc + 1) * CH] for a in yc_d], ystg, "y", yr)
            sel_load(zc, [fm(a)[:, :, tc * CH: tc * CH + CH + 2] for a in zx_d], zstg, "z", "zc")
            sel_load(gbc, [fm(a)[:, :, tc * CH:(tc + 1) * CH] for a in gb_d], gstg, "g", "gbc")
        else:
            ld(ph, "sp", f"lym{tc%2}", yb[:, 0:4, :], fm(yc_d)[:, :, tc * CH:(tc + 1) * CH], [yr])
            ld(ph, "sp", "lz", zc, fm(zx_d)[:, :, tc * CH: tc * CH + CH + 2], ["zc"])
            ld(ph, "sp", "lgb", gbc, fm(gb_d)[:, :, tc * CH:(tc + 1) * CH], ["gbc"])
        for m in range(4):
            ph.I("dve", "tensor_scalar", out=acc, in0=zc[:, m, 2:CH + 2], scalar1=cw[:, m, 2:3], scalar2=None, op0=ALU.mult,
                 reads=["zc", "cw"], writes=["acc"])
            ph.I("dve", "scalar_tensor_tensor", out=acc, in0=zc[:, m, 1:CH + 1], scalar=cw[:, m, 1:2], in1=acc, op0=ALU.mult, op1=ALU.add,
                 reads=["zc", "cw", "acc"], writes=["acc"])
            ph.I("dve", "scalar_tensor_tensor", out=acc, in0=zc[:, m, 0:CH], scalar=cw[:, m, 0:1], in1=acc, op0=ALU.mult, op1=ALU.add,
                 reads=["zc", "cw", "acc"], writes=["acc"])
            ph.I("dve", "tensor_tensor", out=yb[:, 4 + m, :], in0=acc, in1=gbc[:, m, :], op=ALU.mult, reads=["acc", "gbc"], writes=[yr])
        c.linear_fm(Wout_d, 8, 512, yb, yr, CH, c.resid_add(tc * CH))

    if stage >= 2:
        c.mem_setup(memT_d, gkv_d, Wkvk_d, Wkvv_d)
        c.mem_attn(0, Wq_d, Wo_d)

    hlo = c.xq if stage >= 2 else None
    for mc in range(T // MC if stage >= 3 else 0):
        for ts in range(MC // CH):
            cs = slice(mc * MC + ts * CH, mc * MC + (ts + 1) * CH)
            hs = slice(ts * CH, (ts + 1) * CH)
            for k in range(8):
                sqk = c.sq[k % 2]
                ph.I("act", "activation", out=sqk[:], in_=c.xT[:, k, cs], func=AF.Square, reads=["xT"], writes=[f"nsq{k%2}"])
                ph.I("pe", "matmul", c.psN[:], lhsT=c.ones_bf[:], rhs=sqk[:], start=(k == 0), stop=(k == 7), reads=[f"nsq{k%2}", "ones"], writes=["npsN"])
            ph.I("act", "activation", out=c.rstd_s[:], in_=c.psN[:], func=AF.Sqrt, scale=1.0 / 1024.0, bias=EPS, reads=["npsN"], writes=["nrstd_s"])
            ph.I("dve", "reciprocal", out=c.rstd[:], in_=c.rstd_s[:], reads=["nrstd_s"], writes=["nrstd"])
            for k in range(8):
                t = t32[k % 2]
                tr = f"t32{k%2}"
                ph.I("dve", "scalar_tensor_tensor", out=t, in0=c.xT[:, k, cs], scalar=c.gcols[:, 1, k:k + 1], in1=c.rstd[:], op0=ALU.mult, op1=ALU.mult,
                     reads=["xT", "nrstd", "gcol"], writes=[tr])
                ph.I("act", "activation", out=c.hT[:, k, hs], in_=t, func=AF.Copy, reads=[tr], writes=["hT"])
                ph.I("dve", "tensor_tensor", out=hlo[:, k, :], in0=t, in1=c.hT[:, k, hs], op=ALU.subtract, reads=[tr, "hT"], writes=["xq"])
            for s4 in range(4):
                sub = slice(ts * CH + s4 * 128, ts * CH + (s4 + 1) * 128)
                subl = slice(s4 * 128, (s4 + 1) * 128)
                ps, pres = c.nextps()
                n = 0
                for (a_, ar_, sl_, b_, br_) in ((c.hT, "hT", sub, wrh, "wrh"), (hlo, "xq", subl, wrh, "wrh"), (c.hT, "hT", sub, wrl, "wrl")):
                    for k in range(8):
                        ph.I("pe", "matmul", ps[:, 0:8], lhsT=a_[:, k, sl_], rhs=b_[:, k, :], start=(n == 0), stop=(n == 23), reads=[ar_, br_], writes=[pres])
                        n += 1
                ph.I("dve", "tensor_copy", out=lg[:], in_=ps[:, 0:8], reads=[pres], writes=["lg"])
                ph.I("dve", "max", out=mx8[:], in_=lg[:], reads=["lg"], writes=["mx8"])
                ph.I("dve", "tensor_scalar", out=nv1[:], in0=mx8[:, 0:1], scalar1=-1.0, scalar2=None, op0=ALU.mult, reads=["mx8"], writes=["nv1"])
                ph.I("act", "activation", out=ex[:], in_=lg[:], func=AF.Exp, bias=nv1[:, 0:1], reads=["lg", "nv1"], writes=["ex"])
                ph.I("dve", "tensor_scalar", out=msk[:], in0=lg[:], scalar1=mx8[:, 1:2], scalar2=None, op0=ALU.is_ge, reads=["lg", "mx8"], writes=["msk"])
                ph.I("dve", "tensor_tensor", out=ex[:], in0=ex[:], in1=msk[:], op=ALU.mult, reads=["ex", "msk"], writes=["ex"])
                ph.I("dve", "reduce_sum", out=den[:], in_=ex[:], axis=AX.X, reads=["ex"], writes=["den"])
                ph.I("dve", "reciprocal", out=den[:], in_=den[:], reads=["den"], writes=["den"])
                ph.I("dve", "tensor_scalar", out=comb[:, ts * 4 + s4, :], in0=ex[:], scalar1=den[:, 0:1], scalar2=None, op0=ALU.mult,
                     reads=["ex", "den"], writes=["comb"])
        for e in range(NE):
            cb = cbc[e % 2]
            cbr = f"cbc{e%2}"
            for s8 in range(MC // 128):
                d_ = dg[s8 % 2]
                dr = f"dg{s8%2}"
                ph.I("dve", "tensor_scalar", out=d_[:], in0=ident[:], scalar1=comb[:, s8, e:e + 1], scalar2=None, op0=ALU.mult,
                     reads=["ident", "comb"], writes=[dr])
                ph.I("pe", "matmul", c.psL[:, (s8 % 4) * 128:(s8 % 4 + 1) * 128], lhsT=ones32[:], rhs=d_[:], start=True, stop=True,
                     reads=[dr, "ones32"], writes=["psL"])
                if s8 % 4 == 3:
                    ph.I("act", "activation", out=cb[:, (s8 // 4) * CH:(s8 // 4 + 1) * CH], in_=c.psL[:], func=AF.Copy, reads=["psL"], writes=[cbr])
            for cg in range(14):
                wg, wgr = c.wload(Wg_d[e, cg], 8, 256)
                wu, wur = c.wload(Wu_d[e, cg], 8, 256)
                for mi in range(2):
                    for ts in range(MC // CH):
                        psG, pgr = c.nextps()
                        psU, pur = c.nextps()
                        for k in range(8):
                            ph.I("pe", "matmul", psG[:], lhsT=wg[:, k, mi * 128:(mi + 1) * 128], rhs=c.hT[:, k, ts * CH:(ts + 1) * CH],
                                 start=(k == 0), stop=(k == 7), reads=[wgr, "hT"], writes=[pgr])
                        for k in range(8):
                            ph.I("pe", "matmul", psU[:], lhsT=wu[:, k, mi * 128:(mi + 1) * 128], rhs=c.hT[:, k, ts * CH:(ts + 1) * CH],
                                 start=(k == 0), stop=(k == 7), reads=[wur, "hT"], writes=[pur])
                        sg = c.sg[(mi * 2 + ts) % 2]
                        sgr = f"sg{(mi * 2 + ts) % 2}"
                        ph.I("act", "activation", out=sg[:], in_=psG[:], func=AF.Silu, reads=[pgr], writes=[sgr])
                        ph.I("dve", "tensor_tensor", out=c.actT[:, cg * 2 + mi, ts * CH:(ts + 1) * CH], in0=sg[:], in1=psU[:], op=ALU.mult,
                             reads=[sgr, pur], writes=["actT"])

            def ev_down(m, ts, ps, pres, cb=cb, cbr=cbr, mc=mc):
                tm = tmp[(m * 2 + ts) % 2]
                tmr = f"tmp{(m * 2 + ts) % 2}"
                sl = slice(mc * MC + ts * CH, mc * MC + (ts + 1) * CH)
                ph.I("dve", "tensor_tensor", out=tm, in0=ps[:], in1=cb[:, ts * CH:(ts + 1) * CH], op=ALU.mult, reads=[pres, cbr], writes=[tmr])
                ph.I("pool", "tensor_tensor", out=c.xT[:, m, sl], in0=c.xT[:, m, sl], in1=tm, op=ALU.add, reads=[tmr, "xT"], writes=["xT"])
            c.linear_fm(Wd_d[e], NFE, 128, c.actT, "actT", MC, ev_down)

    fo = c.arena[:, 0:4096].rearrange("p (k t) -> p k t", t=CH)
    ph.I("dve", "memset", acc[:, 0:1], 0.0, reads=["actT"], writes=["fo"])
    if stage < 4:
        ld(ph, "sp", "sout", out_d.rearrange("(k p) t -> p k t", p=128), c.xT[:], [], reads=["xT"])
    for tc in range(NCH if stage >= 4 else 0):
        cs = slice(tc * CH, (tc + 1) * CH)
        c.norm(2, cs, fo, "fo")
        ld(ph, "sp", "sout", out_d.rearrange("(k p) t -> p k t", p=128)[:, :, cs], fo, [], reads=["fo"])
    return c.finish()


def host_E(inp, resC, resD):
    gcols = _c(np.stack([pk(inp["norm_mem_q"][1]), pk(inp["norm_ffn"][1]), pk(inp["norm_final"]), pk(inp["norm_final"])], axis=1))
    cw = np.asarray(inp["conv_w"][0], np.float32)
    convw = _c(cw.reshape(3, 4, 128).transpose(2, 1, 0))
    Wg = np.stack([wtiles(inp["moe_w_gate"][0][e], 256) for e in range(NE)])
    Wu = np.stack([wtiles(inp["moe_w_up"][0][e], 256) for e in range(NE)])
    Wd = np.stack([wtiles(inp["moe_w_down"][0][e], 128) for e in range(NE)])
    shared = dict(gcols=gcols, convw=convw, Wout=wtiles(inp["odd_w_out"][0], 512),
                  Wr=_c(np.asarray(inp["router_w"][0], np.float32).reshape(8, 128, 8).transpose(1, 0, 2)),
                  ident=np.eye(128, dtype=np.float32), Wg=Wg, Wu=Wu, Wd=Wd)
    mem = [mem_inputs(inp, 1, b) for b in range(2)]
    maps = []
    for core in range(8):
        b, i = core // 4, core % 4
        yc = np.concatenate([resD[b * 4 + h]["y"][:, i * T:(i + 1) * T] for h in range(4)], axis=0)
        z = resC[core]["z"]
        halo = resC[core - 1]["z"][:, T - 2:] if i > 0 else np.zeros((512, 2), np.float32)
        m = dict(xT=resC[core]["xo"], yc=_c(yc), zext=_c(np.concatenate([halo, z], axis=1)), gb=resC[core]["gb"])
        m.update(shared)
        m.update(mem[b])
        maps.append(m)
    return maps


def build_fused():
    nc = bass.Bass("TRN2", target_bir_lowering=False)
    (dm, notallowed) = get_diff_masks()
    nmask = len(dm[0][0]) + len(dm[-1][0]) + 1

    def din(name, shape, dt=F32):
        return nc.dram_tensor(name, list(shape), dt, kind="ExternalInput").ap()

    def scr(name, shape, dt=F32):
        return nc.dram_tensor(name, list(shape), dt, kind="Internal").ap()

    x_d = din("x", [1024, S])
    A = dict(g=din("A_g", [128, 8]), w=din("A_w", [1024, 2564]), vgain=din("A_vgain", [128, 512]), ws=din("A_ws", [128, 4, 128]),
             bs=din("A_bs", [128, 4, 512]))
    B = dict(bf=din("B_bf", [4, 64, 1]), ident=din("B_ident", [64, 64]), lmat=din("B_lmat", [64, 64]), tri=din("B_tri", [128, 128]))
    C = {n: din("C_" + n, sh, dt) for n, (sh, dt) in C_IN.items() if n not in ("xT", "ymix")}
    D = dict(masks=din("D_masks", [128, nmask, 128]), tab=din("D_tab", [4, 128, 32]), lam=din("D_lam", [128, 4, 64]),
             gain=din("D_gain", [4, 128, 1]))
    E = {n: din("E_" + n, sh, dt) for n, (sh, dt) in E_IN.items() if n not in ("xT", "yc", "zext", "gb")}
    sel_d = din("E_sel", [128, 4])
    out_d = nc.dram_tensor("out", [1024, T], F32, kind="ExternalOutput").ap()

    q_s, k_s, ya_s, yb_s, yc_s = (scr(n, [512, S], BF16) for n in ("q_s", "k_s", "ya_s", "yb_s", "yc_s"))
    v_s, v1_s = scr("v_s", [S, 512], BF16), scr("v1_s", [S, 512], BF16)
    f_s = scr("f_s", [4, S])
    x1_s = scr("x1_s", [1024, S])
    qk_s = scr("qk_s", [1024, S], BF16)
    z_s = scr("z_s", [512, S + 2])
    gb_s = scr("gb_s", [512, S])

    c0 = Ctx("Z", nc=nc)
    zt = c0.sb("zt", [128, 4, 2])
    c0.ph.I("dve", "memset", zt[:], 0.0, writes=["zt"])
    ld(c0.ph, "sp", "sz", z_s.rearrange("(m p) t -> p m t", p=128)[:, :, 0:2], zt[:], [], reads=["zt"])
    c0.finish()

    for blk in range(4):
        cols = slice(blk * T, (blk + 1) * T)
        io = dict(A)
        io.update(xT=x_d[:, cols], ya=ya_s[:, cols], q=q_s[:, cols], k=k_s[:, cols], v=v_s[cols, :], f=f_s[:, cols])
        build_A(Ctx(f"A{blk}", nc=nc), io)
    for h in range(4):
        rows = slice(h * 128, (h + 1) * 128)
        io = dict(q=q_s[rows, :], k=k_s[rows, :], v=v_s.rearrange("(j p) c -> p j c", p=128)[:, :, rows],
                  f2=f_s[h].rearrange("(j p) -> j p", p=128), bf=B["bf"][h], ident=B["ident"], lmat=B["lmat"], tri=B["tri"], y=yb_s[rows, :])
        build_B(Ctx(f"B{h}", nc=nc), io)
    for blk in range(4):
        cols = slice(blk * T, (blk + 1) * T)
        io = dict(C)
        io.update(xT=x_d[:, cols], ymix=(ya_s[:, cols], yb_s[:, cols]), xo=x1_s[:, cols], qk=qk_s[:, cols], v=v1_s[cols, :],
                  z=z_s[:, 2 + blk * T: 2 + (blk + 1) * T], gb=gb_s[:, cols])
        build_C(Tok(f"C{blk}", nc=nc), io)
    for h in range(4):
        r64 = lambda base: qk_s[base + h * 64: base + (h + 1) * 64, :]
        io = dict(q=(r64(0), r64(256)), k=(r64(512), r64(768)), v=v1_s.rearrange("(j p) c -> p j c", p=128)[:, :, h * 128:(h + 1) * 128],
                  masks=D["masks"], tab=D["tab"][h], lam=D["lam"], gain=D["gain"][h], y=yc_s[h * 128:(h + 1) * 128, :])
        build_D(Ctx(f"D{h}", nc=nc), io)
    io = dict(E)
    io.update(xT=[x1_s[:, b * T:(b + 1) * T] for b in range(4)], yc=[yc_s[:, b * T:(b + 1) * T] for b in range(4)],
              zext=[z_s[:, b * T: b * T + T + 2] for b in range(4)], gb=[gb_s[:, b * T:(b + 1) * T] for b in range(4)],
              sel=sel_d, out=out_d)
    build_E(4, Tok("E", arena_words=14336, nc=nc), io)
    return nc


def host_fused(inp):
    x = np.asarray(inp["x"], np.float32)
    (dm, notallowed) = get_diff_masks()
    masks = _c(np.concatenate([dm[0][1], dm[-1][1], notallowed[None]], axis=0).transpose(1, 0, 2))
    bs4 = rep(np.tile(np.asarray(inp["gmlp_b_s"][0], np.float32), (1, 4)))
    sh = {
        "A_g": pk(inp["norm_mix"][0]), "A_w": _c(np.asarray(inp["even_w_in"][0], np.float32)),
        "A_vgain": rep(np.asarray(inp["gmlp_v_gain"][0], np.float32)),
        "A_ws": _c(np.transpose(np.asarray(inp["gmlp_w_s"][0], np.float32), (2, 0, 1))), "A_bs": bs4,
        "B_bf": _c(np.broadcast_to(np.asarray(inp["fox_b_f"][0], np.float32)[:, None, None], (4, 64, 1))),
        "B_ident": np.eye(64, dtype=np.float32), "B_lmat": np.triu(np.ones((64, 64), np.float32), 1),
        "B_tri": np.triu(np.ones((128, 128), np.float32), 0),
        "C_gcols": _c(np.stack([pk(inp["norm_mem_q"][0]), pk(inp["norm_ffn"][0]), pk(inp["norm_mix"][1]), pk(inp["norm_mix"][1])], axis=1)),
        "C_Wout": wtiles(inp["even_w_out"][0], 512), "C_Wg": wtiles(inp["ffn_w_gate"][0], 256), "C_Wu": wtiles(inp["ffn_w_up"][0], 256),
        "C_Wd": wtiles(inp["ffn_w_down"][0], 128), "C_Win": wtiles(inp["odd_w_in"][0], 512),
        "D_masks": masks,
        "D_tab": _c(np.stack([rep(np.asarray(inp["rel_bias"], np.float32)[:, h]) for h in range(4)])),
        "D_lam": _c(np.stack([rep(inp["diff_lambda_q1"][0]), rep(inp["diff_lambda_k1"][0]),
                              rep(inp["diff_lambda_q2"][0]), rep(inp["diff_lambda_k2"][0])], axis=1).astype(np.float32)),
        "D_gain": _c(np.asarray(inp["diff_subln_gain"][0], np.float32).reshape(4, 128, 1)),
        "E_convw": _c(np.asarray(inp["conv_w"][0], np.float32).reshape(3, 4, 128).transpose(2, 1, 0)),
        "E_gcols": _c(np.stack([pk(inp["norm_mem_q"][1]), pk(inp["norm_ffn"][1]), pk(inp["norm_final"]), pk(inp["norm_final"])], axis=1)),
        "E_Wout": wtiles(inp["odd_w_out"][0], 512),
        "E_Wr": _c(np.asarray(inp["router_w"][0], np.float32).reshape(8, 128, 8).transpose(1, 0, 2)),
        "E_ident": np.eye(128, dtype=np.float32),
        "E_Wg": np.stack([wtiles(inp["moe_w_gate"][0][e], 256) for e in range(NE)]),
        "E_Wu": np.stack([wtiles(inp["moe_w_up"][0][e], 256) for e in range(NE)]),
        "E_Wd": np.stack([wtiles(inp["moe_w_down"][0][e], 128) for e in range(NE)]),
    }
    memC = [mem_inputs(inp, 0, b) for b in range(2)]
    memE = [mem_inputs(inp, 1, b) for b in range(2)]
    xT = [_c(x[b].T) for b in range(2)]
    maps = []
    for core in range(8):
        b, i = core // 4, core % 4
        m = dict(sh)
        m["x"] = xT[b]
        for k_, v_ in memC[b].items():
            m["C_" + k_] = v_
        for k_, v_ in memE[b].items():
            m["E_" + k_] = v_
        selv = np.zeros((128, 4), np.float32)
        selv[:, i] = 1.0
        m["E_sel"] = selv
        maps.append(m)
    return maps


def kernel(**inp):
    inp = {k: np.asarray(v) for k, v in inp.items()}
    res = run(get_nc("F", build_fused), host_fused(inp))
    out = np.empty((2, 8192, 1024), np.float32)
    for core in range(8):
        b, i = core // 4, core % 4
        out[b, i * T:(i + 1) * T] = res[core]["out"].T
    return out
```

```python
import contextlib
from concourse.bass_utils import run_bass_kernel_spmd
import numpy as np
import concourse.bass as bass
import concourse.mybir as mybir

F32 = mybir.dt.float32
BF16 = mybir.dt.bfloat16
F32R = mybir.dt.float32r
I32 = mybir.dt.int32
U32 = mybir.dt.uint32
AF = mybir.ActivationFunctionType
ALU = mybir.AluOpType
AX = mybir.AxisListType

ENGS = ("pe", "act", "dve", "pool", "sp")


class Phase:
    def __init__(self, nc, name):
        self.nc = nc
        self.name = name
        self.ops = []
        self.last_writer = {}
        self.readers = {}
        self.dma_keys = {}

    def _add(self, eng, fn, reads, writes, dma_key=None, ndma=0):
        idx = len(self.ops)
        deps = set()
        for r in reads:
            w = self.last_writer.get(r)
            if w is not None:
                deps.add(w)
        for r in writes:
            w = self.last_writer.get(r)
            if w is not None:
                deps.add(w)
            for rd in self.readers.get(r, ()):
                deps.add(rd)
        deps.discard(idx)
        for r in writes:
            self.last_writer[r] = idx
            self.readers[r] = []
        for r in reads:
            if r not in writes:
                self.readers.setdefault(r, []).append(idx)
        self.ops.append(dict(eng=eng, fn=fn, deps=deps, dma_key=dma_key, ndma=ndma,
                             signal=False))
        return idx

    def op(self, eng, fn, reads=(), writes=()):
        return self._add(eng, fn, tuple(reads), tuple(writes))

    def I(self, eng, meth, *a, reads=(), writes=(), **kw):
        return self.op(eng, lambda e: getattr(e, meth)(*a, **kw), reads, writes)

    def dma(self, queue, fn, key, reads=(), writes=(), n=1):
        return self._add(queue, fn, tuple(reads), tuple(writes), dma_key=key, ndma=n)

    def emit(self):
        nc = self.nc
        ops = self.ops
        pos = {}
        cnt = {e: 0 for e in ENGS}
        for i, o in enumerate(ops):
            pos[i] = cnt[o["eng"]]
            cnt[o["eng"]] += 1
        for i, o in enumerate(ops):
            need = set()
            for d in o["deps"]:
                po = ops[d]
                if po["dma_key"] is not None:
                    need.add(d)
                elif po["eng"] == o["eng"]:
                    if o["eng"] != "pe" and pos[i] - pos[d] <= 2 and o["dma_key"] is None:
                        need.add(d)
                    elif o["dma_key"] is not None:
                        need.add(d)
                else:
                    need.add(d)
            o["need"] = need
            for d in need:
                if ops[d]["dma_key"] is None:
                    ops[d]["signal"] = True
        sigcnt = {e: 0 for e in ENGS}
        for o in ops:
            if o["dma_key"] is None and o["signal"]:
                sigcnt[o["eng"]] += 1
                o["sigval"] = sigcnt[o["eng"]]
        dmacnt = {}
        for o in ops:
            if o["dma_key"] is not None:
                dmacnt[o["dma_key"]] = dmacnt.get(o["dma_key"], 0) + o["ndma"]
                o["sigval"] = 16 * dmacnt[o["dma_key"]]
        keys = sorted(dmacnt.keys(), key=str)
        import contextlib
        esem = {e: nc.alloc_semaphore(name=f"{self.name}_{e}") for e in ENGS}
        dsem = {k: nc.alloc_semaphore(name=f"{self.name}_d{j}") for j, k in enumerate(keys)}
        with contextlib.ExitStack() as st:
            block = st.enter_context(nc.Block())
            per_eng = {e: [] for e in ENGS}
            for i, o in enumerate(ops):
                per_eng[o["eng"]].append(i)

            def run(engname, eng):
                seen = {}
                for i in per_eng[engname]:
                    o = ops[i]
                    waits = {}
                    for d in o["need"]:
                        po = ops[d]
                        if po["dma_key"] is not None:
                            k = ("d", po["dma_key"])
                        else:
                            k = ("e", po["eng"])
                        waits[k] = max(waits.get(k, 0), po["sigval"])
                    for k, v in waits.items():
                        if seen.get(k, 0) >= v:
                            continue
                        seen[k] = v
                        s = dsem[k[1]] if k[0] == "d" else esem[k[1]]
                        eng.wait_ge(s, v)
                    if o["dma_key"] is not None:
                        o["fn"](eng, dsem[o["dma_key"]])
                    else:
                        ins = o["fn"](eng)
                        if o["signal"]:
                            ins.then_inc(esem[engname], 1)
                fin = {}
                for i in per_eng[engname]:
                    o = ops[i]
                    if o["dma_key"] is not None:
                        fin[o["dma_key"]] = max(fin.get(o["dma_key"], 0), o["sigval"])
                for k, v in fin.items():
                    if seen.get(("d", k), 0) < v:
                        eng.wait_ge(dsem[k], v)

            @block.tensor
            def _(e):
                run("pe", e)

            @block.scalar
            def _(e):
                run("act", e)

            @block.vector
            def _(e):
                run("dve", e)

            @block.gpsimd
            def _(e):
                run("pool", e)

            @block.sync
            def _(e):
                run("sp", e)
        nc.clear_and_free_semaphores(list(esem.values()) + list(dsem.values()))
        nc.all_engine_barrier()


T = 2048
CH = 512
NCH = T // CH
EPS = 1e-6
NEG = -30000.0


class Ctx:
    def __init__(self, name, nc=None):
        self.nc = nc if nc is not None else bass.Bass("TRN2", target_bir_lowering=False)
        self.name = name
        self.st = contextlib.ExitStack()
        self.ph = Phase(self.nc, name)
        self.n = 0

    def din(self, name, shape, dt=F32):
        return self.nc.dram_tensor(name, list(shape), dt, kind="ExternalInput").ap()

    def dout(self, name, shape, dt=F32):
        return self.nc.dram_tensor(name, list(shape), dt, kind="ExternalOutput").ap()

    def sb(self, name, shape, dt=F32):
        return self.st.enter_context(self.nc.sbuf_tensor("s_" + self.name + "_" + name, list(shape), dt))

    def psum(self, name):
        return self.st.enter_context(self.nc.psum_tensor("p_" + self.name + "_" + name, [128, 512], F32))

    def finish(self):
        self.ph.emit()
        self.st.close()
        return self.nc


def ld(ph, queue, key, out_ap, in_ap, writes, reads=()):
    ph.dma(queue, lambda e, s: e.dma_start(out=out_ap, in_=in_ap).then_inc(s, 16), key,
           reads=reads, writes=writes)


def rmsnorm_fm(ph, tag, src, cs, gcol, xn, ones_bf, sq, psN, rstd_s, rstd, src_res, xn_res,
               nk=8, dim=1024.0):
    for k in range(nk):
        sqk = sq[k % 2]
        ph.op("act", (lambda k, sqk: lambda e: e.activation(out=sqk[:], in_=src[:, k, cs], func=AF.Square))(k, sqk),
              reads=[src_res], writes=[f"{tag}sq{k%2}"])
        ph.op("pe", (lambda k, sqk: lambda e: e.matmul(psN[:], lhsT=ones_bf[:], rhs=sqk[:], start=(k == 0), stop=(k == nk - 1)))(k, sqk),
              reads=[f"{tag}sq{k%2}", "ones"], writes=[f"{tag}psN"])
    ph.op("act", lambda e: e.activation(out=rstd_s[:], in_=psN[:], func=AF.Sqrt, scale=1.0 / dim, bias=EPS),
          reads=[f"{tag}psN"], writes=[f"{tag}rstd_s"])
    ph.op("dve", lambda e: e.reciprocal(out=rstd[:], in_=rstd_s[:]), reads=[f"{tag}rstd_s"], writes=[f"{tag}rstd"])
    for k in range(nk):
        ph.op("dve", (lambda k: lambda e: e.scalar_tensor_tensor(out=xn[:, k, :], in0=src[:, k, cs], scalar=gcol[:, k:k + 1],
                                                                  in1=rstd[:], op0=ALU.mult, op1=ALU.mult))(k),
              reads=[src_res, f"{tag}rstd", "gcol"], writes=[xn_res])


def build_A(c=None, io=None):
    if c is None:
        c = Ctx("A")
        io = dict(xT=c.din("xT", [1024, T]), g=c.din("g", [128, 8]), w=c.din("w", [1024, 2564]), vgain=c.din("vgain", [128, 512]),
                  ws=c.din("ws", [128, 4, 128]), bs=c.din("bs", [128, 4, 512]),
                  ya=c.dout("ya", [512, T], BF16), q=c.dout("q", [512, T], BF16), k=c.dout("k", [512, T], BF16),
                  v=c.dout("v", [T, 512], BF16), f=c.dout("f", [4, T], F32))
    nc, ph = c.nc, c.ph
    xT_d, g_d, w_d, vg_d, ws_d, bs_d = io["xT"], io["g"], io["w"], io["vgain"], io["ws"], io["bs"]
    ya_d, q_d, k_d, v_d, f_d = io["ya"], io["q"], io["k"], io["v"], io["f"]

    xT = c.sb("xT", [128, 8, T])
    w = c.sb("w", [128, 8, 2564], BF16)
    gcol = c.sb("gcol", [128, 8])
    ones_bf = c.sb("ones", [128, 128], BF16)
    sq = [c.sb(f"sq{i}", [128, CH], BF16) for i in range(2)]
    rstd_s = c.sb("rstd_s", [128, CH])
    rstd = c.sb("rstd", [128, CH])
    xn = c.sb("xn", [128, 8, CH], BF16)
    uT = c.sb("uT", [128, 4, CH])
    vgain = c.sb("vgain", [128, 512])
    wsm = c.sb("wsm", [128, 4, 128], BF16)
    bsb = c.sb("bsb", [128, 4, 512])
    vg = c.sb("vg", [128, 512])
    vscr = c.sb("vscr", [128, 128])
    ss = c.sb("ss", [128, 4])
    ssr = c.sb("ssr", [128, 4])
    vn = [c.sb(f"vn{i}", [128, 512], BF16) for i in range(2)]
    tmix = c.sb("tmix", [128, 512])
    yast = c.sb("yast", [128, 4, CH], BF16)
    qst = c.sb("qst", [128, 4, CH], BF16)
    kst = c.sb("kst", [128, 4, CH], BF16)
    vst = c.sb("vst", [128, 4, 512], BF16)
    fst = c.sb("fst", [4, CH])
    psN = c.psum("psN")
    psP = [c.psum(f"psP{i}") for i in range(3)]
    psM = [c.psum(f"psM{i}") for i in range(4)]

    ld(ph, "sp", "lx", xT[:], xT_d.rearrange("(k p) t -> p k t", p=128), ["xT"])
    ld(ph, "sp", "lg", gcol[:], g_d, ["gcol"])
    ld(ph, "sp", "lvg", vgain[:], vg_d, ["vgain"])
    ld(ph, "sp", "lbs", bsb[:], bs_d, ["bsb"])
    wv = w_d.rearrange("(k p) c -> p k c", p=128)
    for gi, (a, b) in enumerate([(0, 512), (512, 1024), (1024, 1536), (1536, 2048), (2048, 2564)]):
        ld(ph, "pool", f"lw{gi}", w[:, :, a:b], wv[:, :, a:b], [f"w{gi}"])
    ld(ph, "pool", "lws", wsm[:], ws_d, ["wsm"])
    ph.op("dve", lambda e: e.memset(ones_bf[:], 1.0), writes=["ones"])
    ph.op("dve", lambda e: e.memset(wsm[64:128, :, 0:64], 0.0), reads=["wsm"], writes=["wsm"])

    pi = [0]

    def nextps():
        pi[0] = (pi[0] + 1) % 3
        return psP[pi[0]], f"psP{pi[0]}"

    def proj_fm(col0, m, wres):
        ps, pres = nextps()
        for k in range(8):
            ph.op("pe", (lambda k, ps: lambda e: e.matmul(ps[:], lhsT=w[:, k, col0 + m * 128: col0 + (m + 1) * 128], rhs=xn[:, k, :],
                                                           start=(k == 0), stop=(k == 7)))(k, ps),
                  reads=["xn", wres], writes=[pres])
        return ps, pres

    for tc in range(NCH):
        cs = slice(tc * CH, (tc + 1) * CH)
        rmsnorm_fm(ph, "n", xT, cs, gcol, xn, ones_bf, sq, psN, rstd_s, rstd, "xT", "xn")
        for m in range(4):
            ps, pres = proj_fm(0, m, "w0")
            ph.op("act", (lambda m, ps: lambda e: e.activation(out=uT[:, m, :], in_=ps[:], func=AF.Gelu_apprx_tanh))(m, ps),
                  reads=[pres], writes=[f"uT{m}"])
        for m in range(4):
            ps, pres = proj_fm(1024, m, "w2")
            ph.op("dve", (lambda m, ps: lambda e: e.tensor_copy(out=qst[:, m, :], in_=ps[:]))(m, ps), reads=[pres], writes=["qst"])
        ld(ph, "sp", "sq", q_d.rearrange("(m p) t -> p m t", p=128)[:, :, cs], qst[:], [], reads=["qst"])
        for m in range(4):
            ps, pres = proj_fm(1536, m, "w3")
            ph.op("act", (lambda m, ps: lambda e: e.activation(out=kst[:, m, :], in_=ps[:], func=AF.Copy))(m, ps), reads=[pres], writes=["kst"])
        ld(ph, "sp", "sk", k_d.rearrange("(m p) t -> p m t", p=128)[:, :, cs], kst[:], [], reads=["kst"])
        ps, pres = nextps()
        for k in range(8):
            ph.op("pe", (lambda k, ps: lambda e: e.matmul(ps[0:4, :], lhsT=w[:, k, 2560:2564], rhs=xn[:, k, :], start=(k == 0), stop=(k == 7)))(k, ps),
                  reads=["xn", "w4"], writes=[pres])
        ph.op("dve", (lambda ps: lambda e: e.tensor_copy(out=fst[:], in_=ps[0:4, :]))(ps), reads=[pres], writes=["fst"])
        ld(ph, "sp", "sf", f_d[:, cs], fst[:], [], reads=["fst"])
        for s in range(4):
            ts_ = slice(s * 128, (s + 1) * 128)
            ps, pres = nextps()
            for k in range(8):
                ph.op("pe", (lambda k, ps, ts_: lambda e: e.matmul(ps[:], lhsT=xn[:, k, ts_], rhs=w[:, k, 512:1024], start=(k == 0), stop=(k == 7)))(k, ps, ts_),
                      reads=["xn", "w1"], writes=[pres])
            ph.op("act", (lambda ps: lambda e: e.activation(out=vg[:], in_=ps[:], func=AF.Gelu_apprx_tanh))(ps), reads=[pres], writes=["vg"])
            for g in range(4):
                ph.op("act", (lambda g: lambda e: e.activation(out=vscr[:], in_=vg[:, g * 128:(g + 1) * 128], func=AF.Square,
                                                               accum_out=ss[:, g:g + 1]))(g),
                      reads=["vg"], writes=["vscr", f"ss{g}"])
            ph.op("act", lambda e: e.activation(out=ssr[:], in_=ss[:], func=AF.Sqrt, scale=1.0 / 128.0, bias=EPS),
                  reads=[f"ss{g}" for g in range(4)], writes=["ssr"])
            ph.op("dve", lambda e: e.reciprocal(out=ss[:], in_=ssr[:]), reads=["ssr"], writes=[f"ss{g}" for g in range(4)])
            vnb = vn[s % 2]
            for g in range(4):
                gs = slice(g * 128, (g + 1) * 128)
                ph.op("dve", (lambda g, gs, vnb: lambda e: e.scalar_tensor_tensor(out=vnb[:, gs], in0=vg[:, gs], scalar=ss[:, g:g + 1], in1=vgain[:, gs],
                                                                                   op0=ALU.mult, op1=ALU.mult))(g, gs, vnb),
                      reads=["vg", f"ss{g}", "vgain"], writes=[f"vn{s%2}"])
            for g in range(4):
                gs = slice(g * 128, (g + 1) * 128)
                ph.op("pe", (lambda g, gs, vnb, ts_: lambda e: e.matmul(psM[g][:, ts_], lhsT=vnb[:, gs], rhs=wsm[:, g, :], start=True, stop=True))(g, gs, vnb, ts_),
                      reads=[f"vn{s%2}", "wsm"], writes=[f"psM{g}"])
            ps, pres = nextps()
            for k in range(8):
                ph.op("pe", (lambda k, ps, ts_: lambda e: e.matmul(ps[:], lhsT=xn[:, k, ts_], rhs=w[:, k, 2048:2560], start=(k == 0), stop=(k == 7)))(k, ps, ts_),
                      reads=["xn", "w4"], writes=[pres])
            ph.op("dve", (lambda ps, s: lambda e: e.tensor_copy(out=vst[:, s, :], in_=ps[:]))(ps, s), reads=[pres], writes=["vst"])
        ld(ph, "sp", "sv", v_d.rearrange("(n p) c -> p n c", p=128)[:, tc * 4:(tc + 1) * 4, :], vst[:], [], reads=["vst"])
        for g in range(4):
            ph.op("dve", (lambda g: lambda e: e.tensor_tensor(out=tmix[:], in0=psM[g][:], in1=bsb[:, g, :], op=ALU.add))(g),
                  reads=[f"psM{g}", "bsb"], writes=["tmix"])
            ph.op("dve", (lambda g: lambda e: e.tensor_tensor(out=yast[:, g, :], in0=tmix[:], in1=uT[:, g, :], op=ALU.mult))(g),
                  reads=["tmix", f"uT{g}"], writes=["yast"])
        ld(ph, "sp", "sy", ya_d.rearrange("(m p) t -> p m t", p=128)[:, :, cs], yast[:], [], reads=["yast"])
    return c.finish()


def _c(a):
    return np.ascontiguousarray(a)


def pk(vec):
    return _c(np.asarray(vec).reshape(-1, 128).T)


def rep(vec, n=128):
    return _c(np.broadcast_to(np.asarray(vec)[None], (n,) + tuple(np.asarray(vec).shape)))


def core_tokens(xfull, core):
    b, i = core // 4, core % 4
    return xfull[b, i * T:(i + 1) * T]


_NC_CACHE = {}


def get_nc(name, builder):
    if name not in _NC_CACHE:
        _NC_CACHE[name] = builder()
    return _NC_CACHE[name]


def run(nc, in_maps):
    res = run_bass_kernel_spmd(nc, in_maps, core_ids=list(range(8)))
    return res.results


def host_A(inp):
    x = np.asarray(inp["x"], np.float32)
    maps = []
    ws = _c(np.transpose(np.asarray(inp["gmlp_w_s"][0], np.float32), (2, 0, 1)))
    bs = np.asarray(inp["gmlp_b_s"][0], np.float32)
    bs4 = rep(np.tile(bs, (1, 4)))
    for core in range(8):
        maps.append(dict(
            xT=_c(core_tokens(x, core).T),
            g=pk(inp["norm_mix"][0]),
            w=_c(np.asarray(inp["even_w_in"][0], np.float32)),
            vgain=rep(np.asarray(inp["gmlp_v_gain"][0], np.float32)),
            ws=ws, bs=bs4))
    return maps


S = 8192
NT = S // 128
NG = S // CH


def build_B(c=None, io=None):
    if c is None:
        c = Ctx("B")
        io = dict(q=c.din("q", [128, S], BF16), k=c.din("k", [128, S], BF16), v=c.din("v", [128, NT, 128], BF16),
                  f2=c.din("f2", [64, 128]), bf=c.din("bf", [64, 1]), ident=c.din("ident", [64, 64]), lmat=c.din("lmat", [64, 64]),
                  tri=c.din("tri", [128, 128]), y=c.dout("y", [128, S], BF16))
    nc, ph = c.nc, c.ph
    q_d, k_d, v_d, f_d, bf_d, id_d, L_d, tri_d, y_d = (io[n] for n in ("q", "k", "v", "f2", "bf", "ident", "lmat", "tri", "y"))

    qT = c.sb("qT", [128, S], BF16)
    kT = c.sb("kT", [128, S], BF16)
    V = c.sb("V", [128, NT, 128], BF16)
    f2 = c.sb("f2", [64, 128])
    bfc = c.sb("bfc", [64, 1])
    ident = c.sb("ident", [64, 64])
    lmat = c.sb("lmat", [64, 64])
    tri = c.sb("tri", [128, 128], BF16)
    ones_bf = c.sb("ones", [128, 128], BF16)
    ones64 = c.sb("ones64", [64, 128])
    cs2 = c.sb("cs2", [64, 128])
    off = c.sb("off", [64, 1])
    dg = c.sb("dg", [64, 64])
    dcol = c.sb("dcol", [128, 64])
    dref = c.sb("dref", [128, 64])
    biasm = c.sb("biasm", [128, NT // 2, NT])
    PT = [c.sb(f"PT{i}", [128, CH], BF16) for i in range(4)]
    rl = c.sb("rl", [128, CH])
    ost = [c.sb(f"ost{i}", [128, CH], BF16) for i in range(2)]
    psS = [c.psum(f"psS{i}") for i in range(3)]
    psO = [c.psum(f"psO{i}") for i in range(2)]
    psL = [c.psum(f"psL{i}") for i in range(2)]
    psX = c.psum("psX")

    ld(ph, "sp", "l1", f2[:], f_d, ["f2"])
    ld(ph, "sp", "l2", bfc[:], bf_d, ["bfc"])
    ld(ph, "sp", "l3", ident[:], id_d, ["ident"])
    ld(ph, "sp", "l4", lmat[:], L_d, ["lmat"])
    ld(ph, "pool", "l5", tri[:], tri_d, ["tri"])
    ld(ph, "sp", "lq", qT[:], q_d, ["qT"])
    ld(ph, "act", "lk", kT[:], k_d, ["kT"])
    ld(ph, "sp", "lv", V[:], v_d, ["V"])
    ph.op("dve", lambda e: e.memset(ones_bf[:], 1.0), writes=["ones"])
    ph.op("dve", lambda e: e.memset(ones64[:], 1.0), writes=["ones64"])

    ph.op("dve", lambda e: e.tensor_scalar(out=bfc[:], in0=bfc[:], scalar1=-1.0, scalar2=None, op0=ALU.mult), reads=["bfc"], writes=["bfc"])
    ph.op("act", lambda e: e.activation(out=f2[:], in_=f2[:], func=AF.Exp, scale=-1.0, bias=bfc[:, 0:1]), reads=["f2", "bfc"], writes=["f2"])
    ph.op("act", lambda e: e.activation(out=f2[:], in_=f2[:], func=AF.Ln, scale=1.0, bias=1.0), reads=["f2"], writes=["f2"])
    ph.op("dve", lambda e: e.tensor_tensor_scan(out=cs2[:], data0=f2[:], data1=f2[:], initial=0.0, op0=ALU.add, op1=ALU.max),
          reads=["f2"], writes=["cs2"])
    ph.op("pe", lambda e: e.matmul(psX[0:64, 0:1], lhsT=lmat[:], rhs=cs2[:, 127:128], start=True, stop=True), reads=["cs2", "lmat"], writes=["psX"])
    ph.op("dve", lambda e: e.tensor_copy(out=off[:], in_=psX[0:64, 0:1]), reads=["psX"], writes=["off"])
    ph.op("dve", lambda e: e.tensor_scalar(out=cs2[:], in0=cs2[:], scalar1=off[:, 0:1], scalar2=None, op0=ALU.add), reads=["cs2", "off"], writes=["cs2"])
    ph.op("pe", lambda e: e.transpose(psX[:, 0:64], cs2[:], ident[:]), reads=["cs2", "ident", "off"], writes=["psX"])
    ph.op("dve", lambda e: e.tensor_copy(out=dcol[:], in_=psX[:, 0:64]), reads=["psX"], writes=["dcol"])
    ph.op("dve", lambda e: e.tensor_scalar(out=dg[:], in0=ident[:], scalar1=cs2[:, 0:1], scalar2=None, op0=ALU.mult), reads=["cs2", "ident"], writes=["dg"])
    ph.op("pe", lambda e: e.matmul(psX[:, 64:128], lhsT=ones64[:], rhs=dg[:], start=True, stop=True), reads=["dg", "ones64", "dcol"], writes=["psX"])
    ph.op("dve", lambda e: e.tensor_copy(out=dref[:], in_=psX[:, 64:128]), reads=["psX"], writes=["dref"])
    for k in range(NT // 2):
        ph.I("dve", "tensor_scalar", out=biasm[:, k, 0:2 * k + 2], in0=dcol[:, 0:2 * k + 2], scalar1=dref[:, 2 * k + 1:2 * k + 2], scalar2=None,
             op0=ALU.subtract, reads=["dcol", "dref"], writes=["biasm"])

    scale = 128.0 ** -0.5
    LA = 2
    its = [(I, j) for I in range(NG) for j in range(4 * I + 4)]
    NIT = len(its)
    NPT = len(PT)

    def front(n):
        I, j = its[n]
        qs = slice(I * CH, (I + 1) * CH)
        pS, pr = psS[n % 3], f"psS{n%3}"
        P, sr = PT[n % NPT], f"PT{n%NPT}"
        ph.I("pe", "matmul", pS[:], lhsT=kT[:, j * 128:(j + 1) * 128], rhs=qT[:, qs], start=True, stop=True, reads=["kT", "qT"], writes=[pr])
        jj = max(j - 4 * I, 0)
        for kk in range(2):
            lo, hi = max(jj * 128, kk * 256), (kk + 1) * 256
            if lo >= hi:
                continue
            ph.I("act", "activation", out=P[:, lo:hi], in_=pS[:, lo:hi], func=AF.Exp, scale=scale, bias=biasm[:, 2 * I + kk, j:j + 1],
                 reads=[pr, "biasm"], writes=[sr])
        if j >= 4 * I:
            bs_ = slice(jj * 128, (jj + 1) * 128)
            ph.I("pool", "tensor_tensor", out=P[:, bs_], in0=P[:, bs_], in1=tri[:], op=ALU.mult, reads=[sr, "tri"], writes=[sr])

    def back(n):
        I, j = its[n]
        qs = slice(I * CH, (I + 1) * CH)
        last = 4 * I + 3
        P, sr = PT[n % NPT], f"PT{n%NPT}"
        pO, pL = psO[I % 2], psL[I % 2]
        c0 = max(j - 4 * I, 0) * 128
        ph.I("pe", "matmul", pO[:, c0:], lhsT=V[:, j, :], rhs=P[:, c0:], start=(j == 0), stop=(j == last), reads=[sr, "V"], writes=[f"psO{I%2}"])
        ph.I("pe", "matmul", pL[:, c0:], lhsT=ones_bf[:], rhs=P[:, c0:], start=(j == 0), stop=(j == last), reads=[sr, "ones"], writes=[f"psL{I%2}"])
        if j == last:
            o = ost[I % 2]
            ph.I("dve", "reciprocal", out=rl[:], in_=pL[:], reads=[f"psL{I%2}"], writes=["rl"])
            ph.I("dve", "tensor_tensor", out=o[:], in0=pO[:], in1=rl[:], op=ALU.mult, reads=[f"psO{I%2}", "rl"], writes=[f"ost{I%2}"])
            ld(ph, "sp", f"so{I%2}", y_d[:, qs], o[:], [], reads=[f"ost{I%2}"])

    for n in range(NIT + LA):
        if n < NIT:
            front(n)
        if n - LA >= 0:
            back(n - LA)
    return c.finish()


def host_B(inp, resA):
    maps = []
    ident = np.eye(64, dtype=np.float32)
    lmat = np.triu(np.ones((64, 64), np.float32), 1)
    tri = np.triu(np.ones((128, 128), np.float32), 0)
    for core in range(8):
        b, h = core // 4, core % 4
        q = np.concatenate([resA[b * 4 + i]["q"][h * 128:(h + 1) * 128] for i in range(4)], axis=1)
        k = np.concatenate([resA[b * 4 + i]["k"][h * 128:(h + 1) * 128] for i in range(4)], axis=1)
        v = np.concatenate([resA[b * 4 + i]["v"][:, h * 128:(h + 1) * 128] for i in range(4)], axis=0)
        f = np.concatenate([resA[b * 4 + i]["f"][h] for i in range(4)], axis=0)
        maps.append(dict(q=_c(q), k=_c(k), v=_c(v.reshape(NT, 128, 128).transpose(1, 0, 2)),
                         f2=_c(f.reshape(64, 128)), bf=np.full((64, 1), inp["fox_b_f"][0][h], np.float32),
                         ident=ident, lmat=lmat, tri=tri))
    return maps


MC = 1024
M_SCALE = 256.0 ** -0.5


def wtiles(W, ncols):
    W = np.asarray(W, np.float32)
    K_, N_ = W.shape
    return _c(W.reshape(K_ // 128, 128, N_ // ncols, ncols).transpose(2, 1, 0, 3))


class Tok(Ctx):
    def __init__(self, name, nwb=4, wb_elems=4096, arena_words=11264, nc=None):
        super().__init__(name, nc=nc)
        c = self
        self.xT = c.sb("xT", [128, 8, T])
        self.ones_bf = c.sb("ones", [128, 128], BF16)
        self.sq = [c.sb(f"sq{i}", [128, CH], BF16) for i in range(2)]
        self.rstd_s = c.sb("rstd_s", [128, CH])
        self.rstd = c.sb("rstd", [128, CH])
        self.gcols = c.sb("gcols", [128, 4, 8])
        self.wb = [c.sb(f"wb{i}", [128, wb_elems], BF16) for i in range(nwb)]
        self.wi = 0
        self.arena = c.sb("arena", [128, arena_words])
        self.actT = self.arena[:].bitcast(BF16).rearrange("p (k t) -> p k t", t=MC)
        self.ymb = [c.sb(f"ymb{i}", [128, 8, CH], BF16) for i in range(2)]
        self.hT = c.sb("hT", [128, 8, MC], BF16)
        self.sg = [c.sb(f"sg{i}", [128, CH], BF16) for i in range(2)]
        self.pp = [c.psum(f"pp{i}") for i in range(6)]
        self.psN = c.psum("psN")
        self.psL = c.psum("psL")
        self.pi = 0
        self.ph.I("dve", "memset", self.ones_bf[:], 1.0, writes=["ones"])

    def nextps(self):
        self.pi = (self.pi + 1) % len(self.pp)
        return self.pp[self.pi], f"pp{self.pi}"

    def wload(self, dram_tile_ap, nk, ncols):
        i = self.wi
        self.wi = (self.wi + 1) % len(self.wb)
        view = self.wb[i][:, 0:nk * ncols].rearrange("p (k c) -> p k c", c=ncols)
        ld(self.ph, "pool", f"wb{i}", view, dram_tile_ap, [f"wb{i}"])
        return view, f"wb{i}"

    def norm(self, gi, cs, xn, xn_res, n=CH):
        rmsnorm_fm(self.ph, "n", self.xT, cs, self.gcols[:, gi, :], xn, self.ones_bf, self.sq, self.psN,
                   self.rstd_s, self.rstd, "xT", xn_res)

    def linear_fm(self, Wt_d, nk, ncols, src, src_res, ntok, evac):
        ph = self.ph
        ncg = Wt_d.shape[0]
        for cg in range(ncg):
            wv, wres = self.wload(Wt_d[cg], nk, ncols)
            for mi in range(ncols // 128):
                for ts in range(ntok // CH):
                    ps, pres = self.nextps()
                    for k in range(nk):
                        ph.I("pe", "matmul", ps[:], lhsT=wv[:, k, mi * 128:(mi + 1) * 128], rhs=src[:, k, ts * CH:(ts + 1) * CH],
                             start=(k == 0), stop=(k == nk - 1), reads=[wres, src_res], writes=[pres])
                    evac(cg * (ncols // 128) + mi, ts, ps, pres)

    def resid_add(self, t0):
        ph, xT = self.ph, self.xT

        def ev(m, ts, ps, pres):
            sl = slice(t0 + ts * CH, t0 + (ts + 1) * CH)
            ph.I("dve", "tensor_tensor", out=xT[:, m, sl], in0=xT[:, m, sl], in1=ps[:], op=ALU.add, reads=[pres, "xT"], writes=["xT"])
        return ev

    def mem_setup(self, memT_d, gkv_d, Wkv_k_d, Wkv_v_d):
        c, ph = self, self.ph
        self.memT = self.arena[:, 0:2048].rearrange("p (k t) -> p k t", t=256)
        self.memn = self.arena[:, 2048:3072].bitcast(BF16).rearrange("p (k t) -> p k t", t=256)
        self.gkv = c.sb("gkv", [128, 8])
        self.KmT = self.arena[:, 3072:4096].bitcast(BF16).rearrange("p (k t) -> p k t", t=256)
        self.Vm = self.arena[:, 4096:5120].bitcast(BF16).rearrange("p (k t) -> p k t", t=1024)
        self.qmT = self.ymb[0]
        self.omT = self.ymb[1]
        self.PmT = [self.arena[:, 9728 + 256 * i: 9728 + 256 * (i + 1)].bitcast(BF16) for i in range(2)]
        self.rl = self.arena[:, 10240:10752]
        self.xq = c.sb("xq", [128, 8, CH], BF16)
        ld(ph, "sp", "lmem", self.memT, memT_d.rearrange("(k p) t -> p k t", p=128), ["memT"])
        ld(ph, "sp", "lgkv", self.gkv[:], gkv_d, ["gkv"])
        for k in range(8):
            sqk = self.sq[k % 2]
            ph.I("act", "activation", out=sqk[:, 0:256], in_=self.memT[:, k, :], func=AF.Square, reads=["memT"], writes=[f"nsq{k%2}"])
            ph.I("pe", "matmul", self.psN[:, 0:256], lhsT=self.ones_bf[:], rhs=sqk[:, 0:256], start=(k == 0), stop=(k == 7),
                 reads=[f"nsq{k%2}", "ones"], writes=["npsN"])
        ph.I("act", "activation", out=self.rstd_s[:, 0:256], in_=self.psN[:, 0:256], func=AF.Sqrt, scale=1.0 / 1024.0, bias=EPS,
             reads=["npsN"], writes=["nrstd_s"])
        ph.I("dve", "reciprocal", out=self.rstd[:, 0:256], in_=self.rstd_s[:, 0:256], reads=["nrstd_s"], writes=["nrstd"])
        for k in range(8):
            ph.I("dve", "scalar_tensor_tensor", out=self.memn[:, k, :], in0=self.memT[:, k, :], scalar=self.gkv[:, k:k + 1],
                 in1=self.rstd[:, 0:256], op0=ALU.mult, op1=ALU.mult, reads=["memT", "nrstd", "gkv"], writes=["memn"])
        for cg in range(2):
            wv, wres = self.wload(Wkv_k_d[cg], 8, 512)
            for mi in range(4):
                ps, pres = self.nextps()
                for k in range(8):
                    ph.I("pe", "matmul", ps[:, 0:256], lhsT=wv[:, k, mi * 128:(mi + 1) * 128], rhs=self.memn[:, k, :],
                         start=(k == 0), stop=(k == 7), reads=[wres, "memn"], writes=[pres])
                ph.I("act", "activation", out=self.KmT[:, cg * 4 + mi, :], in_=ps[:, 0:256], func=AF.Copy, reads=[pres], writes=["KmT"])
        for cg in range(2):
            wv, wres = self.wload(Wkv_v_d[cg], 8, 512)
            for mt in range(2):
                ps, pres = self.nextps()
                for k in range(8):
                    ph.I("pe", "matmul", ps[:], lhsT=self.memn[:, k, mt * 128:(mt + 1) * 128], rhs=wv[:, k, :],
                         start=(k == 0), stop=(k == 7), reads=[wres, "memn"], writes=[pres])
                ph.I("dve", "tensor_copy", out=self.Vm[:, mt, cg * 512:(cg + 1) * 512], in_=ps[:], reads=[pres], writes=["Vm"])

    def mem_attn(self, gi, Wq_d, Wo_d):
        ph = self.ph
        for tc in range(NCH):
            cs = slice(tc * CH, (tc + 1) * CH)
            self.norm(gi, cs, self.xq, "xq")

            def ev_q(m, ts, ps, pres):
                eng = "act" if m % 2 else "dve"
                if eng == "act":
                    ph.I("act", "activation", out=self.qmT[:, m, :], in_=ps[:], func=AF.Copy, reads=[pres], writes=["ymb0"])
                else:
                    ph.I("dve", "tensor_copy", out=self.qmT[:, m, :], in_=ps[:], reads=[pres], writes=["ymb0"])
            self.linear_fm(Wq_d, 8, 512, self.xq, "xq", CH, ev_q)
            for hh in range(4):
                for mt in range(2):
                    ps, pres = self.nextps()
                    for c2 in range(2):
                        ph.I("pe", "matmul", ps[:], lhsT=self.KmT[:, hh * 2 + c2, mt * 128:(mt + 1) * 128], rhs=self.qmT[:, hh * 2 + c2, :],
                             start=(c2 == 0), stop=(c2 == 1), reads=["KmT", "ymb0"], writes=[pres])
                    ph.I("act", "activation", out=self.PmT[mt], in_=ps[:], func=AF.Exp, scale=M_SCALE, reads=[pres], writes=[f"PmT{mt}"])
                for mt in range(2):
                    ph.I("pe", "matmul", self.psL[:], lhsT=self.ones_bf[:], rhs=self.PmT[mt], start=(mt == 0), stop=(mt == 1),
                         reads=[f"PmT{mt}", "ones"], writes=["psL"])
                ph.I("dve", "reciprocal", out=self.rl, in_=self.psL[:], reads=["psL"], writes=["rl"])
                for c2 in range(2):
                    ps, pres = self.nextps()
                    for mt in range(2):
                        ph.I("pe", "matmul", ps[:], lhsT=self.Vm[:, mt, hh * 256 + c2 * 128: hh * 256 + (c2 + 1) * 128], rhs=self.PmT[mt],
                             start=(mt == 0), stop=(mt == 1), reads=["Vm", f"PmT{mt}"], writes=[pres])
                    ph.I("dve", "tensor_tensor", out=self.omT[:, hh * 2 + c2, :], in0=ps[:], in1=self.rl, op=ALU.mult,
                         reads=[pres, "rl"], writes=["ymb1"])
            self.linear_fm(Wo_d, 8, 512, self.omT, "ymb1", CH, self.resid_add(tc * CH))


def out_proj(c, ymix_d, Wout_d):
    ph = c.ph
    for tc in range(NCH):
        yb = c.ymb[tc % 2]
        if isinstance(ymix_d, tuple):
            for hi, part in enumerate(ymix_d):
                ld(ph, "sp", f"lym{tc%2}", yb[:, 4 * hi:4 * hi + 4, :], part.rearrange("(k p) t -> p k t", p=128)[:, :, tc * CH:(tc + 1) * CH],
                   [f"ymb{tc%2}"])
        else:
            ymv = ymix_d.rearrange("(k p) t -> p k t", p=128)
            ld(ph, "sp", f"lym{tc%2}", yb[:], ymv[:, :, tc * CH:(tc + 1) * CH], [f"ymb{tc%2}"])
        c.linear_fm(Wout_d, 8, 512, yb, f"ymb{tc%2}", CH, c.resid_add(tc * CH))


def ffn_swiglu(c, gi, Wg_d, Wu_d, Wd_d, nf, post=None):
    ph = c.ph
    ncg = nf // 2
    for mc in range(T // MC):
        for ts in range(MC // CH):
            cs = slice(mc * MC + ts * CH, mc * MC + (ts + 1) * CH)
            c.norm(gi, cs, c.hT[:, :, ts * CH:(ts + 1) * CH], "hT")
        for cg in range(ncg):
            wg, wgr = c.wload(Wg_d[cg], 8, 256)
            wu, wur = c.wload(Wu_d[cg], 8, 256)
            for mi in range(2):
                for ts in range(MC // CH):
                    psG, pgr = c.nextps()
                    psU, pur = c.nextps()
                    for k in range(8):
                        ph.I("pe", "matmul", psG[:], lhsT=wg[:, k, mi * 128:(mi + 1) * 128], rhs=c.hT[:, k, ts * CH:(ts + 1) * CH],
                             start=(k == 0), stop=(k == 7), reads=[wgr, "hT"], writes=[pgr])
                    for k in range(8):
                        ph.I("pe", "matmul", psU[:], lhsT=wu[:, k, mi * 128:(mi + 1) * 128], rhs=c.hT[:, k, ts * CH:(ts + 1) * CH],
                             start=(k == 0), stop=(k == 7), reads=[wur, "hT"], writes=[pur])
                    sg = c.sg[(mi * 2 + ts) % 2]
                    sgr = f"sg{(mi * 2 + ts) % 2}"
                    ph.I("act", "activation", out=sg[:], in_=psG[:], func=AF.Silu, reads=[pgr], writes=[sgr])
                    ph.I("dve", "tensor_tensor", out=c.actT[:, cg * 2 + mi, ts * CH:(ts + 1) * CH], in0=sg[:], in1=psU[:], op=ALU.mult,
                         reads=[sgr, pur], writes=["actT"])
        c.linear_fm(Wd_d, nf, 128, c.actT, "actT", MC, c.resid_add(mc * MC))


C_IN = dict(xT=([1024, T], F32), ymix=([1024, T], BF16), gcols=([128, 4, 8], F32), Wout=([2, 128, 8, 512], F32), memT=([1024, 256], F32),
            gkv=([128, 8], F32), Wkvk=([2, 128, 8, 512], F32), Wkvv=([2, 128, 8, 512], F32), Wq=([2, 128, 8, 512], F32),
            Wo=([2, 128, 8, 512], F32), Wg=([11, 128, 8, 256], F32), Wu=([11, 128, 8, 256], F32), Wd=([8, 128, 22, 128], F32),
            Win=([6, 128, 8, 512], F32))


def build_C(c=None, io=None):
    if c is None:
        c = Tok("C")
        io = {n: c.din(n, sh, dt) for n, (sh, dt) in C_IN.items()}
        io.update(xo=c.dout("xo", [1024, T]), qk=c.dout("qk", [1024, T], BF16), v=c.dout("v", [T, 512], BF16),
                  z=c.dout("z", [512, T]), gb=c.dout("gb", [512, T]))
    nc, ph = c.nc, c.ph
    xT_d, ymix_d, gcols_d, Wout_d, memT_d, gkv_d, Wkvk_d, Wkvv_d, Wq_d, Wo_d, Wg_d, Wu_d, Wd_d, Win_d = (io[n] for n in C_IN)
    xo_d, qk_d, v_d, z_d, gb_d = io["xo"], io["qk"], io["v"], io["z"], io["gb"]

    ld(ph, "act", "lx", c.xT[:], xT_d.rearrange("(k p) t -> p k t", p=128), ["xT"])
    ld(ph, "sp", "lg", c.gcols[:], gcols_d, ["gcol"])

    out_proj(c, ymix_d, Wout_d)
    c.mem_setup(memT_d, gkv_d, Wkvk_d, Wkvv_d)
    c.mem_attn(0, Wq_d, Wo_d)
    ffn_swiglu(c, 1, Wg_d, Wu_d, Wd_d, 22)
    ld(ph, "act", "sx", xo_d.rearrange("(k p) t -> p k t", p=128), c.xT[:], [], reads=["xT"])

    xn1 = c.xq
    ar = c.arena
    gbst = ar[:, 0:2048].rearrange("p (m t) -> p m t", t=CH)
    zst = ar[:, 2048:4096].rearrange("p (m t) -> p m t", t=CH)
    vst = ar[:, 4096:5120].bitcast(BF16).rearrange("p (m t) -> p m t", t=512)
    qkst = ar[:, 5120:7168].bitcast(BF16).rearrange("p (m t) -> p m t", t=CH)
    hst = ar[:, 7168:9216].rearrange("p (m t) -> p m t", t=CH)
    dummy = c.sb("dummy", [128, 1])
    ph.I("dve", "memset", dummy[:], 0.0, reads=["actT"], writes=["qkst", "gbst", "zst", "vst"] + [f"hst{m}" for m in range(4)])
    for tc in range(NCH):
        cs = slice(tc * CH, (tc + 1) * CH)
        c.norm(2, cs, xn1, "xq")

        def ev_qk(m, ts, ps, pres):
            if m % 2:
                ph.I("act", "activation", out=qkst[:, m, :], in_=ps[:], func=AF.Copy, reads=[pres], writes=["qkst"])
            else:
                ph.I("dve", "tensor_copy", out=qkst[:, m, :], in_=ps[:], reads=[pres], writes=["qkst"])
        c.linear_fm(Win_d[0:2], 8, 512, xn1, "xq", CH, ev_qk)
        ld(ph, "sp", "sqk", qk_d.rearrange("(m p) t -> p m t", p=128)[:, :, cs], qkst, [], reads=["qkst"])
        wv, wres = c.wload(Win_d[2], 8, 512)
        for s in range(4):
            ps, pres = c.nextps()
            for k in range(8):
                ph.I("pe", "matmul", ps[:], lhsT=xn1[:, k, s * 128:(s + 1) * 128], rhs=wv[:, k, :], start=(k == 0), stop=(k == 7),
                     reads=[wres, "xq"], writes=[pres])
            ph.I("act", "activation", out=vst[:, s, :], in_=ps[:], func=AF.Copy, reads=[pres], writes=["vst"])
        ld(ph, "sp", "sv", v_d.rearrange("(n p) c -> p n c", p=128)[:, tc * 4:(tc + 1) * 4, :], vst, [], reads=["vst"])

        def ev_conv(m, ts, ps, pres):
            if m < 4:
                ph.I("act", "activation", out=hst[:, m, :], in_=ps[:], func=AF.Copy, reads=[pres], writes=[f"hst{m}"])
            elif m < 8:
                ph.I("act", "activation", out=gbst[:, m - 4, :], in_=ps[:], func=AF.Copy, reads=[pres], writes=["gbst"])
            else:
                ph.I("dve", "tensor_tensor", out=zst[:, m - 8, :], in0=hst[:, m - 8, :], in1=ps[:], op=ALU.mult,
                     reads=[pres, f"hst{m-8}"], writes=["zst"])
        c.linear_fm(Win_d[3:6], 8, 512, xn1, "xq", CH, ev_conv)
        ld(ph, "sp", "sgb", gb_d.rearrange("(m p) t -> p m t", p=128)[:, :, cs], gbst, [], reads=["gbst"])
        ld(ph, "sp", "sz", z_d.rearrange("(m p) t -> p m t", p=128)[:, :, cs], zst, [], reads=["zst"])
    return c.finish()


def mem_inputs(inp, l, b):
    wkv = np.asarray(inp["mem_w_kv"][l], np.float32)
    return dict(memT=_c(np.asarray(inp["mem"][b], np.float32).T), gkv=pk(inp["norm_mem_kv"][l]),
                Wkvk=wtiles(wkv[:, :1024], 512), Wkvv=wtiles(wkv[:, 1024:], 512),
                Wq=wtiles(inp["mem_w_q"][l], 512), Wo=wtiles(inp["mem_w_o"][l], 512))


def host_C(inp, resA, resB, xT_cores):
    maps = []
    gcols = _c(np.stack([pk(inp["norm_mem_q"][0]), pk(inp["norm_ffn"][0]), pk(inp["norm_mix"][1]), pk(inp["norm_mix"][1])], axis=1))
    shared = dict(gcols=gcols, Wout=wtiles(inp["even_w_out"][0], 512), Wg=wtiles(inp["ffn_w_gate"][0], 256),
                  Wu=wtiles(inp["ffn_w_up"][0], 256), Wd=wtiles(inp["ffn_w_down"][0], 128), Win=wtiles(inp["odd_w_in"][0], 512))
    mem = [mem_inputs(inp, 0, b) for b in range(2)]
    for core in range(8):
        b, i = core // 4, core % 4
        yb = np.concatenate([resB[b * 4 + h]["y"][:, i * T:(i + 1) * T] for h in range(4)], axis=0)
        ymix = np.concatenate([resA[core]["ya"], yb], axis=0)
        m = dict(xT=xT_cores[core], ymix=_c(ymix))
        m.update(shared)
        m.update(mem[b])
        maps.append(m)
    return maps


LAM_INIT = 0.8 - 0.6 * float(np.exp(-0.3 * 1))


def rel_bucket_static(rel):
    import jax.numpy as jnp
    import jax
    with jax.default_device(jax.devices("cpu")[0]):
        rel = jnp.asarray(rel, jnp.int32)
        n_half, max_exact = 16, 8
        ret = jnp.where(rel > 0, n_half, 0)
        n = jnp.abs(rel)
        nf = jnp.maximum(n, 1).astype(jnp.float32)
        large = max_exact + (jnp.log(nf / max_exact) / float(np.log(128 / max_exact)) * (n_half - max_exact)).astype(jnp.int32)
        large = jnp.minimum(large, n_half - 1)
        return np.asarray(ret + jnp.where(n < max_exact, n, large))


def diff_masks():
    p = np.arange(128)[:, None]
    f = np.arange(128)[None, :]
    out = {}
    for delta in (0, -1):
        rel = 128 * delta + p - f
        bk = rel_bucket_static(rel)
        ids = sorted(set(bk.reshape(-1).tolist()))
        masks = np.stack([(bk == b).astype(np.float32) for b in ids])
        out[delta] = (ids, masks)
    notallowed = ((p // 64) > (f // 64)).astype(np.float32)
    return out, notallowed


_DM = {}


def get_diff_masks():
    if "m" not in _DM:
        _DM["m"] = diff_masks()
    return _DM["m"]


def build_D(c=None, io=None):
    (dm, notallowed) = get_diff_masks()
    ids0, ids1 = dm[0][0], dm[-1][0]
    n0, n1 = len(ids0), len(ids1)
    if c is None:
        c = Ctx("D")
        io = dict(q=c.din("q", [128, S], BF16), k=c.din("k", [128, S], BF16), v=c.din("v", [128, NT, 128], BF16),
                  masks=c.din("masks", [128, n0 + n1 + 1, 128]), tab=c.din("tab", [128, 32]), lam=c.din("lam", [128, 4, 64]),
                  gain=c.din("gain", [128, 1]), y=c.dout("y", [128, S], BF16))
    nc, ph = c.nc, c.ph
    q_d, k_d, v_d, mk_d, tab_d, lam_d, gain_d, y_d = (io[n] for n in ("q", "k", "v", "masks", "tab", "lam", "gain", "y"))

    QT = c.sb("QT", [128, S], BF16)
    KT = [c.sb(f"KT{i}", [128, S], BF16) for i in range(2)]
    V = c.sb("V", [128, NT, 128], BF16)
    mk = c.sb("mk", [128, n0 + n1 + 1, 128])
    tab = c.sb("tab", [128, 32])
    lamp = c.sb("lamp", [128, 4, 64])
    gsc = c.sb("gsc", [128, 1])
    B0 = c.sb("B0", [128, 128])
    B1 = c.sb("B1", [128, 128])
    lprod = c.sb("lprod", [128, 2, 64])
    lsum = c.sb("lsum", [128, 2])
    neglam = c.sb("neglam", [128, 1])
    ones_bf = c.sb("ones", [128, 128], BF16)
    tb = [c.sb(f"tb{i}", [128, 128]) for i in range(2)]
    PT = [[c.sb(f"PT{s}{i}", [128, CH], BF16) for i in range(4)] for s in range(2)]
    l1c = c.sb("l1c", [128, CH])
    l2c = c.sb("l2c", [128, CH])
    o1 = c.sb("o1", [128, CH])
    o2 = c.sb("o2", [128, CH])
    sqo = c.sb("sqo", [128, CH], BF16)
    rs = c.sb("rs", [128, CH])
    ost = [c.sb(f"ost{i}", [128, CH], BF16) for i in range(2)]
    psS3 = [c.psum(f"psS{i}") for i in range(3)]
    psN = c.psum("psN")
    psO = [c.psum(f"psO{s}") for s in range(2)]
    psL = [c.psum(f"psL{s}") for s in range(2)]

    ld(ph, "sp", "l1", mk[:], mk_d, ["mk"])
    ld(ph, "sp", "l2", tab[:], tab_d, ["tab"])
    ld(ph, "sp", "l3", lamp[:], lam_d, ["lamp"])
    ld(ph, "sp", "l4", gsc[:], gain_d, ["gsc"])
    if isinstance(q_d, tuple):
        ld(ph, "sp", "lq", QT[0:64, :], q_d[0], ["QT"])
        ld(ph, "sp", "lq", QT[64:128, :], q_d[1], ["QT"])
        ld(ph, "act", "lk0", KT[0][0:64, :], k_d[0], ["KT0"])
        ld(ph, "act", "lk1", KT[1][64:128, :], k_d[1], ["KT1"])
    else:
        ld(ph, "sp", "lq", QT[:], q_d, ["QT"])
        ld(ph, "act", "lk0", KT[0][0:64, :], k_d[0:64, :], ["KT0"])
        ld(ph, "act", "lk1", KT[1][64:128, :], k_d[64:128, :], ["KT1"])
    ph.I("pool", "memset", KT[0][64:128, :], 0.0, writes=["KT0z"])
    ph.I("pool", "memset", KT[1][0:64, :], 0.0, writes=["KT1z"])
    ld(ph, "sp", "lv", V[:], v_d, ["V"])
    ph.I("dve", "memset", ones_bf[:], 1.0, writes=["ones"])
    ph.I("dve", "tensor_scalar", out=B0[:], in0=mk[:, n0 + n1, :], scalar1=NEG, scalar2=None, op0=ALU.mult, reads=["mk"], writes=["B0"])
    for n, b in enumerate(ids0):
        ph.I("dve", "scalar_tensor_tensor", out=B0[:], in0=mk[:, n, :], scalar=tab[:, b:b + 1], in1=B0[:], op0=ALU.mult, op1=ALU.add,
             reads=["mk", "tab", "B0"], writes=["B0"])
    ph.I("dve", "tensor_scalar", out=B1[:], in0=mk[:, n0, :], scalar1=tab[:, ids1[0]:ids1[0] + 1], scalar2=None, op0=ALU.mult,
         reads=["mk", "tab"], writes=["B1"])
    for n, b in enumerate(ids1[1:]):
        ph.I("dve", "scalar_tensor_tensor", out=B1[:], in0=mk[:, n0 + 1 + n, :], scalar=tab[:, b:b + 1], in1=B1[:], op0=ALU.mult, op1=ALU.add,
             reads=["mk", "tab", "B1"], writes=["B1"])
    ph.I("dve", "tensor_tensor", out=lprod[:, 0, :], in0=lamp[:, 0, :], in1=lamp[:, 1, :], op=ALU.mult, reads=["lamp"], writes=["lprod"])
    ph.I("dve", "tensor_tensor", out=lprod[:, 1, :], in0=lamp[:, 2, :], in1=lamp[:, 3, :], op=ALU.mult, reads=["lamp"], writes=["lprod"])
    ph.I("dve", "reduce_sum", out=lsum[:], in_=lprod[:], axis=AX.X, reads=["lprod"], writes=["lsum"])
    ph.I("act", "activation", out=lsum[:], in_=lsum[:], func=AF.Exp, reads=["lsum"], writes=["lsum"])
    ph.I("dve", "tensor_tensor", out=neglam[:], in0=lsum[:, 1:2], in1=lsum[:, 0:1], op=ALU.subtract, reads=["lsum"], writes=["neglam"])
    ph.I("dve", "tensor_scalar", out=neglam[:], in0=neglam[:], scalar1=-LAM_INIT, scalar2=None, op0=ALU.add, reads=["neglam"], writes=["neglam"])
    ph.I("dve", "tensor_scalar", out=gsc[:], in0=gsc[:], scalar1=1.0 - LAM_INIT, scalar2=None, op0=ALU.mult, reads=["gsc"], writes=["gsc"])

    scale = 64.0 ** -0.5
    LA = 2
    its = [(I, j) for I in range(NG) for j in range(4 * I + 4)]
    NIT = len(its)
    NPT = len(PT[0])

    def front(n):
        I, j = its[n]
        qs = slice(I * CH, (I + 1) * CH)
        jj = max(j - 4 * I, 0)
        for s in range(2):
            bi = (2 * n + s) % 3
            pS, pr = psS3[bi], f"psS{bi}"
            P, sr = PT[s][n % NPT], f"PT{s}{n%NPT}"
            ph.I("pe", "matmul", pS[:], lhsT=KT[s][:, j * 128:(j + 1) * 128], rhs=QT[:, qs], start=True, stop=True,
                 reads=[f"KT{s}", f"KT{s}z", "QT"], writes=[pr])
            if j <= 4 * I - 2:
                ph.I("act", "activation", out=P[:], in_=pS[:], func=AF.Exp, scale=scale, bias=tab[:, 15:16], reads=[pr, "tab"], writes=[sr])
            else:
                for ii in range(jj, 4):
                    delta = j - (4 * I + ii)
                    bs_ = slice(ii * 128, (ii + 1) * 128)
                    if delta <= -2:
                        ph.I("act", "activation", out=P[:, bs_], in_=pS[:, bs_], func=AF.Exp, scale=scale, bias=tab[:, 15:16],
                             reads=[pr, "tab"], writes=[sr])
                    else:
                        Bt, br = (B0, "B0") if delta == 0 else (B1, "B1")
                        t = tb[(s + ii) % 2]
                        tr = f"tb{(s + ii) % 2}"
                        ph.I("dve", "scalar_tensor_tensor", out=t[:], in0=pS[:, bs_], scalar=scale, in1=Bt[:], op0=ALU.mult, op1=ALU.add,
                             reads=[pr, br], writes=[tr])
                        ph.I("act", "activation", out=P[:, bs_], in_=t[:], func=AF.Exp, reads=[tr], writes=[sr])

    def back(n):
        I, j = its[n]
        qs = slice(I * CH, (I + 1) * CH)
        last = 4 * I + 3
        c0 = max(j - 4 * I, 0) * 128
        for s in range(2):
            P, sr = PT[s][n % NPT], f"PT{s}{n%NPT}"
            ph.I("pe", "matmul", psO[s][:, c0:], lhsT=V[:, j, :], rhs=P[:, c0:], start=(j == 0), stop=(j == last),
                 reads=[sr, "V"], writes=[f"psO{s}"])
            ph.I("pe", "matmul", psL[s][:, c0:], lhsT=ones_bf[:], rhs=P[:, c0:], start=(j == 0), stop=(j == last),
                 reads=[sr, "ones"], writes=[f"psL{s}"])
        if j != last:
            return
        ph.I("dve", "tensor_copy", out=l1c[:], in_=psL[0][:], reads=["psL0"], writes=["l1c"])
        ph.I("dve", "tensor_copy", out=o1[:], in_=psO[0][:], reads=["psO0"], writes=["o1"])
        ph.I("dve", "tensor_copy", out=l2c[:], in_=psL[1][:], reads=["psL1"], writes=["l2c"])
        ph.I("dve", "tensor_copy", out=o2[:], in_=psO[1][:], reads=["psO1"], writes=["o2"])
        ph.I("dve", "reciprocal", out=l1c[:], in_=l1c[:], reads=["l1c"], writes=["l1c"])
        ph.I("dve", "reciprocal", out=l2c[:], in_=l2c[:], reads=["l2c"], writes=["l2c"])
        ph.I("pool", "tensor_tensor", out=o1[:], in0=o1[:], in1=l1c[:], op=ALU.mult, reads=["o1", "l1c"], writes=["o1"])
        ph.I("pool", "tensor_tensor", out=o2[:], in0=o2[:], in1=l2c[:], op=ALU.mult, reads=["o2", "l2c"], writes=["o2"])
        ph.I("dve", "scalar_tensor_tensor", out=o1[:], in0=o2[:], scalar=neglam[:, 0:1], in1=o1[:], op0=ALU.mult, op1=ALU.add,
             reads=["o1", "o2", "neglam"], writes=["o1"])
        ph.I("act", "activation", out=sqo[:], in_=o1[:], func=AF.Square, reads=["o1"], writes=["sqo"])
        ph.I("pe", "matmul", psN[:], lhsT=ones_bf[:], rhs=sqo[:], start=True, stop=True, reads=["sqo", "ones"], writes=["psN"])
        ph.I("act", "activation", out=rs[:], in_=psN[:], func=AF.Sqrt, scale=1.0 / 128.0, bias=EPS, reads=["psN"], writes=["rs"])
        ph.I("dve", "reciprocal", out=rs[:], in_=rs[:], reads=["rs"], writes=["rs"])
        o = ost[I % 2]
        ph.I("dve", "scalar_tensor_tensor", out=o[:], in0=o1[:], scalar=gsc[:, 0:1], in1=rs[:], op0=ALU.mult, op1=ALU.mult,
             reads=["o1", "rs", "gsc"], writes=[f"ost{I%2}"])
        ld(ph, "sp", f"so{I%2}", y_d[:, qs], o[:], [], reads=[f"ost{I%2}"])

    for n in range(NIT + LA):
        if n < NIT:
            front(n)
        if n - LA >= 0:
            back(n - LA)
    return c.finish()


def host_D(inp, resC):
    (dm, notallowed) = get_diff_masks()
    masks = np.concatenate([dm[0][1], dm[-1][1], notallowed[None]], axis=0)
    masks = _c(masks.transpose(1, 0, 2))
    lam = _c(np.stack([rep(inp["diff_lambda_q1"][0]), rep(inp["diff_lambda_k1"][0]),
                       rep(inp["diff_lambda_q2"][0]), rep(inp["diff_lambda_k2"][0])], axis=1).astype(np.float32))
    maps = []
    for core in range(8):
        b, h = core // 4, core % 4
        qk = np.concatenate([resC[b * 4 + i]["qk"] for i in range(4)], axis=1)
        q = np.concatenate([qk[h * 64:(h + 1) * 64], qk[256 + h * 64:256 + (h + 1) * 64]], axis=0)
        k = np.concatenate([qk[512 + h * 64:512 + (h + 1) * 64], qk[768 + h * 64:768 + (h + 1) * 64]], axis=0)
        v = np.concatenate([resC[b * 4 + i]["v"][:, h * 128:(h + 1) * 128] for i in range(4)], axis=0)
        maps.append(dict(q=_c(q), k=_c(k), v=_c(v.reshape(NT, 128, 128).transpose(1, 0, 2)), masks=masks,
                         tab=rep(np.asarray(inp["rel_bias"], np.float32)[:, h]), lam=lam,
                         gain=_c(np.asarray(inp["diff_subln_gain"][0], np.float32)[h * 128:(h + 1) * 128, None])))
    return maps


NE = 8
NFE = 28


E_IN = dict(xT=([1024, T], F32), yc=([512, T], BF16), zext=([512, T + 2], F32), gb=([512, T], F32), convw=([128, 4, 3], F32),
            gcols=([128, 4, 8], F32), Wout=([2, 128, 8, 512], F32), memT=([1024, 256], F32), gkv=([128, 8], F32),
            Wkvk=([2, 128, 8, 512], F32), Wkvv=([2, 128, 8, 512], F32), Wq=([2, 128, 8, 512], F32), Wo=([2, 128, 8, 512], F32),
            Wr=([128, 8, 8], F32), ident=([128, 128], F32), Wg=([NE, 14, 128, 8, 256], F32), Wu=([NE, 14, 128, 8, 256], F32),
            Wd=([NE, 8, 128, NFE, 128], F32))


def build_E(stage=4, c=None, io=None):
    if c is None:
        c = Tok("E", arena_words=14336)
        io = {n: c.din(n, sh, dt) for n, (sh, dt) in E_IN.items()}
        io["out"] = c.dout("out", [1024, T])
    nc, ph = c.nc, c.ph
    (xT_d, yc_d, zx_d, gb_d, cw_d, gcols_d, Wout_d, memT_d, gkv_d, Wkvk_d, Wkvv_d, Wq_d, Wo_d, Wr_d, id_d, Wg_d, Wu_d, Wd_d) = (io[n] for n in E_IN)
    out_d = io["out"]
    sel_d = io.get("sel")
    fused = sel_d is not None

    cw = c.sb("cw", [128, 4, 3])
    zc = c.arena[:, 5120:5120 + 4 * (CH + 2)].rearrange("p (m t) -> p m t", t=CH + 2)
    gbc = c.arena[:, 7680:9728].rearrange("p (m t) -> p m t", t=CH)
    acc = c.arena[:, 10752:11264]
    ident = c.sb("ident", [128, 128])
    ones32 = c.sb("ones32", [128, 128])
    wr32 = c.sb("wr32", [128, 8, 8])
    wrh = c.sb("wrh", [128, 8, 8], BF16)
    wrl = c.sb("wrl", [128, 8, 8], BF16)
    y1w = c.ymb[1][:].rearrange("p k t -> p (k t)").bitcast(F32)
    y0w = c.ymb[0][:].rearrange("p k t -> p (k t)").bitcast(F32)
    t32 = [y1w[:, i * CH:(i + 1) * CH] for i in range(2)]
    lg = c.sb("lg", [128, 8])
    mx8 = c.sb("mx8", [128, 8])
    nv1 = c.sb("nv1", [128, 1])
    ex = c.sb("ex", [128, 8])
    msk = c.sb("msk", [128, 8])
    den = c.sb("den", [128, 1])
    comb = c.sb("comb", [128, MC // 128, 8])
    dg = [c.sb(f"dg{i}", [128, 128]) for i in range(2)]
    cbc = [y0w[:, i * MC:(i + 1) * MC] for i in range(2)]
    tmp = [y1w[:, (2 + i) * CH:(3 + i) * CH] for i in range(2)]

    def sel_load(dst, srcs, stg, tag, res, queue="sp"):
        for b_ in range(4):
            sb_ = stg[b_ % len(stg)]
            sr_ = f"{tag}stg{b_ % len(stg)}"
            ld(ph, queue, f"l{tag}{b_ % len(stg)}", sb_, srcs[b_], [sr_])
            if b_ == 0:
                ph.I("dve", "tensor_scalar", out=dst, in0=sb_, scalar1=sel[:, 0:1], scalar2=None, op0=ALU.mult, reads=[sr_, "sel"], writes=[res])
            else:
                ph.I("dve", "scalar_tensor_tensor", out=dst, in0=sb_, scalar=sel[:, b_:b_ + 1], in1=dst, op0=ALU.mult, op1=ALU.add,
                     reads=[sr_, "sel", res], writes=[res])

    if fused:
        sel = c.sb("sel", [128, 4])
        ld(ph, "sp", "lsel", sel[:], sel_d, ["sel"])
        xstg = [c.arena[:, i * T:(i + 1) * T] for i in range(2)]
        for k in range(8):
            sel_load(c.xT[:, k, :], [xb.rearrange("(k p) t -> p k t", p=128)[:, k, :] for xb in xT_d], xstg, "x", "xT")
    else:
        ld(ph, "sp", "lx", c.xT[:], xT_d.rearrange("(k p) t -> p k t", p=128), ["xT"])
    ld(ph, "sp", "lg", c.gcols[:], gcols_d, ["gcol"])
    ld(ph, "sp", "lcw", cw[:], cw_d, ["cw"])
    ld(ph, "sp", "lid", ident[:], id_d, ["ident"])
    ld(ph, "sp", "lwr", wr32[:], Wr_d, ["wr32"])
    ph.I("dve", "memset", ones32[:], 1.0, writes=["ones32"])
    ph.I("dve", "tensor_copy", out=wrh[:], in_=wr32[:], reads=["wr32"], writes=["wrh"])
    ph.I("dve", "tensor_tensor", out=wrl[:], in0=wr32[:], in1=wrh[:], op=ALU.subtract, reads=["wr32", "wrh"], writes=["wrl"])

    fm = lambda ap: ap.rearrange("(k p) t -> p k t", p=128)
    if fused:
        zstg = [c.arena[:, i * 2056:(i + 1) * 2056].rearrange("p (m t) -> p m t", t=CH + 2) for i in range(2)]
        gstg = [c.arena[:, 11264:13312].rearrange("p (m t) -> p m t", t=CH)]
        ystg = [c.arena[:, 13312:14336].bitcast(BF16).rearrange("p (m t) -> p m t", t=CH)]
        dmy = c.sb("dmy", [128, 1])
        ph.I("dve", "memset", dmy[:], 0.0, reads=["xstg0", "xstg1"], writes=["zstg0", "zstg1"])
    for tc in range(NCH):
        yb = c.ymb[tc % 2]
        yr = f"ymb{tc%2}"
        if fused:
            sel_load(yb[:, 0:4, :], [fm(a)[:, :, tc * CH:(tc + 1) * CH] for a in yc_d], ystg, "y", yr)
            sel_load(zc, [fm(a)[:, :, tc * CH: tc * CH + CH + 2] for a in zx_d], zstg, "z", "zc")
            sel_load(gbc, [fm(a)[:, :, tc * CH:(tc + 1) * CH] for a in gb_d], gstg, "g", "gbc")
        else:
            ld(ph, "sp", f"lym{tc%2}", yb[:, 0:4, :], fm(yc_d)[:, :, tc * CH:(tc + 1) * CH], [yr])
            ld(ph, "sp", "lz", zc, fm(zx_d)[:, :, tc * CH: tc * CH + CH + 2], ["zc"])
            ld(ph, "sp", "lgb", gbc, fm(gb_d)[:, :, tc * CH:(tc + 1) * CH], ["gbc"])
        for m in range(4):
            ph.I("dve", "tensor_scalar", out=acc, in0=zc[:, m, 2:CH + 2], scalar1=cw[:, m, 2:3], scalar2=None, op0=ALU.mult,
                 reads=["zc", "cw"], writes=["acc"])
            ph.I("dve", "scalar_tensor_tensor", out=acc, in0=zc[:, m, 1:CH + 1], scalar=cw[:, m, 1:2], in1=acc, op0=ALU.mult, op1=ALU.add,
                 reads=["zc", "cw", "acc"], writes=["acc"])
            ph.I("dve", "scalar_tensor_tensor", out=acc, in0=zc[:, m, 0:CH], scalar=cw[:, m, 0:1], in1=acc, op0=ALU.mult, op1=ALU.add,
                 reads=["zc", "cw", "acc"], writes=["acc"])
            ph.I("dve", "tensor_tensor", out=yb[:, 4 + m, :], in0=acc, in1=gbc[:, m, :], op=ALU.mult, reads=["acc", "gbc"], writes=[yr])
        c.linear_fm(Wout_d, 8, 512, yb, yr, CH, c.resid_add(tc * CH))

    if stage >= 2:
        c.mem_setup(memT_d, gkv_d, Wkvk_d, Wkvv_d)
        c.mem_attn(0, Wq_d, Wo_d)

    hlo = c.xq if stage >= 2 else None
    for mc in range(T // MC if stage >= 3 else 0):
        for ts in range(MC // CH):
            cs = slice(mc * MC + ts * CH, mc * MC + (ts + 1) * CH)
            hs = slice(ts * CH, (ts + 1) * CH)
            for k in range(8):
                sqk = c.sq[k % 2]
                ph.I("act", "activation", out=sqk[:], in_=c.xT[:, k, cs], func=AF.Square, reads=["xT"], writes=[f"nsq{k%2}"])
                ph.I("pe", "matmul", c.psN[:], lhsT=c.ones_bf[:], rhs=sqk[:], start=(k == 0), stop=(k == 7), reads=[f"nsq{k%2}", "ones"], writes=["npsN"])
            ph.I("act", "activation", out=c.rstd_s[:], in_=c.psN[:], func=AF.Sqrt, scale=1.0 / 1024.0, bias=EPS, reads=["npsN"], writes=["nrstd_s"])
            ph.I("dve", "reciprocal", out=c.rstd[:], in_=c.rstd_s[:], reads=["nrstd_s"], writes=["nrstd"])
            for k in range(8):
                t = t32[k % 2]
                tr = f"t32{k%2}"
                ph.I("dve", "scalar_tensor_tensor", out=t, in0=c.xT[:, k, cs], scalar=c.gcols[:, 1, k:k + 1], in1=c.rstd[:], op0=ALU.mult, op1=ALU.mult,
                     reads=["xT", "nrstd", "gcol"], writes=[tr])
                ph.I("act", "activation", out=c.hT[:, k, hs], in_=t, func=AF.Copy, reads=[tr], writes=["hT"])
                ph.I("dve", "tensor_tensor", out=hlo[:, k, :], in0=t, in1=c.hT[:, k, hs], op=ALU.subtract, reads=[tr, "hT"], writes=["xq"])
            for s4 in range(4):
                sub = slice(ts * CH + s4 * 128, ts * CH + (s4 + 1) * 128)
                subl = slice(s4 * 128, (s4 + 1) * 128)
                ps, pres = c.nextps()
                n = 0
                for (a_, ar_, sl_, b_, br_) in ((c.hT, "hT", sub, wrh, "wrh"), (hlo, "xq", subl, wrh, "wrh"), (c.hT, "hT", sub, wrl, "wrl")):
                    for k in range(8):
                        ph.I("pe", "matmul", ps[:, 0:8], lhsT=a_[:, k, sl_], rhs=b_[:, k, :], start=(n == 0), stop=(n == 23), reads=[ar_, br_], writes=[pres])
                        n += 1
                ph.I("dve", "tensor_copy", out=lg[:], in_=ps[:, 0:8], reads=[pres], writes=["lg"])
                ph.I("dve", "max", out=mx8[:], in_=lg[:], reads=["lg"], writes=["mx8"])
                ph.I("dve", "tensor_scalar", out=nv1[:], in0=mx8[:, 0:1], scalar1=-1.0, scalar2=None, op0=ALU.mult, reads=["mx8"], writes=["nv1"])
                ph.I("act", "activation", out=ex[:], in_=lg[:], func=AF.Exp, bias=nv1[:, 0:1], reads=["lg", "nv1"], writes=["ex"])
                ph.I("dve", "tensor_scalar", out=msk[:], in0=lg[:], scalar1=mx8[:, 1:2], scalar2=None, op0=ALU.is_ge, reads=["lg", "mx8"], writes=["msk"])
                ph.I("dve", "tensor_tensor", out=ex[:], in0=ex[:], in1=msk[:], op=ALU.mult, reads=["ex", "msk"], writes=["ex"])
                ph.I("dve", "reduce_sum", out=den[:], in_=ex[:], axis=AX.X, reads=["ex"], writes=["den"])
                ph.I("dve", "reciprocal", out=den[:], in_=den[:], reads=["den"], writes=["den"])
                ph.I("dve", "tensor_scalar", out=comb[:, ts * 4 + s4, :], in0=ex[:], scalar1=den[:, 0:1], scalar2=None, op0=ALU.mult,
                     reads=["ex", "den"], writes=["comb"])
        for e in range(NE):
            cb = cbc[e % 2]
            cbr = f"cbc{e%2}"
            for s8 in range(MC // 128):
                d_ = dg[s8 % 2]
                dr = f"dg{s8%2}"
                ph.I("dve", "tensor_scalar", out=d_[:], in0=ident[:], scalar1=comb[:, s8, e:e + 1], scalar2=None, op0=ALU.mult,
                     reads=["ident", "comb"], writes=[dr])
                ph.I("pe", "matmul", c.psL[:, (s8 % 4) * 128:(s8 % 4 + 1) * 128], lhsT=ones32[:], rhs=d_[:], start=True, stop=True,
                     reads=[dr, "ones32"], writes=["psL"])
                if s8 % 4 == 3:
                    ph.I("act", "activation", out=cb[:, (s8 // 4) * CH:(s8 // 4 + 1) * CH], in_=c.psL[:], func=AF.Copy, reads=["psL"], writes=[cbr])
            for cg in range(14):
                wg, wgr = c.wload(Wg_d[e, cg], 8, 256)
                wu, wur = c.wload(Wu_d[e, cg], 8, 256)
                for mi in range(2):
                    for ts in range(MC // CH):
                        psG, pgr = c.nextps()
                        psU, pur = c.nextps()
                        for k in range(8):
                            ph.I("pe", "matmul", psG[:], lhsT=wg[:, k, mi * 128:(mi + 1) * 128], rhs=c.hT[:, k, ts * CH:(ts + 1) * CH],
                                 start=(k == 0), stop=(k == 7), reads=[wgr, "hT"], writes=[pgr])
                        for k in range(8):
                            ph.I("pe", "matmul", psU[:], lhsT=wu[:, k, mi * 128:(mi + 1) * 128], rhs=c.hT[:, k, ts * CH:(ts + 1) * CH],
                                 start=(k == 0), stop=(k == 7), reads=[wur, "hT"], writes=[pur])
                        sg = c.sg[(mi * 2 + ts) % 2]
                        sgr = f"sg{(mi * 2 + ts) % 2}"
                        ph.I("act", "activation", out=sg[:], in_=psG[:], func=AF.Silu, reads=[pgr], writes=[sgr])
                        ph.I("dve", "tensor_tensor", out=c.actT[:, cg * 2 + mi, ts * CH:(ts + 1) * CH], in0=sg[:], in1=psU[:], op=ALU.mult,
                             reads=[sgr, pur], writes=["actT"])

            def ev_down(m, ts, ps, pres, cb=cb, cbr=cbr, mc=mc):
                tm = tmp[(m * 2 + ts) % 2]
                tmr = f"tmp{(m * 2 + ts) % 2}"
                sl = slice(mc * MC + ts * CH, mc * MC + (ts + 1) * CH)
                ph.I("dve", "tensor_tensor", out=tm, in0=ps[:], in1=cb[:, ts * CH:(ts + 1) * CH], op=ALU.mult, reads=[pres, cbr], writes=[tmr])
                ph.I("pool", "tensor_tensor", out=c.xT[:, m, sl], in0=c.xT[:, m, sl], in1=tm, op=ALU.add, reads=[tmr, "xT"], writes=["xT"])
            c.linear_fm(Wd_d[e], NFE, 128, c.actT, "actT", MC, ev_down)

    fo = c.arena[:, 0:4096].rearrange("p (k t) -> p k t", t=CH)
    ph.I("dve", "memset", acc[:, 0:1], 0.0, reads=["actT"], writes=["fo"])
    if stage < 4:
        ld(ph, "sp", "sout", out_d.rearrange("(k p) t -> p k t", p=128), c.xT[:], [], reads=["xT"])
    for tc in range(NCH if stage >= 4 else 0):
        cs = slice(tc * CH, (tc + 1) * CH)
        c.norm(2, cs, fo, "fo")
        ld(ph, "sp", "sout", out_d.rearrange("(k p) t -> p k t", p=128)[:, :, cs], fo, [], reads=["fo"])
    return c.finish()


def host_E(inp, resC, resD):
    gcols = _c(np.stack([pk(inp["norm_mem_q"][1]), pk(inp["norm_ffn"][1]), pk(inp["norm_final"]), pk(inp["norm_final"])], axis=1))
    cw = np.asarray(inp["conv_w"][0], np.float32)
    convw = _c(cw.reshape(3, 4, 128).transpose(2, 1, 0))
    Wg = np.stack([wtiles(inp["moe_w_gate"][0][e], 256) for e in range(NE)])
    Wu = np.stack([wtiles(inp["moe_w_up"][0][e], 256) for e in range(NE)])
    Wd = np.stack([wtiles(inp["moe_w_down"][0][e], 128) for e in range(NE)])
    shared = dict(gcols=gcols, convw=convw, Wout=wtiles(inp["odd_w_out"][0], 512),
                  Wr=_c(np.asarray(inp["router_w"][0], np.float32).reshape(8, 128, 8).transpose(1, 0, 2)),
                  ident=np.eye(128, dtype=np.float32), Wg=Wg, Wu=Wu, Wd=Wd)
    mem = [mem_inputs(inp, 1, b) for b in range(2)]
    maps = []
    for core in range(8):
        b, i = core // 4, core % 4
        yc = np.concatenate([resD[b * 4 + h]["y"][:, i * T:(i + 1) * T] for h in range(4)], axis=0)
        z = resC[core]["z"]
        halo = resC[core - 1]["z"][:, T - 2:] if i > 0 else np.zeros((512, 2), np.float32)
        m = dict(xT=resC[core]["xo"], yc=_c(yc), zext=_c(np.concatenate([halo, z], axis=1)), gb=resC[core]["gb"])
        m.update(shared)
        m.update(mem[b])
        maps.append(m)
    return maps


def build_fused():
    nc = bass.Bass("TRN2", target_bir_lowering=False)
    (dm, notallowed) = get_diff_masks()
    nmask = len(dm[0][0]) + len(dm[-1][0]) + 1

    def din(name, shape, dt=F32):
        return nc.dram_tensor(name, list(shape), dt, kind="ExternalInput").ap()

    def scr(name, shape, dt=F32):
        return nc.dram_tensor(name, list(shape), dt, kind="Internal").ap()

    x_d = din("x", [1024, S])
    A = dict(g=din("A_g", [128, 8]), w=din("A_w", [1024, 2564]), vgain=din("A_vgain", [128, 512]), ws=din("A_ws", [128, 4, 128]),
             bs=din("A_bs", [128, 4, 512]))
    B = dict(bf=din("B_bf", [4, 64, 1]), ident=din("B_ident", [64, 64]), lmat=din("B_lmat", [64, 64]), tri=din("B_tri", [128, 128]))
    C = {n: din("C_" + n, sh, dt) for n, (sh, dt) in C_IN.items() if n not in ("xT", "ymix")}
    D = dict(masks=din("D_masks", [128, nmask, 128]), tab=din("D_tab", [4, 128, 32]), lam=din("D_lam", [128, 4, 64]),
             gain=din("D_gain", [4, 128, 1]))
    E = {n: din("E_" + n, sh, dt) for n, (sh, dt) in E_IN.items() if n not in ("xT", "yc", "zext", "gb")}
    sel_d = din("E_sel", [128, 4])
    out_d = nc.dram_tensor("out", [1024, T], F32, kind="ExternalOutput").ap()

    q_s, k_s, ya_s, yb_s, yc_s = (scr(n, [512, S], BF16) for n in ("q_s", "k_s", "ya_s", "yb_s", "yc_s"))
    v_s, v1_s = scr("v_s", [S, 512], BF16), scr("v1_s", [S, 512], BF16)
    f_s = scr("f_s", [4, S])
    x1_s = scr("x1_s", [1024, S])
    qk_s = scr("qk_s", [1024, S], BF16)
    z_s = scr("z_s", [512, S + 2])
    gb_s = scr("gb_s", [512, S])

    c0 = Ctx("Z", nc=nc)
    zt = c0.sb("zt", [128, 4, 2])
    c0.ph.I("dve", "memset", zt[:], 0.0, writes=["zt"])
    ld(c0.ph, "sp", "sz", z_s.rearrange("(m p) t -> p m t", p=128)[:, :, 0:2], zt[:], [], reads=["zt"])
    c0.finish()

    for blk in range(4):
        cols = slice(blk * T, (blk + 1) * T)
        io = dict(A)
        io.update(xT=x_d[:, cols], ya=ya_s[:, cols], q=q_s[:, cols], k=k_s[:, cols], v=v_s[cols, :], f=f_s[:, cols])
        build_A(Ctx(f"A{blk}", nc=nc), io)
    for h in range(4):
        rows = slice(h * 128, (h + 1) * 128)
        io = dict(q=q_s[rows, :], k=k_s[rows, :], v=v_s.rearrange("(j p) c -> p j c", p=128)[:, :, rows],
                  f2=f_s[h].rearrange("(j p) -> j p", p=128), bf=B["bf"][h], ident=B["ident"], lmat=B["lmat"], tri=B["tri"], y=yb_s[rows, :])
        build_B(Ctx(f"B{h}", nc=nc), io)
    for blk in range(4):
        cols = slice(blk * T, (blk + 1) * T)
        io = dict(C)
        io.update(xT=x_d[:, cols], ymix=(ya_s[:, cols], yb_s[:, cols]), xo=x1_s[:, cols], qk=qk_s[:, cols], v=v1_s[cols, :],
                  z=z_s[:, 2 + blk * T: 2 + (blk + 1) * T], gb=gb_s[:, cols])
        build_C(Tok(f"C{blk}", nc=nc), io)
    for h in range(4):
        r64 = lambda base: qk_s[base + h * 64: base + (h + 1) * 64, :]
        io = dict(q=(r64(0), r64(256)), k=(r64(512), r64(768)), v=v1_s.rearrange("(j p) c -> p j c", p=128)[:, :, h * 128:(h + 1) * 128],
                  masks=D["masks"], tab=D["tab"][h], lam=D["lam"], gain=D["gain"][h], y=yc_s[h * 128:(h + 1) * 128, :])
        build_D(Ctx(f"D{h}", nc=nc), io)
    io = dict(E)
    io.update(xT=[x1_s[:, b * T:(b + 1) * T] for b in range(4)], yc=[yc_s[:, b * T:(b + 1) * T] for b in range(4)],
              zext=[z_s[:, b * T: b * T + T + 2] for b in range(4)], gb=[gb_s[:, b * T:(b + 1) * T] for b in range(4)],
              sel=sel_d, out=out_d)
    build_E(4, Tok("E", arena_words=14336, nc=nc), io)
    return nc


def host_fused(inp):
    x = np.asarray(inp["x"], np.float32)
    (dm, notallowed) = get_diff_masks()
    masks = _c(np.concatenate([dm[0][1], dm[-1][1], notallowed[None]], axis=0).transpose(1, 0, 2))
    bs4 = rep(np.tile(np.asarray(inp["gmlp_b_s"][0], np.float32), (1, 4)))
    sh = {
        "A_g": pk(inp["norm_mix"][0]), "A_w": _c(np.asarray(inp["even_w_in"][0], np.float32)),
        "A_vgain": rep(np.asarray(inp["gmlp_v_gain"][0], np.float32)),
        "A_ws": _c(np.transpose(np.asarray(inp["gmlp_w_s"][0], np.float32), (2, 0, 1))), "A_bs": bs4,
        "B_bf": _c(np.broadcast_to(np.asarray(inp["fox_b_f"][0], np.float32)[:, None, None], (4, 64, 1))),
        "B_ident": np.eye(64, dtype=np.float32), "B_lmat": np.triu(np.ones((64, 64), np.float32), 1),
        "B_tri": np.triu(np.ones((128, 128), np.float32), 0),
        "C_gcols": _c(np.stack([pk(inp["norm_mem_q"][0]), pk(inp["norm_ffn"][0]), pk(inp["norm_mix"][1]), pk(inp["norm_mix"][1])], axis=1)),
        "C_Wout": wtiles(inp["even_w_out"][0], 512), "C_Wg": wtiles(inp["ffn_w_gate"][0], 256), "C_Wu": wtiles(inp["ffn_w_up"][0], 256),
        "C_Wd": wtiles(inp["ffn_w_down"][0], 128), "C_Win": wtiles(inp["odd_w_in"][0], 512),
        "D_masks": masks,
        "D_tab": _c(np.stack([rep(np.asarray(inp["rel_bias"], np.float32)[:, h]) for h in range(4)])),
        "D_lam": _c(np.stack([rep(inp["diff_lambda_q1"][0]), rep(inp["diff_lambda_k1"][0]),
                              rep(inp["diff_lambda_q2"][0]), rep(inp["diff_lambda_k2"][0])], axis=1).astype(np.float32)),
        "D_gain": _c(np.asarray(inp["diff_subln_gain"][0], np.float32).reshape(4, 128, 1)),
        "E_convw": _c(np.asarray(inp["conv_w"][0], np.float32).reshape(3, 4, 128).transpose(2, 1, 0)),
        "E_gcols": _c(np.stack([pk(inp["norm_mem_q"][1]), pk(inp["norm_ffn"][1]), pk(inp["norm_final"]), pk(inp["norm_final"])], axis=1)),
        "E_Wout": wtiles(inp["odd_w_out"][0], 512),
        "E_Wr": _c(np.asarray(inp["router_w"][0], np.float32).reshape(8, 128, 8).transpose(1, 0, 2)),
        "E_ident": np.eye(128, dtype=np.float32),
        "E_Wg": np.stack([wtiles(inp["moe_w_gate"][0][e], 256) for e in range(NE)]),
        "E_Wu": np.stack([wtiles(inp["moe_w_up"][0][e], 256) for e in range(NE)]),
        "E_Wd": np.stack([wtiles(inp["moe_w_down"][0][e], 128) for e in range(NE)]),
    }
    memC = [mem_inputs(inp, 0, b) for b in range(2)]
    memE = [mem_inputs(inp, 1, b) for b in range(2)]
    xT = [_c(x[b].T) for b in range(2)]
    maps = []
    for core in range(8):
        b, i = core // 4, core % 4
        m = dict(sh)
        m["x"] = xT[b]
        for k_, v_ in memC[b].items():
            m["C_" + k_] = v_
        for k_, v_ in memE[b].items():
            m["E_" + k_] = v_
        selv = np.zeros((128, 4), np.float32)
        selv[:, i] = 1.0
        m["E_sel"] = selv
        maps.append(m)
    return maps


def kernel(**inp):
    inp = {k: np.asarray(v) for k, v in inp.items()}
    res = run(get_nc("F", build_fused), host_fused(inp))
    out = np.empty((2, 8192, 1024), np.float32)
    for core in range(8):
        b, i = core // 4, core % 4
        out[b, i * T:(i + 1) * T] = res[core]["out"].T
    return out
```

```python
import contextlib
from concourse.bass_utils import run_bass_kernel_spmd
import numpy as np
import concourse.bass as bass
import concourse.mybir as mybir

F32 = mybir.dt.float32
BF16 = mybir.dt.bfloat16
F32R = mybir.dt.float32r
I32 = mybir.dt.int32
U32 = mybir.dt.uint32
AF = mybir.ActivationFunctionType
ALU = mybir.AluOpType
AX = mybir.AxisListType

ENGS = ("pe", "act", "dve", "pool", "sp")


class Phase:
    def __init__(self, nc, name):
        self.nc = nc
        self.name = name
        self.ops = []
        self.last_writer = {}
        self.readers = {}
        self.dma_keys = {}

    def _add(self, eng, fn, reads, writes, dma_key=None, ndma=0):
        idx = len(self.ops)
        deps = set()
        for r in reads:
            w = self.last_writer.get(r)
            if w is not None:
                deps.add(w)
        for r in writes:
            w = self.last_writer.get(r)
            if w is not None:
                deps.add(w)
            for rd in self.readers.get(r, ()):
                deps.add(rd)
        deps.discard(idx)
        for r in writes:
            self.last_writer[r] = idx
            self.readers[r] = []
        for r in reads:
            if r not in writes:
                self.readers.setdefault(r, []).append(idx)
        self.ops.append(dict(eng=eng, fn=fn, deps=deps, dma_key=dma_key, ndma=ndma,
                             signal=False))
        return idx

    def op(self, eng, fn, reads=(), writes=()):
        return self._add(eng, fn, tuple(reads), tuple(writes))

    def I(self, eng, meth, *a, reads=(), writes=(), **kw):
        return self.op(eng, lambda e: getattr(e, meth)(*a, **kw), reads, writes)

    def dma(self, queue, fn, key, reads=(), writes=(), n=1):
        return self._add(queue, fn, tuple(reads), tuple(writes), dma_key=key, ndma=n)

    def emit(self):
        nc = self.nc
        ops = self.ops
        pos = {}
        cnt = {e: 0 for e in ENGS}
        for i, o in enumerate(ops):
            pos[i] = cnt[o["eng"]]
            cnt[o["eng"]] += 1
        for i, o in enumerate(ops):
            need = set()
            for d in o["deps"]:
                po = ops[d]
                if po["dma_key"] is not None:
                    need.add(d)
                elif po["eng"] == o["eng"]:
                    if o["eng"] != "pe" and pos[i] - pos[d] <= 2 and o["dma_key"] is None:
                        need.add(d)
                    elif o["dma_key"] is not None:
                        need.add(d)
                else:
                    need.add(d)
            o["need"] = need
            for d in need:
                if ops[d]["dma_key"] is None:
                    ops[d]["signal"] = True
        sigcnt = {e: 0 for e in ENGS}
        for o in ops:
            if o["dma_key"] is None and o["signal"]:
                sigcnt[o["eng"]] += 1
                o["sigval"] = sigcnt[o["eng"]]
        dmacnt = {}
        for o in ops:
            if o["dma_key"] is not None:
                dmacnt[o["dma_key"]] = dmacnt.get(o["dma_key"], 0) + o["ndma"]
                o["sigval"] = 16 * dmacnt[o["dma_key"]]
        keys = sorted(dmacnt.keys(), key=str)
        import contextlib
        esem = {e: nc.alloc_semaphore(name=f"{self.name}_{e}") for e in ENGS}
        dsem = {k: nc.alloc_semaphore(name=f"{self.name}_d{j}") for j, k in enumerate(keys)}
        with contextlib.ExitStack() as st:
            block = st.enter_context(nc.Block())
            per_eng = {e: [] for e in ENGS}
            for i, o in enumerate(ops):
                per_eng[o["eng"]].append(i)

            def run(engname, eng):
                seen = {}
                for i in per_eng[engname]:
                    o = ops[i]
                    waits = {}
                    for d in o["need"]:
                        po = ops[d]
                        if po["dma_key"] is not None:
                            k = ("d", po["dma_key"])
                        else:
                            k = ("e", po["eng"])
                        waits[k] = max(waits.get(k, 0), po["sigval"])
                    for k, v in waits.items():
                        if seen.get(k, 0) >= v:
                            continue
                        seen[k] = v
                        s = dsem[k[1]] if k[0] == "d" else esem[k[1]]
                        eng.wait_ge(s, v)
                    if o["dma_key"] is not None:
                        o["fn"](eng, dsem[o["dma_key"]])
                    else:
                        ins = o["fn"](eng)
                        if o["signal"]:
                            ins.then_inc(esem[engname], 1)
                fin = {}
                for i in per_eng[engname]:
                    o = ops[i]
                    if o["dma_key"] is not None:
                        fin[o["dma_key"]] = max(fin.get(o["dma_key"], 0), o["sigval"])
                for k, v in fin.items():
                    if seen.get(("d", k), 0) < v:
                        eng.wait_ge(dsem[k], v)

            @block.tensor
            def _(e):
                run("pe", e)

            @block.scalar
            def _(e):
                run("act", e)

            @block.vector
            def _(e):
                run("dve", e)

            @block.gpsimd
            def _(e):
                run("pool", e)

            @block.sync
            def _(e):
                run("sp", e)
        nc.clear_and_free_semaphores(list(esem.values()) + list(dsem.values()))
        nc.all_engine_barrier()


T = 2048
CH = 512
NCH = T // CH
EPS = 1e-6
NEG = -30000.0


class Ctx:
    def __init__(self, name, nc=None):
        self.nc = nc if nc is not None else bass.Bass("TRN2", target_bir_lowering=False)
        self.name = name
        self.st = contextlib.ExitStack()
        self.ph = Phase(self.nc, name)
        self.n = 0

    def din(self, name, shape, dt=F32):
        return self.nc.dram_tensor(name, list(shape), dt, kind="ExternalInput").ap()

    def dout(self, name, shape, dt=F32):
        return self.nc.dram_tensor(name, list(shape), dt, kind="ExternalOutput").ap()

    def sb(self, name, shape, dt=F32):
        return self.st.enter_context(self.nc.sbuf_tensor("s_" + self.name + "_" + name, list(shape), dt))

    def psum(self, name):
        return self.st.enter_context(self.nc.psum_tensor("p_" + self.name + "_" + name, [128, 512], F32))

    def finish(self):
        self.ph.emit()
        self.st.close()
        return self.nc


def ld(ph, queue, key, out_ap, in_ap, writes, reads=()):
    ph.dma(queue, lambda e, s: e.dma_start(out=out_ap, in_=in_ap).then_inc(s, 16), key,
           reads=reads, writes=writes)


def rmsnorm_fm(ph, tag, src, cs, gcol, xn, ones_bf, sq, psN, rstd_s, rstd, src_res, xn_res,
               nk=8, dim=1024.0):
    for k in range(nk):
        sqk = sq[k % 2]
        ph.op("act", (lambda k, sqk: lambda e: e.activation(out=sqk[:], in_=src[:, k, cs], func=AF.Square))(k, sqk),
              reads=[src_res], writes=[f"{tag}sq{k%2}"])
        ph.op("pe", (lambda k, sqk: lambda e: e.matmul(psN[:], lhsT=ones_bf[:], rhs=sqk[:], start=(k == 0), stop=(k == nk - 1)))(k, sqk),
              reads=[f"{tag}sq{k%2}", "ones"], writes=[f"{tag}psN"])
    ph.op("act", lambda e: e.activation(out=rstd_s[:], in_=psN[:], func=AF.Sqrt, scale=1.0 / dim, bias=EPS),
          reads=[f"{tag}psN"], writes=[f"{tag}rstd_s"])
    ph.op("dve", lambda e: e.reciprocal(out=rstd[:], in_=rstd_s[:]), reads=[f"{tag}rstd_s"], writes=[f"{tag}rstd"])
    for k in range(nk):
        ph.op("dve", (lambda k: lambda e: e.scalar_tensor_tensor(out=xn[:, k, :], in0=src[:, k, cs], scalar=gcol[:, k:k + 1],
                                                                  in1=rstd[:], op0=ALU.mult, op1=ALU.mult))(k),
              reads=[src_res, f"{tag}rstd", "gcol"], writes=[xn_res])


def build_A(c=None, io=None):
    if c is None:
        c = Ctx("A")
        io = dict(xT=c.din("xT", [1024, T]), g=c.din("g", [128, 8]), w=c.din("w", [1024, 2564]), vgain=c.din("vgain", [128, 512]),
                  ws=c.din("ws", [128, 4, 128]), bs=c.din("bs", [128, 4, 512]),
                  ya=c.dout("ya", [512, T], BF16), q=c.dout("q", [512, T], BF16), k=c.dout("k", [512, T], BF16),
                  v=c.dout("v", [T, 512], BF16), f=c.dout("f", [4, T], F32))
    nc, ph = c.nc, c.ph
    xT_d, g_d, w_d, vg_d, ws_d, bs_d = io["xT"], io["g"], io["w"], io["vgain"], io["ws"], io["bs"]
    ya_d, q_d, k_d, v_d, f_d = io["ya"], io["q"], io["k"], io["v"], io["f"]

    xT = c.sb("xT", [128, 8, T])
    w = c.sb("w", [128, 8, 2564], BF16)
    gcol = c.sb("gcol", [128, 8])
    ones_bf = c.sb("ones", [128, 128], BF16)
    sq = [c.sb(f"sq{i}", [128, CH], BF16) for i in range(2)]
    rstd_s = c.sb("rstd_s", [128, CH])
    rstd = c.sb("rstd", [128, CH])
    xn = c.sb("xn", [128, 8, CH], BF16)
    uT = c.sb("uT", [128, 4, CH])
    vgain = c.sb("vgain", [128, 512])
    wsm = c.sb("wsm", [128, 4, 128], BF16)
    bsb = c.sb("bsb", [128, 4, 512])
    vg = c.sb("vg", [128, 512])
    vscr = c.sb("vscr", [128, 128])
    ss = c.sb("ss", [128, 4])
    ssr = c.sb("ssr", [128, 4])
    vn = [c.sb(f"vn{i}", [128, 512], BF16) for i in range(2)]
    tmix = c.sb("tmix", [128, 512])
    yast = c.sb("yast", [128, 4, CH], BF16)
    qst = c.sb("qst", [128, 4, CH], BF16)
    kst = c.sb("kst", [128, 4, CH], BF16)
    vst = c.sb("vst", [128, 4, 512], BF16)
    fst = c.sb("fst", [4, CH])
    psN = c.psum("psN")
    psP = [c.psum(f"psP{i}") for i in range(3)]
    psM = [c.psum(f"psM{i}") for i in range(4)]

    ld(ph, "sp", "lx", xT[:], xT_d.rearrange("(k p) t -> p k t", p=128), ["xT"])
    ld(ph, "sp", "lg", gcol[:], g_d, ["gcol"])
    ld(ph, "sp", "lvg", vgain[:], vg_d, ["vgain"])
    ld(ph, "sp", "lbs", bsb[:], bs_d, ["bsb"])
    wv = w_d.rearrange("(k p) c -> p k c", p=128)
    for gi, (a, b) in enumerate([(0, 512), (512, 1024), (1024, 1536), (1536, 2048), (2048, 2564)]):
        ld(ph, "pool", f"lw{gi}", w[:, :, a:b], wv[:, :, a:b], [f"w{gi}"])
    ld(ph, "pool", "lws", wsm[:], ws_d, ["wsm"])
    ph.op("dve", lambda e: e.memset(ones_bf[:], 1.0), writes=["ones"])
    ph.op("dve", lambda e: e.memset(wsm[64:128, :, 0:64], 0.0), reads=["wsm"], writes=["wsm"])

    pi = [0]

    def nextps():
        pi[0] = (pi[0] + 1) % 3
        return psP[pi[0]], f"psP{pi[0]}"

    def proj_fm(col0, m, wres):
        ps, pres = nextps()
        for k in range(8):
            ph.op("pe", (lambda k, ps: lambda e: e.matmul(ps[:], lhsT=w[:, k, col0 + m * 128: col0 + (m + 1) * 128], rhs=xn[:, k, :],
                                                           start=(k == 0), stop=(k == 7)))(k, ps),
                  reads=["xn", wres], writes=[pres])
        return ps, pres

    for tc in range(NCH):
        cs = slice(tc * CH, (tc + 1) * CH)
        rmsnorm_fm(ph, "n", xT, cs, gcol, xn, ones_bf, sq, psN, rstd_s, rstd, "xT", "xn")
        for m in range(4):
            ps, pres = proj_fm(0, m, "w0")
            ph.op("act", (lambda m, ps: lambda e: e.activation(out=uT[:, m, :], in_=ps[:], func=AF.Gelu_apprx_tanh))(m, ps),
                  reads=[pres], writes=[f"uT{m}"])
        for m in range(4):
            ps, pres = proj_fm(1024, m, "w2")
            ph.op("dve", (lambda m, ps: lambda e: e.tensor_copy(out=qst[:, m, :], in_=ps[:]))(m, ps), reads=[pres], writes=["qst"])
        ld(ph, "sp", "sq", q_d.rearrange("(m p) t -> p m t", p=128)[:, :, cs], qst[:], [], reads=["qst"])
        for m in range(4):
            ps, pres = proj_fm(1536, m, "w3")
            ph.op("act", (lambda m, ps: lambda e: e.activation(out=kst[:, m, :], in_=ps[:], func=AF.Copy))(m, ps), reads=[pres], writes=["kst"])
        ld(ph, "sp", "sk", k_d.rearrange("(m p) t -> p m t", p=128)[:, :, cs], kst[:], [], reads=["kst"])
        ps, pres = nextps()
        for k in range(8):
            ph.op("pe", (lambda k, ps: lambda e: e.matmul(ps[0:4, :], lhsT=w[:, k, 2560:2564], rhs=xn[:, k, :], start=(k == 0), stop=(k == 7)))(k, ps),
                  reads=["xn", "w4"], writes=[pres])
        ph.op("dve", (lambda ps: lambda e: e.tensor_copy(out=fst[:], in_=ps[0:4, :]))(ps), reads=[pres], writes=["fst"])
        ld(ph, "sp", "sf", f_d[:, cs], fst[:], [], reads=["fst"])
        for s in range(4):
            ts_ = slice(s * 128, (s + 1) * 128)
            ps, pres = nextps()
            for k in range(8):
                ph.op("pe", (lambda k, ps, ts_: lambda e: e.matmul(ps[:], lhsT=xn[:, k, ts_], rhs=w[:, k, 512:1024], start=(k == 0), stop=(k == 7)))(k, ps, ts_),
                      reads=["xn", "w1"], writes=[pres])
            ph.op("act", (lambda ps: lambda e: e.activation(out=vg[:], in_=ps[:], func=AF.Gelu_apprx_tanh))(ps), reads=[pres], writes=["vg"])
            for g in range(4):
                ph.op("act", (lambda g: lambda e: e.activation(out=vscr[:], in_=vg[:, g * 128:(g + 1) * 128], func=AF.Square,
                                                               accum_out=ss[:, g:g + 1]))(g),
                      reads=["vg"], writes=["vscr", f"ss{g}"])
            ph.op("act", lambda e: e.activation(out=ssr[:], in_=ss[:], func=AF.Sqrt, scale=1.0 / 128.0, bias=EPS),
                  reads=[f"ss{g}" for g in range(4)], writes=["ssr"])
            ph.op("dve", lambda e: e.reciprocal(out=ss[:], in_=ssr[:]), reads=["ssr"], writes=[f"ss{g}" for g in range(4)])
            vnb = vn[s % 2]
            for g in range(4):
                gs = slice(g * 128, (g + 1) * 128)
                ph.op("dve", (lambda g, gs, vnb: lambda e: e.scalar_tensor_tensor(out=vnb[:, gs], in0=vg[:, gs], scalar=ss[:, g:g + 1], in1=vgain[:, gs],
                                                                                   op0=ALU.mult, op1=ALU.mult))(g, gs, vnb),
                      reads=["vg", f"ss{g}", "vgain"], writes=[f"vn{s%2}"])
            for g in range(4):
                gs = slice(g * 128, (g + 1) * 128)
                ph.op("pe", (lambda g, gs, vnb, ts_: lambda e: e.matmul(psM[g][:, ts_], lhsT=vnb[:, gs], rhs=wsm[:, g, :], start=True, stop=True))(g, gs, vnb, ts_),
                      reads=[f"vn{s%2}", "wsm"], writes=[f"psM{g}"])
            ps, pres = nextps()
            for k in range(8):
                ph.op("pe", (lambda k, ps, ts_: lambda e: e.matmul(ps[:], lhsT=xn[:, k, ts_], rhs=w[:, k, 2048:2560], start=(k == 0), stop=(k == 7)))(k, ps, ts_),
                      reads=["xn", "w4"], writes=[pres])
            ph.op("dve", (lambda ps, s: lambda e: e.tensor_copy(out=vst[:, s, :], in_=ps[:]))(ps, s), reads=[pres], writes=["vst"])
        ld(ph, "sp", "sv", v_d.rearrange("(n p) c -> p n c", p=128)[:, tc * 4:(tc + 1) * 4, :], vst[:], [], reads=["vst"])
        for g in range(4):
            ph.op("dve", (lambda g: lambda e: e.tensor_tensor(out=tmix[:], in0=psM[g][:], in1=bsb[:, g, :], op=ALU.add))(g),
                  reads=[f"psM{g}", "bsb"], writes=["tmix"])
            ph.op("dve", (lambda g: lambda e: e.tensor_tensor(out=yast[:, g, :], in0=tmix[:], in1=uT[:, g, :], op=ALU.mult))(g),
                  reads=["tmix", f"uT{g}"], writes=["yast"])
        ld(ph, "sp", "sy", ya_d.rearrange("(m p) t -> p m t", p=128)[:, :, cs], yast[:], [], reads=["yast"])
    return c.finish()


def _c(a):
    return np.ascontiguousarray(a)


def pk(vec):
    return _c(np.asarray(vec).reshape(-1, 128).T)


def rep(vec, n=128):
    return _c(np.broadcast_to(np.asarray(vec)[None], (n,) + tuple(np.asarray(vec).shape)))


def core_tokens(xfull, core):
    b, i = core // 4, core % 4
    return xfull[b, i * T:(i + 1) * T]


_NC_CACHE = {}


def get_nc(name, builder):
    if name not in _NC_CACHE:
        _NC_CACHE[name] = builder()
    return _NC_CACHE[name]


def run(nc, in_maps):
    res = run_bass_kernel_spmd(nc, in_maps, core_ids=list(range(8)))
    return res.results


def host_A(inp):
    x = np.asarray(inp["x"], np.float32)
    maps = []
    ws = _c(np.transpose(np.asarray(inp["gmlp_w_s"][0], np.float32), (2, 0, 1)))
    bs = np.asarray(inp["gmlp_b_s"][0], np.float32)
    bs4 = rep(np.tile(bs, (1, 4)))
    for core in range(8):
        maps.append(dict(
            xT=_c(core_tokens(x, core).T),
            g=pk(inp["norm_mix"][0]),
            w=_c(np.asarray(inp["even_w_in"][0], np.float32)),
            vgain=rep(np.asarray(inp["gmlp_v_gain"][0], np.float32)),
            ws=ws, bs=bs4))
    return maps


S = 8192
NT = S // 128
NG = S // CH


def build_B(c=None, io=None):
    if c is None:
        c = Ctx("B")
        io = dict(q=c.din("q", [128, S], BF16), k=c.din("k", [128, S], BF16), v=c.din("v", [128, NT, 128], BF16),
                  f2=c.din("f2", [64, 128]), bf=c.din("bf", [64, 1]), ident=c.din("ident", [64, 64]), lmat=c.din("lmat", [64, 64]),
                  tri=c.din("tri", [128, 128]), y=c.dout("y", [128, S], BF16))
    nc, ph = c.nc, c.ph
    q_d, k_d, v_d, f_d, bf_d, id_d, L_d, tri_d, y_d = (io[n] for n in ("q", "k", "v", "f2", "bf", "ident", "lmat", "tri", "y"))

    qT = c.sb("qT", [128, S], BF16)
    kT = c.sb("kT", [128, S], BF16)
    V = c.sb("V", [128, NT, 128], BF16)
    f2 = c.sb("f2", [64, 128])
    bfc = c.sb("bfc", [64, 1])
    ident = c.sb("ident", [64, 64])
    lmat = c.sb("lmat", [64, 64])
    tri = c.sb("tri", [128, 128], BF16)
    ones_bf = c.sb("ones", [128, 128], BF16)
    ones64 = c.sb("ones64", [64, 128])
    cs2 = c.sb("cs2", [64, 128])
    off = c.sb("off", [64, 1])
    dg = c.sb("dg", [64, 64])
    dcol = c.sb("dcol", [128, 64])
    dref = c.sb("dref", [128, 64])
    biasm = c.sb("biasm", [128, NT // 2, NT])
    PT = [c.sb(f"PT{i}", [128, CH], BF16) for i in range(4)]
    rl = c.sb("rl", [128, CH])
    ost = [c.sb(f"ost{i}", [128, CH], BF16) for i in range(2)]
    psS = [c.psum(f"psS{i}") for i in range(3)]
    psO = [c.psum(f"psO{i}") for i in range(2)]
    psL = [c.psum(f"psL{i}") for i in range(2)]
    psX = c.psum("psX")

    ld(ph, "sp", "l1", f2[:], f_d, ["f2"])
    ld(ph, "sp", "l2", bfc[:], bf_d, ["bfc"])
    ld(ph, "sp", "l3", ident[:], id_d, ["ident"])
    ld(ph, "sp", "l4", lmat[:], L_d, ["lmat"])
    ld(ph, "pool", "l5", tri[:], tri_d, ["tri"])
    ld(ph, "sp", "lq", qT[:], q_d, ["qT"])
    ld(ph, "act", "lk", kT[:], k_d, ["kT"])
    ld(ph, "sp", "lv", V[:], v_d, ["V"])
    ph.op("dve", lambda e: e.memset(ones_bf[:], 1.0), writes=["ones"])
    ph.op("dve", lambda e: e.memset(ones64[:], 1.0), writes=["ones64"])

    ph.op("dve", lambda e: e.tensor_scalar(out=bfc[:], in0=bfc[:], scalar1=-1.0, scalar2=None, op0=ALU.mult), reads=["bfc"], writes=["bfc"])
    ph.op("act", lambda e: e.activation(out=f2[:], in_=f2[:], func=AF.Exp, scale=-1.0, bias=bfc[:, 0:1]), reads=["f2", "bfc"], writes=["f2"])
    ph.op("act", lambda e: e.activation(out=f2[:], in_=f2[:], func=AF.Ln, scale=1.0, bias=1.0), reads=["f2"], writes=["f2"])
    ph.op("dve", lambda e: e.tensor_tensor_scan(out=cs2[:], data0=f2[:], data1=f2[:], initial=0.0, op0=ALU.add, op1=ALU.max),
          reads=["f2"], writes=["cs2"])
    ph.op("pe", lambda e: e.matmul(psX[0:64, 0:1], lhsT=lmat[:], rhs=cs2[:, 127:128], start=True, stop=True), reads=["cs2", "lmat"], writes=["psX"])
    ph.op("dve", lambda e: e.tensor_copy(out=off[:], in_=psX[0:64, 0:1]), reads=["psX"], writes=["off"])
    ph.op("dve", lambda e: e.tensor_scalar(out=cs2[:], in0=cs2[:], scalar1=off[:, 0:1], scalar2=None, op0=ALU.add), reads=["cs2", "off"], writes=["cs2"])
    ph.op("pe", lambda e: e.transpose(psX[:, 0:64], cs2[:], ident[:]), reads=["cs2", "ident", "off"], writes=["psX"])
    ph.op("dve", lambda e: e.tensor_copy(out=dcol[:], in_=psX[:, 0:64]), reads=["psX"], writes=["dcol"])
    ph.op("dve", lambda e: e.tensor_scalar(out=dg[:], in0=ident[:], scalar1=cs2[:, 0:1], scalar2=None, op0=ALU.mult), reads=["cs2", "ident"], writes=["dg"])
    ph.op("pe", lambda e: e.matmul(psX[:, 64:128], lhsT=ones64[:], rhs=dg[:], start=True, stop=True), reads=["dg", "ones64", "dcol"], writes=["psX"])
    ph.op("dve", lambda e: e.tensor_copy(out=dref[:], in_=psX[:, 64:128]), reads=["psX"], writes=["dref"])
    for k in range(NT // 2):
        ph.I("dve", "tensor_scalar", out=biasm[:, k, 0:2 * k + 2], in0=dcol[:, 0:2 * k + 2], scalar1=dref[:, 2 * k + 1:2 * k + 2], scalar2=None,
             op0=ALU.subtract, reads=["dcol", "dref"], writes=["biasm"])

    scale = 128.0 ** -0.5
    LA = 2
    its = [(I, j) for I in range(NG) for j in range(4 * I + 4)]
    NIT = len(its)
    NPT = len(PT)

    def front(n):
        I, j = its[n]
        qs = slice(I * CH, (I + 1) * CH)
        pS, pr = psS[n % 3], f"psS{n%3}"
        P, sr = PT[n % NPT], f"PT{n%NPT}"
        ph.I("pe", "matmul", pS[:], lhsT=kT[:, j * 128:(j + 1) * 128], rhs=qT[:, qs], start=True, stop=True, reads=["kT", "qT"], writes=[pr])
        jj = max(j - 4 * I, 0)
        for kk in range(2):
            lo, hi = max(jj * 128, kk * 256), (kk + 1) * 256
            if lo >= hi:
                continue
            ph.I("act", "activation", out=P[:, lo:hi], in_=pS[:, lo:hi], func=AF.Exp, scale=scale, bias=biasm[:, 2 * I + kk, j:j + 1],
                 reads=[pr, "biasm"], writes=[sr])
        if j >= 4 * I:
            bs_ = slice(jj * 128, (jj + 1) * 128)
            ph.I("pool", "tensor_tensor", out=P[:, bs_], in0=P[:, bs_], in1=tri[:], op=ALU.mult, reads=[sr, "tri"], writes=[sr])

    def back(n):
        I, j = its[n]
        qs = slice(I * CH, (I + 1) * CH)
        last = 4 * I + 3
        P, sr = PT[n % NPT], f"PT{n%NPT}"
        pO, pL = psO[I % 2], psL[I % 2]
        c0 = max(j - 4 * I, 0) * 128
        ph.I("pe", "matmul", pO[:, c0:], lhsT=V[:, j, :], rhs=P[:, c0:], start=(j == 0), stop=(j == last), reads=[sr, "V"], writes=[f"psO{I%2}"])
        ph.I("pe", "matmul", pL[:, c0:], lhsT=ones_bf[:], rhs=P[:, c0:], start=(j == 0), stop=(j == last), reads=[sr, "ones"], writes=[f"psL{I%2}"])
        if j == last:
            o = ost[I % 2]
            ph.I("dve", "reciprocal", out=rl[:], in_=pL[:], reads=[f"psL{I%2}"], writes=["rl"])
            ph.I("dve", "tensor_tensor", out=o[:], in0=pO[:], in1=rl[:], op=ALU.mult, reads=[f"psO{I%2}", "rl"], writes=[f"ost{I%2}"])
            ld(ph, "sp", f"so{I%2}", y_d[:, qs], o[:], [], reads=[f"ost{I%2}"])

    for n in range(NIT + LA):
        if n < NIT:
            front(n)
        if n - LA >= 0:
            back(n - LA)
    return c.finish()


def host_B(inp, resA):
    maps = []
    ident = np.eye(64, dtype=np.float32)
    lmat = np.triu(np.ones((64, 64), np.float32), 1)
    tri = np.triu(np.ones((128, 128), np.float32), 0)
    for core in range(8):
        b, h = core // 4, core % 4
        q = np.concatenate([resA[b * 4 + i]["q"][h * 128:(h + 1) * 128] for i in range(4)], axis=1)
        k = np.concatenate([resA[b * 4 + i]["k"][h * 128:(h + 1) * 128] for i in range(4)], axis=1)
        v = np.concatenate([resA[b * 4 + i]["v"][:, h * 128:(h + 1) * 128] for i in range(4)], axis=0)
        f = np.concatenate([resA[b * 4 + i]["f"][h] for i in range(4)], axis=0)
        maps.append(dict(q=_c(q), k=_c(k), v=_c(v.reshape(NT, 128, 128).transpose(1, 0, 2)),
                         f2=_c(f.reshape(64, 128)), bf=np.full((64, 1), inp["fox_b_f"][0][h], np.float32),
                         ident=ident, lmat=lmat, tri=tri))
    return maps


MC = 1024
M_SCALE = 256.0 ** -0.5


def wtiles(W, ncols):
    W = np.asarray(W, np.float32)
    K_, N_ = W.shape
    return _c(W.reshape(K_ // 128, 128, N_ // ncols, ncols).transpose(2, 1, 0, 3))


class Tok(Ctx):
    def __init__(self, name, nwb=4, wb_elems=4096, arena_words=11264, nc=None):
        super().__init__(name, nc=nc)
        c = self
        self.xT = c.sb("xT", [128, 8, T])
        self.ones_bf = c.sb("ones", [128, 128], BF16)
        self.sq = [c.sb(f"sq{i}", [128, CH], BF16) for i in range(2)]
        self.rstd_s = c.sb("rstd_s", [128, CH])
        self.rstd = c.sb("rstd", [128, CH])
        self.gcols = c.sb("gcols", [128, 4, 8])
        self.wb = [c.sb(f"wb{i}", [128, wb_elems], BF16) for i in range(nwb)]
        self.wi = 0
        self.arena = c.sb("arena", [128, arena_words])
        self.actT = self.arena[:].bitcast(BF16).rearrange("p (k t) -> p k t", t=MC)
        self.ymb = [c.sb(f"ymb{i}", [128, 8, CH], BF16) for i in range(2)]
        self.hT = c.sb("hT", [128, 8, MC], BF16)
        self.sg = [c.sb(f"sg{i}", [128, CH], BF16) for i in range(2)]
        self.pp = [c.psum(f"pp{i}") for i in range(6)]
        self.psN = c.psum("psN")
        self.psL = c.psum("psL")
        self.pi = 0
        self.ph.I("dve", "memset", self.ones_bf[:], 1.0, writes=["ones"])

    def nextps(self):
        self.pi = (self.pi + 1) % len(self.pp)
        return self.pp[self.pi], f"pp{self.pi}"

    def wload(self, dram_tile_ap, nk, ncols):
        i = self.wi
        self.wi = (self.wi + 1) % len(self.wb)
        view = self.wb[i][:, 0:nk * ncols].rearrange("p (k c) -> p k c", c=ncols)
        ld(self.ph, "pool", f"wb{i}", view, dram_tile_ap, [f"wb{i}"])
        return view, f"wb{i}"

    def norm(self, gi, cs, xn, xn_res, n=CH):
        rmsnorm_fm(self.ph, "n", self.xT, cs, self.gcols[:, gi, :], xn, self.ones_bf, self.sq, self.psN,
                   self.rstd_s, self.rstd, "xT", xn_res)

    def linear_fm(self, Wt_d, nk, ncols, src, src_res, ntok, evac):
        ph = self.ph
        ncg = Wt_d.shape[0]
        for cg in range(ncg):
            wv, wres = self.wload(Wt_d[cg], nk, ncols)
            for mi in range(ncols // 128):
                for ts in range(ntok // CH):
                    ps, pres = self.nextps()
                    for k in range(nk):
                        ph.I("pe", "matmul", ps[:], lhsT=wv[:, k, mi * 128:(mi + 1) * 128], rhs=src[:, k, ts * CH:(ts + 1) * CH],
                             start=(k == 0), stop=(k == nk - 1), reads=[wres, src_res], writes=[pres])
                    evac(cg * (ncols // 128) + mi, ts, ps, pres)

    def resid_add(self, t0):
        ph, xT = self.ph, self.xT

        def ev(m, ts, ps, pres):
            sl = slice(t0 + ts * CH, t0 + (ts + 1) * CH)
            ph.I("dve", "tensor_tensor", out=xT[:, m, sl], in0=xT[:, m, sl], in1=ps[:], op=ALU.add, reads=[pres, "xT"], writes=["xT"])
        return ev

    def mem_setup(self, memT_d, gkv_d, Wkv_k_d, Wkv_v_d):
        c, ph = self, self.ph
        self.memT = self.arena[:, 0:2048].rearrange("p (k t) -> p k t", t=256)
        self.memn = self.arena[:, 2048:3072].bitcast(BF16).rearrange("p (k t) -> p k t", t=256)
        self.gkv = c.sb("gkv", [128, 8])
        self.KmT = self.arena[:, 3072:4096].bitcast(BF16).rearrange("p (k t) -> p k t", t=256)
        self.Vm = self.arena[:, 4096:5120].bitcast(BF16).rearrange("p (k t) -> p k t", t=1024)
        self.qmT = self.ymb[0]
        self.omT = self.ymb[1]
        self.PmT = [self.arena[:, 9728 + 256 * i: 9728 + 256 * (i + 1)].bitcast(BF16) for i in range(2)]
        self.rl = self.arena[:, 10240:10752]
        self.xq = c.sb("xq", [128, 8, CH], BF16)
        ld(ph, "sp", "lmem", self.memT, memT_d.rearrange("(k p) t -> p k t", p=128), ["memT"])
        ld(ph, "sp", "lgkv", self.gkv[:], gkv_d, ["gkv"])
        for k in range(8):
            sqk = self.sq[k % 2]
            ph.I("act", "activation", out=sqk[:, 0:256], in_=self.memT[:, k, :], func=AF.Square, reads=["memT"], writes=[f"nsq{k%2}"])
            ph.I("pe", "matmul", self.psN[:, 0:256], lhsT=self.ones_bf[:], rhs=sqk[:, 0:256], start=(k == 0), stop=(k == 7),
                 reads=[f"nsq{k%2}", "ones"], writes=["npsN"])
        ph.I("act", "activation", out=self.rstd_s[:, 0:256], in_=self.psN[:, 0:256], func=AF.Sqrt, scale=1.0 / 1024.0, bias=EPS,
             reads=["npsN"], writes=["nrstd_s"])
        ph.I("dve", "reciprocal", out=self.rstd[:, 0:256], in_=self.rstd_s[:, 0:256], reads=["nrstd_s"], writes=["nrstd"])
        for k in range(8):
            ph.I("dve", "scalar_tensor_tensor", out=self.memn[:, k, :], in0=self.memT[:, k, :], scalar=self.gkv[:, k:k + 1],
                 in1=self.rstd[:, 0:256], op0=ALU.mult, op1=ALU.mult, reads=["memT", "nrstd", "gkv"], writes=["memn"])
        for cg in range(2):
            wv, wres = self.wload(Wkv_k_d[cg], 8, 512)
            for mi in range(4):
                ps, pres = self.nextps()
                for k in range(8):
                    ph.I("pe", "matmul", ps[:, 0:256], lhsT=wv[:, k, mi * 128:(mi + 1) * 128], rhs=self.memn[:, k, :],
                         start=(k == 0), stop=(k == 7), reads=[wres, "memn"], writes=[pres])
                ph.I("act", "activation", out=self.KmT[:, cg * 4 + mi, :], in_=ps[:, 0:256], func=AF.Copy, reads=[pres], writes=["KmT"])
        for cg in range(2):
            wv, wres = self.wload(Wkv_v_d[cg], 8, 512)
            for mt in range(2):
                ps, pres = self.nextps()
                for k in range(8):
                    ph.I("pe", "matmul", ps[:], lhsT=self.memn[:, k, mt * 128:(mt + 1) * 128], rhs=wv[:, k, :],
                         start=(k == 0), stop=(k == 7), reads=[wres, "memn"], writes=[pres])
                ph.I("dve", "tensor_copy", out=self.Vm[:, mt, cg * 512:(cg + 1) * 512], in_=ps[:], reads=[pres], writes=["Vm"])

    def mem_attn(self, gi, Wq_d, Wo_d):
        ph = self.ph
        xqb = [self.hT[:, :, 0:CH], self.hT[:, :, CH:2 * CH]]
        self.norm(gi, slice(0, CH), xqb[0], "xqb0")
        for tc in range(NCH):
            cs = slice(tc * CH, (tc + 1) * CH)
            xq, xqr = xqb[tc % 2], f"xqb{tc%2}"

            def ev_q(m, ts, ps, pres):
                eng = "act" if m % 2 else "dve"
                if eng == "act":
                    ph.I("act", "activation", out=self.qmT[:, m, :], in_=ps[:], func=AF.Copy, reads=[pres], writes=["ymb0"])
                else:
                    ph.I("dve", "tensor_copy", out=self.qmT[:, m, :], in_=ps[:], reads=[pres], writes=["ymb0"])
            self.linear_fm(Wq_d, 8, 512, xq, xqr, CH, ev_q)
            if tc + 1 < NCH:
                self.norm(gi, slice((tc + 1) * CH, (tc + 2) * CH), xqb[(tc + 1) % 2], f"xqb{(tc+1)%2}")
            for hh in range(4):
                for mt in range(2):
                    ps, pres = self.nextps()
                    for c2 in range(2):
                        ph.I("pe", "matmul", ps[:], lhsT=self.KmT[:, hh * 2 + c2, mt * 128:(mt + 1) * 128], rhs=self.qmT[:, hh * 2 + c2, :],
                             start=(c2 == 0), stop=(c2 == 1), reads=["KmT", "ymb0"], writes=[pres])
                    ph.I("act", "activation", out=self.PmT[mt], in_=ps[:], func=AF.Exp, scale=M_SCALE, reads=[pres], writes=[f"PmT{mt}"])
                for mt in range(2):
                    ph.I("pe", "matmul", self.psL[:], lhsT=self.ones_bf[:], rhs=self.PmT[mt], start=(mt == 0), stop=(mt == 1),
                         reads=[f"PmT{mt}", "ones"], writes=["psL"])
                ph.I("dve", "reciprocal", out=self.rl, in_=self.psL[:], reads=["psL"], writes=["rl"])
                for c2 in range(2):
                    ps, pres = self.nextps()
                    for mt in range(2):
                        ph.I("pe", "matmul", ps[:], lhsT=self.Vm[:, mt, hh * 256 + c2 * 128: hh * 256 + (c2 + 1) * 128], rhs=self.PmT[mt],
                             start=(mt == 0), stop=(mt == 1), reads=["Vm", f"PmT{mt}"], writes=[pres])
                    ph.I("dve", "tensor_tensor", out=self.omT[:, hh * 2 + c2, :], in0=ps[:], in1=self.rl, op=ALU.mult,
                         reads=[pres, "rl"], writes=["ymb1"])
            self.linear_fm(Wo_d, 8, 512, self.omT, "ymb1", CH, self.resid_add(tc * CH))


def out_proj(c, ymix_d, Wout_d):
    ph = c.ph
    for tc in range(NCH):
        yb = c.ymb[tc % 2]
        if isinstance(ymix_d, tuple):
            for hi, part in enumerate(ymix_d):
                ld(ph, "sp", f"lym{tc%2}", yb[:, 4 * hi:4 * hi + 4, :], part.rearrange("(k p) t -> p k t", p=128)[:, :, tc * CH:(tc + 1) * CH],
                   [f"ymb{tc%2}"])
        else:
            ymv = ymix_d.rearrange("(k p) t -> p k t", p=128)
            ld(ph, "sp", f"lym{tc%2}", yb[:], ymv[:, :, tc * CH:(tc + 1) * CH], [f"ymb{tc%2}"])
        c.linear_fm(Wout_d, 8, 512, yb, f"ymb{tc%2}", CH, c.resid_add(tc * CH))


def ffn_swiglu(c, gi, Wg_d, Wu_d, Wd_d, nf, post=None):
    ph = c.ph
    ncg = nf // 2
    for mc in range(T // MC):
        for ts in range(MC // CH):
            cs = slice(mc * MC + ts * CH, mc * MC + (ts + 1) * CH)
            c.norm(gi, cs, c.hT[:, :, ts * CH:(ts + 1) * CH], "hT")
        for cg in range(ncg):
            wg, wgr = c.wload(Wg_d[cg], 8, 256)
            wu, wur = c.wload(Wu_d[cg], 8, 256)
            for mi in range(2):
                for ts in range(MC // CH):
                    psG, pgr = c.nextps()
                    psU, pur = c.nextps()
                    for k in range(8):
                        ph.I("pe", "matmul", psG[:], lhsT=wg[:, k, mi * 128:(mi + 1) * 128], rhs=c.hT[:, k, ts * CH:(ts + 1) * CH],
                             start=(k == 0), stop=(k == 7), reads=[wgr, "hT"], writes=[pgr])
                    for k in range(8):
                        ph.I("pe", "matmul", psU[:], lhsT=wu[:, k, mi * 128:(mi + 1) * 128], rhs=c.hT[:, k, ts * CH:(ts + 1) * CH],
                             start=(k == 0), stop=(k == 7), reads=[wur, "hT"], writes=[pur])
                    sg = c.sg[(mi * 2 + ts) % 2]
                    sgr = f"sg{(mi * 2 + ts) % 2}"
                    ph.I("act", "activation", out=sg[:], in_=psG[:], func=AF.Silu, reads=[pgr], writes=[sgr])
                    ph.I("dve", "tensor_tensor", out=c.actT[:, cg * 2 + mi, ts * CH:(ts + 1) * CH], in0=sg[:], in1=psU[:], op=ALU.mult,
                         reads=[sgr, pur], writes=["actT"])
        c.linear_fm(Wd_d, nf, 128, c.actT, "actT", MC, c.resid_add(mc * MC))


C_IN = dict(xT=([1024, T], F32), ymix=([1024, T], BF16), gcols=([128, 4, 8], F32), Wout=([2, 128, 8, 512], F32), memT=([1024, 256], F32),
            gkv=([128, 8], F32), Wkvk=([2, 128, 8, 512], F32), Wkvv=([2, 128, 8, 512], F32), Wq=([2, 128, 8, 512], F32),
            Wo=([2, 128, 8, 512], F32), Wg=([11, 128, 8, 256], F32), Wu=([11, 128, 8, 256], F32), Wd=([8, 128, 22, 128], F32),
            Win=([6, 128, 8, 512], F32))


def build_C(c=None, io=None):
    if c is None:
        c = Tok("C")
        io = {n: c.din(n, sh, dt) for n, (sh, dt) in C_IN.items()}
        io.update(xo=c.dout("xo", [1024, T]), qk=c.dout("qk", [1024, T], BF16), v=c.dout("v", [T, 512], BF16),
                  z=c.dout("z", [512, T]), gb=c.dout("gb", [512, T]))
    nc, ph = c.nc, c.ph
    xT_d, ymix_d, gcols_d, Wout_d, memT_d, gkv_d, Wkvk_d, Wkvv_d, Wq_d, Wo_d, Wg_d, Wu_d, Wd_d, Win_d = (io[n] for n in C_IN)
    xo_d, qk_d, v_d, z_d, gb_d = io["xo"], io["qk"], io["v"], io["z"], io["gb"]

    ld(ph, "act", "lx", c.xT[:], xT_d.rearrange("(k p) t -> p k t", p=128), ["xT"])
    ld(ph, "sp", "lg", c.gcols[:], gcols_d, ["gcol"])

    out_proj(c, ymix_d, Wout_d)
    c.mem_setup(memT_d, gkv_d, Wkvk_d, Wkvv_d)
    c.mem_attn(0, Wq_d, Wo_d)
    ffn_swiglu(c, 1, Wg_d, Wu_d, Wd_d, 22)
    ld(ph, "act", "sx", xo_d.rearrange("(k p) t -> p k t", p=128), c.xT[:], [], reads=["xT"])

    xn1 = c.xq
    ar = c.arena
    gbst = ar[:, 0:2048].rearrange("p (m t) -> p m t", t=CH)
    zst = ar[:, 2048:4096].rearrange("p (m t) -> p m t", t=CH)
    vst = ar[:, 4096:5120].bitcast(BF16).rearrange("p (m t) -> p m t", t=512)
    qkst = ar[:, 5120:7168].bitcast(BF16).rearrange("p (m t) -> p m t", t=CH)
    hst = ar[:, 7168:9216].rearrange("p (m t) -> p m t", t=CH)
    dummy = c.sb("dummy", [128, 1])
    ph.I("dve", "memset", dummy[:], 0.0, reads=["actT"], writes=["qkst", "gbst", "zst", "vst"] + [f"hst{m}" for m in range(4)])
    xnb = [c.hT[:, :, 0:CH], c.hT[:, :, CH:2 * CH]]
    c.norm(2, slice(0, CH), xnb[0], "xnb0")
    for tc in range(NCH):
        cs = slice(tc * CH, (tc + 1) * CH)
        xn1, xnr = xnb[tc % 2], f"xnb{tc%2}"

        def ev_qk(m, ts, ps, pres):
            if m % 2:
                ph.I("act", "activation", out=qkst[:, m, :], in_=ps[:], func=AF.Copy, reads=[pres], writes=["qkst"])
            else:
                ph.I("dve", "tensor_copy", out=qkst[:, m, :], in_=ps[:], reads=[pres], writes=["qkst"])
        c.linear_fm(Win_d[0:2], 8, 512, xn1, xnr, CH, ev_qk)
        if tc + 1 < NCH:
            c.norm(2, slice((tc + 1) * CH, (tc + 2) * CH), xnb[(tc + 1) % 2], f"xnb{(tc+1)%2}")
        ld(ph, "sp", "sqk", qk_d.rearrange("(m p) t -> p m t", p=128)[:, :, cs], qkst, [], reads=["qkst"])
        wv, wres = c.wload(Win_d[2], 8, 512)
        for s in range(4):
            ps, pres = c.nextps()
            for k in range(8):
                ph.I("pe", "matmul", ps[:], lhsT=xn1[:, k, s * 128:(s + 1) * 128], rhs=wv[:, k, :], start=(k == 0), stop=(k == 7),
                     reads=[wres, xnr], writes=[pres])
            ph.I("act", "activation", out=vst[:, s, :], in_=ps[:], func=AF.Copy, reads=[pres], writes=["vst"])
        ld(ph, "sp", "sv", v_d.rearrange("(n p) c -> p n c", p=128)[:, tc * 4:(tc + 1) * 4, :], vst, [], reads=["vst"])

        def ev_conv(m, ts, ps, pres):
            if m < 4:
                ph.I("act", "activation", out=hst[:, m, :], in_=ps[:], func=AF.Copy, reads=[pres], writes=[f"hst{m}"])
            elif m < 8:
                ph.I("act", "activation", out=gbst[:, m - 4, :], in_=ps[:], func=AF.Copy, reads=[pres], writes=["gbst"])
            else:
                ph.I("dve", "tensor_tensor", out=zst[:, m - 8, :], in0=hst[:, m - 8, :], in1=ps[:], op=ALU.mult,
                     reads=[pres, f"hst{m-8}"], writes=["zst"])
        c.linear_fm(Win_d[3:6], 8, 512, xn1, xnr, CH, ev_conv)
        ld(ph, "sp", "sgb", gb_d.rearrange("(m p) t -> p m t", p=128)[:, :, cs], gbst, [], reads=["gbst"])
        ld(ph, "sp", "sz", z_d.rearrange("(m p) t -> p m t", p=128)[:, :, cs], zst, [], reads=["zst"])
    return c.finish()


def mem_inputs(inp, l, b):
    wkv = np.asarray(inp["mem_w_kv"][l], np.float32)
    return dict(memT=_c(np.asarray(inp["mem"][b], np.float32).T), gkv=pk(inp["norm_mem_kv"][l]),
                Wkvk=wtiles(wkv[:, :1024], 512), Wkvv=wtiles(wkv[:, 1024:], 512),
                Wq=wtiles(inp["mem_w_q"][l], 512), Wo=wtiles(inp["mem_w_o"][l], 512))


def host_C(inp, resA, resB, xT_cores):
    maps = []
    gcols = _c(np.stack([pk(inp["norm_mem_q"][0]), pk(inp["norm_ffn"][0]), pk(inp["norm_mix"][1]), pk(inp["norm_mix"][1])], axis=1))
    shared = dict(gcols=gcols, Wout=wtiles(inp["even_w_out"][0], 512), Wg=wtiles(inp["ffn_w_gate"][0], 256),
                  Wu=wtiles(inp["ffn_w_up"][0], 256), Wd=wtiles(inp["ffn_w_down"][0], 128), Win=wtiles(inp["odd_w_in"][0], 512))
    mem = [mem_inputs(inp, 0, b) for b in range(2)]
    for core in range(8):
        b, i = core // 4, core % 4
        yb = np.concatenate([resB[b * 4 + h]["y"][:, i * T:(i + 1) * T] for h in range(4)], axis=0)
        ymix = np.concatenate([resA[core]["ya"], yb], axis=0)
        m = dict(xT=xT_cores[core], ymix=_c(ymix))
        m.update(shared)
        m.update(mem[b])
        maps.append(m)
    return maps


LAM_INIT = 0.8 - 0.6 * float(np.exp(-0.3 * 1))


def rel_bucket_static(rel):
    import jax.numpy as jnp
    import jax
    with jax.default_device(jax.devices("cpu")[0]):
        rel = jnp.asarray(rel, jnp.int32)
        n_half, max_exact = 16, 8
        ret = jnp.where(rel > 0, n_half, 0)
        n = jnp.abs(rel)
        nf = jnp.maximum(n, 1).astype(jnp.float32)
        large = max_exact + (jnp.log(nf / max_exact) / float(np.log(128 / max_exact)) * (n_half - max_exact)).astype(jnp.int32)
        large = jnp.minimum(large, n_half - 1)
        return np.asarray(ret + jnp.where(n < max_exact, n, large))


def diff_masks():
    p = np.arange(128)[:, None]
    f = np.arange(128)[None, :]
    out = {}
    for delta in (0, -1):
        rel = 128 * delta + p - f
        bk = rel_bucket_static(rel)
        ids = sorted(set(bk.reshape(-1).tolist()))
        masks = np.stack([(bk == b).astype(np.float32) for b in ids])
        out[delta] = (ids, masks)
    notallowed = ((p // 64) > (f // 64)).astype(np.float32)
    return out, notallowed


_DM = {}


def get_diff_masks():
    if "m" not in _DM:
        _DM["m"] = diff_masks()
    return _DM["m"]


def build_D(c=None, io=None):
    (dm, notallowed) = get_diff_masks()
    ids0, ids1 = dm[0][0], dm[-1][0]
    n0, n1 = len(ids0), len(ids1)
    if c is None:
        c = Ctx("D")
        io = dict(q=c.din("q", [128, S], BF16), k=c.din("k", [128, S], BF16), v=c.din("v", [128, NT, 128], BF16),
                  masks=c.din("masks", [128, n0 + n1 + 1, 128]), tab=c.din("tab", [128, 32]), lam=c.din("lam", [128, 4, 64]),
                  gain=c.din("gain", [128, 1]), y=c.dout("y", [128, S], BF16))
    nc, ph = c.nc, c.ph
    q_d, k_d, v_d, mk_d, tab_d, lam_d, gain_d, y_d = (io[n] for n in ("q", "k", "v", "masks", "tab", "lam", "gain", "y"))

    QT = c.sb("QT", [128, S], BF16)
    KT = [c.sb(f"KT{i}", [128, S], BF16) for i in range(2)]
    V = c.sb("V", [128, NT, 128], BF16)
    mk = c.sb("mk", [128, n0 + n1 + 1, 128])
    tab = c.sb("tab", [128, 32])
    lamp = c.sb("lamp", [128, 4, 64])
    gsc = c.sb("gsc", [128, 1])
    B0 = c.sb("B0", [128, 128])
    B1 = c.sb("B1", [128, 128])
    lprod = c.sb("lprod", [128, 2, 64])
    lsum = c.sb("lsum", [128, 2])
    neglam = c.sb("neglam", [128, 1])
    ones_bf = c.sb("ones", [128, 128], BF16)
    tb = [c.sb(f"tb{i}", [128, 128]) for i in range(2)]
    PT = [[c.sb(f"PT{s}{i}", [128, CH], BF16) for i in range(6)] for s in range(2)]
    l1c = c.sb("l1c", [128, CH])
    l2c = c.sb("l2c", [128, CH])
    o1 = c.sb("o1", [128, CH])
    o2 = c.sb("o2", [128, CH])
    sqo = c.sb("sqo", [128, CH], BF16)
    rs = c.sb("rs", [128, CH])
    ost = [c.sb(f"ost{i}", [128, CH], BF16) for i in range(2)]
    psS3 = [c.psum(f"psS{i}") for i in range(3)]
    psN = c.psum("psN")
    psO = [c.psum(f"psO{s}") for s in range(2)]
    psL = [c.psum(f"psL{s}") for s in range(2)]

    ld(ph, "sp", "l1", mk[:], mk_d, ["mk"])
    ld(ph, "sp", "l2", tab[:], tab_d, ["tab"])
    ld(ph, "sp", "l3", lamp[:], lam_d, ["lamp"])
    ld(ph, "sp", "l4", gsc[:], gain_d, ["gsc"])
    if isinstance(q_d, tuple):
        ld(ph, "sp", "lq", QT[0:64, :], q_d[0], ["QT"])
        ld(ph, "sp", "lq", QT[64:128, :], q_d[1], ["QT"])
        ld(ph, "act", "lk0", KT[0][0:64, :], k_d[0], ["KT0"])
        ld(ph, "act", "lk1", KT[1][64:128, :], k_d[1], ["KT1"])
    else:
        ld(ph, "sp", "lq", QT[:], q_d, ["QT"])
        ld(ph, "act", "lk0", KT[0][0:64, :], k_d[0:64, :], ["KT0"])
        ld(ph, "act", "lk1", KT[1][64:128, :], k_d[64:128, :], ["KT1"])
    ph.I("pool", "memset", KT[0][64:128, :], 0.0, writes=["KT0z"])
    ph.I("pool", "memset", KT[1][0:64, :], 0.0, writes=["KT1z"])
    ld(ph, "sp", "lv", V[:], v_d, ["V"])
    ph.I("dve", "memset", ones_bf[:], 1.0, writes=["ones"])
    ph.I("dve", "tensor_scalar", out=B0[:], in0=mk[:, n0 + n1, :], scalar1=NEG, scalar2=None, op0=ALU.mult, reads=["mk"], writes=["B0"])
    for n, b in enumerate(ids0):
        ph.I("dve", "scalar_tensor_tensor", out=B0[:], in0=mk[:, n, :], scalar=tab[:, b:b + 1], in1=B0[:], op0=ALU.mult, op1=ALU.add,
             reads=["mk", "tab", "B0"], writes=["B0"])
    ph.I("dve", "tensor_scalar", out=B1[:], in0=mk[:, n0, :], scalar1=tab[:, ids1[0]:ids1[0] + 1], scalar2=None, op0=ALU.mult,
         reads=["mk", "tab"], writes=["B1"])
    for n, b in enumerate(ids1[1:]):
        ph.I("dve", "scalar_tensor_tensor", out=B1[:], in0=mk[:, n0 + 1 + n, :], scalar=tab[:, b:b + 1], in1=B1[:], op0=ALU.mult, op1=ALU.add,
             reads=["mk", "tab", "B1"], writes=["B1"])
    ph.I("dve", "tensor_tensor", out=lprod[:, 0, :], in0=lamp[:, 0, :], in1=lamp[:, 1, :], op=ALU.mult, reads=["lamp"], writes=["lprod"])
    ph.I("dve", "tensor_tensor", out=lprod[:, 1, :], in0=lamp[:, 2, :], in1=lamp[:, 3, :], op=ALU.mult, reads=["lamp"], writes=["lprod"])
    ph.I("dve", "reduce_sum", out=lsum[:], in_=lprod[:], axis=AX.X, reads=["lprod"], writes=["lsum"])
    ph.I("act", "activation", out=lsum[:], in_=lsum[:], func=AF.Exp, reads=["lsum"], writes=["lsum"])
    ph.I("dve", "tensor_tensor", out=neglam[:], in0=lsum[:, 1:2], in1=lsum[:, 0:1], op=ALU.subtract, reads=["lsum"], writes=["neglam"])
    ph.I("dve", "tensor_scalar", out=neglam[:], in0=neglam[:], scalar1=-LAM_INIT, scalar2=None, op0=ALU.add, reads=["neglam"], writes=["neglam"])
    ph.I("dve", "tensor_scalar", out=gsc[:], in0=gsc[:], scalar1=1.0 - LAM_INIT, scalar2=None, op0=ALU.mult, reads=["gsc"], writes=["gsc"])

    scale = 64.0 ** -0.5
    LA = 4
    its = [(I, j) for I in range(NG) for j in range(4 * I + 4)]
    NIT = len(its)
    NPT = len(PT[0])

    def front(n):
        I, j = its[n]
        qs = slice(I * CH, (I + 1) * CH)
        jj = max(j - 4 * I, 0)
        for s in range(2):
            bi = (2 * n + s) % 3
            pS, pr = psS3[bi], f"psS{bi}"
            P, sr = PT[s][n % NPT], f"PT{s}{n%NPT}"
            ph.I("pe", "matmul", pS[:], lhsT=KT[s][:, j * 128:(j + 1) * 128], rhs=QT[:, qs], start=True, stop=True,
                 reads=[f"KT{s}", f"KT{s}z", "QT"], writes=[pr])
            if j <= 4 * I - 2:
                ph.I("act", "activation", out=P[:], in_=pS[:], func=AF.Exp, scale=scale, bias=tab[:, 15:16], reads=[pr, "tab"], writes=[sr])
            else:
                for ii in range(jj, 4):
                    delta = j - (4 * I + ii)
                    bs_ = slice(ii * 128, (ii + 1) * 128)
                    if delta <= -2:
                        ph.I("act", "activation", out=P[:, bs_], in_=pS[:, bs_], func=AF.Exp, scale=scale, bias=tab[:, 15:16],
                             reads=[pr, "tab"], writes=[sr])
                    else:
                        Bt, br = (B0, "B0") if delta == 0 else (B1, "B1")
                        t = tb[(s + ii) % 2]
                        tr = f"tb{(s + ii) % 2}"
                        ph.I("dve", "scalar_tensor_tensor", out=t[:], in0=pS[:, bs_], scalar=scale, in1=Bt[:], op0=ALU.mult, op1=ALU.add,
                             reads=[pr, br], writes=[tr])
                        ph.I("act", "activation", out=P[:, bs_], in_=t[:], func=AF.Exp, reads=[tr], writes=[sr])

    def back(n):
        I, j = its[n]
        qs = slice(I * CH, (I + 1) * CH)
        last = 4 * I + 3
        c0 = max(j - 4 * I, 0) * 128
        for s in range(2):
            P, sr = PT[s][n % NPT], f"PT{s}{n%NPT}"
            ph.I("pe", "matmul", psO[s][:, c0:], lhsT=V[:, j, :], rhs=P[:, c0:], start=(j == 0), stop=(j == last),
                 reads=[sr, "V"], writes=[f"psO{s}"])
            ph.I("pe", "matmul", psL[s][:, c0:], lhsT=ones_bf[:], rhs=P[:, c0:], start=(j == 0), stop=(j == last),
                 reads=[sr, "ones"], writes=[f"psL{s}"])
        if j != last:
            return
        ph.I("dve", "tensor_copy", out=l1c[:], in_=psL[0][:], reads=["psL0"], writes=["l1c"])
        ph.I("dve", "tensor_copy", out=o1[:], in_=psO[0][:], reads=["psO0"], writes=["o1"])
        ph.I("dve", "tensor_copy", out=l2c[:], in_=psL[1][:], reads=["psL1"], writes=["l2c"])
        ph.I("dve", "tensor_copy", out=o2[:], in_=psO[1][:], reads=["psO1"], writes=["o2"])
        ph.I("dve", "reciprocal", out=l1c[:], in_=l1c[:], reads=["l1c"], writes=["l1c"])
        ph.I("dve", "reciprocal", out=l2c[:], in_=l2c[:], reads=["l2c"], writes=["l2c"])
        ph.I("pool", "tensor_tensor", out=o1[:], in0=o1[:], in1=l1c[:], op=ALU.mult, reads=["o1", "l1c"], writes=["o1"])
        ph.I("pool", "tensor_tensor", out=o2[:], in0=o2[:], in1=l2c[:], op=ALU.mult, reads=["o2", "l2c"], writes=["o2"])
        ph.I("dve", "scalar_tensor_tensor", out=o1[:], in0=o2[:], scalar=neglam[:, 0:1], in1=o1[:], op0=ALU.mult, op1=ALU.add,
             reads=["o1", "o2", "neglam"], writes=["o1"])
        def part2(I=I, qs=qs):
            ph.I("act", "activation", out=sqo[:], in_=o1[:], func=AF.Square, reads=["o1"], writes=["sqo"])
            ph.I("pe", "matmul", psN[:], lhsT=ones_bf[:], rhs=sqo[:], start=True, stop=True, reads=["sqo", "ones"], writes=["psN"])
            ph.I("act", "activation", out=rs[:], in_=psN[:], func=AF.Sqrt, scale=1.0 / 128.0, bias=EPS, reads=["psN"], writes=["rs"])
            ph.I("dve", "reciprocal", out=rs[:], in_=rs[:], reads=["rs"], writes=["rs"])
            o = ost[I % 2]
            ph.I("dve", "scalar_tensor_tensor", out=o[:], in0=o1[:], scalar=gsc[:, 0:1], in1=rs[:], op0=ALU.mult, op1=ALU.mult,
                 reads=["o1", "rs", "gsc"], writes=[f"ost{I%2}"])
            ld(ph, "sp", f"so{I%2}", y_d[:, qs], o[:], [], reads=[f"ost{I%2}"])
        pending.append([n + DEFER, part2])

    DEFER = 8
    pending = []

    def flush(upto):
        while pending and pending[0][0] <= upto:
            pending.pop(0)[1]()

    for n in range(NIT + LA):
        if n < NIT:
            front(n)
        if n - LA >= 0:
            m = n - LA
            if its[m][1] == 4 * its[m][0] + 3:
                flush(10 ** 9)
            back(m)
            flush(m)
    flush(10 ** 9)
    return c.finish()


def host_D(inp, resC):
    (dm, notallowed) = get_diff_masks()
    masks = np.concatenate([dm[0][1], dm[-1][1], notallowed[None]], axis=0)
    masks = _c(masks.transpose(1, 0, 2))
    lam = _c(np.stack([rep(inp["diff_lambda_q1"][0]), rep(inp["diff_lambda_k1"][0]),
                       rep(inp["diff_lambda_q2"][0]), rep(inp["diff_lambda_k2"][0])], axis=1).astype(np.float32))
    maps = []
    for core in range(8):
        b, h = core // 4, core % 4
        qk = np.concatenate([resC[b * 4 + i]["qk"] for i in range(4)], axis=1)
        q = np.concatenate([qk[h * 64:(h + 1) * 64], qk[256 + h * 64:256 + (h + 1) * 64]], axis=0)
        k = np.concatenate([qk[512 + h * 64:512 + (h + 1) * 64], qk[768 + h * 64:768 + (h + 1) * 64]], axis=0)
        v = np.concatenate([resC[b * 4 + i]["v"][:, h * 128:(h + 1) * 128] for i in range(4)], axis=0)
        maps.append(dict(q=_c(q), k=_c(k), v=_c(v.reshape(NT, 128, 128).transpose(1, 0, 2)), masks=masks,
                         tab=rep(np.asarray(inp["rel_bias"], np.float32)[:, h]), lam=lam,
                         gain=_c(np.asarray(inp["diff_subln_gain"][0], np.float32)[h * 128:(h + 1) * 128, None])))
    return maps


NE = 8
NFE = 28


E_IN = dict(xT=([1024, T], F32), yc=([512, T], BF16), zext=([512, T + 2], F32), gb=([512, T], F32), convw=([128, 4, 3], F32),
            gcols=([128, 4, 8], F32), Wout=([2, 128, 8, 512], F32), memT=([1024, 256], F32), gkv=([128, 8], F32),
            Wkvk=([2, 128, 8, 512], F32), Wkvv=([2, 128, 8, 512], F32), Wq=([2, 128, 8, 512], F32), Wo=([2, 128, 8, 512], F32),
            Wr=([128, 8, 8], F32), ident=([128, 128], F32), Wg=([NE, 14, 128, 8, 256], F32), Wu=([NE, 14, 128, 8, 256], F32),
            Wd=([NE, 8, 128, NFE, 128], F32))


def build_E(stage=4, c=None, io=None):
    if c is None:
        c = Tok("E", arena_words=14336)
        io = {n: c.din(n, sh, dt) for n, (sh, dt) in E_IN.items()}
        io["out"] = c.dout("out", [1024, T])
    nc, ph = c.nc, c.ph
    (xT_d, yc_d, zx_d, gb_d, cw_d, gcols_d, Wout_d, memT_d, gkv_d, Wkvk_d, Wkvv_d, Wq_d, Wo_d, Wr_d, id_d, Wg_d, Wu_d, Wd_d) = (io[n] for n in E_IN)
    out_d = io["out"]
    sel_d = io.get("sel")
    fused = sel_d is not None

    cw = c.sb("cw", [128, 4, 3])
    zc = c.arena[:, 5120:5120 + 4 * (CH + 2)].rearrange("p (m t) -> p m t", t=CH + 2)
    gbc = c.arena[:, 7680:9728].rearrange("p (m t) -> p m t", t=CH)
    acc = c.arena[:, 10752:11264]
    ident = c.sb("ident", [128, 128])
    ones32 = c.sb("ones32", [128, 128])
    wr32 = c.sb("wr32", [128, 8, 8])
    wrh = c.sb("wrh", [128, 8, 8], BF16)
    wrl = c.sb("wrl", [128, 8, 8], BF16)
    y1w = c.ymb[1][:].rearrange("p k t -> p (k t)").bitcast(F32)
    y0w = c.ymb[0][:].rearrange("p k t -> p (k t)").bitcast(F32)
    t32 = [y1w[:, i * CH:(i + 1) * CH] for i in range(2)]
    lg = c.sb("lg", [128, 8])
    mx8 = c.sb("mx8", [128, 8])
    nv1 = c.sb("nv1", [128, 1])
    ex = c.sb("ex", [128, 8])
    msk = c.sb("msk", [128, 8])
    den = c.sb("den", [128, 1])
    comb = c.sb("comb", [128, MC // 128, 8])
    dg = [c.sb(f"dg{i}", [128, 128]) for i in range(2)]
    cbc = [y0w[:, i * MC:(i + 1) * MC] for i in range(2)]
    tmp = [y1w[:, (2 + i) * CH:(3 + i) * CH] for i in range(2)]

    def sel_load(dst, srcs, stg, tag, res, queue="sp"):
        for b_ in range(4):
            sb_ = stg[b_ % len(stg)]
            sr_ = f"{tag}stg{b_ % len(stg)}"
            ld(ph, queue, f"l{tag}{b_ % len(stg)}", sb_, srcs[b_], [sr_])
            if b_ == 0:
                ph.I("dve", "tensor_scalar", out=dst, in0=sb_, scalar1=sel[:, 0:1], scalar2=None, op0=ALU.mult, reads=[sr_, "sel"], writes=[res])
            else:
                ph.I("dve", "scalar_tensor_tensor", out=dst, in0=sb_, scalar=sel[:, b_:b_ + 1], in1=dst, op0=ALU.mult, op1=ALU.add,
                     reads=[sr_, "sel", res], writes=[res])

    if fused:
        sel = c.sb("sel", [128, 4])
        ld(ph, "sp", "lsel", sel[:], sel_d, ["sel"])
        xstg = [c.arena[:, i * T:(i + 1) * T] for i in range(2)]
        for k in range(8):
            sel_load(c.xT[:, k, :], [xb.rearrange("(k p) t -> p k t", p=128)[:, k, :] for xb in xT_d], xstg, "x", "xT")
    else:
        ld(ph, "sp", "lx", c.xT[:], xT_d.rearrange("(k p) t -> p k t", p=128), ["xT"])
    ld(ph, "sp", "lg", c.gcols[:], gcols_d, ["gcol"])
    ld(ph, "sp", "lcw", cw[:], cw_d, ["cw"])
    ld(ph, "sp", "lid", ident[:], id_d, ["ident"])
    ld(ph, "sp", "lwr", wr32[:], Wr_d, ["wr32"])
    ph.I("dve", "memset", ones32[:], 1.0, writes=["ones32"])
    ph.I("dve", "tensor_copy", out=wrh[:], in_=wr32[:], reads=["wr32"], writes=["wrh"])
    ph.I("dve", "tensor_tensor", out=wrl[:], in0=wr32[:], in1=wrh[:], op=ALU.subtract, reads=["wr32", "wrh"], writes=["wrl"])

    fm = lambda ap: ap.rearrange("(k p) t -> p k t", p=128)
    if fused:
        zstg = [c.arena[:, i * 2056:(i + 1) * 2056].rearrange("p (m t) -> p m t", t=CH + 2) for i in range(2)]
        gstg = [c.arena[:, 11264:13312].rearrange("p (m t) -> p m t", t=CH)]
        ystg = [c.arena[:, 13312:14336].bitcast(BF16).rearrange("p (m t) -> p m t", t=CH)]
        dmy = c.sb("dmy", [128, 1])
        ph.I("dve", "memset", dmy[:], 0.0, reads=["xstg0", "xstg1"], writes=["zstg0", "zstg1"])
    for tc in range(NCH):
        yb = c.ymb[tc % 2]
        yr = f"ymb{tc%2}"
        if fused:
            sel_load(yb[:, 0:4, :], [fm(a)[:, :, tc * CH:(tc + 1) * CH] for a in yc_d], ystg, "y", yr)
            sel_load(zc, [fm(a)[:, :, tc * CH: tc * CH + CH + 2] for a in zx_d], zstg, "z", "zc")
            sel_load(gbc, [fm(a)[:, :, tc * CH:(tc + 1) * CH] for a in gb_d], gstg, "g", "gbc")
        else:
            ld(ph, "sp", f"lym{tc%2}", yb[:, 0:4, :], fm(yc_d)[:, :, tc * CH:(tc + 1) * CH], [yr])
            ld(ph, "sp", "lz", zc, fm(zx_d)[:, :, tc * CH: tc * CH + CH + 2], ["zc"])
            ld(ph, "sp", "lgb", gbc, fm(gb_d)[:, :, tc * CH:(tc + 1) * CH], ["gbc"])
        for m in range(4):
            ph.I("dve", "tensor_scalar", out=acc, in0=zc[:, m, 2:CH + 2], scalar1=cw[:, m, 2:3], scalar2=None, op0=ALU.mult,
                 reads=["zc", "cw"], writes=["acc"])
            ph.I("dve", "scalar_tensor_tensor", out=acc, in0=zc[:, m, 1:CH + 1], scalar=cw[:, m, 1:2], in1=acc, op0=ALU.mult, op1=ALU.add,
                 reads=["zc", "cw", "acc"], writes=["acc"])
            ph.I("dve", "scalar_tensor_tensor", out=acc, in0=zc[:, m, 0:CH], scalar=cw[:, m, 0:1], in1=acc, op0=ALU.mult, op1=ALU.add,
                 reads=["zc", "cw", "acc"], writes=["acc"])
            ph.I("dve", "tensor_tensor", out=yb[:, 4 + m, :], in0=acc, in1=gbc[:, m, :], op=ALU.mult, reads=["acc", "gbc"], writes=[yr])
        c.linear_fm(Wout_d, 8, 512, yb, yr, CH, c.resid_add(tc * CH))

    if stage >= 2:
        c.mem_setup(memT_d, gkv_d, Wkvk_d, Wkvv_d)
        c.mem_attn(0, Wq_d, Wo_d)

    hlo = c.xq if stage >= 2 else None
    for mc in range(T // MC if stage >= 3 else 0):
        for ts in range(MC // CH):
            cs = slice(mc * MC + ts * CH, mc * MC + (ts + 1) * CH)
            hs = slice(ts * CH, (ts + 1) * CH)
            for k in range(8):
                sqk = c.sq[k % 2]
                ph.I("act", "activation", out=sqk[:], in_=c.xT[:, k, cs], func=AF.Square, reads=["xT"], writes=[f"nsq{k%2}"])
                ph.I("pe", "matmul", c.psN[:], lhsT=c.ones_bf[:], rhs=sqk[:], start=(k == 0), stop=(k == 7), reads=[f"nsq{k%2}", "ones"], writes=["npsN"])
            ph.I("act", "activation", out=c.rstd_s[:], in_=c.psN[:], func=AF.Sqrt, scale=1.0 / 1024.0, bias=EPS, reads=["npsN"], writes=["nrstd_s"])
            ph.I("dve", "reciprocal", out=c.rstd[:], in_=c.rstd_s[:], reads=["nrstd_s"], writes=["nrstd"])
            for k in range(8):
                t = t32[k % 2]
                tr = f"t32{k%2}"
                ph.I("dve", "scalar_tensor_tensor", out=t, in0=c.xT[:, k, cs], scalar=c.gcols[:, 1, k:k + 1], in1=c.rstd[:], op0=ALU.mult, op1=ALU.mult,
                     reads=["xT", "nrstd", "gcol"], writes=[tr])
                ph.I("act", "activation", out=c.hT[:, k, hs], in_=t, func=AF.Copy, reads=[tr], writes=["hT"])
                ph.I("dve", "tensor_tensor", out=hlo[:, k, :], in0=t, in1=c.hT[:, k, hs], op=ALU.subtract, reads=[tr, "hT"], writes=["xq"])
            for s4 in range(4):
                sub = slice(ts * CH + s4 * 128, ts * CH + (s4 + 1) * 128)
                subl = slice(s4 * 128, (s4 + 1) * 128)
                ps, pres = c.nextps()
                n = 0
                for (a_, ar_, sl_, b_, br_) in ((c.hT, "hT", sub, wrh, "wrh"), (hlo, "xq", subl, wrh, "wrh"), (c.hT, "hT", sub, wrl, "wrl")):
                    for k in range(8):
                        ph.I("pe", "matmul", ps[:, 0:8], lhsT=a_[:, k, sl_], rhs=b_[:, k, :], start=(n == 0), stop=(n == 23), reads=[ar_, br_], writes=[pres])
                        n += 1
                ph.I("dve", "tensor_copy", out=lg[:], in_=ps[:, 0:8], reads=[pres], writes=["lg"])
                ph.I("dve", "max", out=mx8[:], in_=lg[:], reads=["lg"], writes=["mx8"])
                ph.I("dve", "tensor_scalar", out=nv1[:], in0=mx8[:, 0:1], scalar1=-1.0, scalar2=None, op0=ALU.mult, reads=["mx8"], writes=["nv1"])
                ph.I("act", "activation", out=ex[:], in_=lg[:], func=AF.Exp, bias=nv1[:, 0:1], reads=["lg", "nv1"], writes=["ex"])
                ph.I("dve", "tensor_scalar", out=msk[:], in0=lg[:], scalar1=mx8[:, 1:2], scalar2=None, op0=ALU.is_ge, reads=["lg", "mx8"], writes=["msk"])
                ph.I("dve", "tensor_tensor", out=ex[:], in0=ex[:], in1=msk[:], op=ALU.mult, reads=["ex", "msk"], writes=["ex"])
                ph.I("dve", "reduce_sum", out=den[:], in_=ex[:], axis=AX.X, reads=["ex"], writes=["den"])
                ph.I("dve", "reciprocal", out=den[:], in_=den[:], reads=["den"], writes=["den"])
                ph.I("dve", "tensor_scalar", out=comb[:, ts * 4 + s4, :], in0=ex[:], scalar1=den[:, 0:1], scalar2=None, op0=ALU.mult,
                     reads=["ex", "den"], writes=["comb"])
        for e in range(NE):
            cb = cbc[e % 2]
            cbr = f"cbc{e%2}"
            for s8 in range(MC // 128):
                d_ = dg[s8 % 2]
                dr = f"dg{s8%2}"
                ph.I("dve", "tensor_scalar", out=d_[:], in0=ident[:], scalar1=comb[:, s8, e:e + 1], scalar2=None, op0=ALU.mult,
                     reads=["ident", "comb"], writes=[dr])
                ph.I("pe", "matmul", c.psL[:, (s8 % 4) * 128:(s8 % 4 + 1) * 128], lhsT=ones32[:], rhs=d_[:], start=True, stop=True,
                     reads=[dr, "ones32"], writes=["psL"])
                if s8 % 4 == 3:
                    ph.I("act", "activation", out=cb[:, (s8 // 4) * CH:(s8 // 4 + 1) * CH], in_=c.psL[:], func=AF.Copy, reads=["psL"], writes=[cbr])
            for cg in range(14):
                wg, wgr = c.wload(Wg_d[e, cg], 8, 256)
                wu, wur = c.wload(Wu_d[e, cg], 8, 256)
                for mi in range(2):
                    for ts in range(MC // CH):
                        psG, pgr = c.nextps()
                        psU, pur = c.nextps()
                        for k in range(8):
                            ph.I("pe", "matmul", psG[:], lhsT=wg[:, k, mi * 128:(mi + 1) * 128], rhs=c.hT[:, k, ts * CH:(ts + 1) * CH],
                                 start=(k == 0), stop=(k == 7), reads=[wgr, "hT"], writes=[pgr])
                        for k in range(8):
                            ph.I("pe", "matmul", psU[:], lhsT=wu[:, k, mi * 128:(mi + 1) * 128], rhs=c.hT[:, k, ts * CH:(ts + 1) * CH],
                                 start=(k == 0), stop=(k == 7), reads=[wur, "hT"], writes=[pur])
                        sg = c.sg[(mi * 2 + ts) % 2]
                        sgr = f"sg{(mi * 2 + ts) % 2}"
                        ph.I("act", "activation", out=sg[:], in_=psG[:], func=AF.Silu, reads=[pgr], writes=[sgr])
                        ph.I("dve", "tensor_tensor", out=c.actT[:, cg * 2 + mi, ts * CH:(ts + 1) * CH], in0=sg[:], in1=psU[:], op=ALU.mult,
                             reads=[sgr, pur], writes=["actT"])

            def ev_down(m, ts, ps, pres, cb=cb, cbr=cbr, mc=mc):
                tm = tmp[(m * 2 + ts) % 2]
                tmr = f"tmp{(m * 2 + ts) % 2}"
                sl = slice(mc * MC + ts * CH, mc * MC + (ts + 1) * CH)
                ph.I("dve", "tensor_tensor", out=tm, in0=ps[:], in1=cb[:, ts * CH:(ts + 1) * CH], op=ALU.mult, reads=[pres, cbr], writes=[tmr])
                ph.I("pool", "tensor_tensor", out=c.xT[:, m, sl], in0=c.xT[:, m, sl], in1=tm, op=ALU.add, reads=[tmr, "xT"], writes=["xT"])
            c.linear_fm(Wd_d[e], NFE, 128, c.actT, "actT", MC, ev_down)

    fo = c.arena[:, 0:4096].rearrange("p (k t) -> p k t", t=CH)
    ph.I("dve", "memset", acc[:, 0:1], 0.0, reads=["actT"], writes=["fo"])
    if stage < 4:
        ld(ph, "sp", "sout", out_d.rearrange("(k p) t -> p k t", p=128), c.xT[:], [], reads=["xT"])
    for tc in range(NCH if stage >= 4 else 0):
        cs = slice(tc * CH, (tc + 1) * CH)
        c.norm(2, cs, fo, "fo")
        ld(ph, "sp", "sout", out_d.rearrange("(k p) t -> p k t", p=128)[:, :, cs], fo, [], reads=["fo"])
    return c.finish()


def host_E(inp, resC, resD):
    gcols = _c(np.stack([pk(inp["norm_mem_q"][1]), pk(inp["norm_ffn"][1]), pk(inp["norm_final"]), pk(inp["norm_final"])], axis=1))
    cw = np.asarray(inp["conv_w"][0], np.float32)
    convw = _c(cw.reshape(3, 4, 128).transpose(2, 1, 0))
    Wg = np.stack([wtiles(inp["moe_w_gate"][0][e], 256) for e in range(NE)])
    Wu = np.stack([wtiles(inp["moe_w_up"][0][e], 256) for e in range(NE)])
    Wd = np.stack([wtiles(inp["moe_w_down"][0][e], 128) for e in range(NE)])
    shared = dict(gcols=gcols, convw=convw, Wout=wtiles(inp["odd_w_out"][0], 512),
                  Wr=_c(np.asarray(inp["router_w"][0], np.float32).reshape(8, 128, 8).transpose(1, 0, 2)),
                  ident=np.eye(128, dtype=np.float32), Wg=Wg, Wu=Wu, Wd=Wd)
    mem = [mem_inputs(inp, 1, b) for b in range(2)]
    maps = []
    for core in range(8):
        b, i = core // 4, core % 4
        yc = np.concatenate([resD[b * 4 + h]["y"][:, i * T:(i + 1) * T] for h in range(4)], axis=0)
        z = resC[core]["z"]
        halo = resC[core - 1]["z"][:, T - 2:] if i > 0 else np.zeros((512, 2), np.float32)
        m = dict(xT=resC[core]["xo"], yc=_c(yc), zext=_c(np.concatenate([halo, z], axis=1)), gb=resC[core]["gb"])
        m.update(shared)
        m.update(mem[b])
        maps.append(m)
    return maps


def build_fused():
    nc = bass.Bass("TRN2", target_bir_lowering=False)
    (dm, notallowed) = get_diff_masks()
    nmask = len(dm[0][0]) + len(dm[-1][0]) + 1

    def din(name, shape, dt=F32):
        return nc.dram_tensor(name, list(shape), dt, kind="ExternalInput").ap()

    def scr(name, shape, dt=F32):
        return nc.dram_tensor(name, list(shape), dt, kind="Internal").ap()

    x_d = din("x", [1024, S])
    A = dict(g=din("A_g", [128, 8]), w=din("A_w", [1024, 2564]), vgain=din("A_vgain", [128, 512]), ws=din("A_ws", [128, 4, 128]),
             bs=din("A_bs", [128, 4, 512]))
    B = dict(bf=din("B_bf", [4, 64, 1]), ident=din("B_ident", [64, 64]), lmat=din("B_lmat", [64, 64]), tri=din("B_tri", [128, 128]))
    C = {n: din("C_" + n, sh, dt) for n, (sh, dt) in C_IN.items() if n not in ("xT", "ymix")}
    D = dict(masks=din("D_masks", [128, nmask, 128]), tab=din("D_tab", [4, 128, 32]), lam=din("D_lam", [128, 4, 64]),
             gain=din("D_gain", [4, 128, 1]))
    E = {n: din("E_" + n, sh, dt) for n, (sh, dt) in E_IN.items() if n not in ("xT", "yc", "zext", "gb")}
    sel_d = din("E_sel", [128, 4])
    out_d = nc.dram_tensor("out", [1024, T], F32, kind="ExternalOutput").ap()

    q_s, k_s, ya_s, yb_s, yc_s = (scr(n, [512, S], BF16) for n in ("q_s", "k_s", "ya_s", "yb_s", "yc_s"))
    v_s, v1_s = scr("v_s", [S, 512], BF16), scr("v1_s", [S, 512], BF16)
    f_s = scr("f_s", [4, S])
    x1_s = scr("x1_s", [1024, S])
    qk_s = scr("qk_s", [1024, S], BF16)
    z_s = scr("z_s", [512, S + 2])
    gb_s = scr("gb_s", [512, S])

    c0 = Ctx("Z", nc=nc)
    zt = c0.sb("zt", [128, 4, 2])
    c0.ph.I("dve", "memset", zt[:], 0.0, writes=["zt"])
    ld(c0.ph, "sp", "sz", z_s.rearrange("(m p) t -> p m t", p=128)[:, :, 0:2], zt[:], [], reads=["zt"])
    c0.finish()

    for blk in range(4):
        cols = slice(blk * T, (blk + 1) * T)
        io = dict(A)
        io.update(xT=x_d[:, cols], ya=ya_s[:, cols], q=q_s[:, cols], k=k_s[:, cols], v=v_s[cols, :], f=f_s[:, cols])
        build_A(Ctx(f"A{blk}", nc=nc), io)
    for h in range(4):
        rows = slice(h * 128, (h + 1) * 128)
        io = dict(q=q_s[rows, :], k=k_s[rows, :], v=v_s.rearrange("(j p) c -> p j c", p=128)[:, :, rows],
                  f2=f_s[h].rearrange("(j p) -> j p", p=128), bf=B["bf"][h], ident=B["ident"], lmat=B["lmat"], tri=B["tri"], y=yb_s[rows, :])
        build_B(Ctx(f"B{h}", nc=nc), io)
    for blk in range(4):
        cols = slice(blk * T, (blk + 1) * T)
        io = dict(C)
        io.update(xT=x_d[:, cols], ymix=(ya_s[:, cols], yb_s[:, cols]), xo=x1_s[:, cols], qk=qk_s[:, cols], v=v1_s[cols, :],
                  z=z_s[:, 2 + blk * T: 2 + (blk + 1) * T], gb=gb_s[:, cols])
        build_C(Tok(f"C{blk}", nc=nc), io)
    for h in range(4):
        r64 = lambda base: qk_s[base + h * 64: base + (h + 1) * 64, :]
        io = dict(q=(r64(0), r64(256)), k=(r64(512), r64(768)), v=v1_s.rearrange("(j p) c -> p j c", p=128)[:, :, h * 128:(h + 1) * 128],
                  masks=D["masks"], tab=D["tab"][h], lam=D["lam"], gain=D["gain"][h], y=yc_s[h * 128:(h + 1) * 128, :])
        build_D(Ctx(f"D{h}", nc=nc), io)
    io = dict(E)
    io.update(xT=[x1_s[:, b * T:(b + 1) * T] for b in range(4)], yc=[yc_s[:, b * T:(b + 1) * T] for b in range(4)],
              zext=[z_s[:, b * T: b * T + T + 2] for b in range(4)], gb=[gb_s[:, b * T:(b + 1) * T] for b in range(4)],
              sel=sel_d, out=out_d)
    build_E(4, Tok("E", arena_words=14336, nc=nc), io)
    return nc


def host_fused(inp):
    x = np.asarray(inp["x"], np.float32)
    (dm, notallowed) = get_diff_masks()
    masks = _c(np.concatenate([dm[0][1], dm[-1][1], notallowed[None]], axis=0).transpose(1, 0, 2))
    bs4 = rep(np.tile(np.asarray(inp["gmlp_b_s"][0], np.float32), (1, 4)))
    sh = {
        "A_g": pk(inp["norm_mix"][0]), "A_w": _c(np.asarray(inp["even_w_in"][0], np.float32)),
        "A_vgain": rep(np.asarray(inp["gmlp_v_gain"][0], np.float32)),
        "A_ws": _c(np.transpose(np.asarray(inp["gmlp_w_s"][0], np.float32), (2, 0, 1))), "A_bs": bs4,
        "B_bf": _c(np.broadcast_to(np.asarray(inp["fox_b_f"][0], np.float32)[:, None, None], (4, 64, 1))),
        "B_ident": np.eye(64, dtype=np.float32), "B_lmat": np.triu(np.ones((64, 64), np.float32), 1),
        "B_tri": np.triu(np.ones((128, 128), np.float32), 0),
        "C_gcols": _c(np.stack([pk(inp["norm_mem_q"][0]), pk(inp["norm_ffn"][0]), pk(inp["norm_mix"][1]), pk(inp["norm_mix"][1])], axis=1)),
        "C_Wout": wtiles(inp["even_w_out"][0], 512), "C_Wg": wtiles(inp["ffn_w_gate"][0], 256), "C_Wu": wtiles(inp["ffn_w_up"][0], 256),
        "C_Wd": wtiles(inp["ffn_w_down"][0], 128), "C_Win": wtiles(inp["odd_w_in"][0], 512),
        "D_masks": masks,
        "D_tab": _c(np.stack([rep(np.asarray(inp["rel_bias"], np.float32)[:, h]) for h in range(4)])),
        "D_lam": _c(np.stack([rep(inp["diff_lambda_q1"][0]), rep(inp["diff_lambda_k1"][0]),
                              rep(inp["diff_lambda_q2"][0]), rep(inp["diff_lambda_k2"][0])], axis=1).astype(np.float32)),
        "D_gain": _c(np.asarray(inp["diff_subln_gain"][0], np.float32).reshape(4, 128, 1)),
        "E_convw": _c(np.asarray(inp["conv_w"][0], np.float32).reshape(3, 4, 128).transpose(2, 1, 0)),
        "E_gcols": _c(np.stack([pk(inp["norm_mem_q"][1]), pk(inp["norm_ffn"][1]), pk(inp["norm_final"]), pk(inp["norm_final"])], axis=1)),
        "E_Wout": wtiles(inp["odd_w_out"][0], 512),
        "E_Wr": _c(np.asarray(inp["router_w"][0], np.float32).reshape(8, 128, 8).transpose(1, 0, 2)),
        "E_ident": np.eye(128, dtype=np.float32),
        "E_Wg": np.stack([wtiles(inp["moe_w_gate"][0][e], 256) for e in range(NE)]),
        "E_Wu": np.stack([wtiles(inp["moe_w_up"][0][e], 256) for e in range(NE)]),
        "E_Wd": np.stack([wtiles(inp["moe_w_down"][0][e], 128) for e in range(NE)]),
    }
    memC = [mem_inputs(inp, 0, b) for b in range(2)]
    memE = [mem_inputs(inp, 1, b) for b in range(2)]
    xT = [_c(x[b].T) for b in range(2)]
    maps = []
    for core in range(8):
        b, i = core // 4, core % 4
        m = dict(sh)
        m["x"] = xT[b]
        for k_, v_ in memC[b].items():
            m["C_" + k_] = v_
        for k_, v_ in memE[b].items():
            m["E_" + k_] = v_
        selv = np.zeros((128, 4), np.float32)
        selv[:, i] = 1.0
        m["E_sel"] = selv
        maps.append(m)
    return maps


def kernel(**inp):
    inp = {k: np.asarray(v) for k, v in inp.items()}
    res = run(get_nc("F", build_fused), host_fused(inp))
    out = np.empty((2, 8192, 1024), np.float32)
    for core in range(8):
        b, i = core // 4, core % 4
        out[b, i * T:(i + 1) * T] = res[core]["out"].T
    return out
```

```python
import contextlib
from concourse.bass_utils import run_bass_kernel_spmd
import numpy as np
import concourse.bass as bass
import concourse.mybir as mybir

F32 = mybir.dt.float32
BF16 = mybir.dt.bfloat16
F32R = mybir.dt.float32r
I32 = mybir.dt.int32
U32 = mybir.dt.uint32
AF = mybir.ActivationFunctionType
ALU = mybir.AluOpType
AX = mybir.AxisListType

ENGS = ("pe", "act", "dve", "pool", "sp")


class Phase:
    def __init__(self, nc, name):
        self.nc = nc
        self.name = name
        self.ops = []
        self.last_writer = {}
        self.readers = {}
        self.dma_keys = {}

    def _add(self, eng, fn, reads, writes, dma_key=None, ndma=0):
        idx = len(self.ops)
        deps = set()
        for r in reads:
            w = self.last_writer.get(r)
            if w is not None:
                deps.add(w)
        for r in writes:
            w = self.last_writer.get(r)
            if w is not None:
                deps.add(w)
            for rd in self.readers.get(r, ()):
                deps.add(rd)
        deps.discard(idx)
        for r in writes:
            self.last_writer[r] = idx
            self.readers[r] = []
        for r in reads:
            if r not in writes:
                self.readers.setdefault(r, []).append(idx)
        self.ops.append(dict(eng=eng, fn=fn, deps=deps, dma_key=dma_key, ndma=ndma,
                             signal=False))
        return idx

    def op(self, eng, fn, reads=(), writes=()):
        return self._add(eng, fn, tuple(reads), tuple(writes))

    def I(self, eng, meth, *a, reads=(), writes=(), **kw):
        return self.op(eng, lambda e: getattr(e, meth)(*a, **kw), reads, writes)

    def dma(self, queue, fn, key, reads=(), writes=(), n=1):
        return self._add(queue, fn, tuple(reads), tuple(writes), dma_key=key, ndma=n)

    def emit(self):
        nc = self.nc
        ops = self.ops
        pos = {}
        cnt = {e: 0 for e in ENGS}
        for i, o in enumerate(ops):
            pos[i] = cnt[o["eng"]]
            cnt[o["eng"]] += 1
        for i, o in enumerate(ops):
            need = set()
            for d in o["deps"]:
                po = ops[d]
                if po["dma_key"] is not None:
                    need.add(d)
                elif po["eng"] == o["eng"]:
                    if o["eng"] != "pe" and pos[i] - pos[d] <= 2 and o["dma_key"] is None:
                        need.add(d)
                    elif o["dma_key"] is not None:
                        need.add(d)
                else:
                    need.add(d)
            o["need"] = need
            for d in need:
                if ops[d]["dma_key"] is None:
                    ops[d]["signal"] = True
        sigcnt = {e: 0 for e in ENGS}
        for o in ops:
            if o["dma_key"] is None and o["signal"]:
                sigcnt[o["eng"]] += 1
                o["sigval"] = sigcnt[o["eng"]]
        dmacnt = {}
        for o in ops:
            if o["dma_key"] is not None:
                dmacnt[o["dma_key"]] = dmacnt.get(o["dma_key"], 0) + o["ndma"]
                o["sigval"] = 16 * dmacnt[o["dma_key"]]
        keys = sorted(dmacnt.keys(), key=str)
        import contextlib
        esem = {e: nc.alloc_semaphore(name=f"{self.name}_{e}") for e in ENGS}
        dsem = {k: nc.alloc_semaphore(name=f"{self.name}_d{j}") for j, k in enumerate(keys)}
        with contextlib.ExitStack() as st:
            block = st.enter_context(nc.Block())
            per_eng = {e: [] for e in ENGS}
            for i, o in enumerate(ops):
                per_eng[o["eng"]].append(i)

            def run(engname, eng):
                seen = {}
                for i in per_eng[engname]:
                    o = ops[i]
                    waits = {}
                    for d in o["need"]:
                        po = ops[d]
                        if po["dma_key"] is not None:
                            k = ("d", po["dma_key"])
                        else:
                            k = ("e", po["eng"])
                        waits[k] = max(waits.get(k, 0), po["sigval"])
                    for k, v in waits.items():
                        if seen.get(k, 0) >= v:
                            continue
                        seen[k] = v
                        s = dsem[k[1]] if k[0] == "d" else esem[k[1]]
                        eng.wait_ge(s, v)
                    if o["dma_key"] is not None:
                        o["fn"](eng, dsem[o["dma_key"]])
                    else:
                        ins = o["fn"](eng)
                        if o["signal"]:
                            ins.then_inc(esem[engname], 1)
                fin = {}
                for i in per_eng[engname]:
                    o = ops[i]
                    if o["dma_key"] is not None:
                        fin[o["dma_key"]] = max(fin.get(o["dma_key"], 0), o["sigval"])
                for k, v in fin.items():
                    if seen.get(("d", k), 0) < v:
                        eng.wait_ge(dsem[k], v)

            @block.tensor
            def _(e):
                run("pe", e)

            @block.scalar
            def _(e):
                run("act", e)

            @block.vector
            def _(e):
                run("dve", e)

            @block.gpsimd
            def _(e):
                run("pool", e)

            @block.sync
            def _(e):
                run("sp", e)
        nc.clear_and_free_semaphores(list(esem.values()) + list(dsem.values()))
        nc.all_engine_barrier()


T = 2048
CH = 512
NCH = T // CH
EPS = 1e-6
NEG = -30000.0


class Ctx:
    def __init__(self, name, nc=None):
        self.nc = nc if nc is not None else bass.Bass("TRN2", target_bir_lowering=False)
        self.name = name
        self.st = contextlib.ExitStack()
        self.ph = Phase(self.nc, name)
        self.n = 0

    def din(self, name, shape, dt=F32):
        return self.nc.dram_tensor(name, list(shape), dt, kind="ExternalInput").ap()

    def dout(self, name, shape, dt=F32):
        return self.nc.dram_tensor(name, list(shape), dt, kind="ExternalOutput").ap()

    def sb(self, name, shape, dt=F32):
        return self.st.enter_context(self.nc.sbuf_tensor("s_" + self.name + "_" + name, list(shape), dt))

    def psum(self, name):
        return self.st.enter_context(self.nc.psum_tensor("p_" + self.name + "_" + name, [128, 512], F32))

    def finish(self):
        self.ph.emit()
        self.st.close()
        return self.nc


def ld(ph, queue, key, out_ap, in_ap, writes, reads=()):
    ph.dma(queue, lambda e, s: e.dma_start(out=out_ap, in_=in_ap).then_inc(s, 16), key,
           reads=reads, writes=writes)


def rmsnorm_fm(ph, tag, src, cs, gcol, xn, ones_bf, sq, psN, rstd_s, rstd, src_res, xn_res,
               nk=8, dim=1024.0):
    for k in range(nk):
        sqk = sq[k % 2]
        ph.op("act", (lambda k, sqk: lambda e: e.activation(out=sqk[:], in_=src[:, k, cs], func=AF.Square))(k, sqk),
              reads=[src_res], writes=[f"{tag}sq{k%2}"])
        ph.op("pe", (lambda k, sqk: lambda e: e.matmul(psN[:], lhsT=ones_bf[:], rhs=sqk[:], start=(k == 0), stop=(k == nk - 1)))(k, sqk),
              reads=[f"{tag}sq{k%2}", "ones"], writes=[f"{tag}psN"])
    ph.op("act", lambda e: e.activation(out=rstd_s[:], in_=psN[:], func=AF.Sqrt, scale=1.0 / dim, bias=EPS),
          reads=[f"{tag}psN"], writes=[f"{tag}rstd_s"])
    ph.op("dve", lambda e: e.reciprocal(out=rstd[:], in_=rstd_s[:]), reads=[f"{tag}rstd_s"], writes=[f"{tag}rstd"])
    for k in range(nk):
        ph.op("dve", (lambda k: lambda e: e.scalar_tensor_tensor(out=xn[:, k, :], in0=src[:, k, cs], scalar=gcol[:, k:k + 1],
                                                                  in1=rstd[:], op0=ALU.mult, op1=ALU.mult))(k),
              reads=[src_res, f"{tag}rstd", "gcol"], writes=[xn_res])


def build_A(c=None, io=None):
    if c is None:
        c = Ctx("A")
        io = dict(xT=c.din("xT", [1024, T]), g=c.din("g", [128, 8]), w=c.din("w", [1024, 2564]), vgain=c.din("vgain", [128, 512]),
                  ws=c.din("ws", [128, 4, 128]), bs=c.din("bs", [128, 4, 512]),
                  ya=c.dout("ya", [512, T], BF16), q=c.dout("q", [512, T], BF16), k=c.dout("k", [512, T], BF16),
                  v=c.dout("v", [T, 512], BF16), f=c.dout("f", [4, T], F32))
    nc, ph = c.nc, c.ph
    xT_d, g_d, w_d, vg_d, ws_d, bs_d = io["xT"], io["g"], io["w"], io["vgain"], io["ws"], io["bs"]
    ya_d, q_d, k_d, v_d, f_d = io["ya"], io["q"], io["k"], io["v"], io["f"]

    xT = c.sb("xT", [128, 8, T])
    w = c.sb("w", [128, 8, 2564], BF16)
    gcol = c.sb("gcol", [128, 8])
    ones_bf = c.sb("ones", [128, 128], BF16)
    sq = [c.sb(f"sq{i}", [128, CH], BF16) for i in range(2)]
    rstd_s = c.sb("rstd_s", [128, CH])
    rstd = c.sb("rstd", [128, CH])
    xn = c.sb("xn", [128, 8, CH], BF16)
    uT = c.sb("uT", [128, 4, CH])
    vgain = c.sb("vgain", [128, 512])
    wsm = c.sb("wsm", [128, 4, 128], BF16)
    bsb = c.sb("bsb", [128, 4, 512])
    vg = c.sb("vg", [128, 512])
    vscr = c.sb("vscr", [128, 128])
    ss = c.sb("ss", [128, 4])
    ssr = c.sb("ssr", [128, 4])
    vn = [c.sb(f"vn{i}", [128, 512], BF16) for i in range(2)]
    tmix = c.sb("tmix", [128, 512])
    yast = c.sb("yast", [128, 4, CH], BF16)
    qst = c.sb("qst", [128, 4, CH], BF16)
    kst = c.sb("kst", [128, 4, CH], BF16)
    vst = c.sb("vst", [128, 4, 512], BF16)
    fst = c.sb("fst", [4, CH])
    psN = c.psum("psN")
    psP = [c.psum(f"psP{i}") for i in range(3)]
    psM = [c.psum(f"psM{i}") for i in range(4)]

    ld(ph, "sp", "lx", xT[:], xT_d.rearrange("(k p) t -> p k t", p=128), ["xT"])
    ld(ph, "sp", "lg", gcol[:], g_d, ["gcol"])
    ld(ph, "sp", "lvg", vgain[:], vg_d, ["vgain"])
    ld(ph, "sp", "lbs", bsb[:], bs_d, ["bsb"])
    wv = w_d.rearrange("(k p) c -> p k c", p=128)
    for gi, (a, b) in enumerate([(0, 512), (512, 1024), (1024, 1536), (1536, 2048), (2048, 2564)]):
        ld(ph, "pool", f"lw{gi}", w[:, :, a:b], wv[:, :, a:b], [f"w{gi}"])
    ld(ph, "pool", "lws", wsm[:], ws_d, ["wsm"])
    ph.op("dve", lambda e: e.memset(ones_bf[:], 1.0), writes=["ones"])
    ph.op("dve", lambda e: e.memset(wsm[64:128, :, 0:64], 0.0), reads=["wsm"], writes=["wsm"])

    pi = [0]

    def nextps():
        pi[0] = (pi[0] + 1) % 3
        return psP[pi[0]], f"psP{pi[0]}"

    def proj_fm(col0, m, wres):
        ps, pres = nextps()
        for k in range(8):
            ph.op("pe", (lambda k, ps: lambda e: e.matmul(ps[:], lhsT=w[:, k, col0 + m * 128: col0 + (m + 1) * 128], rhs=xn[:, k, :],
                                                           start=(k == 0), stop=(k == 7)))(k, ps),
                  reads=["xn", wres], writes=[pres])
        return ps, pres

    for tc in range(NCH):
        cs = slice(tc * CH, (tc + 1) * CH)
        rmsnorm_fm(ph, "n", xT, cs, gcol, xn, ones_bf, sq, psN, rstd_s, rstd, "xT", "xn")
        for m in range(4):
            ps, pres = proj_fm(0, m, "w0")
            ph.op("act", (lambda m, ps: lambda e: e.activation(out=uT[:, m, :], in_=ps[:], func=AF.Gelu_apprx_tanh))(m, ps),
                  reads=[pres], writes=[f"uT{m}"])
        for m in range(4):
            ps, pres = proj_fm(1024, m, "w2")
            ph.op("dve", (lambda m, ps: lambda e: e.tensor_copy(out=qst[:, m, :], in_=ps[:]))(m, ps), reads=[pres], writes=["qst"])
        ld(ph, "sp", "sq", q_d.rearrange("(m p) t -> p m t", p=128)[:, :, cs], qst[:], [], reads=["qst"])
        for m in range(4):
            ps, pres = proj_fm(1536, m, "w3")
            ph.op("act", (lambda m, ps: lambda e: e.activation(out=kst[:, m, :], in_=ps[:], func=AF.Copy))(m, ps), reads=[pres], writes=["kst"])
        ld(ph, "sp", "sk", k_d.rearrange("(m p) t -> p m t", p=128)[:, :, cs], kst[:], [], reads=["kst"])
        ps, pres = nextps()
        for k in range(8):
            ph.op("pe", (lambda k, ps: lambda e: e.matmul(ps[0:4, :], lhsT=w[:, k, 2560:2564], rhs=xn[:, k, :], start=(k == 0), stop=(k == 7)))(k, ps),
                  reads=["xn", "w4"], writes=[pres])
        ph.op("dve", (lambda ps: lambda e: e.tensor_copy(out=fst[:], in_=ps[0:4, :]))(ps), reads=[pres], writes=["fst"])
        ld(ph, "sp", "sf", f_d[:, cs], fst[:], [], reads=["fst"])
        for s in range(4):
            ts_ = slice(s * 128, (s + 1) * 128)
            ps, pres = nextps()
            for k in range(8):
                ph.op("pe", (lambda k, ps, ts_: lambda e: e.matmul(ps[:], lhsT=xn[:, k, ts_], rhs=w[:, k, 512:1024], start=(k == 0), stop=(k == 7)))(k, ps, ts_),
                      reads=["xn", "w1"], writes=[pres])
            ph.op("act", (lambda ps: lambda e: e.activation(out=vg[:], in_=ps[:], func=AF.Gelu_apprx_tanh))(ps), reads=[pres], writes=["vg"])
            for g in range(4):
                ph.op("act", (lambda g: lambda e: e.activation(out=vscr[:], in_=vg[:, g * 128:(g + 1) * 128], func=AF.Square,
                                                               accum_out=ss[:, g:g + 1]))(g),
                      reads=["vg"], writes=["vscr", f"ss{g}"])
            ph.op("act", lambda e: e.activation(out=ssr[:], in_=ss[:], func=AF.Sqrt, scale=1.0 / 128.0, bias=EPS),
                  reads=[f"ss{g}" for g in range(4)], writes=["ssr"])
            ph.op("dve", lambda e: e.reciprocal(out=ss[:], in_=ssr[:]), reads=["ssr"], writes=[f"ss{g}" for g in range(4)])
            vnb = vn[s % 2]
            for g in range(4):
                gs = slice(g * 128, (g + 1) * 128)
                ph.op("dve", (lambda g, gs, vnb: lambda e: e.scalar_tensor_tensor(out=vnb[:, gs], in0=vg[:, gs], scalar=ss[:, g:g + 1], in1=vgain[:, gs],
                                                                                   op0=ALU.mult, op1=ALU.mult))(g, gs, vnb),
                      reads=["vg", f"ss{g}", "vgain"], writes=[f"vn{s%2}"])
            for g in range(4):
                gs = slice(g * 128, (g + 1) * 128)
                ph.op("pe", (lambda g, gs, vnb, ts_: lambda e: e.matmul(psM[g][:, ts_], lhsT=vnb[:, gs], rhs=wsm[:, g, :], start=True, stop=True))(g, gs, vnb, ts_),
                      reads=[f"vn{s%2}", "wsm"], writes=[f"psM{g}"])
            ps, pres = nextps()
            for k in range(8):
                ph.op("pe", (lambda k, ps, ts_: lambda e: e.matmul(ps[:], lhsT=xn[:, k, ts_], rhs=w[:, k, 2048:2560], start=(k == 0), stop=(k == 7)))(k, ps, ts_),
                      reads=["xn", "w4"], writes=[pres])
            ph.op("dve", (lambda ps, s: lambda e: e.tensor_copy(out=vst[:, s, :], in_=ps[:]))(ps, s), reads=[pres], writes=["vst"])
        ld(ph, "sp", "sv", v_d.rearrange("(n p) c -> p n c", p=128)[:, tc * 4:(tc + 1) * 4, :], vst[:], [], reads=["vst"])
        for g in range(4):
            ph.op("dve", (lambda g: lambda e: e.tensor_tensor(out=tmix[:], in0=psM[g][:], in1=bsb[:, g, :], op=ALU.add))(g),
                  reads=[f"psM{g}", "bsb"], writes=["tmix"])
            ph.op("dve", (lambda g: lambda e: e.tensor_tensor(out=yast[:, g, :], in0=tmix[:], in1=uT[:, g, :], op=ALU.mult))(g),
                  reads=["tmix", f"uT{g}"], writes=["yast"])
        ld(ph, "sp", "sy", ya_d.rearrange("(m p) t -> p m t", p=128)[:, :, cs], yast[:], [], reads=["yast"])
    return c.finish()


def _c(a):
    return np.ascontiguousarray(a)


def pk(vec):
    return _c(np.asarray(vec).reshape(-1, 128).T)


def rep(vec, n=128):
    return _c(np.broadcast_to(np.asarray(vec)[None], (n,) + tuple(np.asarray(vec).shape)))


def core_tokens(xfull, core):
    b, i = core // 4, core % 4
    return xfull[b, i * T:(i + 1) * T]


_NC_CACHE = {}


def get_nc(name, builder):
    if name not in _NC_CACHE:
        _NC_CACHE[name] = builder()
    return _NC_CACHE[name]


def run(nc, in_maps):
    res = run_bass_kernel_spmd(nc, in_maps, core_ids=list(range(8)))
    return res.results


def host_A(inp):
    x = np.asarray(inp["x"], np.float32)
    maps = []
    ws = _c(np.transpose(np.asarray(inp["gmlp_w_s"][0], np.float32), (2, 0, 1)))
    bs = np.asarray(inp["gmlp_b_s"][0], np.float32)
    bs4 = rep(np.tile(bs, (1, 4)))
    for core in range(8):
        maps.append(dict(
            xT=_c(core_tokens(x, core).T),
            g=pk(inp["norm_mix"][0]),
            w=_c(np.asarray(inp["even_w_in"][0], np.float32)),
            vgain=rep(np.asarray(inp["gmlp_v_gain"][0], np.float32)),
            ws=ws, bs=bs4))
    return maps


S = 8192
NT = S // 128
NG = S // CH


def build_B(c=None, io=None):
    if c is None:
        c = Ctx("B")
        io = dict(q=c.din("q", [128, S], BF16), k=c.din("k", [128, S], BF16), v=c.din("v", [128, NT, 128], BF16),
                  f2=c.din("f2", [64, 128]), bf=c.din("bf", [64, 1]), ident=c.din("ident", [64, 64]), lmat=c.din("lmat", [64, 64]),
                  tri=c.din("tri", [128, 128]), y=c.dout("y", [128, S], BF16))
    nc, ph = c.nc, c.ph
    q_d, k_d, v_d, f_d, bf_d, id_d, L_d, tri_d, y_d = (io[n] for n in ("q", "k", "v", "f2", "bf", "ident", "lmat", "tri", "y"))

    qT = c.sb("qT", [128, S], BF16)
    kT = c.sb("kT", [128, S], BF16)
    V = c.sb("V", [128, NT, 128], BF16)
    f2 = c.sb("f2", [64, 128])
    bfc = c.sb("bfc", [64, 1])
    ident = c.sb("ident", [64, 64])
    lmat = c.sb("lmat", [64, 64])
    tri = c.sb("tri", [128, 128], BF16)
    ones_bf = c.sb("ones", [128, 128], BF16)
    ones64 = c.sb("ones64", [64, 128])
    cs2 = c.sb("cs2", [64, 128])
    off = c.sb("off", [64, 1])
    dg = c.sb("dg", [64, 64])
    dcol = c.sb("dcol", [128, 64])
    dref = c.sb("dref", [128, 64])
    biasm = c.sb("biasm", [128, NT // 2, NT])
    PT = [c.sb(f"PT{i}", [128, CH], BF16) for i in range(4)]
    rl = c.sb("rl", [128, CH])
    ost = [c.sb(f"ost{i}", [128, CH], BF16) for i in range(2)]
    psS = [c.psum(f"psS{i}") for i in range(3)]
    psO = [c.psum(f"psO{i}") for i in range(2)]
    psL = [c.psum(f"psL{i}") for i in range(2)]
    psX = c.psum("psX")

    ld(ph, "sp", "l1", f2[:], f_d, ["f2"])
    ld(ph, "sp", "l2", bfc[:], bf_d, ["bfc"])
    ld(ph, "sp", "l3", ident[:], id_d, ["ident"])
    ld(ph, "sp", "l4", lmat[:], L_d, ["lmat"])
    ld(ph, "pool", "l5", tri[:], tri_d, ["tri"])
    ld(ph, "sp", "lq", qT[:], q_d, ["qT"])
    ld(ph, "act", "lk", kT[:], k_d, ["kT"])
    ld(ph, "sp", "lv", V[:], v_d, ["V"])
    ph.op("dve", lambda e: e.memset(ones_bf[:], 1.0), writes=["ones"])
    ph.op("dve", lambda e: e.memset(ones64[:], 1.0), writes=["ones64"])

    ph.op("dve", lambda e: e.tensor_scalar(out=bfc[:], in0=bfc[:], scalar1=-1.0, scalar2=None, op0=ALU.mult), reads=["bfc"], writes=["bfc"])
    ph.op("act", lambda e: e.activation(out=f2[:], in_=f2[:], func=AF.Exp, scale=-1.0, bias=bfc[:, 0:1]), reads=["f2", "bfc"], writes=["f2"])
    ph.op("act", lambda e: e.activation(out=f2[:], in_=f2[:], func=AF.Ln, scale=1.0, bias=1.0), reads=["f2"], writes=["f2"])
    ph.op("dve", lambda e: e.tensor_tensor_scan(out=cs2[:], data0=f2[:], data1=f2[:], initial=0.0, op0=ALU.add, op1=ALU.max),
          reads=["f2"], writes=["cs2"])
    ph.op("pe", lambda e: e.matmul(psX[0:64, 0:1], lhsT=lmat[:], rhs=cs2[:, 127:128], start=True, stop=True), reads=["cs2", "lmat"], writes=["psX"])
    ph.op("dve", lambda e: e.tensor_copy(out=off[:], in_=psX[0:64, 0:1]), reads=["psX"], writes=["off"])
    ph.op("dve", lambda e: e.tensor_scalar(out=cs2[:], in0=cs2[:], scalar1=off[:, 0:1], scalar2=None, op0=ALU.add), reads=["cs2", "off"], writes=["cs2"])
    ph.op("pe", lambda e: e.transpose(psX[:, 0:64], cs2[:], ident[:]), reads=["cs2", "ident", "off"], writes=["psX"])
    ph.op("dve", lambda e: e.tensor_copy(out=dcol[:], in_=psX[:, 0:64]), reads=["psX"], writes=["dcol"])
    ph.op("dve", lambda e: e.tensor_scalar(out=dg[:], in0=ident[:], scalar1=cs2[:, 0:1], scalar2=None, op0=ALU.mult), reads=["cs2", "ident"], writes=["dg"])
    ph.op("pe", lambda e: e.matmul(psX[:, 64:128], lhsT=ones64[:], rhs=dg[:], start=True, stop=True), reads=["dg", "ones64", "dcol"], writes=["psX"])
    ph.op("dve", lambda e: e.tensor_copy(out=dref[:], in_=psX[:, 64:128]), reads=["psX"], writes=["dref"])
    for k in range(NT // 2):
        ph.I("dve", "tensor_scalar", out=biasm[:, k, 0:2 * k + 2], in0=dcol[:, 0:2 * k + 2], scalar1=dref[:, 2 * k + 1:2 * k + 2], scalar2=None,
             op0=ALU.subtract, reads=["dcol", "dref"], writes=["biasm"])

    scale = 128.0 ** -0.5
    LA = 2
    its = [(I, j) for I in range(NG) for j in range(4 * I + 4)]
    NIT = len(its)
    NPT = len(PT)

    def front(n):
        I, j = its[n]
        qs = slice(I * CH, (I + 1) * CH)
        pS, pr = psS[n % 3], f"psS{n%3}"
        P, sr = PT[n % NPT], f"PT{n%NPT}"
        ph.I("pe", "matmul", pS[:], lhsT=kT[:, j * 128:(j + 1) * 128], rhs=qT[:, qs], start=True, stop=True, reads=["kT", "qT"], writes=[pr])
        jj = max(j - 4 * I, 0)
        for kk in range(2):
            lo, hi = max(jj * 128, kk * 256), (kk + 1) * 256
            if lo >= hi:
                continue
            ph.I("act", "activation", out=P[:, lo:hi], in_=pS[:, lo:hi], func=AF.Exp, scale=scale, bias=biasm[:, 2 * I + kk, j:j + 1],
                 reads=[pr, "biasm"], writes=[sr])
        if j >= 4 * I:
            bs_ = slice(jj * 128, (jj + 1) * 128)
            ph.I("pool", "tensor_tensor", out=P[:, bs_], in0=P[:, bs_], in1=tri[:], op=ALU.mult, reads=[sr, "tri"], writes=[sr])

    def back(n):
        I, j = its[n]
        qs = slice(I * CH, (I + 1) * CH)
        last = 4 * I + 3
        P, sr = PT[n % NPT], f"PT{n%NPT}"
        pO, pL = psO[I % 2], psL[I % 2]
        c0 = max(j - 4 * I, 0) * 128
        ph.I("pe", "matmul", pO[:, c0:], lhsT=V[:, j, :], rhs=P[:, c0:], start=(j == 0), stop=(j == last), reads=[sr, "V"], writes=[f"psO{I%2}"])
        ph.I("pe", "matmul", pL[:, c0:], lhsT=ones_bf[:], rhs=P[:, c0:], start=(j == 0), stop=(j == last), reads=[sr, "ones"], writes=[f"psL{I%2}"])
        if j == last:
            o = ost[I % 2]
            ph.I("dve", "reciprocal", out=rl[:], in_=pL[:], reads=[f"psL{I%2}"], writes=["rl"])
            ph.I("dve", "tensor_tensor", out=o[:], in0=pO[:], in1=rl[:], op=ALU.mult, reads=[f"psO{I%2}", "rl"], writes=[f"ost{I%2}"])
            ld(ph, "sp", f"so{I%2}", y_d[:, qs], o[:], [], reads=[f"ost{I%2}"])

    for n in range(NIT + LA):
        if n < NIT:
            front(n)
        if n - LA >= 0:
            back(n - LA)
    return c.finish()


def host_B(inp, resA):
    maps = []
    ident = np.eye(64, dtype=np.float32)
    lmat = np.triu(np.ones((64, 64), np.float32), 1)
    tri = np.triu(np.ones((128, 128), np.float32), 0)
    for core in range(8):
        b, h = core // 4, core % 4
        q = np.concatenate([resA[b * 4 + i]["q"][h * 128:(h + 1) * 128] for i in range(4)], axis=1)
        k = np.concatenate([resA[b * 4 + i]["k"][h * 128:(h + 1) * 128] for i in range(4)], axis=1)
        v = np.concatenate([resA[b * 4 + i]["v"][:, h * 128:(h + 1) * 128] for i in range(4)], axis=0)
        f = np.concatenate([resA[b * 4 + i]["f"][h] for i in range(4)], axis=0)
        maps.append(dict(q=_c(q), k=_c(k), v=_c(v.reshape(NT, 128, 128).transpose(1, 0, 2)),
                         f2=_c(f.reshape(64, 128)), bf=np.full((64, 1), inp["fox_b_f"][0][h], np.float32),
                         ident=ident, lmat=lmat, tri=tri))
    return maps


MC = 1024
M_SCALE = 256.0 ** -0.5


def wtiles(W, ncols):
    W = np.asarray(W, np.float32)
    K_, N_ = W.shape
    return _c(W.reshape(K_ // 128, 128, N_ // ncols, ncols).transpose(2, 1, 0, 3))


class Tok(Ctx):
    def __init__(self, name, nwb=4, wb_elems=4096, arena_words=11264, nc=None):
        super().__init__(name, nc=nc)
        c = self
        self.xT = c.sb("xT", [128, 8, T])
        self.ones_bf = c.sb("ones", [128, 128], BF16)
        self.sq = [c.sb(f"sq{i}", [128, CH], BF16) for i in range(2)]
        self.rstd_s = c.sb("rstd_s", [128, CH])
        self.rstd = c.sb("rstd", [128, CH])
        self.gcols = c.sb("gcols", [128, 4, 8])
        self.wb = [c.sb(f"wb{i}", [128, wb_elems], BF16) for i in range(nwb)]
        self.wi = 0
        self.arena = c.sb("arena", [128, arena_words])
        self.actT = self.arena[:].bitcast(BF16).rearrange("p (k t) -> p k t", t=MC)
        self.ymb = [c.sb(f"ymb{i}", [128, 8, CH], BF16) for i in range(2)]
        self.hT = c.sb("hT", [128, 8, MC], BF16)
        self.sg = [c.sb(f"sg{i}", [128, CH], BF16) for i in range(2)]
        self.pp = [c.psum(f"pp{i}") for i in range(6)]
        self.psN = c.psum("psN")
        self.psL = c.psum("psL")
        self.pi = 0
        self.ph.I("dve", "memset", self.ones_bf[:], 1.0, writes=["ones"])

    def nextps(self):
        self.pi = (self.pi + 1) % len(self.pp)
        return self.pp[self.pi], f"pp{self.pi}"

    def wload(self, dram_tile_ap, nk, ncols):
        i = self.wi
        self.wi = (self.wi + 1) % len(self.wb)
        view = self.wb[i][:, 0:nk * ncols].rearrange("p (k c) -> p k c", c=ncols)
        ld(self.ph, "pool", f"wb{i}", view, dram_tile_ap, [f"wb{i}"])
        return view, f"wb{i}"

    def norm(self, gi, cs, xn, xn_res, n=CH):
        rmsnorm_fm(self.ph, "n", self.xT, cs, self.gcols[:, gi, :], xn, self.ones_bf, self.sq, self.psN,
                   self.rstd_s, self.rstd, "xT", xn_res)

    def linear_fm(self, Wt_d, nk, ncols, src, src_res, ntok, evac):
        ph = self.ph
        ncg = Wt_d.shape[0]
        for cg in range(ncg):
            wv, wres = self.wload(Wt_d[cg], nk, ncols)
            for mi in range(ncols // 128):
                for ts in range(ntok // CH):
                    ps, pres = self.nextps()
                    for k in range(nk):
                        ph.I("pe", "matmul", ps[:], lhsT=wv[:, k, mi * 128:(mi + 1) * 128], rhs=src[:, k, ts * CH:(ts + 1) * CH],
                             start=(k == 0), stop=(k == nk - 1), reads=[wres, src_res], writes=[pres])
                    evac(cg * (ncols // 128) + mi, ts, ps, pres)

    def resid_add(self, t0):
        ph, xT = self.ph, self.xT

        def ev(m, ts, ps, pres):
            sl = slice(t0 + ts * CH, t0 + (ts + 1) * CH)
            ph.I("dve", "tensor_tensor", out=xT[:, m, sl], in0=xT[:, m, sl], in1=ps[:], op=ALU.add, reads=[pres, "xT"], writes=["xT"])
        return ev

    def mem_setup(self, memT_d, gkv_d, Wkv_k_d, Wkv_v_d):
        c, ph = self, self.ph
        self.memT = self.arena[:, 0:2048].rearrange("p (k t) -> p k t", t=256)
        self.memn = self.arena[:, 2048:3072].bitcast(BF16).rearrange("p (k t) -> p k t", t=256)
        self.gkv = c.sb("gkv", [128, 8])
        self.KmT = self.arena[:, 3072:4096].bitcast(BF16).rearrange("p (k t) -> p k t", t=256)
        self.Vm = self.arena[:, 4096:5120].bitcast(BF16).rearrange("p (k t) -> p k t", t=1024)
        self.qmT = self.ymb[0]
        self.omT = self.ymb[1]
        self.PmT = [self.arena[:, 9728 + 256 * i: 9728 + 256 * (i + 1)].bitcast(BF16) for i in range(2)]
        self.rl = self.arena[:, 10240:10752]
        self.xq = c.sb("xq", [128, 8, CH], BF16)
        ld(ph, "sp", "lmem", self.memT, memT_d.rearrange("(k p) t -> p k t", p=128), ["memT"])
        ld(ph, "sp", "lgkv", self.gkv[:], gkv_d, ["gkv"])
        for k in range(8):
            sqk = self.sq[k % 2]
            ph.I("act", "activation", out=sqk[:, 0:256], in_=self.memT[:, k, :], func=AF.Square, reads=["memT"], writes=[f"nsq{k%2}"])
            ph.I("pe", "matmul", self.psN[:, 0:256], lhsT=self.ones_bf[:], rhs=sqk[:, 0:256], start=(k == 0), stop=(k == 7),
                 reads=[f"nsq{k%2}", "ones"], writes=["npsN"])
        ph.I("act", "activation", out=self.rstd_s[:, 0:256], in_=self.psN[:, 0:256], func=AF.Sqrt, scale=1.0 / 1024.0, bias=EPS,
             reads=["npsN"], writes=["nrstd_s"])
        ph.I("dve", "reciprocal", out=self.rstd[:, 0:256], in_=self.rstd_s[:, 0:256], reads=["nrstd_s"], writes=["nrstd"])
        for k in range(8):
            ph.I("dve", "scalar_tensor_tensor", out=self.memn[:, k, :], in0=self.memT[:, k, :], scalar=self.gkv[:, k:k + 1],
                 in1=self.rstd[:, 0:256], op0=ALU.mult, op1=ALU.mult, reads=["memT", "nrstd", "gkv"], writes=["memn"])
        for cg in range(2):
            wv, wres = self.wload(Wkv_k_d[cg], 8, 512)
            for mi in range(4):
                ps, pres = self.nextps()
                for k in range(8):
                    ph.I("pe", "matmul", ps[:, 0:256], lhsT=wv[:, k, mi * 128:(mi + 1) * 128], rhs=self.memn[:, k, :],
                         start=(k == 0), stop=(k == 7), reads=[wres, "memn"], writes=[pres])
                ph.I("act", "activation", out=self.KmT[:, cg * 4 + mi, :], in_=ps[:, 0:256], func=AF.Copy, reads=[pres], writes=["KmT"])
        for cg in range(2):
            wv, wres = self.wload(Wkv_v_d[cg], 8, 512)
            for mt in range(2):
                ps, pres = self.nextps()
                for k in range(8):
                    ph.I("pe", "matmul", ps[:], lhsT=self.memn[:, k, mt * 128:(mt + 1) * 128], rhs=wv[:, k, :],
                         start=(k == 0), stop=(k == 7), reads=[wres, "memn"], writes=[pres])
                ph.I("dve", "tensor_copy", out=self.Vm[:, mt, cg * 512:(cg + 1) * 512], in_=ps[:], reads=[pres], writes=["Vm"])

    def mem_attn(self, gi, Wq_d, Wo_d):
        ph = self.ph
        xqb = [self.hT[:, :, 0:CH], self.hT[:, :, CH:2 * CH]]
        self.norm(gi, slice(0, CH), xqb[0], "xqb0")
        for tc in range(NCH):
            cs = slice(tc * CH, (tc + 1) * CH)
            xq, xqr = xqb[tc % 2], f"xqb{tc%2}"

            def ev_q(m, ts, ps, pres):
                eng = "act" if m % 2 else "dve"
                if eng == "act":
                    ph.I("act", "activation", out=self.qmT[:, m, :], in_=ps[:], func=AF.Copy, reads=[pres], writes=["ymb0"])
                else:
                    ph.I("dve", "tensor_copy", out=self.qmT[:, m, :], in_=ps[:], reads=[pres], writes=["ymb0"])
            self.linear_fm(Wq_d, 8, 512, xq, xqr, CH, ev_q)
            if tc + 1 < NCH:
                self.norm(gi, slice((tc + 1) * CH, (tc + 2) * CH), xqb[(tc + 1) % 2], f"xqb{(tc+1)%2}")
            for hh in range(4):
                for mt in range(2):
                    ps, pres = self.nextps()
                    for c2 in range(2):
                        ph.I("pe", "matmul", ps[:], lhsT=self.KmT[:, hh * 2 + c2, mt * 128:(mt + 1) * 128], rhs=self.qmT[:, hh * 2 + c2, :],
                             start=(c2 == 0), stop=(c2 == 1), reads=["KmT", "ymb0"], writes=[pres])
                    ph.I("act", "activation", out=self.PmT[mt], in_=ps[:], func=AF.Exp, scale=M_SCALE, reads=[pres], writes=[f"PmT{mt}"])
                for mt in range(2):
                    ph.I("pe", "matmul", self.psL[:], lhsT=self.ones_bf[:], rhs=self.PmT[mt], start=(mt == 0), stop=(mt == 1),
                         reads=[f"PmT{mt}", "ones"], writes=["psL"])
                ph.I("dve", "reciprocal", out=self.rl, in_=self.psL[:], reads=["psL"], writes=["rl"])
                for c2 in range(2):
                    ps, pres = self.nextps()
                    for mt in range(2):
                        ph.I("pe", "matmul", ps[:], lhsT=self.Vm[:, mt, hh * 256 + c2 * 128: hh * 256 + (c2 + 1) * 128], rhs=self.PmT[mt],
                             start=(mt == 0), stop=(mt == 1), reads=["Vm", f"PmT{mt}"], writes=[pres])
                    ph.I("dve", "tensor_tensor", out=self.omT[:, hh * 2 + c2, :], in0=ps[:], in1=self.rl, op=ALU.mult,
                         reads=[pres, "rl"], writes=["ymb1"])
            self.linear_fm(Wo_d, 8, 512, self.omT, "ymb1", CH, self.resid_add(tc * CH))


def out_proj(c, ymix_d, Wout_d):
    ph = c.ph
    for tc in range(NCH):
        yb = c.ymb[tc % 2]
        if isinstance(ymix_d, tuple):
            for hi, part in enumerate(ymix_d):
                ld(ph, "sp", f"lym{tc%2}", yb[:, 4 * hi:4 * hi + 4, :], part.rearrange("(k p) t -> p k t", p=128)[:, :, tc * CH:(tc + 1) * CH],
                   [f"ymb{tc%2}"])
        else:
            ymv = ymix_d.rearrange("(k p) t -> p k t", p=128)
            ld(ph, "sp", f"lym{tc%2}", yb[:], ymv[:, :, tc * CH:(tc + 1) * CH], [f"ymb{tc%2}"])
        c.linear_fm(Wout_d, 8, 512, yb, f"ymb{tc%2}", CH, c.resid_add(tc * CH))


def ffn_swiglu(c, gi, Wg_d, Wu_d, Wd_d, nf, post=None):
    ph = c.ph
    ncg = nf // 2
    for mc in range(T // MC):
        for ts in range(MC // CH):
            cs = slice(mc * MC + ts * CH, mc * MC + (ts + 1) * CH)
            c.norm(gi, cs, c.hT[:, :, ts * CH:(ts + 1) * CH], "hT")
        for cg in range(ncg):
            wg, wgr = c.wload(Wg_d[cg], 8, 256)
            wu, wur = c.wload(Wu_d[cg], 8, 256)
            for mi in range(2):
                for ts in range(MC // CH):
                    psG, pgr = c.nextps()
                    psU, pur = c.nextps()
                    for k in range(8):
                        ph.I("pe", "matmul", psG[:], lhsT=wg[:, k, mi * 128:(mi + 1) * 128], rhs=c.hT[:, k, ts * CH:(ts + 1) * CH],
                             start=(k == 0), stop=(k == 7), reads=[wgr, "hT"], writes=[pgr])
                    for k in range(8):
                        ph.I("pe", "matmul", psU[:], lhsT=wu[:, k, mi * 128:(mi + 1) * 128], rhs=c.hT[:, k, ts * CH:(ts + 1) * CH],
                             start=(k == 0), stop=(k == 7), reads=[wur, "hT"], writes=[pur])
                    sg = c.sg[(mi * 2 + ts) % 2]
                    sgr = f"sg{(mi * 2 + ts) % 2}"
                    ph.I("act", "activation", out=sg[:], in_=psG[:], func=AF.Silu, reads=[pgr], writes=[sgr])
                    ph.I("dve", "tensor_tensor", out=c.actT[:, cg * 2 + mi, ts * CH:(ts + 1) * CH], in0=sg[:], in1=psU[:], op=ALU.mult,
                         reads=[sgr, pur], writes=["actT"])
        c.linear_fm(Wd_d, nf, 128, c.actT, "actT", MC, c.resid_add(mc * MC))


C_IN = dict(xT=([1024, T], F32), ymix=([1024, T], BF16), gcols=([128, 4, 8], F32), Wout=([2, 128, 8, 512], F32), memT=([1024, 256], F32),
            gkv=([128, 8], F32), Wkvk=([2, 128, 8, 512], F32), Wkvv=([2, 128, 8, 512], F32), Wq=([2, 128, 8, 512], F32),
            Wo=([2, 128, 8, 512], F32), Wg=([11, 128, 8, 256], F32), Wu=([11, 128, 8, 256], F32), Wd=([8, 128, 22, 128], F32),
            Win=([6, 128, 8, 512], F32))


def build_C(c=None, io=None):
    if c is None:
        c = Tok("C")
        io = {n: c.din(n, sh, dt) for n, (sh, dt) in C_IN.items()}
        io.update(xo=c.dout("xo", [1024, T]), qk=c.dout("qk", [1024, T], BF16), v=c.dout("v", [T, 512], BF16),
                  z=c.dout("z", [512, T]), gb=c.dout("gb", [512, T]))
    nc, ph = c.nc, c.ph
    xT_d, ymix_d, gcols_d, Wout_d, memT_d, gkv_d, Wkvk_d, Wkvv_d, Wq_d, Wo_d, Wg_d, Wu_d, Wd_d, Win_d = (io[n] for n in C_IN)
    xo_d, qk_d, v_d, z_d, gb_d = io["xo"], io["qk"], io["v"], io["z"], io["gb"]

    ld(ph, "act", "lx", c.xT[:], xT_d.rearrange("(k p) t -> p k t", p=128), ["xT"])
    ld(ph, "sp", "lg", c.gcols[:], gcols_d, ["gcol"])

    out_proj(c, ymix_d, Wout_d)
    c.mem_setup(memT_d, gkv_d, Wkvk_d, Wkvv_d)
    c.mem_attn(0, Wq_d, Wo_d)
    ffn_swiglu(c, 1, Wg_d, Wu_d, Wd_d, 22)
    ld(ph, "act", "sx", xo_d.rearrange("(k p) t -> p k t", p=128), c.xT[:], [], reads=["xT"])

    xn1 = c.xq
    ar = c.arena
    gbst = ar[:, 0:2048].rearrange("p (m t) -> p m t", t=CH)
    zst = ar[:, 2048:4096].rearrange("p (m t) -> p m t", t=CH)
    vst = ar[:, 4096:5120].bitcast(BF16).rearrange("p (m t) -> p m t", t=512)
    qkst = ar[:, 5120:7168].bitcast(BF16).rearrange("p (m t) -> p m t", t=CH)
    hst = ar[:, 7168:9216].rearrange("p (m t) -> p m t", t=CH)
    dummy = c.sb("dummy", [128, 1])
    ph.I("dve", "memset", dummy[:], 0.0, reads=["actT"], writes=["qkst", "gbst", "zst", "vst"] + [f"hst{m}" for m in range(4)])
    xnb = [c.hT[:, :, 0:CH], c.hT[:, :, CH:2 * CH]]
    c.norm(2, slice(0, CH), xnb[0], "xnb0")
    for tc in range(NCH):
        cs = slice(tc * CH, (tc + 1) * CH)
        xn1, xnr = xnb[tc % 2], f"xnb{tc%2}"

        def ev_qk(m, ts, ps, pres):
            if m % 2:
                ph.I("act", "activation", out=qkst[:, m, :], in_=ps[:], func=AF.Copy, reads=[pres], writes=["qkst"])
            else:
                ph.I("dve", "tensor_copy", out=qkst[:, m, :], in_=ps[:], reads=[pres], writes=["qkst"])
        c.linear_fm(Win_d[0:2], 8, 512, xn1, xnr, CH, ev_qk)
        if tc + 1 < NCH:
            c.norm(2, slice((tc + 1) * CH, (tc + 2) * CH), xnb[(tc + 1) % 2], f"xnb{(tc+1)%2}")
        ld(ph, "sp", "sqk", qk_d.rearrange("(m p) t -> p m t", p=128)[:, :, cs], qkst, [], reads=["qkst"])
        wv, wres = c.wload(Win_d[2], 8, 512)
        for s in range(4):
            ps, pres = c.nextps()
            for k in range(8):
                ph.I("pe", "matmul", ps[:], lhsT=xn1[:, k, s * 128:(s + 1) * 128], rhs=wv[:, k, :], start=(k == 0), stop=(k == 7),
                     reads=[wres, xnr], writes=[pres])
            ph.I("act", "activation", out=vst[:, s, :], in_=ps[:], func=AF.Copy, reads=[pres], writes=["vst"])
        ld(ph, "sp", "sv", v_d.rearrange("(n p) c -> p n c", p=128)[:, tc * 4:(tc + 1) * 4, :], vst, [], reads=["vst"])

        def ev_conv(m, ts, ps, pres):
            if m < 4:
                ph.I("act", "activation", out=hst[:, m, :], in_=ps[:], func=AF.Copy, reads=[pres], writes=[f"hst{m}"])
            elif m < 8:
                ph.I("act", "activation", out=gbst[:, m - 4, :], in_=ps[:], func=AF.Copy, reads=[pres], writes=["gbst"])
            else:
                ph.I("dve", "tensor_tensor", out=zst[:, m - 8, :], in0=hst[:, m - 8, :], in1=ps[:], op=ALU.mult,
                     reads=[pres, f"hst{m-8}"], writes=["zst"])
        c.linear_fm(Win_d[3:6], 8, 512, xn1, xnr, CH, ev_conv)
        ld(ph, "sp", "sgb", gb_d.rearrange("(m p) t -> p m t", p=128)[:, :, cs], gbst, [], reads=["gbst"])
        ld(ph, "sp", "sz", z_d.rearrange("(m p) t -> p m t", p=128)[:, :, cs], zst, [], reads=["zst"])
    return c.finish()


def mem_inputs(inp, l, b):
    wkv = np.asarray(inp["mem_w_kv"][l], np.float32)
    return dict(memT=_c(np.asarray(inp["mem"][b], np.float32).T), gkv=pk(inp["norm_mem_kv"][l]),
                Wkvk=wtiles(wkv[:, :1024], 512), Wkvv=wtiles(wkv[:, 1024:], 512),
                Wq=wtiles(inp["mem_w_q"][l], 512), Wo=wtiles(inp["mem_w_o"][l], 512))


def host_C(inp, resA, resB, xT_cores):
    maps = []
    gcols = _c(np.stack([pk(inp["norm_mem_q"][0]), pk(inp["norm_ffn"][0]), pk(inp["norm_mix"][1]), pk(inp["norm_mix"][1])], axis=1))
    shared = dict(gcols=gcols, Wout=wtiles(inp["even_w_out"][0], 512), Wg=wtiles(inp["ffn_w_gate"][0], 256),
                  Wu=wtiles(inp["ffn_w_up"][0], 256), Wd=wtiles(inp["ffn_w_down"][0], 128), Win=wtiles(inp["odd_w_in"][0], 512))
    mem = [mem_inputs(inp, 0, b) for b in range(2)]
    for core in range(8):
        b, i = core // 4, core % 4
        yb = np.concatenate([resB[b * 4 + h]["y"][:, i * T:(i + 1) * T] for h in range(4)], axis=0)
        ymix = np.concatenate([resA[core]["ya"], yb], axis=0)
        m = dict(xT=xT_cores[core], ymix=_c(ymix))
        m.update(shared)
        m.update(mem[b])
        maps.append(m)
    return maps


LAM_INIT = 0.8 - 0.6 * float(np.exp(-0.3 * 1))


def rel_bucket_static(rel):
    import jax.numpy as jnp
    import jax
    with jax.default_device(jax.devices("cpu")[0]):
        rel = jnp.asarray(rel, jnp.int32)
        n_half, max_exact = 16, 8
        ret = jnp.where(rel > 0, n_half, 0)
        n = jnp.abs(rel)
        nf = jnp.maximum(n, 1).astype(jnp.float32)
        large = max_exact + (jnp.log(nf / max_exact) / float(np.log(128 / max_exact)) * (n_half - max_exact)).astype(jnp.int32)
        large = jnp.minimum(large, n_half - 1)
        return np.asarray(ret + jnp.where(n < max_exact, n, large))


def diff_masks():
    p = np.arange(128)[:, None]
    f = np.arange(128)[None, :]
    out = {}
    for delta in (0, -1):
        rel = 128 * delta + p - f
        bk = rel_bucket_static(rel)
        ids = sorted(set(bk.reshape(-1).tolist()))
        masks = np.stack([(bk == b).astype(np.float32) for b in ids])
        out[delta] = (ids, masks)
    notallowed = ((p // 64) > (f // 64)).astype(np.float32)
    return out, notallowed


_DM = {}


def get_diff_masks():
    if "m" not in _DM:
        _DM["m"] = diff_masks()
    return _DM["m"]


def build_D(c=None, io=None):
    (dm, notallowed) = get_diff_masks()
    ids0, ids1 = dm[0][0], dm[-1][0]
    n0, n1 = len(ids0), len(ids1)
    if c is None:
        c = Ctx("D")
        io = dict(q=c.din("q", [128, S], BF16), k=c.din("k", [128, S], BF16), v=c.din("v", [128, NT, 128], BF16),
                  masks=c.din("masks", [128, n0 + n1 + 1, 128]), tab=c.din("tab", [128, 32]), lam=c.din("lam", [128, 4, 64]),
                  gain=c.din("gain", [128, 1]), y=c.dout("y", [128, S], BF16))
    nc, ph = c.nc, c.ph
    q_d, k_d, v_d, mk_d, tab_d, lam_d, gain_d, y_d = (io[n] for n in ("q", "k", "v", "masks", "tab", "lam", "gain", "y"))

    QT = c.sb("QT", [128, S], BF16)
    KT = [c.sb(f"KT{i}", [128, S], BF16) for i in range(2)]
    V = c.sb("V", [128, NT, 128], BF16)
    mk = c.sb("mk", [128, n0 + n1 + 1, 128])
    tab = c.sb("tab", [128, 32])
    lamp = c.sb("lamp", [128, 4, 64])
    gsc = c.sb("gsc", [128, 1])
    B0 = c.sb("B0", [128, 128])
    B1 = c.sb("B1", [128, 128])
    lprod = c.sb("lprod", [128, 2, 64])
    lsum = c.sb("lsum", [128, 2])
    neglam = c.sb("neglam", [128, 1])
    ones_bf = c.sb("ones", [128, 128], BF16)
    tb = [c.sb(f"tb{i}", [128, 128]) for i in range(2)]
    PT = [[c.sb(f"PT{s}{i}", [128, CH], BF16) for i in range(6)] for s in range(2)]
    l1c = c.sb("l1c", [128, CH])
    l2c = c.sb("l2c", [128, CH])
    o1 = c.sb("o1", [128, CH])
    o2 = c.sb("o2", [128, CH])
    sqo = c.sb("sqo", [128, CH], BF16)
    rs = c.sb("rs", [128, CH])
    ost = [c.sb(f"ost{i}", [128, CH], BF16) for i in range(2)]
    psS3 = [c.psum(f"psS{i}") for i in range(3)]
    psN = c.psum("psN")
    psO = [c.psum(f"psO{s}") for s in range(2)]
    psL = [c.psum(f"psL{s}") for s in range(2)]

    ld(ph, "sp", "l1", mk[:], mk_d, ["mk"])
    ld(ph, "sp", "l2", tab[:], tab_d, ["tab"])
    ld(ph, "sp", "l3", lamp[:], lam_d, ["lamp"])
    ld(ph, "sp", "l4", gsc[:], gain_d, ["gsc"])
    if isinstance(q_d, tuple):
        ld(ph, "sp", "lq", QT[0:64, :], q_d[0], ["QT"])
        ld(ph, "sp", "lq", QT[64:128, :], q_d[1], ["QT"])
        ld(ph, "act", "lk0", KT[0][0:64, :], k_d[0], ["KT0"])
        ld(ph, "act", "lk1", KT[1][64:128, :], k_d[1], ["KT1"])
    else:
        ld(ph, "sp", "lq", QT[:], q_d, ["QT"])
        ld(ph, "act", "lk0", KT[0][0:64, :], k_d[0:64, :], ["KT0"])
        ld(ph, "act", "lk1", KT[1][64:128, :], k_d[64:128, :], ["KT1"])
    ph.I("pool", "memset", KT[0][64:128, :], 0.0, writes=["KT0z"])
    ph.I("pool", "memset", KT[1][0:64, :], 0.0, writes=["KT1z"])
    ld(ph, "sp", "lv", V[:], v_d, ["V"])
    ph.I("dve", "memset", ones_bf[:], 1.0, writes=["ones"])
    ph.I("dve", "tensor_scalar", out=B0[:], in0=mk[:, n0 + n1, :], scalar1=NEG, scalar2=None, op0=ALU.mult, reads=["mk"], writes=["B0"])
    for n, b in enumerate(ids0):
        ph.I("dve", "scalar_tensor_tensor", out=B0[:], in0=mk[:, n, :], scalar=tab[:, b:b + 1], in1=B0[:], op0=ALU.mult, op1=ALU.add,
             reads=["mk", "tab", "B0"], writes=["B0"])
    ph.I("dve", "tensor_scalar", out=B1[:], in0=mk[:, n0, :], scalar1=tab[:, ids1[0]:ids1[0] + 1], scalar2=None, op0=ALU.mult,
         reads=["mk", "tab"], writes=["B1"])
    for n, b in enumerate(ids1[1:]):
        ph.I("dve", "scalar_tensor_tensor", out=B1[:], in0=mk[:, n0 + 1 + n, :], scalar=tab[:, b:b + 1], in1=B1[:], op0=ALU.mult, op1=ALU.add,
             reads=["mk", "tab", "B1"], writes=["B1"])
    ph.I("dve", "tensor_tensor", out=lprod[:, 0, :], in0=lamp[:, 0, :], in1=lamp[:, 1, :], op=ALU.mult, reads=["lamp"], writes=["lprod"])
    ph.I("dve", "tensor_tensor", out=lprod[:, 1, :], in0=lamp[:, 2, :], in1=lamp[:, 3, :], op=ALU.mult, reads=["lamp"], writes=["lprod"])
    ph.I("dve", "reduce_sum", out=lsum[:], in_=lprod[:], axis=AX.X, reads=["lprod"], writes=["lsum"])
    ph.I("act", "activation", out=lsum[:], in_=lsum[:], func=AF.Exp, reads=["lsum"], writes=["lsum"])
    ph.I("dve", "tensor_tensor", out=neglam[:], in0=lsum[:, 1:2], in1=lsum[:, 0:1], op=ALU.subtract, reads=["lsum"], writes=["neglam"])
    ph.I("dve", "tensor_scalar", out=neglam[:], in0=neglam[:], scalar1=-LAM_INIT, scalar2=None, op0=ALU.add, reads=["neglam"], writes=["neglam"])
    ph.I("dve", "tensor_scalar", out=gsc[:], in0=gsc[:], scalar1=1.0 - LAM_INIT, scalar2=None, op0=ALU.mult, reads=["gsc"], writes=["gsc"])

    scale = 64.0 ** -0.5
    LA = 4
    its = [(I, j) for I in range(NG) for j in range(4 * I + 4)]
    NIT = len(its)
    NPT = len(PT[0])

    def front(n):
        I, j = its[n]
        qs = slice(I * CH, (I + 1) * CH)
        jj = max(j - 4 * I, 0)
        for s in range(2):
            bi = (2 * n + s) % 3
            pS, pr = psS3[bi], f"psS{bi}"
            P, sr = PT[s][n % NPT], f"PT{s}{n%NPT}"
            ph.I("pe", "matmul", pS[:], lhsT=KT[s][:, j * 128:(j + 1) * 128], rhs=QT[:, qs], start=True, stop=True,
                 reads=[f"KT{s}", f"KT{s}z", "QT"], writes=[pr])
            if j <= 4 * I - 2:
                ph.I("act", "activation", out=P[:], in_=pS[:], func=AF.Exp, scale=scale, bias=tab[:, 15:16], reads=[pr, "tab"], writes=[sr])
            else:
                for ii in range(jj, 4):
                    delta = j - (4 * I + ii)
                    bs_ = slice(ii * 128, (ii + 1) * 128)
                    if delta <= -2:
                        ph.I("act", "activation", out=P[:, bs_], in_=pS[:, bs_], func=AF.Exp, scale=scale, bias=tab[:, 15:16],
                             reads=[pr, "tab"], writes=[sr])
                    else:
                        Bt, br = (B0, "B0") if delta == 0 else (B1, "B1")
                        t = tb[(s + ii) % 2]
                        tr = f"tb{(s + ii) % 2}"
                        ph.I("dve", "scalar_tensor_tensor", out=t[:], in0=pS[:, bs_], scalar=scale, in1=Bt[:], op0=ALU.mult, op1=ALU.add,
                             reads=[pr, br], writes=[tr])
                        ph.I("act", "activation", out=P[:, bs_], in_=t[:], func=AF.Exp, reads=[tr], writes=[sr])

    def back(n):
        I, j = its[n]
        qs = slice(I * CH, (I + 1) * CH)
        last = 4 * I + 3
        c0 = max(j - 4 * I, 0) * 128
        for s in range(2):
            P, sr = PT[s][n % NPT], f"PT{s}{n%NPT}"
            ph.I("pe", "matmul", psO[s][:, c0:], lhsT=V[:, j, :], rhs=P[:, c0:], start=(j == 0), stop=(j == last),
                 reads=[sr, "V"], writes=[f"psO{s}"])
            ph.I("pe", "matmul", psL[s][:, c0:], lhsT=ones_bf[:], rhs=P[:, c0:], start=(j == 0), stop=(j == last),
                 reads=[sr, "ones"], writes=[f"psL{s}"])
        if j != last:
            return
        ph.I("dve", "tensor_copy", out=l1c[:], in_=psL[0][:], reads=["psL0"], writes=["l1c"])
        ph.I("dve", "tensor_copy", out=o1[:], in_=psO[0][:], reads=["psO0"], writes=["o1"])
        ph.I("dve", "tensor_copy", out=l2c[:], in_=psL[1][:], reads=["psL1"], writes=["l2c"])
        ph.I("dve", "tensor_copy", out=o2[:], in_=psO[1][:], reads=["psO1"], writes=["o2"])
        ph.I("dve", "reciprocal", out=l1c[:], in_=l1c[:], reads=["l1c"], writes=["l1c"])
        ph.I("dve", "reciprocal", out=l2c[:], in_=l2c[:], reads=["l2c"], writes=["l2c"])
        ph.I("pool", "tensor_tensor", out=o1[:], in0=o1[:], in1=l1c[:], op=ALU.mult, reads=["o1", "l1c"], writes=["o1"])
        ph.I("pool", "tensor_tensor", out=o2[:], in0=o2[:], in1=l2c[:], op=ALU.mult, reads=["o2", "l2c"], writes=["o2"])
        ph.I("dve", "scalar_tensor_tensor", out=o1[:], in0=o2[:], scalar=neglam[:, 0:1], in1=o1[:], op0=ALU.mult, op1=ALU.add,
             reads=["o1", "o2", "neglam"], writes=["o1"])
        def part2(I=I, qs=qs):
            ph.I("act", "activation", out=sqo[:], in_=o1[:], func=AF.Square, reads=["o1"], writes=["sqo"])
            ph.I("pe", "matmul", psN[:], lhsT=ones_bf[:], rhs=sqo[:], start=True, stop=True, reads=["sqo", "ones"], writes=["psN"])
            ph.I("act", "activation", out=rs[:], in_=psN[:], func=AF.Sqrt, scale=1.0 / 128.0, bias=EPS, reads=["psN"], writes=["rs"])
            ph.I("dve", "reciprocal", out=rs[:], in_=rs[:], reads=["rs"], writes=["rs"])
            o = ost[I % 2]
            ph.I("dve", "scalar_tensor_tensor", out=o[:], in0=o1[:], scalar=gsc[:, 0:1], in1=rs[:], op0=ALU.mult, op1=ALU.mult,
                 reads=["o1", "rs", "gsc"], writes=[f"ost{I%2}"])
            ld(ph, "sp", f"so{I%2}", y_d[:, qs], o[:], [], reads=[f"ost{I%2}"])
        pending.append([n + DEFER, part2])

    DEFER = 8
    pending = []

    def flush(upto):
        while pending and pending[0][0] <= upto:
            pending.pop(0)[1]()

    for n in range(NIT + LA):
        if n < NIT:
            front(n)
        if n - LA >= 0:
            m = n - LA
            if its[m][1] == 4 * its[m][0] + 3:
                flush(10 ** 9)
            back(m)
            flush(m)
    flush(10 ** 9)
    return c.finish()


def host_D(inp, resC):
    (dm, notallowed) = get_diff_masks()
    masks = np.concatenate([dm[0][1], dm[-1][1], notallowed[None]], axis=0)
    masks = _c(masks.transpose(1, 0, 2))
    lam = _c(np.stack([rep(inp["diff_lambda_q1"][0]), rep(inp["diff_lambda_k1"][0]),
                       rep(inp["diff_lambda_q2"][0]), rep(inp["diff_lambda_k2"][0])], axis=1).astype(np.float32))
    maps = []
    for core in range(8):
        b, h = core // 4, core % 4
        qk = np.concatenate([resC[b * 4 + i]["qk"] for i in range(4)], axis=1)
        q = np.concatenate([qk[h * 64:(h + 1) * 64], qk[256 + h * 64:256 + (h + 1) * 64]], axis=0)
        k = np.concatenate([qk[512 + h * 64:512 + (h + 1) * 64], qk[768 + h * 64:768 + (h + 1) * 64]], axis=0)
        v = np.concatenate([resC[b * 4 + i]["v"][:, h * 128:(h + 1) * 128] for i in range(4)], axis=0)
        maps.append(dict(q=_c(q), k=_c(k), v=_c(v.reshape(NT, 128, 128).transpose(1, 0, 2)), masks=masks,
                         tab=rep(np.asarray(inp["rel_bias"], np.float32)[:, h]), lam=lam,
                         gain=_c(np.asarray(inp["diff_subln_gain"][0], np.float32)[h * 128:(h + 1) * 128, None])))
    return maps


NE = 8
NFE = 28


E_IN = dict(xT=([1024, T], F32), yc=([512, T], BF16), zext=([512, T + 2], F32), gb=([512, T], F32), convw=([128, 4, 3], F32),
            gcols=([128, 4, 8], F32), Wout=([2, 128, 8, 512], F32), memT=([1024, 256], F32), gkv=([128, 8], F32),
            Wkvk=([2, 128, 8, 512], F32), Wkvv=([2, 128, 8, 512], F32), Wq=([2, 128, 8, 512], F32), Wo=([2, 128, 8, 512], F32),
            Wr=([128, 8, 8], F32), ident=([128, 128], F32), Wg=([NE, 14, 128, 8, 256], F32), Wu=([NE, 14, 128, 8, 256], F32),
            Wd=([NE, 8, 128, NFE, 128], F32))


def build_E(stage=4, c=None, io=None):
    if c is None:
        c = Tok("E", arena_words=14336)
        io = {n: c.din(n, sh, dt) for n, (sh, dt) in E_IN.items()}
        io["out"] = c.dout("out", [1024, T])
    nc, ph = c.nc, c.ph
    (xT_d, yc_d, zx_d, gb_d, cw_d, gcols_d, Wout_d, memT_d, gkv_d, Wkvk_d, Wkvv_d, Wq_d, Wo_d, Wr_d, id_d, Wg_d, Wu_d, Wd_d) = (io[n] for n in E_IN)
    out_d = io["out"]
    sel_d = io.get("sel")
    fused = sel_d is not None

    cw = c.sb("cw", [128, 4, 3])
    zc = c.arena[:, 5120:5120 + 4 * (CH + 2)].rearrange("p (m t) -> p m t", t=CH + 2)
    gbc = c.arena[:, 7680:9728].rearrange("p (m t) -> p m t", t=CH)
    acc = c.arena[:, 10752:11264]
    ident = c.sb("ident", [128, 128])
    ones32 = c.sb("ones32", [128, 128])
    wr32 = c.sb("wr32", [128, 8, 8])
    wrh = c.sb("wrh", [128, 8, 8], BF16)
    wrl = c.sb("wrl", [128, 8, 8], BF16)
    y1w = c.ymb[1][:].rearrange("p k t -> p (k t)").bitcast(F32)
    y0w = c.ymb[0][:].rearrange("p k t -> p (k t)").bitcast(F32)
    t32 = [y1w[:, i * CH:(i + 1) * CH] for i in range(2)]
    lg = c.sb("lg", [128, 8])
    mx8 = c.sb("mx8", [128, 8])
    nv1 = c.sb("nv1", [128, 1])
    ex = c.sb("ex", [128, 8])
    msk = c.sb("msk", [128, 8])
    den = c.sb("den", [128, 1])
    comb = c.sb("comb", [128, MC // 128, 8])
    dg = [c.sb(f"dg{i}", [128, 128]) for i in range(2)]
    cbc = [y0w[:, i * MC:(i + 1) * MC] for i in range(2)]
    tmp = [y1w[:, (2 + i) * CH:(3 + i) * CH] for i in range(2)]

    def sel_load(dst, srcs, stg, tag, res, queue="sp"):
        for b_ in range(4):
            sb_ = stg[b_ % len(stg)]
            sr_ = f"{tag}stg{b_ % len(stg)}"
            ld(ph, queue, f"l{tag}{b_ % len(stg)}", sb_, srcs[b_], [sr_])
            if b_ == 0:
                ph.I("dve", "tensor_scalar", out=dst, in0=sb_, scalar1=sel[:, 0:1], scalar2=None, op0=ALU.mult, reads=[sr_, "sel"], writes=[res])
            else:
                ph.I("dve", "scalar_tensor_tensor", out=dst, in0=sb_, scalar=sel[:, b_:b_ + 1], in1=dst, op0=ALU.mult, op1=ALU.add,
                     reads=[sr_, "sel", res], writes=[res])

    if fused:
        sel = c.sb("sel", [128, 4])
        ld(ph, "sp", "lsel", sel[:], sel_d, ["sel"])
        xstg = [c.arena[:, i * T:(i + 1) * T] for i in range(2)]
        for k in range(8):
            sel_load(c.xT[:, k, :], [xb.rearrange("(k p) t -> p k t", p=128)[:, k, :] for xb in xT_d], xstg, "x", "xT")
    else:
        ld(ph, "sp", "lx", c.xT[:], xT_d.rearrange("(k p) t -> p k t", p=128), ["xT"])
    ld(ph, "sp", "lg", c.gcols[:], gcols_d, ["gcol"])
    ld(ph, "sp", "lcw", cw[:], cw_d, ["cw"])
    ld(ph, "sp", "lid", ident[:], id_d, ["ident"])
    ld(ph, "sp", "lwr", wr32[:], Wr_d, ["wr32"])
    ph.I("dve", "memset", ones32[:], 1.0, writes=["ones32"])
    ph.I("dve", "tensor_copy", out=wrh[:], in_=wr32[:], reads=["wr32"], writes=["wrh"])
    ph.I("dve", "tensor_tensor", out=wrl[:], in0=wr32[:], in1=wrh[:], op=ALU.subtract, reads=["wr32", "wrh"], writes=["wrl"])

    fm = lambda ap: ap.rearrange("(k p) t -> p k t", p=128)
    if fused:
        zstg = [c.arena[:, i * 2056:(i + 1) * 2056].rearrange("p (m t) -> p m t", t=CH + 2) for i in range(2)]
        gstg = [c.arena[:, 11264:13312].rearrange("p (m t) -> p m t", t=CH)]
        ystg = [c.arena[:, 13312:14336].bitcast(BF16).rearrange("p (m t) -> p m t", t=CH)]
        dmy = c.sb("dmy", [128, 1])
        ph.I("dve", "memset", dmy[:], 0.0, reads=["xstg0", "xstg1"], writes=["zstg0", "zstg1"])
    for tc in range(NCH):
        yb = c.ymb[tc % 2]
        yr = f"ymb{tc%2}"
        if fused:
            sel_load(yb[:, 0:4, :], [fm(a)[:, :, tc * CH:(tc + 1) * CH] for a in yc_d], ystg, "y", yr)
            sel_load(zc, [fm(a)[:, :, tc * CH: tc * CH + CH + 2] for a in zx_d], zstg, "z", "zc")
            sel_load(gbc, [fm(a)[:, :, tc * CH:(tc + 1) * CH] for a in gb_d], gstg, "g", "gbc")
        else:
            ld(ph, "sp", f"lym{tc%2}", yb[:, 0:4, :], fm(yc_d)[:, :, tc * CH:(tc + 1) * CH], [yr])
            ld(ph, "sp", "lz", zc, fm(zx_d)[:, :, tc * CH: tc * CH + CH + 2], ["zc"])
            ld(ph, "sp", "lgb", gbc, fm(gb_d)[:, :, tc * CH:(tc + 1) * CH], ["gbc"])
        for m in range(4):
            ph.I("dve", "tensor_scalar", out=acc, in0=zc[:, m, 2:CH + 2], scalar1=cw[:, m, 2:3], scalar2=None, op0=ALU.mult,
                 reads=["zc", "cw"], writes=["acc"])
            ph.I("dve", "scalar_tensor_tensor", out=acc, in0=zc[:, m, 1:CH + 1], scalar=cw[:, m, 1:2], in1=acc, op0=ALU.mult, op1=ALU.add,
                 reads=["zc", "cw", "acc"], writes=["acc"])
            ph.I("dve", "scalar_tensor_tensor", out=acc, in0=zc[:, m, 0:CH], scalar=cw[:, m, 0:1], in1=acc, op0=ALU.mult, op1=ALU.add,
                 reads=["zc", "cw", "acc"], writes=["acc"])
            ph.I("dve", "tensor_tensor", out=yb[:, 4 + m, :], in0=acc, in1=gbc[:, m, :], op=ALU.mult, reads=["acc", "gbc"], writes=[yr])
        c.linear_fm(Wout_d, 8, 512, yb, yr, CH, c.resid_add(tc * CH))

    if stage >= 2:
        c.mem_setup(memT_d, gkv_d, Wkvk_d, Wkvv_d)
        c.mem_attn(0, Wq_d, Wo_d)

    hlo = c.xq if stage >= 2 else None
    for mc in range(T // MC if stage >= 3 else 0):
        for ts in range(MC // CH):
            cs = slice(mc * MC + ts * CH, mc * MC + (ts + 1) * CH)
            hs = slice(ts * CH, (ts + 1) * CH)
            for k in range(8):
                sqk = c.sq[k % 2]
                ph.I("act", "activation", out=sqk[:], in_=c.xT[:, k, cs], func=AF.Square, reads=["xT"], writes=[f"nsq{k%2}"])
                ph.I("pe", "matmul", c.psN[:], lhsT=c.ones_bf[:], rhs=sqk[:], start=(k == 0), stop=(k == 7), reads=[f"nsq{k%2}", "ones"], writes=["npsN"])
            ph.I("act", "activation", out=c.rstd_s[:], in_=c.psN[:], func=AF.Sqrt, scale=1.0 / 1024.0, bias=EPS, reads=["npsN"], writes=["nrstd_s"])
            ph.I("dve", "reciprocal", out=c.rstd[:], in_=c.rstd_s[:], reads=["nrstd_s"], writes=["nrstd"])
            for k in range(8):
                t = t32[k % 2]
                tr = f"t32{k%2}"
                ph.I("dve", "scalar_tensor_tensor", out=t, in0=c.xT[:, k, cs], scalar=c.gcols[:, 1, k:k + 1], in1=c.rstd[:], op0=ALU.mult, op1=ALU.mult,
                     reads=["xT", "nrstd", "gcol"], writes=[tr])
                ph.I("act", "activation", out=c.hT[:, k, hs], in_=t, func=AF.Copy, reads=[tr], writes=["hT"])
                ph.I("dve", "tensor_tensor", out=hlo[:, k, :], in0=t, in1=c.hT[:, k, hs], op=ALU.subtract, reads=[tr, "hT"], writes=["xq"])
            for s4 in range(4):
                sub = slice(ts * CH + s4 * 128, ts * CH + (s4 + 1) * 128)
                subl = slice(s4 * 128, (s4 + 1) * 128)
                ps, pres = c.nextps()
                n = 0
                for (a_, ar_, sl_, b_, br_) in ((c.hT, "hT", sub, wrh, "wrh"), (hlo, "xq", subl, wrh, "wrh"), (c.hT, "hT", sub, wrl, "wrl")):
                    for k in range(8):
                        ph.I("pe", "matmul", ps[:, 0:8], lhsT=a_[:, k, sl_], rhs=b_[:, k, :], start=(n == 0), stop=(n == 23), reads=[ar_, br_], writes=[pres])
                        n += 1
                ph.I("dve", "tensor_copy", out=lg[:], in_=ps[:, 0:8], reads=[pres], writes=["lg"])
                ph.I("dve", "max", out=mx8[:], in_=lg[:], reads=["lg"], writes=["mx8"])
                ph.I("dve", "tensor_scalar", out=nv1[:], in0=mx8[:, 0:1], scalar1=-1.0, scalar2=None, op0=ALU.mult, reads=["mx8"], writes=["nv1"])
                ph.I("act", "activation", out=ex[:], in_=lg[:], func=AF.Exp, bias=nv1[:, 0:1], reads=["lg", "nv1"], writes=["ex"])
                ph.I("dve", "tensor_scalar", out=msk[:], in0=lg[:], scalar1=mx8[:, 1:2], scalar2=None, op0=ALU.is_ge, reads=["lg", "mx8"], writes=["msk"])
                ph.I("dve", "tensor_tensor", out=ex[:], in0=ex[:], in1=msk[:], op=ALU.mult, reads=["ex", "msk"], writes=["ex"])
                ph.I("dve", "reduce_sum", out=den[:], in_=ex[:], axis=AX.X, reads=["ex"], writes=["den"])
                ph.I("dve", "reciprocal", out=den[:], in_=den[:], reads=["den"], writes=["den"])
                ph.I("dve", "tensor_scalar", out=comb[:, ts * 4 + s4, :], in0=ex[:], scalar1=den[:, 0:1], scalar2=None, op0=ALU.mult,
                     reads=["ex", "den"], writes=["comb"])
        for e in range(NE):
            cb = cbc[e % 2]
            cbr = f"cbc{e%2}"
            for s8 in range(MC // 128):
                d_ = dg[s8 % 2]
                dr = f"dg{s8%2}"
                ph.I("dve", "tensor_scalar", out=d_[:], in0=ident[:], scalar1=comb[:, s8, e:e + 1], scalar2=None, op0=ALU.mult,
                     reads=["ident", "comb"], writes=[dr])
                ph.I("pe", "matmul", c.psL[:, (s8 % 4) * 128:(s8 % 4 + 1) * 128], lhsT=ones32[:], rhs=d_[:], start=True, stop=True,
                     reads=[dr, "ones32"], writes=["psL"])
                if s8 % 4 == 3:
                    ph.I("act", "activation", out=cb[:, (s8 // 4) * CH:(s8 // 4 + 1) * CH], in_=c.psL[:], func=AF.Copy, reads=["psL"], writes=[cbr])
            for cg in range(14):
                wg, wgr = c.wload(Wg_d[e, cg], 8, 256)
                wu, wur = c.wload(Wu_d[e, cg], 8, 256)
                for mi in range(2):
                    for ts in range(MC // CH):
                        psG, pgr = c.nextps()
                        psU, pur = c.nextps()
                        for k in range(8):
                            ph.I("pe", "matmul", psG[:], lhsT=wg[:, k, mi * 128:(mi + 1) * 128], rhs=c.hT[:, k, ts * CH:(ts + 1) * CH],
                                 start=(k == 0), stop=(k == 7), reads=[wgr, "hT"], writes=[pgr])
                        for k in range(8):
                            ph.I("pe", "matmul", psU[:], lhsT=wu[:, k, mi * 128:(mi + 1) * 128], rhs=c.hT[:, k, ts * CH:(ts + 1) * CH],
                                 start=(k == 0), stop=(k == 7), reads=[wur, "hT"], writes=[pur])
                        sg = c.sg[(mi * 2 + ts) % 2]
                        sgr = f"sg{(mi * 2 + ts) % 2}"
                        ph.I("act", "activation", out=sg[:], in_=psG[:], func=AF.Silu, reads=[pgr], writes=[sgr])
                        ph.I("dve", "tensor_tensor", out=c.actT[:, cg * 2 + mi, ts * CH:(ts + 1) * CH], in0=sg[:], in1=psU[:], op=ALU.mult,
                             reads=[sgr, pur], writes=["actT"])

            def ev_down(m, ts, ps, pres, cb=cb, cbr=cbr, mc=mc):
                tm = tmp[(m * 2 + ts) % 2]
                tmr = f"tmp{(m * 2 + ts) % 2}"
                sl = slice(mc * MC + ts * CH, mc * MC + (ts + 1) * CH)
                ph.I("dve", "tensor_tensor", out=tm, in0=ps[:], in1=cb[:, ts * CH:(ts + 1) * CH], op=ALU.mult, reads=[pres, cbr], writes=[tmr])
                ph.I("pool", "tensor_tensor", out=c.xT[:, m, sl], in0=c.xT[:, m, sl], in1=tm, op=ALU.add, reads=[tmr, "xT"], writes=["xT"])
            c.linear_fm(Wd_d[e], NFE, 128, c.actT, "actT", MC, ev_down)

    fo = c.arena[:, 0:4096].rearrange("p (k t) -> p k t", t=CH)
    ph.I("dve", "memset", acc[:, 0:1], 0.0, reads=["actT"], writes=["fo"])
    if stage < 4:
        ld(ph, "sp", "sout", out_d.rearrange("(k p) t -> p k t", p=128), c.xT[:], [], reads=["xT"])
    for tc in range(NCH if stage >= 4 else 0):
        cs = slice(tc * CH, (tc + 1) * CH)
        c.norm(2, cs, fo, "fo")
        ld(ph, "sp", "sout", out_d.rearrange("(k p) t -> p k t", p=128)[:, :, cs], fo, [], reads=["fo"])
    return c.finish()


def host_E(inp, resC, resD):
    gcols = _c(np.stack([pk(inp["norm_mem_q"][1]), pk(inp["norm_ffn"][1]), pk(inp["norm_final"]), pk(inp["norm_final"])], axis=1))
    cw = np.asarray(inp["conv_w"][0], np.float32)
    convw = _c(cw.reshape(3, 4, 128).transpose(2, 1, 0))
    Wg = np.stack([wtiles(inp["moe_w_gate"][0][e], 256) for e in range(NE)])
    Wu = np.stack([wtiles(inp["moe_w_up"][0][e], 256) for e in range(NE)])
    Wd = np.stack([wtiles(inp["moe_w_down"][0][e], 128) for e in range(NE)])
    shared = dict(gcols=gcols, convw=convw, Wout=wtiles(inp["odd_w_out"][0], 512),
                  Wr=_c(np.asarray(inp["router_w"][0], np.float32).reshape(8, 128, 8).transpose(1, 0, 2)),
                  ident=np.eye(128, dtype=np.float32), Wg=Wg, Wu=Wu, Wd=Wd)
    mem = [mem_inputs(inp, 1, b) for b in range(2)]
    maps = []
    for core in range(8):
        b, i = core // 4, core % 4
        yc = np.concatenate([resD[b * 4 + h]["y"][:, i * T:(i + 1) * T] for h in range(4)], axis=0)
        z = resC[core]["z"]
        halo = resC[core - 1]["z"][:, T - 2:] if i > 0 else np.zeros((512, 2), np.float32)
        m = dict(xT=resC[core]["xo"], yc=_c(yc), zext=_c(np.concatenate([halo, z], axis=1)), gb=resC[core]["gb"])
        m.update(shared)
        m.update(mem[b])
        maps.append(m)
    return maps


def build_fused():
    nc = bass.Bass("TRN2", target_bir_lowering=False)
    (dm, notallowed) = get_diff_masks()
    nmask = len(dm[0][0]) + len(dm[-1][0]) + 1

    def din(name, shape, dt=F32):
        return nc.dram_tensor(name, list(shape), dt, kind="ExternalInput").ap()

    def scr(name, shape, dt=F32):
        return nc.dram_tensor(name, list(shape), dt, kind="Internal").ap()

    x_d = din("x", [1024, S])
    A = dict(g=din("A_g", [128, 8]), w=din("A_w", [1024, 2564]), vgain=din("A_vgain", [128, 512]), ws=din("A_ws", [128, 4, 128]),
             bs=din("A_bs", [128, 4, 512]))
    B = dict(bf=din("B_bf", [4, 64, 1]), ident=din("B_ident", [64, 64]), lmat=din("B_lmat", [64, 64]), tri=din("B_tri", [128, 128]))
    C = {n: din("C_" + n, sh, dt) for n, (sh, dt) in C_IN.items() if n not in ("xT", "ymix")}
    D = dict(masks=din("D_masks", [128, nmask, 128]), tab=din("D_tab", [4, 128, 32]), lam=din("D_lam", [128, 4, 64]),
             gain=din("D_gain", [4, 128, 1]))
    E = {n: din("E_" + n, sh, dt) for n, (sh, dt) in E_IN.items() if n not in ("xT", "yc", "zext", "gb")}
    sel_d = din("E_sel", [128, 4])
    out_d = nc.dram_tensor("out", [1024, T], F32, kind="ExternalOutput").ap()

    q_s, k_s, ya_s, yb_s, yc_s = (scr(n, [512, S], BF16) for n in ("q_s", "k_s", "ya_s", "yb_s", "yc_s"))
    v_s, v1_s = scr("v_s", [S, 512], BF16), scr("v1_s", [S, 512], BF16)
    f_s = scr("f_s", [4, S])
    x1_s = scr("x1_s", [1024, S])
    qk_s = scr("qk_s", [1024, S], BF16)
    z_s = scr("z_s", [512, S + 2])
    gb_s = scr("gb_s", [512, S])

    c0 = Ctx("Z", nc=nc)
    zt = c0.sb("zt", [128, 4, 2])
    c0.ph.I("dve", "memset", zt[:], 0.0, writes=["zt"])
    ld(c0.ph, "sp", "sz", z_s.rearrange("(m p) t -> p m t", p=128)[:, :, 0:2], zt[:], [], reads=["zt"])
    c0.finish()

    for blk in range(4):
        cols = slice(blk * T, (blk + 1) * T)
        io = dict(A)
        io.update(xT=x_d[:, cols], ya=ya_s[:, cols], q=q_s[:, cols], k=k_s[:, cols], v=v_s[cols, :], f=f_s[:, cols])
        build_A(Ctx(f"A{blk}", nc=nc), io)
    for h in range(4):
        rows = slice(h * 128, (h + 1) * 128)
        io = dict(q=q_s[rows, :], k=k_s[rows, :], v=v_s.rearrange("(j p) c -> p j c", p=128)[:, :, rows],
                  f2=f_s[h].rearrange("(j p) -> j p", p=128), bf=B["bf"][h], ident=B["ident"], lmat=B["lmat"], tri=B["tri"], y=yb_s[rows, :])
        build_B(Ctx(f"B{h}", nc=nc), io)
    for blk in range(4):
        cols = slice(blk * T, (blk + 1) * T)
        io = dict(C)
        io.update(xT=x_d[:, cols], ymix=(ya_s[:, cols], yb_s[:, cols]), xo=x1_s[:, cols], qk=qk_s[:, cols], v=v1_s[cols, :],
                  z=z_s[:, 2 + blk * T: 2 + (blk + 1) * T], gb=gb_s[:, cols])
        build_C(Tok(f"C{blk}", nc=nc), io)
    for h in range(4):
        r64 = lambda base: qk_s[base + h * 64: base + (h + 1) * 64, :]
        io = dict(q=(r64(0), r64(256)), k=(r64(512), r64(768)), v=v1_s.rearrange("(j p) c -> p j c", p=128)[:, :, h * 128:(h + 1) * 128],
                  masks=D["masks"], tab=D["tab"][h], lam=D["lam"], gain=D["gain"][h], y=yc_s[h * 128:(h + 1) * 128, :])
        build_D(Ctx(f"D{h}", nc=nc), io)
    io = dict(E)
    io.update(xT=[x1_s[:, b * T:(b + 1) * T] for b in range(4)], yc=[yc_s[:, b * T:(b + 1) * T] for b in range(4)],
              zext=[z_s[:, b * T: b * T + T + 2] for b in range(4)], gb=[gb_s[:, b * T:(b + 1) * T] for b in range(4)],
              sel=sel_d, out=out_d)
    build_E(4, Tok("E", arena_words=14336, nc=nc), io)
    return nc


def host_fused(inp):
    x = np.asarray(inp["x"], np.float32)
    (dm, notallowed) = get_diff_masks()
    masks = _c(np.concatenate([dm[0][1], dm[-1][1], notallowed[None]], axis=0).transpose(1, 0, 2))
    bs4 = rep(np.tile(np.asarray(inp["gmlp_b_s"][0], np.float32), (1, 4)))
    sh = {
        "A_g": pk(inp["norm_mix"][0]), "A_w": _c(np.asarray(inp["even_w_in"][0], np.float32)),
        "A_vgain": rep(np.asarray(inp["gmlp_v_gain"][0], np.float32)),
        "A_ws": _c(np.transpose(np.asarray(inp["gmlp_w_s"][0], np.float32), (2, 0, 1))), "A_bs": bs4,
        "B_bf": _c(np.broadcast_to(np.asarray(inp["fox_b_f"][0], np.float32)[:, None, None], (4, 64, 1))),
        "B_ident": np.eye(64, dtype=np.float32), "B_lmat": np.triu(np.ones((64, 64), np.float32), 1),
        "B_tri": np.triu(np.ones((128, 128), np.float32), 0),
        "C_gcols": _c(np.stack([pk(inp["norm_mem_q"][0]), pk(inp["norm_ffn"][0]), pk(inp["norm_mix"][1]), pk(inp["norm_mix"][1])], axis=1)),
        "C_Wout": wtiles(inp["even_w_out"][0], 512), "C_Wg": wtiles(inp["ffn_w_gate"][0], 256), "C_Wu": wtiles(inp["ffn_w_up"][0], 256),
        "C_Wd": wtiles(inp["ffn_w_down"][0], 128), "C_Win": wtiles(inp["odd_w_in"][0], 512),
        "D_masks": masks,
        "D_tab": _c(np.stack([rep(np.asarray(inp["rel_bias"], np.float32)[:, h]) for h in range(4)])),
        "D_lam": _c(np.stack([rep(inp["diff_lambda_q1"][0]), rep(inp["diff_lambda_k1"][0]),
                              rep(inp["diff_lambda_q2"][0]), rep(inp["diff_lambda_k2"][0])], axis=1).astype(np.float32)),
        "D_gain": _c(np.asarray(inp["diff_subln_gain"][0], np.float32).reshape(4, 128, 1)),
        "E_convw": _c(np.asarray(inp["conv_w"][0], np.float32).reshape(3, 4, 128).transpose(2, 1, 0)),
        "E_gcols": _c(np.stack([pk(inp["norm_mem_q"][1]), pk(inp["norm_ffn"][1]), pk(inp["norm_final"]), pk(inp["norm_final"])], axis=1)),
        "E_Wout": wtiles(inp["odd_w_out"][0], 512),
        "E_Wr": _c(np.asarray(inp["router_w"][0], np.float32).reshape(8, 128, 8).transpose(1, 0, 2)),
        "E_ident": np.eye(128, dtype=np.float32),
        "E_Wg": np.stack([wtiles(inp["moe_w_gate"][0][e], 256) for e in range(NE)]),
        "E_Wu": np.stack([wtiles(inp["moe_w_up"][0][e], 256) for e in range(NE)]),
        "E_Wd": np.stack([wtiles(inp["moe_w_down"][0][e], 128) for e in range(NE)]),
    }
    memC = [mem_inputs(inp, 0, b) for b in range(2)]
    memE = [mem_inputs(inp, 1, b) for b in range(2)]
    xT = [_c(x[b].T) for b in range(2)]
    maps = []
    for core in range(8):
        b, i = core // 4, core % 4
        m = dict(sh)
        m["x"] = xT[b]
        for k_, v_ in memC[b].items():
            m["C_" + k_] = v_
        for k_, v_ in memE[b].items():
            m["E_" + k_] = v_
        selv = np.zeros((128, 4), np.float32)
        selv[:, i] = 1.0
        m["E_sel"] = selv
        maps.append(m)
    return maps


def kernel(**inp):
    inp = {k: np.asarray(v) for k, v in inp.items()}
    x = np.asarray(inp["x"], np.float32)
    resA = run(get_nc("A", build_A), host_A(inp))
    resB = run(get_nc("B", build_B), host_B(inp, resA))
    xT_cores = [_c(core_tokens(x, core).T) for core in range(8)]
    resC = run(get_nc("C", build_C), host_C(inp, resA, resB, xT_cores))
    resD = run(get_nc("D", build_D), host_D(inp, resC))
    resE = run(get_nc("E", build_E), host_E(inp, resC, resD))
    out = np.empty((2, 8192, 1024), np.float32)
    for core in range(8):
        b, i = core // 4, core % 4
        out[b, i * T:(i + 1) * T] = resE[core]["out"].T
    return out
```
